# Optimizing a Trainium2 kernel written in Bass

```python
import jax
import jax.numpy as jnp
from jax import lax
import numpy as np

D_MODEL = 1024
BATCH = 8
SEQ = 2048
DEPTH = 1

ATT_HEADS = 16
ATT_HEAD_DIM = 64
ATT_W = ATT_HEADS * ATT_HEAD_DIM
DILATED_PATTERNS = ((128, 1), (512, 4), (2048, 16))
ROPE_THETA = 10000.0
RET_HEADS = 4
RET_QK_DIM = 256
RET_V_DIM = 512
RET_QK_W = RET_HEADS * RET_QK_DIM
RET_V_W = RET_HEADS * RET_V_DIM
RET_CHUNK = 128
N_GROUPS = 4
EXPERTS_PER_GROUP = 8
N_EXPERTS = N_GROUPS * EXPERTS_PER_GROUP
TOP_K_IN_GROUP = 2
EXPERT_FF = 256
EPS = 1e-6
IN_SPLITS = (ATT_W, ATT_W, ATT_W, RET_QK_W, RET_QK_W, RET_V_W, RET_V_W, D_MODEL, D_MODEL)
IN_WIDTH = sum(IN_SPLITS)

kernel_name = 'hybrid_dilated_retention_hmoe_block'


def _split_points():
    pts, acc = [], 0
    for s in IN_SPLITS[:-1]:
        acc += s
        pts.append(acc)
    return pts


def _rmsnorm(x, g):
    xf = x.astype(jnp.float32)
    y = xf * lax.rsqrt(jnp.mean(xf * xf, axis=-1, keepdims=True) + EPS)
    return (y * g.astype(jnp.float32)).astype(x.dtype)


def _rotate(x, pos, inv_freq):
    ang = pos[:, None] * inv_freq[None, :]
    cos, sin = jnp.cos(ang), jnp.sin(ang)
    x1, x2 = jnp.split(x, 2, axis=-1)
    return jnp.concatenate([x1 * cos - x2 * sin, x2 * cos + x1 * sin], axis=-1)


def _heads(t, n, d):
    B, S, _ = t.shape
    return t.reshape(B, S, n, d).transpose(0, 2, 1, 3)


def _merge_heads(t):
    B, H, S, d = t.shape
    return t.transpose(0, 2, 1, 3).reshape(B, S, H * d)


def _dilated_pattern(q, k, v, window, dilation):
    B, H, S, hd = q.shape
    L = window // dilation
    Sr = S // dilation
    nb = -(-Sr // L)
    pad = nb * L - Sr

    def to_blocks(t):
        t = t.reshape(B, H, Sr, dilation, hd).transpose(0, 1, 3, 2, 4)
        t = jnp.pad(t, ((0, 0), (0, 0), (0, 0), (0, pad), (0, 0)))
        return t.reshape(B, H, dilation, nb, L, hd)

    def with_prev(t):
        prev = jnp.pad(t[:, :, :, :-1], ((0, 0), (0, 0), (0, 0), (1, 0), (0, 0), (0, 0)))
        return jnp.concatenate([prev, t], axis=4)

    qb = to_blocks(q)
    k2 = with_prev(to_blocks(k))
    v2 = with_prev(to_blocks(v))
    s = jnp.einsum('bhrnqd,bhrnkd->bhrnqk', qb, k2) * (hd ** -0.5)
    qpos = jnp.arange(nb)[:, None] * L + jnp.arange(L)[None, :]
    kpos = jnp.arange(nb)[:, None] * L - L + jnp.arange(2 * L)[None, :]
    dist = qpos[:, :, None] - kpos[:, None, :]
    mask = (dist >= 0) & (dist <= L) & (kpos[:, None, :] >= 0)
    s = jnp.where(mask, s, -jnp.inf)
    m = jnp.max(s, axis=-1, keepdims=True)
    p = jnp.exp(s - m)
    den = jnp.sum(p, axis=-1, keepdims=True)
    o = jnp.einsum('bhrnqk,bhrnkd->bhrnqd', p, v2) / den

    def from_blocks(t):
        c = t.shape[-1]
        t = t.reshape(B, H, dilation, nb * L, c)[:, :, :, :Sr]
        return t.transpose(0, 1, 3, 2, 4).reshape(B, H, S, c)

    return from_blocks(o), from_blocks(m), from_blocks(den)


def _dilated_attention(q, k, v):
    outs, maxes, dens = [], [], []
    for window, dilation in DILATED_PATTERNS:
        o, m, d = _dilated_pattern(q, k, v, window, dilation)
        outs.append(o)
        maxes.append(m)
        dens.append(d)
    m_all = functools_max(maxes)
    weights = [d * jnp.exp(m - m_all) for d, m in zip(dens, maxes)]
    num = sum(w * o for w, o in zip(weights, outs))
    return num / sum(weights)


def functools_max(arrs):
    out = arrs[0]
    for a in arrs[1:]:
        out = jnp.maximum(out, a)
    return out


def _retention(q, k, v, log_gamma):
    B, H, S, dk = q.shape
    dv = v.shape[-1]
    C = min(RET_CHUNK, S)
    nc = S // C
    qc = q.reshape(B, H, nc, C, dk)
    kc = k.reshape(B, H, nc, C, dk)
    vc = v.reshape(B, H, nc, C, dv)
    idx = jnp.arange(C, dtype=jnp.float32)
    diff = idx[:, None] - idx[None, :]
    lg = log_gamma[:, None, None]
    decay = jnp.where(diff >= 0, jnp.exp(lg * jnp.maximum(diff, 0.0)), 0.0)
    zeta = jnp.exp(log_gamma[:, None] * (C - 1 - idx))
    xi = jnp.exp(log_gamma[:, None] * (idx + 1))
    gamma_c = jnp.exp(log_gamma * C)
    inner = jnp.einsum('bhnik,bhnjk->bhnij', qc, kc) * decay[None, :, None]
    y = jnp.einsum('bhnij,bhnjv->bhniv', inner, vc)
    kv = jnp.einsum('bhnjk,bhnjv->bhnkv', kc * zeta[None, :, None, :, None], vc)

    def step(state, kv_n):
        return state * gamma_c[None, :, None, None] + kv_n, state

    init = jnp.zeros((B, H, dk, dv), dtype=kv.dtype)
    _, prev = lax.scan(step, init, jnp.moveaxis(kv, 2, 0))
    prev = jnp.moveaxis(prev, 0, 2)
    y = y + jnp.einsum('bhnik,bhnkv->bhniv', qc * xi[None, :, None, :, None], prev)
    return y.reshape(B, H, S, dv)


def _mixer(xn, w_in, b_gate, g_q, g_k, w_att, g_ret, w_ret, w_out):
    dt = xn.dtype
    f32 = jnp.float32
    B, S, _ = xn.shape
    qa, ka, va, qr, kr, vr, gr, ga, gb = jnp.split(xn @ w_in, _split_points(), axis=-1)
    pos = jnp.arange(S, dtype=f32)
    inv_a = ROPE_THETA ** (-jnp.arange(0, ATT_HEAD_DIM, 2, dtype=f32) / ATT_HEAD_DIM)
    q = _rotate(_rmsnorm(_heads(qa, ATT_HEADS, ATT_HEAD_DIM).astype(f32), g_q), pos, inv_a)
    k = _rotate(_rmsnorm(_heads(ka, ATT_HEADS, ATT_HEAD_DIM).astype(f32), g_k), pos, inv_a)
    v = _heads(va, ATT_HEADS, ATT_HEAD_DIM).astype(f32)
    y_att = _merge_heads(_dilated_attention(q, k, v)).astype(dt) @ w_att
    inv_r = 1.0 / (ROPE_THETA ** jnp.linspace(0.0, 1.0, RET_QK_DIM // 2, dtype=f32))
    q = _rotate(_heads(qr, RET_HEADS, RET_QK_DIM).astype(f32), pos, inv_r)
    k = _rotate(_heads(kr, RET_HEADS, RET_QK_DIM).astype(f32), pos, inv_r) * (RET_QK_DIM ** -0.5)
    v = _heads(vr, RET_HEADS, RET_V_DIM).astype(f32)
    log_gamma = jnp.log(1.0 - jnp.exp2(-5.0 - jnp.arange(RET_HEADS, dtype=f32)))
    y = _rmsnorm(_retention(q, k, v, log_gamma), g_ret[:, None, :])
    y_ret = (_merge_heads(y).astype(dt) * jax.nn.silu(gr)) @ w_ret
    gate_a = jax.nn.sigmoid(ga + b_gate[:D_MODEL])
    gate_b = jax.nn.sigmoid(gb + b_gate[D_MODEL:])
    return (gate_a * y_att + gate_b * y_ret) @ w_out


def _hmoe(xn, w_rg, b_rg, w_re, b_re, w1, w3, w2):
    B, S, D = xn.shape
    t = xn.reshape(B * S, D)
    T = t.shape[0]
    group_logits = (t @ w_rg + b_rg).astype(jnp.float32)
    group_prob = jax.nn.softmax(group_logits, axis=-1)
    g_sel = jnp.argmax(group_logits, axis=-1)
    p_group = jnp.take_along_axis(group_prob, g_sel[:, None], axis=-1)
    exp_logits = (t @ w_re + b_re).astype(jnp.float32).reshape(T, N_GROUPS, EXPERTS_PER_GROUP)
    in_group = jnp.take_along_axis(exp_logits, g_sel[:, None, None], axis=1)[:, 0]
    top_v, top_i = lax.top_k(in_group, TOP_K_IN_GROUP)
    top_w = jax.nn.softmax(top_v, axis=-1) * p_group
    expert_id = g_sel[:, None] * EXPERTS_PER_GROUP + top_i
    gate = jnp.sum(jax.nn.one_hot(expert_id, N_EXPERTS, dtype=jnp.float32) * top_w[..., None], axis=1)
    gate = gate.astype(t.dtype)
    y = jnp.zeros_like(t)
    for e in range(N_EXPERTS):
        h = jax.nn.silu(t @ w1[e]) * (t @ w3[e])
        y = y + gate[:, e:e + 1] * (h @ w2[e])
    return y.reshape(B, S, D)


def setup_inputs(seed: int = 0) -> dict:
    key = jax.random.key(seed)
    ks = jax.random.split(key, 20)
    f32 = jnp.float32

    def nrm(k, shape, scale):
        return jax.random.normal(k, shape, f32) * scale

    return {
        'x': nrm(ks[0], (BATCH, SEQ, D_MODEL), 1.0),
        'g_norm_mix': 1.0 + nrm(ks[1], (DEPTH, D_MODEL), 0.02),
        'w_in': nrm(ks[2], (DEPTH, D_MODEL, IN_WIDTH), D_MODEL ** -0.5),
        'b_merge_gate': nrm(ks[3], (DEPTH, 2 * D_MODEL), 0.02),
        'g_q': 1.0 + nrm(ks[4], (DEPTH, ATT_HEAD_DIM), 0.02),
        'g_k': 1.0 + nrm(ks[5], (DEPTH, ATT_HEAD_DIM), 0.02),
        'w_branch_att': nrm(ks[6], (DEPTH, ATT_W, D_MODEL), ATT_W ** -0.5),
        'g_ret_norm': 1.0 + nrm(ks[7], (DEPTH, RET_HEADS, RET_V_DIM), 0.02),
        'w_branch_ret': nrm(ks[8], (DEPTH, RET_V_W, D_MODEL), RET_V_W ** -0.5),
        'w_out': nrm(ks[9], (DEPTH, D_MODEL, D_MODEL), D_MODEL ** -0.5),
        'g_norm_ffn': 1.0 + nrm(ks[10], (DEPTH, D_MODEL), 0.02),
        'w_router_group': nrm(ks[11], (DEPTH, D_MODEL, N_GROUPS), D_MODEL ** -0.5),
        'b_router_group': nrm(ks[12], (DEPTH, N_GROUPS), 0.01),
        'w_router_expert': nrm(ks[13], (DEPTH, D_MODEL, N_EXPERTS), D_MODEL ** -0.5),
        'b_router_expert': nrm(ks[14], (DEPTH, N_EXPERTS), 0.01),
        'w1': nrm(ks[15], (DEPTH, N_EXPERTS, D_MODEL, EXPERT_FF), D_MODEL ** -0.5),
        'w3': nrm(ks[16], (DEPTH, N_EXPERTS, D_MODEL, EXPERT_FF), D_MODEL ** -0.5),
        'w2': nrm(ks[17], (DEPTH, N_EXPERTS, EXPERT_FF, D_MODEL), EXPERT_FF ** -0.5),
    }


def reference(x, g_norm_mix, w_in, b_merge_gate, g_q, g_k, w_branch_att, g_ret_norm,
              w_branch_ret, w_out, g_norm_ffn, w_router_group, b_router_group,
              w_router_expert, b_router_expert, w1, w3, w2):
    for l in range(DEPTH):
        x = x + _mixer(_rmsnorm(x, g_norm_mix[l]), w_in[l], b_merge_gate[l], g_q[l], g_k[l],
                       w_branch_att[l], g_ret_norm[l], w_branch_ret[l], w_out[l])
        x = x + _hmoe(_rmsnorm(x, g_norm_ffn[l]), w_router_group[l], b_router_group[l],
                      w_router_expert[l], b_router_expert[l], w1[l], w3[l], w2[l])
    return x
```

```python
import numpy as np
import ml_dtypes
import concourse.bass as bass
import concourse.mybir as mybir
from concourse.bass_utils import run_bass_kernel_spmd
from contextlib import ExitStack

F32 = mybir.dt.float32
BF16 = mybir.dt.bfloat16
ALU = mybir.AluOpType
AF = mybir.ActivationFunctionType
AX = mybir.AxisListType

S = 2048
D = 1024
NT = 16
NG = 4
EPS = 1e-6
ENGS = ("pe", "act", "dve", "pool", "sp")


class Res:
    __slots__ = ("name", "writer", "readers")

    def __init__(self, name):
        self.name = name
        self.writer = None
        self.readers = []


class Op:
    __slots__ = ("eng", "fn", "deps", "signal", "value", "dma", "semkey", "idx")


class Prog:
    def __init__(self, nc, es):
        self.nc = nc
        self.es = es
        self.ops = {e: [] for e in ENGS}
        self.dma_sems = {}
        self.nres = 0
        self.excl = set()
        self.all_res = []
        self.last_barrier = None

    def res(self, name=None):
        self.nres += 1
        r = Res(name or f"r{self.nres}")
        r.writer = self.last_barrier
        self.all_res.append(r)
        return r

    def sb(self, name, shape, dt):
        return self.es.enter_context(self.nc.sbuf_tensor(name, list(shape), dt))

    def op(self, eng, meth, args=(), kw=None, reads=(), writes=(), dma=False, semkey=None):
        o = Op()
        o.eng = eng
        o.fn = (meth, tuple(args), dict(kw or {}))
        o.signal = False
        o.value = None
        o.dma = dma
        o.semkey = semkey
        if eng in ("act", "dve") and self.excl:
            extra = [r for r in reads if id(r) in self.excl and all(r is not w for w in writes)]
            if extra:
                writes = list(writes) + extra
        deps = {}
        for r in reads:
            if r.writer is not None:
                deps[id(r.writer)] = (r.writer, True)
        for w in writes:
            if w.writer is not None and id(w.writer) not in deps:
                deps[id(w.writer)] = (w.writer, False)
            for rd in w.readers:
                if id(rd) not in deps:
                    deps[id(rd)] = (rd, False)
        dl = []
        for d, raw in deps.values():
            if d is o:
                continue
            if (not d.dma) and (not dma) and d.eng == eng:
                if eng in ("pe", "sp"):
                    continue
            dl.append(d)
            d.signal = True
        o.deps = dl
        for r in reads:
            r.readers.append(o)
        for w in writes:
            w.writer = o
            w.readers = []
        if dma:
            if semkey not in self.dma_sems:
                h = self.es.enter_context(self.nc.semaphore(f"dq{len(self.dma_sems)}"))
                self.dma_sems[semkey] = [h, 0]
            ent = self.dma_sems[semkey]
            ent[1] += 16
            o.value = ent[1]
        o.idx = len(self.ops[eng])
        self.ops[eng].append(o)
        return o

    def dma(self, out, in_, reads=(), writes=(), semkey=None, eng="sp", **kw):
        kw = dict(kw)
        kw["out"] = out
        kw["in_"] = in_
        return self.op(eng, "dma_start", (), kw, reads, writes, dma=True, semkey=semkey)

    def barrier(self, scratch_ap):
        allr = list(self.all_res)
        o = self.op("dve", "memset", (scratch_ap, 0.0), None, reads=allr, writes=allr)
        self.last_barrier = o
        return o

    def emit(self):
        nc = self.nc
        esem = {e: self.es.enter_context(nc.semaphore(f"e_{e}")) for e in ENGS if e != "sp"}
        for e in ENGS:
            c = 0
            for o in self.ops[e]:
                if o.dma:
                    continue
                if o.signal:
                    c += 1
                    o.value = c
        ops = self.ops
        dma_sems = self.dma_sems

        def run(e, engobj):
            waited = {}
            for o in ops[e]:
                need = {}
                for d in o.deps:
                    if d.dma:
                        key = ("d", d.semkey)
                        h = dma_sems[d.semkey][0]
                    else:
                        key = ("e", d.eng)
                        h = esem[d.eng]
                    if key not in need or need[key][1] < d.value:
                        need[key] = (h, d.value)
                for key, (h, v) in need.items():
                    if waited.get(key, 0) >= v:
                        continue
                    waited[key] = v
                    engobj.wait_ge(h, v)
                meth, a, kw = o.fn
                if meth is None:
                    continue
                ins = getattr(engobj, meth)(*a, **kw)
                if o.dma:
                    ins.then_inc(dma_sems[o.semkey][0], 16)
                elif o.signal:
                    ins.then_inc(esem[e], 1)

        with nc.Block() as block:
            @block.tensor
            def _(eng):
                run("pe", eng)

            @block.scalar
            def _(eng):
                run("act", eng)

            @block.vector
            def _(eng):
                run("dve", eng)

            @block.gpsimd
            def _(eng):
                run("pool", eng)

            @block.sync
            def _(eng):
                run("sp", eng)


def _host_consts():
    f32 = np.float32
    t = np.arange(S, dtype=f32)
    inv_a = (f32(10000.0) ** (-np.arange(0, 64, 2, dtype=f32) / f32(64))).astype(f32)
    ang = (t[None, :] * inv_a[:, None]).astype(f32)
    p = np.arange(128)
    CA = np.cos(ang)[p % 32].astype(f32)
    sgn = np.where((p % 64) < 32, -1.0, 1.0).astype(f32)
    SA = (np.sin(ang)[p % 32] * sgn[:, None]).astype(f32)
    inv_r = (f32(1.0) / (f32(10000.0) ** np.linspace(0.0, 1.0, 128, dtype=f32))).astype(f32)
    angr = (t[None, :] * inv_r[:, None]).astype(f32)
    CR = np.cos(angr).astype(f32)
    SR = np.sin(angr).astype(f32)
    lg = np.log(f32(1.0) - np.exp2(f32(-5.0) - np.arange(4, dtype=f32))).astype(f32)
    idx = np.arange(128, dtype=f32)
    diff = idx[None, :] - idx[:, None]
    decT = np.zeros((128, 4, 128), f32)
    for h in range(4):
        decT[:, h, :] = np.where(diff >= 0, np.exp(lg[h] * np.maximum(diff, 0.0)), 0.0) / 16.0
    zeta = (np.exp(lg[None, :] * (127.0 - idx[:, None])) / 16.0).astype(f32)
    xi = np.exp(lg[:, None] * (idx[None, :] + 1.0)).astype(f32)
    xib = np.broadcast_to(xi[None], (128, 4, 128)).astype(f32)
    gamma_c = np.exp(lg * 128.0).astype(f32)
    ident = np.eye(128, dtype=f32)
    cf = {
        "CA": CA, "SA": SA, "CR": CR, "SR": SR,
        "decT": decT.reshape(128, 512), "zeta": zeta, "xib": xib.reshape(128, 512), "ident": ident,
    }
    partner = np.where((p % 64) < 32, p + 32, p - 32)
    perm = np.zeros((128, 128), f32)
    perm[partner, p] = 1.0
    bones = (p[:, None] // 64 == p[None, :] // 64).astype(f32)
    kq = np.arange(128)
    mcur = (kq[None, :] >= kq[:, None]).astype(f32)
    mprev = (kq[:, None] >= kq[None, :]).astype(f32)
    mcur4 = np.tile(mcur, (1, 4))
    mprev4 = np.tile(mprev, (1, 4))
    sel = np.zeros((128, 32, 128), f32)
    for e in range(32):
        sel[e, e, :] = 1.0
        sel[32 + e, e, :] = 1.0
    cb = {
        "perm": perm, "bones": bones, "mcur4": mcur4, "mprev4": mprev4,
        "identb": ident, "sel": sel.reshape(128, 4096),
    }
    return cf, cb, gamma_c


def _pack(dct, order, dtype):
    offs = {}
    cols = 0
    for k in order:
        offs[k] = (cols, dct[k].shape[1])
        cols += dct[k].shape[1]
    arr = np.zeros((128, cols), dtype=dtype)
    for k in order:
        a, n = offs[k]
        arr[:, a:a + n] = dct[k].astype(dtype)
    return arr, offs


CFS_ORDER = ["decT", "zeta", "xib", "ident"]
CBS_ORDER = ["perm", "bones", "mcur4", "mprev4", "identb"]


ARENA_BYTES = 152 * 1024


def build_nc(cfs_offs, cbs_offs, cfs_cols, cbs_cols, gamma_c, debug=False, stop_after=None):
    nc = bass.Bass("TRN2", target_bir_lowering=False)

    def din(name, shape, dt=F32):
        return nc.dram_tensor(name, list(shape), dt, kind="ExternalInput").ap()

    x_d = din("x", [S, D])
    gmix_d = din("g_norm_mix", [D])
    w_in_d = din("w_in", [D, 11264])
    bgate_d = din("b_merge_gate", [2 * D])
    gq_d = din("g_q", [64])
    gk_d = din("g_k", [64])
    watt_d = din("w_branch_att", [D, D])
    gret_d = din("g_ret_norm", [4, 512])
    wret_d = din("w_branch_ret", [2 * D, D])
    wout_d = din("w_out", [D, D])
    gffn_d = din("g_norm_ffn", [D])
    wrg_d = din("w_router_group", [D, 4])
    brg_d = din("b_router_group", [4])
    wre_d = din("w_router_expert", [D, 32])
    bre_d = din("b_router_expert", [32])
    w1_d = din("w1", [32, D, 256])
    w3_d = din("w3", [32, D, 256])
    w2_d = din("w2", [32, 256, D])
    cfs_d = din("cfs", [128, cfs_cols])
    cbs_d = din("cbs", [128, cbs_cols], BF16)
    catt_d = din("catt", [128, 2 * S])
    cret_d = din("cret", [128, 2 * S])
    csel_d = din("csel", [128, 4096], BF16)
    out_d = nc.dram_tensor("out", [S, D], F32, kind="ExternalOutput").ap()
    skind = "ExternalOutput" if debug else "Internal"
    vx_d = nc.dram_tensor("vx", [S, 8, 256], BF16, kind=skind).ap()
    zatt_d = nc.dram_tensor("zatt", [D, S], BF16, kind=skind).ap()
    zret_d = nc.dram_tensor("zret", [2 * D, S], BF16, kind=skind).ap()
    dbg = {}
    if debug:
        dbg["xnT"] = nc.dram_tensor("d_xnT", [128, 8 * S], BF16, kind="ExternalOutput").ap()
        dbg["x1"] = nc.dram_tensor("d_x1", [S, D], F32, kind="ExternalOutput").ap()
        dbg["logits"] = nc.dram_tensor("d_logits", [128, NT * 36], F32, kind="ExternalOutput").ap()
        dbg["gate"] = nc.dram_tensor("d_gate", [128, NT * 32], F32, kind="ExternalOutput").ap()

    with ExitStack() as es:
        p = Prog(nc, es)
        ps = es.enter_context(nc.psum_tensor("ps", [128, 8, 512], F32))
        Rps = [p.res(f"psb{b}") for b in range(8)]
        p.excl.update(id(r) for r in Rps)

        def psb(b):
            return ps[:, b, :].bitcast(BF16)

        def OP(eng, meth, *a, reads=(), writes=(), **kw):
            return p.op(eng, meth, a, kw, reads, writes)

        def MM(out, lhsT, rhs, start, stop, reads, writes, **kw):
            return p.op("pe", "matmul", (out,), dict(lhsT=lhsT, rhs=rhs, start=start, stop=stop, **kw), reads, writes)

        arena = p.sb("arena", [128, ARENA_BYTES // 2], BF16)
        apos = [0]

        def areset(pos=0):
            apos[0] = pos

        def alloc(shape, dt):
            n = 1
            for s_ in shape[1:]:
                n *= s_
            nbytes = n * (4 if dt == F32 else 2)
            nbytes = (nbytes + 63) // 64 * 64
            a = apos[0]
            assert a + nbytes <= ARENA_BYTES, ("arena overflow", a, nbytes)
            apos[0] = a + nbytes
            v = arena[0:shape[0], a // 2:(a + n * (4 if dt == F32 else 2)) // 2]
            if dt == F32:
                v = v.bitcast(F32)
            if len(shape) == 3:
                v = v.rearrange("p (a b) -> p a b", a=shape[1])
            elif len(shape) == 4:
                v = v.rearrange("p (a b c) -> p a b c", a=shape[1], b=shape[2])
            return v

        cfs = p.sb("cfs_sb", [128, cfs_cols], F32)
        cbs = p.sb("cbs_sb", [128, cbs_cols], BF16)
        Rc = p.res("consts")
        Rcs = []

        def cres():
            r = p.res()
            Rcs.append(r)
            return [r]

        def CF(k):
            a, n = cfs_offs[k]
            return cfs[:, a:a + n]

        def CB(k):
            a, n = cbs_offs[k]
            return cbs[:, a:a + n]

        p.dma(cfs[:], cfs_d, writes=cres(), semkey="c")
        p.dma(cbs[:], cbs_d, writes=cres(), semkey="c")
        gmix = p.sb("gmix", [128, 8], F32)
        gffn = p.sb("gffn", [128, 8], F32)
        bga = p.sb("bga", [128, 8], F32)
        bgb = p.sb("bgb", [128, 8], F32)
        gqk = p.sb("gqk", [128, 4], F32)
        epsc = p.sb("epsc", [128, 1], F32)
        scratch = p.sb("scratch", [128, 1], F32)
        p.dma(gmix[:], gmix_d.rearrange("(k p) -> p k", p=128), writes=cres(), semkey="c", allow_slow_non_contiguous=True)
        p.dma(gffn[:], gffn_d.rearrange("(k p) -> p k", p=128), writes=cres(), semkey="c", allow_slow_non_contiguous=True)
        p.dma(bga[:], bgate_d[0:D].rearrange("(k p) -> p k", p=128), writes=cres(), semkey="c", allow_slow_non_contiguous=True)
        p.dma(bgb[:], bgate_d[D:2 * D].rearrange("(k p) -> p k", p=128), writes=cres(), semkey="c", allow_slow_non_contiguous=True)
        for ci, gd in ((0, gq_d), (2, gk_d)):
            for half in range(2):
                p.dma(gqk[half * 64:(half + 1) * 64, ci:ci + 1], gd.rearrange("(p o) -> p o", o=1), writes=cres(), semkey="c")
                for q4 in range(2):
                    p.dma(gqk[half * 64 + q4 * 32: half * 64 + q4 * 32 + 32, ci + 1:ci + 2],
                          gd[(1 - q4) * 32:(1 - q4) * 32 + 32].rearrange("(p o) -> p o", o=1), writes=cres(), semkey="c")
        OP("dve", "memset", epsc[:], EPS, reads=Rcs, writes=[Rc])

        ident = CF("ident")
        identb = CB("identb")
        perm, bones = CB("perm"), CB("bones")
        mcur4, mprev4 = CB("mcur4"), CB("mprev4")
        decT = CF("decT").rearrange("p (h i) -> p h i", h=4)
        zeta = CF("zeta")
        xib = CF("xib").rearrange("p (h i) -> p h i", h=4)

        Rfinal = []
        xnT = p.sb("xnT", [128, 8, S], BF16)
        RxnT = [p.res(f"xnT{t}") for t in range(NT)]
        ss = p.sb("ss", [128, NT], F32)
        rt = p.sb("rt", [128, NT], F32)
        rstd = p.sb("rstd", [128, NT], F32)
        Rxin = [p.res(f"xin{i}") for i in range(2)]
        xs = [p.sb(f"xs{i}", [128, D], F32) for i in range(2)]
        Rxs = [p.res(f"xs{i}") for i in range(2)]
        junk = p.sb("junk", [128, D], BF16)
        Rjunk = p.res("junk")
        logit = p.sb("logit", [128, NT, 36], F32)
        Rlogit = [p.res(f"logit{t}") for t in range(NT)]

        def norm_tile_gen(src, Rsrc, t, gcol, Rstat_t, banks, lo=None, stats_done=False):
            i = t % 2
            if not stats_done:
                OP("act", "activation", out=junk[:], in_=src, func=AF.Square, accum_out=ss[:, t:t + 1],
                   reads=[Rsrc, Rc], writes=[Rjunk, Rstat_t])
                OP("act", "activation", out=rt[:, t:t + 1], in_=ss[:, t:t + 1], func=AF.Sqrt, scale=1.0 / D, bias=epsc[:],
                   reads=[Rstat_t, Rc], writes=[Rstat_t])
                OP("dve", "reciprocal", out=rstd[:, t:t + 1], in_=rt[:, t:t + 1], reads=[Rstat_t], writes=[Rstat_t])
            OP("dve", "tensor_scalar", out=xs[i][:], in0=src, scalar1=rstd[:, t:t + 1], scalar2=None, op0=ALU.mult,
               reads=[Rsrc, Rstat_t], writes=[Rxs[i]])
            yield
            for hb in range(2):
                b = banks[hb]
                for j in range(4):
                    k = hb * 4 + j
                    OP("pe", "transpose", out=ps[:, b, j * 128:(j + 1) * 128], in_=xs[i][:, k * 128:(k + 1) * 128],
                       identity=ident, reads=[Rxs[i], Rc], writes=[Rps[b]])
                gb_ = gcol[:, hb * 4:hb * 4 + 4].unsqueeze(2).to_broadcast([128, 4, 128])
                pin = ps[:, b, :].rearrange("p (j c) -> p j c", j=4)
                dsl = xnT[:, hb * 4:hb * 4 + 4, t * 128:(t + 1) * 128]
                if lo is None:
                    OP("dve", "tensor_tensor", out=dsl, in0=pin, in1=gb_, op=ALU.mult,
                       reads=[Rps[b], Rc], writes=[RxnT[t]])
                else:
                    vtmp, Rvtmp, lodst, Rlo = lo
                    OP("dve", "tensor_tensor", out=vtmp[:, hb * 4:hb * 4 + 4, :], in0=pin, in1=gb_, op=ALU.mult,
                       reads=[Rps[b], Rc], writes=[Rvtmp[hb]])
                    OP("act", "activation", out=dsl, in_=vtmp[:, hb * 4:hb * 4 + 4, :], func=AF.Copy,
                       reads=[Rvtmp[hb]], writes=[RxnT[t]])
                    OP("pool", "tensor_tensor", out=lodst[:, hb * 4:hb * 4 + 4, :], in0=vtmp[:, hb * 4:hb * 4 + 4, :],
                       in1=dsl, op=ALU.subtract, reads=[Rvtmp[hb], RxnT[t]], writes=[Rlo[hb]])
            yield

        def finish():
            p.op("sp", None, reads=list(Rfinal))
            p.emit()
            return nc

        def wsrc(wd, c0, c1):
            return wd[:, c0:c1].rearrange("(k p) n -> p k n", p=128)

        def load_w(dst_ap, src_ap, R, key):
            return p.dma(dst_ap, src_ap, writes=[R], semkey=key, eng="pool")

        Rstat = [p.res(f"stat{t}") for t in range(NT)]
        areset()
        xin = [alloc([128, D], F32) for i in range(4)]
        Rxin = [p.res(f"xin{i}") for i in range(4)]
        def vproj_tile(t):
            i = t % 2
            for cg in range(2):
                b = 4 + cg
                for k in range(8):
                    MM(ps[:, b, :], xnT[:, k, t * 128:(t + 1) * 128], wbig[cg][:, k, :], k == 0, k == 7,
                       [RxnT[t], Rwbig[cg]], [Rps[b]])
                pv = ps[:, b, :].rearrange("p (pr e d) -> p pr e d", e=2, d=64)
                OP("act", "activation", out=vst[i][:, cg * 4:(cg + 1) * 4, 0:64], in_=pv[:, :, 0, :], func=AF.Copy,
                   reads=[Rps[b]], writes=[Rvst[i]])
                OP("dve", "tensor_copy", out=vst[i][:, cg * 4:(cg + 1) * 4, 192:256], in_=pv[:, :, 1, :],
                   reads=[Rps[b]], writes=[Rvst[i]])
            p.dma(vx_d[t * 128:(t + 1) * 128, :, :], vst[i], reads=[Rvst[i]], writes=[Rvx[t]], semkey=("vx", t % 2))

        wbig = [alloc([128, 8, 512], BF16) for i in range(2)]
        Rwbig = [p.res(f"wbig{i}") for i in range(2)]
        vst = [alloc([128, 8, 256], BF16) for i in range(2)]
        Rvst = [p.res(f"vst{i}") for i in range(2)]
        Rvx = [p.res(f"vx{t}") for t in range(NT)]
        for i in range(2):
            OP("pool", "memset", vst[i], 1.0, writes=[Rvst[i]])
        for cg in range(2):
            load_w(wbig[cg], wsrc(w_in_d, 2048 + cg * 512, 2048 + (cg + 1) * 512), Rwbig[cg], ("wbig", cg))
        gens = {}
        do_v = stop_after != "A"
        for t in range(min(3, NT)):
            p.dma(xin[t % 4], x_d[t * 128:(t + 1) * 128, :], writes=[Rxin[t % 4]], semkey=("xin", t % 4))
        for step in range(NT + 2):
            if step < NT:
                t = step
                if t + 3 < NT:
                    p.dma(xin[(t + 3) % 4], x_d[(t + 3) * 128:(t + 4) * 128, :], writes=[Rxin[(t + 3) % 4]],
                          semkey=("xin", (t + 3) % 4))
                gens[t] = norm_tile_gen(xin[t % 4], Rxin[t % 4], t, gmix, Rstat[t], (6, 7))
                next(gens[t])
            if 0 <= step - 1 < NT:
                next(gens[step - 1])
            if do_v and 0 <= step - 2 < NT:
                vproj_tile(step - 2)
        if debug:
            Rfinal.append(p.res())
            p.dma(dbg["xnT"], xnT[:].rearrange("p k s -> p (k s)"), reads=RxnT, writes=[Rfinal[-1]], semkey="dbg0")
        if stop_after == "A":
            Rfinal.append(p.res("fin"))
            p.dma(out_d[0:128, :], xin[1], reads=[Rxin[1]], writes=[Rfinal[-1]], semkey="fin")
            return finish()

        if stop_after == "V":
            Rfinal.extend(Rvx)
            return finish()

        p.barrier(scratch[:])
        areset()
        catt = alloc([128, 2 * S], F32)
        Rcatt = p.res("catt")
        p.dma(catt, catt_d, writes=[Rcatt], semkey="catt")
        CA, SA = catt[:, 0:S], catt[:, S:2 * S]
        wqk = [[alloc([128, 8, 128], BF16) for j in range(2)] for i in range(2)]
        Rwqk = [[p.res(f"wqk{i}_{j}") for j in range(2)] for i in range(2)]
        QKT = [[alloc([128, S], BF16) for j in range(2)] for i in range(2)]
        RQK = [[[p.res(f"QK{i}_{j}_{g}") for g in range(NG)] for j in range(2)] for i in range(2)]
        vxs = [[alloc([128, 16, 256], BF16) for o in range(3)] for i in range(2)]
        Rvxs = [[p.res(f"vxs{i}_{o}") for o in range(3)] for i in range(2)]
        qbf = alloc([128, 512], BF16)
        sqb = alloc([128, 512], BF16)
        rtq = alloc([128, 512], F32)
        rsq = alloc([128, 512], F32)
        t1 = alloc([128, 512], F32)
        t2 = alloc([128, 512], F32)
        Rqbf, Rsqb, Rrtq, Rrsq, Rt1, Rt2 = (p.res(n) for n in ("qbf", "sqb", "rtq", "rsq", "t1", "t2"))
        Pb = [alloc([128, 512], BF16) for i in range(3)]
        RPb = [p.res(f"Pb{i}") for i in range(3)]
        rden = alloc([128, S], F32)
        Rrden = [p.res(f"rden{b}") for b in range(4)]
        zpair = [alloc([128, S], BF16) for i in range(2)]
        Rzpair = [p.res(f"zpair{i}") for i in range(2)]
        Rzatt = [p.res(f"zatt{i}") for i in range(8)]

        def load_pair_weights(pr):
            i = pr % 2
            load_w(wqk[i][0], wsrc(w_in_d, pr * 128, (pr + 1) * 128), Rwqk[i][0], ("wqk", i, 0))
            load_w(wqk[i][1], wsrc(w_in_d, 1024 + pr * 128, 1024 + (pr + 1) * 128), Rwqk[i][1], ("wqk", i, 1))

        def load_pair_v(pr):
            i = pr % 2
            src = vx_d[:, pr, :]
            p.dma(vxs[i][0], src.rearrange("(n i) m -> i n m", i=128), reads=Rvx, writes=[Rvxs[i][0]], semkey=("vxs", i, 0))
            s1 = src.rearrange("(n i c) m -> c i n m", i=128, c=4)
            for c in range(4):
                p.dma(vxs[i][1][:, c * 4:(c + 1) * 4, :], s1[c], reads=Rvx, writes=[Rvxs[i][1]], semkey=("vxs", i, 1))
            p.dma(vxs[i][2], src.rearrange("(i c) m -> i c m", c=16), reads=Rvx, writes=[Rvxs[i][2]], semkey=("vxs", i, 2))

        def colap(T, spec, r0):
            base, r, c = spec
            if r == 1:
                return T[r0:r0 + 64, base:base + 128]
            return T[r0:r0 + 64, base:base + 128 * r].rearrange("p (n r) -> p n r", r=r)[:, :, c]

        def prep_gen(pr):
            i = pr % 2
            for j, gi in ((0, 0), (1, 2)):
                dstT = QKT[i][j]
                for g in range(NG):
                    gs = slice(g * 512, (g + 1) * 512)
                    for k in range(8):
                        MM(ps[:, 6, :], wqk[i][j][:, k, :], xnT[:, k, gs], k == 0, k == 7,
                           [Rwqk[i][j]] + RxnT[g * 4:(g + 1) * 4], [Rps[6]])
                    OP("act", "activation", out=qbf, in_=ps[:, 6, :], func=AF.Copy, reads=[Rps[6]], writes=[Rqbf])
                    OP("dve", "scalar_tensor_tensor", out=t1, in0=ps[:, 6, :], scalar=gqk[:, gi:gi + 1], in1=CA[:, gs],
                       op0=ALU.mult, op1=ALU.mult, reads=[Rps[6], Rc, Rcatt], writes=[Rt1])
                    OP("pool", "tensor_tensor", out=sqb, in0=qbf, in1=qbf, op=ALU.mult, reads=[Rqbf], writes=[Rsqb])
                    yield
                    MM(ps[:, 7, :], bones, sqb, True, True, [Rsqb, Rc], [Rps[7]])
                    MM(ps[:, 6, :], perm, qbf, True, True, [Rqbf, Rc], [Rps[6]])
                    OP("act", "activation", out=rtq, in_=ps[:, 7, :], func=AF.Ln, scale=1.0 / 64, bias=epsc[:],
                       reads=[Rps[7], Rc], writes=[Rrtq])
                    OP("act", "activation", out=rsq, in_=rtq, func=AF.Exp, scale=-0.5, reads=[Rrtq], writes=[Rrsq])
                    OP("dve", "scalar_tensor_tensor", out=t2, in0=ps[:, 6, :], scalar=gqk[:, gi + 1:gi + 2], in1=SA[:, gs],
                       op0=ALU.mult, op1=ALU.mult, reads=[Rps[6], Rc, Rcatt], writes=[Rt2])
                    OP("pool", "tensor_tensor", out=t1, in0=t1, in1=t2, op=ALU.add, reads=[Rt1, Rt2], writes=[Rt1])
                    OP("dve", "tensor_tensor", out=dstT[:, gs], in0=t1, in1=rsq, op=ALU.mult,
                       reads=[Rt1, Rrsq], writes=[RQK[i][j][g]])
                    yield

        def att_batches(pr):
            def cols(r, c, n):
                return (n * 128 * r, r, c)
            tiles_cur, tiles_prev = [], []
            for n in range(16):
                tiles_cur.append((cols(1, 0, n), cols(1, 0, n), 0, n, ("n", n // 4, (n % 4) * 128, 1), n % 4 == 0))
            for c in range(4):
                for n in range(4):
                    tiles_cur.append((cols(4, c, n), cols(4, c, n), 1, c * 4 + n, ("n", n, c, 4), False))
            for c in range(16):
                tiles_cur.append((cols(16, c, 0), cols(16, c, 0), 2, c, ("s", c), False))
            for n in range(1, 16):
                tiles_prev.append((cols(1, 0, n - 1), cols(1, 0, n), 0, n - 1, ("n", n // 4, (n % 4) * 128, 1), False))
            for c in range(4):
                for n in range(1, 4):
                    tiles_prev.append((cols(4, c, n - 1), cols(4, c, n), 1, c * 4 + n - 1, ("n", n, c, 4), False))
            batches = []
            for lst, msk in ((tiles_cur, mcur4), (tiles_prev, mprev4)):
                for s0 in range(0, len(lst), 4):
                    batches.append((lst[s0:s0 + 4], msk))
            items = []
            for eh in range(2):
                for bi, (tl, msk) in enumerate(batches):
                    items.append((eh, bi, tl, msk, bi == len(batches) - 1))
            return items

        gcount = [0]

        def emit_S(pr, item):
            i = pr % 2
            eh, bi, tl, msk, last = item
            r0 = eh * 64
            gb = gcount[0]
            gcount[0] += 1
            sb_ = 4 + (gb % 2)
            pbi = gb % 3
            nt_ = len(tl)
            QT, KT = QKT[i]
            RQKall = RQK[i][0] + RQK[i][1]
            for ti, (kc, qc, vo, vb, osp, st) in enumerate(tl):
                MM(ps[:, sb_, ti * 128:(ti + 1) * 128], colap(KT, kc, r0), colap(QT, qc, r0), True, True,
                   RQKall, [Rps[sb_]])
            OP("act", "activation", out=Pb[pbi][:, 0:nt_ * 128], in_=ps[:, sb_, 0:nt_ * 128], func=AF.Exp, scale=0.125,
               reads=[Rps[sb_]], writes=[RPb[pbi]])
            OP("dve", "tensor_tensor", out=Pb[pbi][:, 0:nt_ * 128], in0=Pb[pbi][:, 0:nt_ * 128],
               in1=msk[:, 0:nt_ * 128], op=ALU.mult, reads=[RPb[pbi], Rc], writes=[RPb[pbi]])
            return pbi

        def emit_PV(pr, item, pbi):
            i = pr % 2
            eh, bi, tl, msk, last = item
            r0 = eh * 64
            zp = zpair[i]
            for ti, (kc, qc, vo, vb, osp, st) in enumerate(tl):
                lhsT = vxs[i][vo][:, vb, eh * 128:(eh + 1) * 128]
                if osp[0] == "n":
                    _, bank, off, r = osp
                    if r == 1:
                        oap = ps[:, bank, off:off + 128]
                    else:
                        oap = ps[:, bank, :].rearrange("p (n r) -> p n r", r=r)[:, :, off]
                    MM(oap, lhsT, Pb[pbi][:, ti * 128:(ti + 1) * 128], st, False,
                       [RPb[pbi], Rvxs[i][vo]], [Rps[bank]], skip_group_check=True)
                else:
                    c = osp[1]
                    for bank in range(4):
                        oap = ps[:, bank, :].rearrange("p (n r) -> p n r", r=16)[:, :, c]
                        MM(oap, lhsT, Pb[pbi][:, ti * 128 + bank * 32: ti * 128 + bank * 32 + 32], False, False,
                           [RPb[pbi], Rvxs[i][vo]], [Rps[bank]], skip_group_check=True)
            if last:
                d0 = 64 - r0
                for bank in range(4):
                    bs = slice(bank * 512, (bank + 1) * 512)
                    OP("act", "activation", out=rden[r0:r0 + 64, bs], in_=ps[d0:d0 + 64, bank, :], func=AF.Ln,
                       reads=[Rps[bank]], writes=[Rrden[bank]])
                    OP("act", "activation", out=rden[r0:r0 + 64, bs], in_=rden[r0:r0 + 64, bs], func=AF.Exp, scale=-1.0,
                       reads=[Rrden[bank]], writes=[Rrden[bank]])
                    OP("dve", "tensor_tensor", out=zp[r0:r0 + 64, bs], in0=ps[r0:r0 + 64, bank, :],
                       in1=rden[r0:r0 + 64, bs], op=ALU.mult, reads=[Rps[bank], Rrden[bank]], writes=[Rzpair[i]])

        load_pair_weights(0)
        load_pair_weights(1)
        load_pair_v(0)
        for _ in prep_gen(0):
            pass
        for pr in range(8):
            i = pr % 2
            if pr + 1 < 8:
                load_pair_v(pr + 1)
            nxt = prep_gen(pr + 1) if pr + 1 < 8 else None
            items = att_batches(pr)
            pq = [emit_S(pr, items[0]), emit_S(pr, items[1])]
            for ii, item in enumerate(items):
                if ii + 2 < len(items):
                    pq.append(emit_S(pr, items[ii + 2]))
                emit_PV(pr, item, pq.pop(0))
                if nxt is not None and ii % 2 == 1:
                    next(nxt, None)
            if nxt is not None:
                for _ in nxt:
                    pass
            if pr + 2 < 8:
                load_pair_weights(pr + 2)
            p.dma(zatt_d[pr * 128:(pr + 1) * 128, :], zpair[i], reads=[Rzpair[i]], writes=[Rzatt[pr]], semkey=("zatt", i))
        if stop_after == "ATT":
            Rfinal.extend(Rzatt)
            return finish()

        p.barrier(scratch[:])
        areset()
        cret = alloc([128, 2 * S], F32)
        Rcret = p.res("cret")
        p.dma(cret, cret_d, writes=[Rcret], semkey="cret")
        CR, SR = cret[:, 0:S], cret[:, S:2 * S]
        gretb = alloc([128, 4, 512], F32)
        Rgretb = p.res("gretb")
        p.dma(gretb.rearrange("p h v -> p (h v)"), gret_d.rearrange("h v -> (h v)").partition_broadcast(128),
              writes=[Rgretb], semkey="gretb")
        wq_r = [alloc([128, 8, 256], BF16) for i in range(2)]
        wk_r = [alloc([128, 8, 256], BF16) for i in range(2)]
        wv_r = [alloc([128, 8, 512], BF16) for i in range(2)]
        wg_r = [alloc([128, 8, 512], BF16) for i in range(2)]
        Rwr = [[p.res(f"wr{i}_{j}") for j in range(4)] for i in range(2)]
        QrT = [alloc([128, 2, 512], BF16) for i in range(2)]
        KrT = [alloc([128, 2, 512], BF16) for i in range(2)]
        Qxi = [alloc([128, 2, 512], BF16) for i in range(2)]
        RQr = [p.res(f"QrT{i}") for i in range(2)]
        RKr = [p.res(f"KrT{i}") for i in range(2)]
        RQx = [p.res(f"Qxi{i}") for i in range(2)]
        sgr = [alloc([128, 4, 512], BF16) for i in range(2)]
        Rsgr = [p.res(f"sgr{i}") for i in range(2)]
        vr_sb = [alloc([128, 512], BF16) for i in range(2)]
        Rvr = [p.res(f"vr{i}") for i in range(2)]
        kz = [alloc([128, 256], BF16) for i in range(2)]
        Rkz = [p.res(f"kz{i}") for i in range(2)]
        inm = [alloc([128, 128], BF16) for i in range(2)]
        Rinm = [p.res(f"inm{i}") for i in range(2)]
        yn = [alloc([128, 512], BF16) for i in range(2)]
        Ryn = [p.res(f"yn{i}") for i in range(2)]
        zst = [alloc([128, 4, 512], BF16) for i in range(2)]
        Rzst = [p.res(f"zst{i}") for i in range(2)]
        state = alloc([128, 2, 512], F32)
        state_bf = alloc([128, 2, 512], BF16)
        Rstate = [p.res(f"state{c}") for c in range(2)]
        Rstbf = [p.res(f"stbf{c}") for c in range(2)]
        tmp = [alloc([128, 512], F32) for i in range(4)]
        Rtmp = [p.res(f"rtmp{i}") for i in range(4)]
        ssr = alloc([128, 64], F32)
        rtr = alloc([128, 64], F32)
        rsr = alloc([128, 64], F32)
        Rzret = []

        def load_head_weights(h):
            i = h % 2
            load_w(wq_r[i], wsrc(w_in_d, 3072 + h * 256, 3072 + (h + 1) * 256), Rwr[i][0], ("wr", i, 0))
            load_w(wk_r[i], wsrc(w_in_d, 4096 + h * 256, 4096 + (h + 1) * 256), Rwr[i][1], ("wr", i, 1))
            load_w(wv_r[i], wsrc(w_in_d, 5120 + h * 512, 5120 + (h + 1) * 512), Rwr[i][2], ("wr", i, 2))
            load_w(wg_r[i], wsrc(w_in_d, 7168 + h * 512, 7168 + (h + 1) * 512), Rwr[i][3], ("wr", i, 3))

        vr3 = vr_sb + [alloc([128, 512], BF16)]
        Rvr3 = Rvr + [p.res("vr2")]
        kz3 = kz + [alloc([128, 256], BF16)]
        Rkz3 = Rkz + [p.res("kz2")]
        inm3 = inm + [alloc([128, 128], BF16)]
        Rinm3 = Rinm + [p.res("inm2")]
        INNER = ps[:, 3, 0:128]
        ysb = [alloc([128, 512], F32) for i in range(2)]
        Rysb = [p.res(f"ysb{i}") for i in range(2)]

        def proj_pieces(h, g):
            wi = h % 2
            gi = g % 2
            gs = slice(g * 512, (g + 1) * 512)
            RxG = RxnT[g * 4:(g + 1) * 4]

            def qk(which):
                wsb = (wq_r, wk_r)[which][wi]
                dst = (QrT, KrT)[which][gi]
                Rdst = (RQr, RKr)[which][gi]
                for c in range(2):
                    for k in range(8):
                        MM(ps[:, c, :], wsb[:, k, c * 128:(c + 1) * 128], xnT[:, k, gs], k == 0, k == 7,
                           [Rwr[wi][which]] + RxG, [Rps[c]])
                OP("dve", "tensor_tensor", out=tmp[0], in0=ps[:, 0, :], in1=CR[:, gs], op=ALU.mult,
                   reads=[Rps[0], Rcret], writes=[Rtmp[0]])
                OP("dve", "tensor_tensor", out=tmp[1], in0=ps[:, 1, :], in1=SR[:, gs], op=ALU.mult,
                   reads=[Rps[1], Rcret], writes=[Rtmp[1]])
                OP("pool", "tensor_tensor", out=dst[:, 0, :], in0=tmp[0], in1=tmp[1], op=ALU.subtract,
                   reads=[Rtmp[0], Rtmp[1]], writes=[Rdst])
                OP("dve", "tensor_tensor", out=tmp[2], in0=ps[:, 1, :], in1=CR[:, gs], op=ALU.mult,
                   reads=[Rps[1], Rcret], writes=[Rtmp[2]])
                OP("dve", "tensor_tensor", out=tmp[3], in0=ps[:, 0, :], in1=SR[:, gs], op=ALU.mult,
                   reads=[Rps[0], Rcret], writes=[Rtmp[3]])
                OP("pool", "tensor_tensor", out=dst[:, 1, :], in0=tmp[2], in1=tmp[3], op=ALU.add,
                   reads=[Rtmp[2], Rtmp[3]], writes=[Rdst])
                if which == 0:
                    xb = xib[:, h, :].unsqueeze(1).to_broadcast([128, 4, 128])
                    for c in range(2):
                        OP("pool", "tensor_tensor", out=Qxi[gi][:, c, :].rearrange("p (n i) -> p n i", n=4),
                           in0=dst[:, c, :].rearrange("p (n i) -> p n i", n=4), in1=xb, op=ALU.mult,
                           reads=[Rdst, Rc], writes=[RQx[gi]])

            def gate(vc):
                b = vc % 2
                for k in range(8):
                    MM(ps[:, b, :], wg_r[wi][:, k, vc * 128:(vc + 1) * 128], xnT[:, k, gs], k == 0, k == 7,
                       [Rwr[wi][3]] + RxG, [Rps[b]])
                OP("act", "activation", out=sgr[gi][:, vc, :], in_=ps[:, b, :], func=AF.Silu,
                   reads=[Rps[b]], writes=[Rsgr[gi]])

            return [lambda: qk(0), lambda: qk(1), lambda: gate(0), lambda: gate(1), lambda: gate(2), lambda: gate(3)]

        def stage_A(q):
            h, gn = q // 16, q % 16
            g, n = gn // 4, gn % 4
            wi, gi, b3 = h % 2, g % 2, q % 3
            cs = slice(n * 128, (n + 1) * 128)
            for k in range(8):
                MM(ps[:, 2, :], xnT[:, k, gn * 128:(gn + 1) * 128], wv_r[wi][:, k, :], k == 0, k == 7,
                   [Rwr[wi][2], RxnT[gn]], [Rps[2]])
            OP("act", "activation", out=vr3[b3], in_=ps[:, 2, :], func=AF.Copy, reads=[Rps[2]], writes=[Rvr3[b3]])
            for c in range(2):
                OP("pe", "transpose", out=psb(4)[:, 512 + c * 128: 512 + (c + 1) * 128], in_=KrT[gi][:, c, cs],
                   identity=identb, reads=[RKr[gi], Rc], writes=[Rps[4]])
            OP("act", "activation", out=kz3[b3], in_=psb(4)[:, 512:768], func=AF.Copy, scale=zeta[:, h:h + 1],
               reads=[Rps[4], Rc], writes=[Rkz3[b3]])
            for c in range(2):
                MM(INNER, KrT[gi][:, c, cs], QrT[gi][:, c, cs], c == 0, c == 1, [RKr[gi], RQr[gi]], [Rps[3]])
            OP("dve", "tensor_tensor", out=inm3[b3], in0=INNER, in1=decT[:, h, :], op=ALU.mult,
               reads=[Rps[3], Rc], writes=[Rinm3[b3]])

        Rst_q = {}

        def stage_B(q):
            h, gn = q // 16, q % 16
            g, n = gn // 4, gn % 4
            gi, b3, ci = g % 2, q % 3, q % 2
            cs = slice(n * 128, (n + 1) * 128)
            yb = 5
            st_i = q
            gC = float(gamma_c[h])
            MM(ps[:, yb, :], inm3[b3], vr3[b3], True, gn == 0, [Rinm3[b3], Rvr3[b3]], [Rps[yb]])
            if gn > 0:
                for c in range(2):
                    MM(ps[:, yb, :], Qxi[gi][:, c, cs], state_bf[:, c, :], False, c == 1, [RQx[gi], Rstbf[c]], [Rps[yb]])
            if gn < 15:
                for c in range(2):
                    MM(ps[:, 6 + c, :], kz3[b3][:, c * 128:(c + 1) * 128], vr3[b3], True, True,
                       [Rkz3[b3], Rvr3[b3]], [Rps[6 + c]])
                    if gn == 0:
                        OP("dve", "tensor_copy", out=state[:, c, :], in_=ps[:, 6 + c, :],
                           reads=[Rps[6 + c]], writes=[Rstate[c]])
                    else:
                        OP("dve", "scalar_tensor_tensor", out=state[:, c, :], in0=state[:, c, :], scalar=gC,
                           in1=ps[:, 6 + c, :], op0=ALU.mult, op1=ALU.add,
                           reads=[Rps[6 + c], Rstate[c]], writes=[Rstate[c]])
                    OP("pool", "tensor_copy", out=state_bf[:, c, :], in_=state[:, c, :],
                       reads=[Rstate[c]], writes=[Rstbf[c]])
            Rst = p.res()
            OP("act", "activation", out=ysb[ci], in_=ps[:, yb, :], func=AF.Copy, reads=[Rps[yb]], writes=[Rysb[ci]])
            OP("act", "activation", out=junk[:, 0:512], in_=ysb[ci], func=AF.Square,
               accum_out=ssr[:, st_i:st_i + 1], reads=[Rysb[ci]], writes=[Rjunk, Rst])
            OP("act", "activation", out=rtr[:, st_i:st_i + 1], in_=ssr[:, st_i:st_i + 1], func=AF.Sqrt,
               scale=1.0 / 512, bias=epsc[:], reads=[Rst, Rc], writes=[Rst])
            OP("dve", "reciprocal", out=rsr[:, st_i:st_i + 1], in_=rtr[:, st_i:st_i + 1], reads=[Rst], writes=[Rst])
            OP("act", "activation", out=yn[ci], in_=ysb[ci], func=AF.Copy, scale=rsr[:, st_i:st_i + 1],
               reads=[Rysb[ci], Rst], writes=[Ryn[ci]])
            OP("pool", "tensor_tensor", out=yn[ci], in0=yn[ci], in1=gretb[:, h, :], op=ALU.mult,
               reads=[Ryn[ci], Rgretb], writes=[Ryn[ci]])

        def stage_C(q):
            h, gn = q // 16, q % 16
            g, n = gn // 4, gn % 4
            gi, ci = g % 2, q % 2
            cs = slice(n * 128, (n + 1) * 128)
            gs = slice(g * 512, (g + 1) * 512)
            for vc in range(4):
                OP("pe", "transpose", out=psb(4)[:, vc * 128:(vc + 1) * 128], in_=yn[ci][:, vc * 128:(vc + 1) * 128],
                   identity=identb, reads=[Ryn[ci], Rc], writes=[Rps[4]])
            OP("dve", "tensor_tensor", out=zst[gi][:, :, cs], in0=psb(4)[:, 0:512].rearrange("p (v i) -> p v i", v=4),
               in1=sgr[gi][:, :, cs], op=ALU.mult, reads=[Rps[4], Rsgr[gi]], writes=[Rzst[gi]])
            if n == 3:
                Rz = p.res()
                Rzret.append(Rz)
                p.dma(zret_d[h * 512:(h + 1) * 512, gs].rearrange("(vc p) s -> p vc s", p=128), zst[gi],
                      reads=[Rzst[gi]], writes=[Rz], semkey=("zret", gi))

        load_head_weights(0)
        load_head_weights(1)
        units = [(h, g) for h in range(4) for g in range(NG)]
        for f in proj_pieces(*units[0]):
            f()
        pending = []
        for s_ in range(64 + 2):
            if s_ < 64:
                u, n = s_ // 4, s_ % 4
                if n == 0:
                    pending = proj_pieces(*units[u + 1]) if u + 1 < 16 else []
                if s_ % 16 == 1 and 2 <= s_ // 16 + 1 < 4:
                    load_head_weights(s_ // 16 + 1)
                stage_A(s_)
            if 0 <= s_ - 1 < 64:
                stage_B(s_ - 1)
            if 0 <= s_ - 2 < 64:
                stage_C(s_ - 2)
            if s_ < 64:
                take = 1 if n < 2 else 2
                for f in pending[:take]:
                    f()
                pending = pending[take:]
        if stop_after == "RET":
            Rfinal.extend(Rzret + Rzatt)
            return finish()

        p.barrier(scratch[:])
        areset()
        yacc = alloc([128, NT, D], F32)
        Ryacc = [p.res(f"yacc{t}") for t in range(NT)]
        moe_base = apos[0]
        za = alloc([128, 8, 512], BF16)
        zr = alloc([128, 16, 512], BF16)
        Rza, Rzr = p.res("za"), p.res("zr")
        wa = [alloc([128, 8, 128], BF16) for i in range(2)]
        wr = [alloc([128, 16, 128], BF16) for i in range(2)]
        wga = [alloc([128, 8, 128], BF16) for i in range(2)]
        wgb = [alloc([128, 8, 128], BF16) for i in range(2)]
        Rwm = [[p.res(f"wm{i}_{j}") for j in range(4)] for i in range(2)]
        mT2 = [alloc([128, 8, 512], BF16) for i in range(2)]
        RmT2 = [[p.res(f"mT{i}_{k}") for k in range(8)] for i in range(2)]
        wout = alloc([128, 8, 512], BF16)
        Rwout = p.res("wout")
        sga = alloc([128, 512], F32)
        sgb = alloc([128, 512], F32)
        ta = alloc([128, 512], F32)
        tb = alloc([128, 512], F32)
        Rsga, Rsgb, Rta, Rtb = (p.res(n) for n in ("sga", "sgb", "ta", "tb"))
        vtmp = alloc([128, 8, 128], F32)
        Rvtmp = [p.res(f"vtmp{i}") for i in range(2)]
        xlo = [alloc([128, 8, 128], BF16) for i in range(2)]
        Rxlo = [[p.res(f"xlo{i}_{hb}") for hb in range(2)] for i in range(2)]
        wrf = alloc([128, 8, 36], F32)
        wrh = alloc([128, 8, 36], BF16)
        wrl = alloc([128, 8, 36], BF16)
        Rwrf, Rwrh, Rwrl = p.res("wrf"), p.res("wrh"), p.res("wrl")
        p.dma(wrf[:, :, 0:4], wrg_d.rearrange("(k p) n -> p k n", p=128), writes=[Rwrf], semkey="wrf")
        p.dma(wrf[:, :, 4:36], wre_d.rearrange("(k p) n -> p k n", p=128), writes=[Rwrf], semkey="wrf")
        OP("dve", "tensor_copy", out=wrh, in_=wrf, reads=[Rwrf], writes=[Rwrh])
        OP("dve", "tensor_tensor", out=wrl, in0=wrf, in1=wrh, op=ALU.subtract, reads=[Rwrf, Rwrh], writes=[Rwrl])

        def load_merge_weights(fc, slot):
            cs_ = slice(fc * 128, (fc + 1) * 128)
            load_w(wa[slot], watt_d[:, cs_].rearrange("(k p) n -> p k n", p=128), Rwm[slot][0], ("wm", slot, 0))
            load_w(wr[slot], wret_d[:, cs_].rearrange("(k p) n -> p k n", p=128), Rwm[slot][1], ("wm", slot, 1))
            load_w(wga[slot], wsrc(w_in_d, 9216 + fc * 128, 9216 + (fc + 1) * 128), Rwm[slot][2], ("wm", slot, 2))
            load_w(wgb[slot], wsrc(w_in_d, 10240 + fc * 128, 10240 + (fc + 1) * 128), Rwm[slot][3], ("wm", slot, 3))

        Rstat2 = [p.res(f"stat2_{t}") for t in range(NT)]

        def post_gen(g):
            mT = mT2[g % 2]
            RmT = RmT2[g % 2]
            for tt in range(4):
                gt = g * 4 + tt
                p.dma(yacc[:, gt, :], x_d[gt * 128:(gt + 1) * 128, :], writes=[Ryacc[gt]], semkey=("xres", gt))
            for half in range(2):
                load_w(wout, wsrc(wout_d, half * 512, (half + 1) * 512), Rwout, "wout")
                for tt in range(4):
                    gt = g * 4 + tt
                    b = 4 + (tt % 2)
                    for k in range(8):
                        MM(ps[:, b, :], mT[:, k, tt * 128:(tt + 1) * 128], wout[:, k, :], k == 0, k == 7,
                           [RmT[k], Rwout], [Rps[b]])
                    OP("dve", "tensor_tensor", out=yacc[:, gt, half * 512:(half + 1) * 512], in0=ps[:, b, :],
                       in1=yacc[:, gt, half * 512:(half + 1) * 512], op=ALU.add, reads=[Rps[b], Ryacc[gt]], writes=[Ryacc[gt]])
                    yield
            Rsg = Rstat2[g * 4:(g + 1) * 4]
            for tt in range(4):
                gt = g * 4 + tt
                OP("act", "activation", out=junk[:], in_=yacc[:, gt, :], func=AF.Square, accum_out=ss[:, gt:gt + 1],
                   reads=[Ryacc[gt], Rc], writes=[Rjunk, Rstat2[gt]])
            OP("act", "activation", out=rt[:, g * 4:(g + 1) * 4], in_=ss[:, g * 4:(g + 1) * 4], func=AF.Sqrt, scale=1.0 / D,
               bias=epsc[:], reads=Rsg + [Rc], writes=Rsg)
            OP("dve", "reciprocal", out=rstd[:, g * 4:(g + 1) * 4], in_=rt[:, g * 4:(g + 1) * 4], reads=Rsg, writes=Rsg)
            yield
            for tt in range(4):
                gt = g * 4 + tt
                i = gt % 2
                for _ in norm_tile_gen(yacc[:, gt, :], Ryacc[gt], gt, gffn, Rstat2[gt], (6, 7),
                                       lo=(vtmp, Rvtmp, xlo[i], Rxlo[i]), stats_done=True):
                    yield
                n_mm = 0
                for (lh, rw, Rl, Rw_) in ((xnT[:, :, gt * 128:(gt + 1) * 128], wrh, [RxnT[gt]], Rwrh),
                                          (xnT[:, :, gt * 128:(gt + 1) * 128], wrl, [RxnT[gt]], Rwrl),
                                          (xlo[i], wrh, Rxlo[i], Rwrh)):
                    for k in range(8):
                        MM(ps[:, 5, 0:36], lh[:, k, :], rw[:, k, :], n_mm == 0, n_mm == 23, Rl + [Rw_], [Rps[5]])
                        n_mm += 1
                OP("dve", "tensor_copy", out=logit[:, gt, :], in_=ps[:, 5, 0:36], reads=[Rps[5]], writes=[Rlogit[gt]])
                yield

        widx = 0
        load_merge_weights(0, 0)
        post = None
        for g in range(NG):
            gs = slice(g * 512, (g + 1) * 512)
            RxG = RxnT[g * 4:(g + 1) * 4]
            mT = mT2[g % 2]
            RmT = RmT2[g % 2]
            p.dma(za, zatt_d[:, gs].rearrange("(k p) s -> p k s", p=128), reads=Rzatt, writes=[Rza], semkey="za")
            p.dma(zr, zret_d[:, gs].rearrange("(k p) s -> p k s", p=128), reads=Rzret, writes=[Rzr], semkey="zr")
            for fc in range(8):
                slot = widx % 2
                widx += 1
                nfc, ng_ = (fc + 1) % 8, g + (1 if fc == 7 else 0)
                if ng_ < NG:
                    load_merge_weights(nfc, widx % 2)
                for k in range(8):
                    MM(ps[:, 2, :], wga[slot][:, k, :], xnT[:, k, gs], k == 0, k == 7, [Rwm[slot][2]] + RxG, [Rps[2]])
                if post is not None:
                    next(post, None)
                for k in range(8):
                    MM(ps[:, 3, :], wgb[slot][:, k, :], xnT[:, k, gs], k == 0, k == 7, [Rwm[slot][3]] + RxG, [Rps[3]])
                if post is not None:
                    next(post, None)
                for k in range(8):
                    MM(ps[:, 0, :], wa[slot][:, k, :], za[:, k, :], k == 0, k == 7, [Rwm[slot][0], Rza], [Rps[0]])
                if post is not None:
                    next(post, None)
                for k in range(16):
                    MM(ps[:, 1, :], wr[slot][:, k, :], zr[:, k, :], k == 0, k == 15, [Rwm[slot][1], Rzr], [Rps[1]])
                OP("act", "activation", out=sga, in_=ps[:, 2, :], func=AF.Sigmoid, bias=bga[:, fc:fc + 1],
                   reads=[Rps[2], Rc], writes=[Rsga])
                OP("act", "activation", out=sgb, in_=ps[:, 3, :], func=AF.Sigmoid, bias=bgb[:, fc:fc + 1],
                   reads=[Rps[3], Rc], writes=[Rsgb])
                OP("dve", "tensor_tensor", out=ta, in0=ps[:, 0, :], in1=sga, op=ALU.mult, reads=[Rps[0], Rsga], writes=[Rta])
                OP("dve", "tensor_tensor", out=tb, in0=ps[:, 1, :], in1=sgb, op=ALU.mult, reads=[Rps[1], Rsgb], writes=[Rtb])
                OP("pool", "tensor_tensor", out=mT[:, fc, :], in0=ta, in1=tb, op=ALU.add, reads=[Rta, Rtb], writes=[RmT[fc]])
            if post is not None:
                for _ in post:
                    pass
            post = post_gen(g)
        for _ in post:
            pass
        if debug:
            Rfinal.append(p.res())
            p.dma(dbg["x1"].rearrange("(t p) d -> p t d", p=128), yacc, reads=Ryacc, writes=[Rfinal[-1]], semkey="dbg1")
            Rfinal.append(p.res())
            p.dma(dbg["logits"], logit[:].rearrange("p t e -> p (t e)"), reads=Rlogit, writes=[Rfinal[-1]], semkey="dbg2")
        if stop_after == "MERGE":
            Rfinal.extend(Ryacc + Rlogit)
            return finish()

        p.barrier(scratch[:])
        areset(moe_base)
        Rr = p.res("route")
        RW = [Rr]
        gT = alloc([128, S], BF16)
        RgT = p.res("gT")
        OP("pool", "memset", gT[64:128, :], 0.0, writes=[RgT])
        route_base = apos[0]
        brb = alloc([128, 36], F32)
        p.dma(brb[:, 0:4], brg_d.partition_broadcast(128), writes=RW, semkey="brb")
        p.dma(brb[:, 4:36], bre_d.partition_broadcast(128), writes=RW, semkey="brb")
        L = alloc([128, NT, 36], F32)
        gmax = alloc([128, NT], F32)
        ohg = alloc([128, NT, 4], F32)
        tg4 = alloc([128, NT, 4], F32)
        den = alloc([128, NT], F32)
        pg = alloc([128, NT], F32)
        sel4 = alloc([128, NT, 4, 8], F32)
        ing = alloc([128, NT, 8], F32)
        ing2 = alloc([128, NT, 8], F32)
        m1 = alloc([128, NT], F32)
        m2 = alloc([128, NT], F32)
        oh1 = alloc([128, NT, 8], F32)
        oh2 = alloc([128, NT, 8], F32)
        dd = alloc([128, NT], F32)
        e2 = alloc([128, NT], F32)
        w1_ = alloc([128, NT], F32)
        w2_ = alloc([128, NT], F32)
        ge = alloc([128, NT, 8], F32)
        ge2 = alloc([128, NT, 8], F32)
        gate = alloc([128, NT, 4, 8], F32)
        glo = alloc([32, S], BF16)

        def bc3(a2, n):
            return a2.unsqueeze(2).to_broadcast([128, NT, n])

        def DV(meth, **kw):
            return OP("dve", meth, reads=RW + Rlogit, writes=RW, **kw)

        DV("tensor_tensor", out=L, in0=logit[:], in1=brb.unsqueeze(1).to_broadcast([128, NT, 36]), op=ALU.add)
        gl = L[:, :, 0:4]
        el = L[:, :, 4:36].rearrange("p t (g e) -> p t g e", g=4)
        DV("tensor_reduce", out=gmax, in_=gl, axis=AX.X, op=ALU.max)
        DV("tensor_tensor", out=ohg, in0=gl, in1=bc3(gmax, 4), op=ALU.is_equal)
        DV("tensor_tensor", out=tg4, in0=gl, in1=bc3(gmax, 4), op=ALU.subtract)
        OP("act", "activation", out=tg4, in_=tg4, func=AF.Exp, reads=RW, writes=RW)
        DV("tensor_reduce", out=den, in_=tg4, axis=AX.X, op=ALU.add)
        DV("reciprocal", out=pg, in_=den)
        DV("tensor_tensor", out=sel4, in0=el, in1=ohg.unsqueeze(3).to_broadcast([128, NT, 4, 8]), op=ALU.mult)
        DV("tensor_reduce", out=ing, in_=sel4.rearrange("p t g e -> p t e g"), axis=AX.X, op=ALU.add)
        DV("tensor_reduce", out=m1, in_=ing, axis=AX.X, op=ALU.max)
        DV("tensor_tensor", out=oh1, in0=ing, in1=bc3(m1, 8), op=ALU.is_equal)
        DV("scalar_tensor_tensor", out=ing2, in0=oh1, scalar=-1.0e30, in1=ing, op0=ALU.mult, op1=ALU.add)
        DV("tensor_reduce", out=m2, in_=ing2, axis=AX.X, op=ALU.max)
        DV("tensor_tensor", out=oh2, in0=ing2, in1=bc3(m2, 8), op=ALU.is_equal)
        DV("tensor_tensor", out=dd, in0=m2, in1=m1, op=ALU.subtract)
        OP("act", "activation", out=e2, in_=dd, func=AF.Exp, reads=RW, writes=RW)
        DV("tensor_scalar", out=w1_, in0=e2, scalar1=1.0, scalar2=None, op0=ALU.add)
        DV("reciprocal", out=w1_, in_=w1_)
        DV("tensor_tensor", out=w2_, in0=e2, in1=w1_, op=ALU.mult)
        DV("tensor_tensor", out=w1_, in0=w1_, in1=pg, op=ALU.mult)
        DV("tensor_tensor", out=w2_, in0=w2_, in1=pg, op=ALU.mult)
        DV("tensor_tensor", out=ge, in0=oh1, in1=bc3(w1_, 8), op=ALU.mult)
        DV("tensor_tensor", out=ge2, in0=oh2, in1=bc3(w2_, 8), op=ALU.mult)
        DV("tensor_tensor", out=ge, in0=ge, in1=ge2, op=ALU.add)
        DV("tensor_tensor", out=gate, in0=ohg.unsqueeze(3).to_broadcast([128, NT, 4, 8]),
           in1=ge.unsqueeze(2).to_broadcast([128, NT, 4, 8]), op=ALU.mult)
        if debug:
            Rfinal.append(p.res())
            p.dma(dbg["gate"], gate.rearrange("p t g e -> p (t g e)"), reads=RW, writes=[Rfinal[-1]], semkey="dbg3")
        for t in range(NT):
            OP("pe", "transpose", out=ps[0:32, t // 4, (t % 4) * 128:(t % 4 + 1) * 128],
               in_=gate[:, t, :, :].rearrange("p g e -> p (g e)"), identity=ident, reads=RW + [Rc], writes=[Rps[t // 4]])
        gps = ps[0:32, 0:4, :].rearrange("p a b -> p (a b)")
        OP("act", "activation", out=gT[0:32, :], in_=gps, func=AF.Copy, reads=Rps[0:4], writes=[RgT])
        OP("dve", "tensor_tensor", out=glo, in0=gps, in1=gT[0:32, :], op=ALU.subtract, reads=Rps[0:4] + [RgT], writes=RW)
        OP("dve", "tensor_copy", out=gT[32:64, :], in_=glo, reads=RW, writes=[RgT])
        if stop_after == "ROUTE":
            Rfinal.extend(Ryacc + [RgT] + RW)
            return finish()

        p.barrier(scratch[:])
        areset(route_base)
        csel = alloc([128, 4096], BF16)
        Rcsel = p.res("csel")
        p.dma(csel, csel_d, writes=[Rcsel], semkey="csel")
        NSLOT = 3
        NS2 = 4
        w1s = [alloc([128, 8, 256], BF16) for i in range(NSLOT)]
        w3s = [alloc([128, 8, 256], BF16) for i in range(NSLOT)]
        w2s = [alloc([128, 2, D], BF16) for i in range(NS2)]
        Rw1 = [p.res(f"w1s{i}") for i in range(NSLOT)]
        Rw3 = [p.res(f"w3s{i}") for i in range(NSLOT)]
        Rw2 = [p.res(f"w2s{i}") for i in range(NS2)]
        hg = [alloc([128, 2, S], BF16) for i in range(2)]
        Rhg = [[p.res(f"hg{i}_{g}") for g in range(NG)] for i in range(2)]
        s_sb = [alloc([128, 512], F32) for i in range(2)]
        u_sb = [alloc([128, 512], F32) for i in range(2)]
        gbc = [alloc([128, 512], F32) for i in range(2)]
        Rs = [p.res(f"s{i}") for i in range(2)]
        Ru = [p.res(f"u{i}") for i in range(2)]
        Rgbc = [p.res(f"gbc{i}") for i in range(2)]

        def load_expert(e):
            sl = e % NSLOT
            load_w(w1s[sl], w1_d[e].rearrange("(k p) n -> p k n", p=128), Rw1[sl], ("w1s", sl))
            load_w(w3s[sl], w3_d[e].rearrange("(k p) n -> p k n", p=128), Rw3[sl], ("w3s", sl))
            load_w(w2s[e % NS2], w2_d[e].rearrange("(k p) n -> p k n", p=128), Rw2[e % NS2], ("w2s", e % NS2))

        ybank = [0]

        def down_unit(e, t, half):
            hi_ = e % 2
            b = 5 + (ybank[0] % 3)
            ybank[0] += 1
            for fc in range(2):
                MM(ps[:, b, :], hg[hi_][:, fc, t * 128:(t + 1) * 128], w2s[e % NS2][:, fc, half * 512:(half + 1) * 512],
                   fc == 0, fc == 1, [Rhg[hi_][t // 4], Rw2[e % NS2]], [Rps[b]])
            OP("dve", "tensor_tensor", out=yacc[:, t, half * 512:(half + 1) * 512],
               in0=ps[:, b, :], in1=yacc[:, t, half * 512:(half + 1) * 512], op=ALU.add,
               reads=[Rps[b], Ryacc[t]], writes=[Ryacc[t]])

        load_expert(0)
        load_expert(1)
        gcnt = 0
        for e in range(32):
            sl = e % NSLOT
            hi_ = e % 2
            if e + 2 < 32:
                load_expert(e + 2)
            for g in range(NG):
                gs = slice(g * 512, (g + 1) * 512)
                RxG = RxnT[g * 4:(g + 1) * 4]
                gb2 = gcnt % 2
                gcnt += 1
                dq = []
                if e > 0:
                    dq = [(t, half) for t in range(g * 4, (g + 1) * 4) for half in range(2)]
                MM(ps[:, 4, :], csel[:, e * 128:(e + 1) * 128], gT[:, gs], True, True, [Rcsel, RgT], [Rps[4]])
                OP("act", "activation", out=gbc[gb2], in_=ps[:, 4, :], func=AF.Copy, reads=[Rps[4]], writes=[Rgbc[gb2]])
                for wi_, (wsb, Rw_) in enumerate(((w1s[sl], Rw1[sl]), (w3s[sl], Rw3[sl]))):
                    for fc in range(2):
                        b = wi_ * 2 + fc
                        for k in range(8):
                            MM(ps[:, b, :], wsb[:, k, fc * 128:(fc + 1) * 128], xnT[:, k, gs], k == 0, k == 7,
                               [Rw_] + RxG, [Rps[b]])
                        for (t, half) in dq[:2]:
                            down_unit(e - 1, t, half)
                        dq = dq[2:]
                for fc in range(2):
                    OP("act", "activation", out=s_sb[fc], in_=ps[:, fc, :], func=AF.Silu, reads=[Rps[fc]], writes=[Rs[fc]])
                    OP("dve", "tensor_tensor", out=u_sb[fc], in0=ps[:, 2 + fc, :], in1=s_sb[fc], op=ALU.mult,
                       reads=[Rps[2 + fc], Rs[fc]], writes=[Ru[fc]])
                    OP("pool", "tensor_tensor", out=hg[hi_][:, fc, gs], in0=u_sb[fc], in1=gbc[gb2], op=ALU.mult,
                       reads=[Ru[fc], Rgbc[gb2]], writes=[Rhg[hi_][g]])
        for t in range(NT):
            for half in range(2):
                down_unit(31, t, half)
        for t in range(NT):
            Rf = p.res()
            Rfinal.append(Rf)
            p.dma(out_d[t * 128:(t + 1) * 128, :], yacc[:, t, :], reads=[Ryacc[t]], writes=[Rf], semkey=("out", t % 4))
        return finish()


_IN_KEYS = ["g_norm_mix", "w_in", "b_merge_gate", "g_q", "g_k", "w_branch_att", "g_ret_norm", "w_branch_ret",
            "w_out", "g_norm_ffn", "w_router_group", "b_router_group", "w_router_expert", "b_router_expert",
            "w1", "w3", "w2"]


def _run(inputs, debug=False, stop_after=None, cores=8, trace=False):
    cfd, cbd, gamma_c = _host_consts()
    cfs_arr, cfs_offs = _pack(cfd, CFS_ORDER, np.float32)
    cbs_arr, cbs_offs = _pack(cbd, CBS_ORDER, ml_dtypes.bfloat16)
    nc = build_nc(cfs_offs, cbs_offs, cfs_arr.shape[1], cbs_arr.shape[1], gamma_c, debug=debug, stop_after=stop_after)
    shared = {}
    for k in _IN_KEYS:
        a = np.asarray(inputs[k], dtype=np.float32)
        shared[k] = np.ascontiguousarray(a[0])
    shared["cfs"] = cfs_arr
    shared["cbs"] = cbs_arr
    shared["catt"] = np.ascontiguousarray(np.concatenate([cfd["CA"], cfd["SA"]], axis=1))
    shared["cret"] = np.ascontiguousarray(np.concatenate([cfd["CR"], cfd["SR"]], axis=1))
    shared["csel"] = np.ascontiguousarray(cbd["sel"].astype(ml_dtypes.bfloat16))
    x = np.asarray(inputs["x"], dtype=np.float32)
    in_maps = []
    for c in range(cores):
        m = dict(shared)
        m["x"] = np.ascontiguousarray(x[c])
        in_maps.append(m)
    res = run_bass_kernel_spmd(nc, in_maps, core_ids=list(range(cores)), trace=trace)
    return res


def kernel(**inputs):
    res = _run(inputs)
    out = np.stack([np.asarray(r["out"], dtype=np.float32) for r in res.results], axis=0)
    return out
```

```python
import numpy as np
import ml_dtypes
import concourse.bass as bass
import concourse.mybir as mybir
from concourse.bass_utils import run_bass_kernel_spmd
from contextlib import ExitStack

F32 = mybir.dt.float32
BF16 = mybir.dt.bfloat16
ALU = mybir.AluOpType
AF = mybir.ActivationFunctionType
AX = mybir.AxisListType

S = 2048
D = 1024
NT = 16
NG = 4
EPS = 1e-6
ENGS = ("pe", "act", "dve", "pool", "sp")


class Res:
    __slots__ = ("name", "writer", "readers")

    def __init__(self, name):
        self.name = name
        self.writer = None
        self.readers = []


class Op:
    __slots__ = ("eng", "fn", "deps", "signal", "value", "dma", "semkey", "idx")


class Prog:
    def __init__(self, nc, es):
        self.nc = nc
        self.es = es
        self.ops = {e: [] for e in ENGS}
        self.dma_sems = {}
        self.nres = 0
        self.excl = set()
        self.all_res = []
        self.last_barrier = None

    def res(self, name=None):
        self.nres += 1
        r = Res(name or f"r{self.nres}")
        r.writer = self.last_barrier
        self.all_res.append(r)
        return r

    def sb(self, name, shape, dt):
        return self.es.enter_context(self.nc.sbuf_tensor(name, list(shape), dt))

    def op(self, eng, meth, args=(), kw=None, reads=(), writes=(), dma=False, semkey=None):
        o = Op()
        o.eng = eng
        o.fn = (meth, tuple(args), dict(kw or {}))
        o.signal = False
        o.value = None
        o.dma = dma
        o.semkey = semkey
        if eng in ("act", "dve") and self.excl:
            extra = [r for r in reads if id(r) in self.excl and all(r is not w for w in writes)]
            if extra:
                writes = list(writes) + extra
        deps = {}
        for r in reads:
            if r.writer is not None:
                deps[id(r.writer)] = (r.writer, True)
        for w in writes:
            if w.writer is not None and id(w.writer) not in deps:
                deps[id(w.writer)] = (w.writer, False)
            for rd in w.readers:
                if id(rd) not in deps:
                    deps[id(rd)] = (rd, False)
        dl = []
        for d, raw in deps.values():
            if d is o:
                continue
            if (not d.dma) and (not dma) and d.eng == eng:
                if eng in ("pe", "sp"):
                    continue
            dl.append(d)
            d.signal = True
        o.deps = dl
        for r in reads:
            r.readers.append(o)
        for w in writes:
            w.writer = o
            w.readers = []
        if dma:
            if semkey not in self.dma_sems:
                h = self.es.enter_context(self.nc.semaphore(f"dq{len(self.dma_sems)}"))
                self.dma_sems[semkey] = [h, 0]
            ent = self.dma_sems[semkey]
            ent[1] += 16
            o.value = ent[1]
        o.idx = len(self.ops[eng])
        self.ops[eng].append(o)
        return o

    def dma(self, out, in_, reads=(), writes=(), semkey=None, eng="sp", **kw):
        kw = dict(kw)
        kw["out"] = out
        kw["in_"] = in_
        return self.op(eng, "dma_start", (), kw, reads, writes, dma=True, semkey=semkey)

    def barrier(self, scratch_ap):
        allr = list(self.all_res)
        o = self.op("dve", "memset", (scratch_ap, 0.0), None, reads=allr, writes=allr)
        self.last_barrier = o
        return o

    def emit(self):
        nc = self.nc
        esem = {e: self.es.enter_context(nc.semaphore(f"e_{e}")) for e in ENGS if e != "sp"}
        for e in ENGS:
            c = 0
            for o in self.ops[e]:
                if o.dma:
                    continue
                if o.signal:
                    c += 1
                    o.value = c
        ops = self.ops
        dma_sems = self.dma_sems

        def run(e, engobj):
            waited = {}
            for o in ops[e]:
                need = {}
                for d in o.deps:
                    if d.dma:
                        key = ("d", d.semkey)
                        h = dma_sems[d.semkey][0]
                    else:
                        key = ("e", d.eng)
                        h = esem[d.eng]
                    if key not in need or need[key][1] < d.value:
                        need[key] = (h, d.value)
                for key, (h, v) in need.items():
                    if waited.get(key, 0) >= v:
                        continue
                    waited[key] = v
                    engobj.wait_ge(h, v)
                meth, a, kw = o.fn
                if meth is None:
                    continue
                ins = getattr(engobj, meth)(*a, **kw)
                if o.dma:
                    ins.then_inc(dma_sems[o.semkey][0], 16)
                elif o.signal:
                    ins.then_inc(esem[e], 1)

        with nc.Block() as block:
            @block.tensor
            def _(eng):
                run("pe", eng)

            @block.scalar
            def _(eng):
                run("act", eng)

            @block.vector
            def _(eng):
                run("dve", eng)

            @block.gpsimd
            def _(eng):
                run("pool", eng)

            @block.sync
            def _(eng):
                run("sp", eng)


def _host_consts():
    f32 = np.float32
    t = np.arange(S, dtype=f32)
    inv_a = (f32(10000.0) ** (-np.arange(0, 64, 2, dtype=f32) / f32(64))).astype(f32)
    ang = (t[None, :] * inv_a[:, None]).astype(f32)
    p = np.arange(128)
    CA = np.cos(ang)[p % 32].astype(f32)
    sgn = np.where((p % 64) < 32, -1.0, 1.0).astype(f32)
    SA = (np.sin(ang)[p % 32] * sgn[:, None]).astype(f32)
    inv_r = (f32(1.0) / (f32(10000.0) ** np.linspace(0.0, 1.0, 128, dtype=f32))).astype(f32)
    angr = (t[None, :] * inv_r[:, None]).astype(f32)
    CR = np.cos(angr).astype(f32)
    SR = np.sin(angr).astype(f32)
    lg = np.log(f32(1.0) - np.exp2(f32(-5.0) - np.arange(4, dtype=f32))).astype(f32)
    idx = np.arange(128, dtype=f32)
    diff = idx[None, :] - idx[:, None]
    decT = np.zeros((128, 4, 128), f32)
    for h in range(4):
        decT[:, h, :] = np.where(diff >= 0, np.exp(lg[h] * np.maximum(diff, 0.0)), 0.0) / 16.0
    zeta = (np.exp(lg[None, :] * (127.0 - idx[:, None])) / 16.0).astype(f32)
    xi = np.exp(lg[:, None] * (idx[None, :] + 1.0)).astype(f32)
    xib = np.broadcast_to(xi[None], (128, 4, 128)).astype(f32)
    gamma_c = np.exp(lg * 128.0).astype(f32)
    ident = np.eye(128, dtype=f32)
    cf = {
        "CA": CA, "SA": SA, "CR": CR, "SR": SR,
        "decT": decT.reshape(128, 512), "zeta": zeta, "xib": xib.reshape(128, 512), "ident": ident,
    }
    partner = np.where((p % 64) < 32, p + 32, p - 32)
    perm = np.zeros((128, 128), f32)
    perm[partner, p] = 1.0
    bones = (p[:, None] // 64 == p[None, :] // 64).astype(f32)
    kq = np.arange(128)
    mcur = (kq[None, :] >= kq[:, None]).astype(f32)
    mprev = (kq[:, None] >= kq[None, :]).astype(f32)
    mcur4 = np.tile(mcur, (1, 4))
    mprev4 = np.tile(mprev, (1, 4))
    sel = np.zeros((128, 32, 128), f32)
    for e in range(32):
        sel[e, e, :] = 1.0
        sel[32 + e, e, :] = 1.0
    cb = {
        "perm": perm, "bones": bones, "mcur4": mcur4, "mprev4": mprev4,
        "identb": ident, "sel": sel.reshape(128, 4096),
    }
    return cf, cb, gamma_c


def _pack(dct, order, dtype):
    offs = {}
    cols = 0
    for k in order:
        offs[k] = (cols, dct[k].shape[1])
        cols += dct[k].shape[1]
    arr = np.zeros((128, cols), dtype=dtype)
    for k in order:
        a, n = offs[k]
        arr[:, a:a + n] = dct[k].astype(dtype)
    return arr, offs


CFS_ORDER = ["decT", "zeta", "xib", "ident"]
CBS_ORDER = ["perm", "bones", "mcur4", "mprev4", "identb"]


ARENA_BYTES = 152 * 1024


def build_nc(cfs_offs, cbs_offs, cfs_cols, cbs_cols, gamma_c, debug=False, stop_after=None):
    nc = bass.Bass("TRN2", target_bir_lowering=False)

    def din(name, shape, dt=F32):
        return nc.dram_tensor(name, list(shape), dt, kind="ExternalInput").ap()

    x_d = din("x", [S, D])
    gmix_d = din("g_norm_mix", [D])
    w_in_d = din("w_in", [D, 11264])
    bgate_d = din("b_merge_gate", [2 * D])
    gq_d = din("g_q", [64])
    gk_d = din("g_k", [64])
    watt_d = din("w_branch_att", [D, D])
    gret_d = din("g_ret_norm", [4, 512])
    wret_d = din("w_branch_ret", [2 * D, D])
    wout_d = din("w_out", [D, D])
    gffn_d = din("g_norm_ffn", [D])
    wrg_d = din("w_router_group", [D, 4])
    brg_d = din("b_router_group", [4])
    wre_d = din("w_router_expert", [D, 32])
    bre_d = din("b_router_expert", [32])
    w1_d = din("w1", [32, D, 256])
    w3_d = din("w3", [32, D, 256])
    w2_d = din("w2", [32, 256, D])
    cfs_d = din("cfs", [128, cfs_cols])
    cbs_d = din("cbs", [128, cbs_cols], BF16)
    catt_d = din("catt", [128, 2 * S])
    cret_d = din("cret", [128, 2 * S])
    csel_d = din("csel", [128, 4096], BF16)
    out_d = nc.dram_tensor("out", [S, D], F32, kind="ExternalOutput").ap()
    skind = "ExternalOutput" if debug else "Internal"
    vx_d = nc.dram_tensor("vx", [S, 8, 256], BF16, kind=skind).ap()
    zatt_d = nc.dram_tensor("zatt", [D, S], BF16, kind=skind).ap()
    zret_d = nc.dram_tensor("zret", [2 * D, S], BF16, kind=skind).ap()
    dbg = {}
    if debug:
        dbg["xnT"] = nc.dram_tensor("d_xnT", [128, 8 * S], BF16, kind="ExternalOutput").ap()
        dbg["x1"] = nc.dram_tensor("d_x1", [S, D], F32, kind="ExternalOutput").ap()
        dbg["logits"] = nc.dram_tensor("d_logits", [128, NT * 36], F32, kind="ExternalOutput").ap()
        dbg["gate"] = nc.dram_tensor("d_gate", [128, NT * 32], F32, kind="ExternalOutput").ap()

    with ExitStack() as es:
        p = Prog(nc, es)
        ps = es.enter_context(nc.psum_tensor("ps", [128, 8, 512], F32))
        Rps = [p.res(f"psb{b}") for b in range(8)]
        p.excl.update(id(r) for r in Rps)

        def psb(b):
            return ps[:, b, :].bitcast(BF16)

        def OP(eng, meth, *a, reads=(), writes=(), **kw):
            return p.op(eng, meth, a, kw, reads, writes)

        def MM(out, lhsT, rhs, start, stop, reads, writes, **kw):
            return p.op("pe", "matmul", (out,), dict(lhsT=lhsT, rhs=rhs, start=start, stop=stop, **kw), reads, writes)

        arena = p.sb("arena", [128, ARENA_BYTES // 2], BF16)
        apos = [0]

        def areset(pos=0):
            apos[0] = pos

        def alloc(shape, dt):
            n = 1
            for s_ in shape[1:]:
                n *= s_
            nbytes = n * (4 if dt == F32 else 2)
            nbytes = (nbytes + 63) // 64 * 64
            a = apos[0]
            assert a + nbytes <= ARENA_BYTES, ("arena overflow", a, nbytes)
            apos[0] = a + nbytes
            v = arena[0:shape[0], a // 2:(a + n * (4 if dt == F32 else 2)) // 2]
            if dt == F32:
                v = v.bitcast(F32)
            if len(shape) == 3:
                v = v.rearrange("p (a b) -> p a b", a=shape[1])
            elif len(shape) == 4:
                v = v.rearrange("p (a b c) -> p a b c", a=shape[1], b=shape[2])
            return v

        cfs = p.sb("cfs_sb", [128, cfs_cols], F32)
        cbs = p.sb("cbs_sb", [128, cbs_cols], BF16)
        Rc = p.res("consts")
        Rcs = []

        def cres():
            r = p.res()
            Rcs.append(r)
            return [r]

        def CF(k):
            a, n = cfs_offs[k]
            return cfs[:, a:a + n]

        def CB(k):
            a, n = cbs_offs[k]
            return cbs[:, a:a + n]

        p.dma(cfs[:], cfs_d, writes=cres(), semkey="c")
        p.dma(cbs[:], cbs_d, writes=cres(), semkey="c")
        gmix = p.sb("gmix", [128, 8], F32)
        gffn = p.sb("gffn", [128, 8], F32)
        bga = p.sb("bga", [128, 8], F32)
        bgb = p.sb("bgb", [128, 8], F32)
        gqk = p.sb("gqk", [128, 4], F32)
        epsc = p.sb("epsc", [128, 1], F32)
        scratch = p.sb("scratch", [128, 1], F32)
        p.dma(gmix[:], gmix_d.rearrange("(k p) -> p k", p=128), writes=cres(), semkey="c", allow_slow_non_contiguous=True, eng="act")
        p.dma(gffn[:], gffn_d.rearrange("(k p) -> p k", p=128), writes=cres(), semkey="c", allow_slow_non_contiguous=True, eng="act")
        p.dma(bga[:], bgate_d[0:D].rearrange("(k p) -> p k", p=128), writes=cres(), semkey="c", allow_slow_non_contiguous=True, eng="act")
        p.dma(bgb[:], bgate_d[D:2 * D].rearrange("(k p) -> p k", p=128), writes=cres(), semkey="c", allow_slow_non_contiguous=True, eng="act")
        for ci, gd in ((0, gq_d), (2, gk_d)):
            for half in range(2):
                p.dma(gqk[half * 64:(half + 1) * 64, ci:ci + 1], gd.rearrange("(p o) -> p o", o=1), writes=cres(), semkey="c", eng="act")
                for q4 in range(2):
                    p.dma(gqk[half * 64 + q4 * 32: half * 64 + q4 * 32 + 32, ci + 1:ci + 2],
                          gd[(1 - q4) * 32:(1 - q4) * 32 + 32].rearrange("(p o) -> p o", o=1), writes=cres(), semkey="c", eng="act")
        OP("dve", "memset", epsc[:], EPS, reads=Rcs, writes=[Rc])

        ident = CF("ident")
        identb = CB("identb")
        perm, bones = CB("perm"), CB("bones")
        mcur4, mprev4 = CB("mcur4"), CB("mprev4")
        decT = CF("decT").rearrange("p (h i) -> p h i", h=4)
        zeta = CF("zeta")
        xib = CF("xib").rearrange("p (h i) -> p h i", h=4)

        Rfinal = []
        xnT = p.sb("xnT", [128, 8, S], BF16)
        RxnT = [p.res(f"xnT{t}") for t in range(NT)]
        ss = p.sb("ss", [128, NT], F32)
        rt = p.sb("rt", [128, NT], F32)
        rstd = p.sb("rstd", [128, NT], F32)
        Rxin = [p.res(f"xin{i}") for i in range(2)]
        xs = [p.sb(f"xs{i}", [128, D], F32) for i in range(2)]
        Rxs = [p.res(f"xs{i}") for i in range(2)]
        junk = p.sb("junk", [128, D], BF16)
        Rjunk = p.res("junk")
        logit = p.sb("logit", [128, NT, 36], F32)
        Rlogit = [p.res(f"logit{t}") for t in range(NT)]

        def norm_tile_gen(src, Rsrc, t, gcol, Rstat_t, banks, lo=None, stats_done=False):
            i = t % 2
            if not stats_done:
                OP("act", "activation", out=junk[:], in_=src, func=AF.Square, accum_out=ss[:, t:t + 1],
                   reads=[Rsrc, Rc], writes=[Rjunk, Rstat_t])
                OP("act", "activation", out=rt[:, t:t + 1], in_=ss[:, t:t + 1], func=AF.Sqrt, scale=1.0 / D, bias=epsc[:],
                   reads=[Rstat_t, Rc], writes=[Rstat_t])
                OP("dve", "reciprocal", out=rstd[:, t:t + 1], in_=rt[:, t:t + 1], reads=[Rstat_t], writes=[Rstat_t])
            OP("dve", "tensor_scalar", out=xs[i][:], in0=src, scalar1=rstd[:, t:t + 1], scalar2=None, op0=ALU.mult,
               reads=[Rsrc, Rstat_t], writes=[Rxs[i]])
            yield
            for hb in range(2):
                b = banks[hb]
                for j in range(4):
                    k = hb * 4 + j
                    OP("pe", "transpose", out=ps[:, b, j * 128:(j + 1) * 128], in_=xs[i][:, k * 128:(k + 1) * 128],
                       identity=ident, reads=[Rxs[i], Rc], writes=[Rps[b]])
                gb_ = gcol[:, hb * 4:hb * 4 + 4].unsqueeze(2).to_broadcast([128, 4, 128])
                pin = ps[:, b, :].rearrange("p (j c) -> p j c", j=4)
                dsl = xnT[:, hb * 4:hb * 4 + 4, t * 128:(t + 1) * 128]
                if lo is None:
                    OP("dve", "tensor_tensor", out=dsl, in0=pin, in1=gb_, op=ALU.mult,
                       reads=[Rps[b], Rc], writes=[RxnT[t]])
                else:
                    vtmp, Rvtmp, lodst, Rlo = lo
                    OP("dve", "tensor_tensor", out=vtmp[:, hb * 4:hb * 4 + 4, :], in0=pin, in1=gb_, op=ALU.mult,
                       reads=[Rps[b], Rc], writes=[Rvtmp[hb]])
                    OP("act", "activation", out=dsl, in_=vtmp[:, hb * 4:hb * 4 + 4, :], func=AF.Copy,
                       reads=[Rvtmp[hb]], writes=[RxnT[t]])
                    OP("pool", "tensor_tensor", out=lodst[:, hb * 4:hb * 4 + 4, :], in0=vtmp[:, hb * 4:hb * 4 + 4, :],
                       in1=dsl, op=ALU.subtract, reads=[Rvtmp[hb], RxnT[t]], writes=[Rlo[hb]])
            yield

        def finish():
            p.op("sp", None, reads=list(Rfinal))
            p.emit()
            return nc

        def wsrc(wd, c0, c1):
            return wd[:, c0:c1].rearrange("(k p) n -> p k n", p=128)

        def load_w(dst_ap, src_ap, R, key):
            return p.dma(dst_ap, src_ap, writes=[R], semkey=key, eng="pool")

        Rstat = [p.res(f"stat{t}") for t in range(NT)]
        areset()
        xin = [alloc([128, D], F32) for i in range(4)]
        Rxin = [p.res(f"xin{i}") for i in range(4)]
        def vproj_tile(t):
            i = t % 2
            for cg in range(2):
                b = 4 + cg
                for k in range(8):
                    MM(ps[:, b, :], xnT[:, k, t * 128:(t + 1) * 128], wbig[cg][:, k, :], k == 0, k == 7,
                       [RxnT[t], Rwbig[cg]], [Rps[b]])
                pv = ps[:, b, :].rearrange("p (pr e d) -> p pr e d", e=2, d=64)
                OP("act", "activation", out=vst[i][:, cg * 4:(cg + 1) * 4, 0:64], in_=pv[:, :, 0, :], func=AF.Copy,
                   reads=[Rps[b]], writes=[Rvst[i]])
                OP("dve", "tensor_copy", out=vst[i][:, cg * 4:(cg + 1) * 4, 192:256], in_=pv[:, :, 1, :],
                   reads=[Rps[b]], writes=[Rvst[i]])
            p.dma(vx_d[t * 128:(t + 1) * 128, :, :], vst[i], reads=[Rvst[i]], writes=[Rvx[t]], semkey=("vx", t % 2))

        wbig = [alloc([128, 8, 512], BF16) for i in range(2)]
        Rwbig = [p.res(f"wbig{i}") for i in range(2)]
        vst = [alloc([128, 8, 256], BF16) for i in range(2)]
        Rvst = [p.res(f"vst{i}") for i in range(2)]
        Rvx = [p.res(f"vx{t}") for t in range(NT)]
        for i in range(2):
            OP("pool", "memset", vst[i], 1.0, writes=[Rvst[i]])
        for cg in range(2):
            load_w(wbig[cg], wsrc(w_in_d, 2048 + cg * 512, 2048 + (cg + 1) * 512), Rwbig[cg], ("wbig", cg))
        gens = {}
        do_v = stop_after != "A"
        for t in range(min(3, NT)):
            p.dma(xin[t % 4], x_d[t * 128:(t + 1) * 128, :], writes=[Rxin[t % 4]], semkey=("xin", t % 4))
        for step in range(NT + 2):
            if step < NT:
                t = step
                if t + 3 < NT:
                    p.dma(xin[(t + 3) % 4], x_d[(t + 3) * 128:(t + 4) * 128, :], writes=[Rxin[(t + 3) % 4]],
                          semkey=("xin", (t + 3) % 4))
                gens[t] = norm_tile_gen(xin[t % 4], Rxin[t % 4], t, gmix, Rstat[t], (6, 7))
                next(gens[t])
            if 0 <= step - 1 < NT:
                next(gens[step - 1])
            if do_v and 0 <= step - 2 < NT:
                vproj_tile(step - 2)
        if debug:
            Rfinal.append(p.res())
            p.dma(dbg["xnT"], xnT[:].rearrange("p k s -> p (k s)"), reads=RxnT, writes=[Rfinal[-1]], semkey="dbg0")
        if stop_after == "A":
            Rfinal.append(p.res("fin"))
            p.dma(out_d[0:128, :], xin[1], reads=[Rxin[1]], writes=[Rfinal[-1]], semkey="fin")
            return finish()

        if stop_after == "V":
            Rfinal.extend(Rvx)
            return finish()

        p.barrier(scratch[:])
        areset()
        catt = alloc([128, 2 * S], F32)
        Rcatt = p.res("catt")
        p.dma(catt, catt_d, writes=[Rcatt], semkey="catt")
        CA, SA = catt[:, 0:S], catt[:, S:2 * S]
        wqk = [[alloc([128, 8, 128], BF16) for j in range(2)] for i in range(2)]
        Rwqk = [[p.res(f"wqk{i}_{j}") for j in range(2)] for i in range(2)]
        QKT = [[alloc([128, S], BF16) for j in range(2)] for i in range(2)]
        RQK = [[[p.res(f"QK{i}_{j}_{g}") for g in range(NG)] for j in range(2)] for i in range(2)]
        vxs = [[alloc([128, 16, 256], BF16) for o in range(3)] for i in range(2)]
        Rvxs = [[p.res(f"vxs{i}_{o}") for o in range(3)] for i in range(2)]
        qbf = alloc([128, 512], BF16)
        sqb = alloc([128, 512], BF16)
        rtq = alloc([128, 512], F32)
        rsq = alloc([128, 512], F32)
        t1 = alloc([128, 512], F32)
        t2 = alloc([128, 512], F32)
        Rqbf, Rsqb, Rrtq, Rrsq, Rt1, Rt2 = (p.res(n) for n in ("qbf", "sqb", "rtq", "rsq", "t1", "t2"))
        Pb = [alloc([128, 512], BF16) for i in range(3)]
        RPb = [p.res(f"Pb{i}") for i in range(3)]
        rden = alloc([128, S], F32)
        Rrden = [p.res(f"rden{b}") for b in range(4)]
        zpair = [alloc([128, S], BF16) for i in range(2)]
        Rzpair = [p.res(f"zpair{i}") for i in range(2)]
        Rzatt = [p.res(f"zatt{i}") for i in range(8)]

        def load_pair_weights(pr):
            i = pr % 2
            load_w(wqk[i][0], wsrc(w_in_d, pr * 128, (pr + 1) * 128), Rwqk[i][0], ("wqk", i, 0))
            load_w(wqk[i][1], wsrc(w_in_d, 1024 + pr * 128, 1024 + (pr + 1) * 128), Rwqk[i][1], ("wqk", i, 1))

        def load_pair_v(pr):
            i = pr % 2
            src = vx_d[:, pr, :]
            p.dma(vxs[i][0], src.rearrange("(n i) m -> i n m", i=128), reads=Rvx, writes=[Rvxs[i][0]], semkey=("vxs", i, 0))
            s1 = src.rearrange("(n i c) m -> c i n m", i=128, c=4)
            for c in range(4):
                p.dma(vxs[i][1][:, c * 4:(c + 1) * 4, :], s1[c], reads=Rvx, writes=[Rvxs[i][1]], semkey=("vxs", i, 1))
            p.dma(vxs[i][2], src.rearrange("(i c) m -> i c m", c=16), reads=Rvx, writes=[Rvxs[i][2]], semkey=("vxs", i, 2))

        def colap(T, spec, r0):
            base, r, c = spec
            if r == 1:
                return T[r0:r0 + 64, base:base + 128]
            return T[r0:r0 + 64, base:base + 128 * r].rearrange("p (n r) -> p n r", r=r)[:, :, c]

        def prep_gen(pr):
            i = pr % 2
            for j, gi in ((0, 0), (1, 2)):
                dstT = QKT[i][j]
                for g in range(NG):
                    gs = slice(g * 512, (g + 1) * 512)
                    for k in range(8):
                        MM(ps[:, 6, :], wqk[i][j][:, k, :], xnT[:, k, gs], k == 0, k == 7,
                           [Rwqk[i][j]] + RxnT[g * 4:(g + 1) * 4], [Rps[6]])
                    OP("act", "activation", out=qbf, in_=ps[:, 6, :], func=AF.Copy, reads=[Rps[6]], writes=[Rqbf])
                    OP("dve", "scalar_tensor_tensor", out=t1, in0=ps[:, 6, :], scalar=gqk[:, gi:gi + 1], in1=CA[:, gs],
                       op0=ALU.mult, op1=ALU.mult, reads=[Rps[6], Rc, Rcatt], writes=[Rt1])
                    OP("pool", "tensor_tensor", out=sqb, in0=qbf, in1=qbf, op=ALU.mult, reads=[Rqbf], writes=[Rsqb])
                    yield
                    MM(ps[:, 7, :], bones, sqb, True, True, [Rsqb, Rc], [Rps[7]])
                    MM(ps[:, 6, :], perm, qbf, True, True, [Rqbf, Rc], [Rps[6]])
                    OP("act", "activation", out=rtq, in_=ps[:, 7, :], func=AF.Ln, scale=1.0 / 64, bias=epsc[:],
                       reads=[Rps[7], Rc], writes=[Rrtq])
                    OP("act", "activation", out=rsq, in_=rtq, func=AF.Exp, scale=-0.5, reads=[Rrtq], writes=[Rrsq])
                    OP("dve", "scalar_tensor_tensor", out=t2, in0=ps[:, 6, :], scalar=gqk[:, gi + 1:gi + 2], in1=SA[:, gs],
                       op0=ALU.mult, op1=ALU.mult, reads=[Rps[6], Rc, Rcatt], writes=[Rt2])
                    OP("pool", "tensor_tensor", out=t1, in0=t1, in1=t2, op=ALU.add, reads=[Rt1, Rt2], writes=[Rt1])
                    OP("dve", "tensor_tensor", out=dstT[:, gs], in0=t1, in1=rsq, op=ALU.mult,
                       reads=[Rt1, Rrsq], writes=[RQK[i][j][g]])
                    yield

        def att_batches(pr):
            def cols(r, c, n):
                return (n * 128 * r, r, c)
            tiles_cur, tiles_prev = [], []
            for n in range(16):
                tiles_cur.append((cols(1, 0, n), cols(1, 0, n), 0, n, ("n", n // 4, (n % 4) * 128, 1), n % 4 == 0))
            for c in range(4):
                for n in range(4):
                    tiles_cur.append((cols(4, c, n), cols(4, c, n), 1, c * 4 + n, ("n", n, c, 4), False))
            for c in range(16):
                tiles_cur.append((cols(16, c, 0), cols(16, c, 0), 2, c, ("s", c), False))
            for n in range(1, 16):
                tiles_prev.append((cols(1, 0, n - 1), cols(1, 0, n), 0, n - 1, ("n", n // 4, (n % 4) * 128, 1), False))
            for c in range(4):
                for n in range(1, 4):
                    tiles_prev.append((cols(4, c, n - 1), cols(4, c, n), 1, c * 4 + n - 1, ("n", n, c, 4), False))
            batches = []
            for lst, msk in ((tiles_cur, mcur4), (tiles_prev, mprev4)):
                for s0 in range(0, len(lst), 4):
                    batches.append((lst[s0:s0 + 4], msk))
            items = []
            for eh in range(2):
                for bi, (tl, msk) in enumerate(batches):
                    items.append((eh, bi, tl, msk, bi == len(batches) - 1))
            return items

        gcount = [0]

        def emit_S(pr, item):
            i = pr % 2
            eh, bi, tl, msk, last = item
            r0 = eh * 64
            gb = gcount[0]
            gcount[0] += 1
            sb_ = 4 + (gb % 2)
            pbi = gb % 3
            nt_ = len(tl)
            QT, KT = QKT[i]
            RQKall = RQK[i][0] + RQK[i][1]
            for ti, (kc, qc, vo, vb, osp, st) in enumerate(tl):
                MM(ps[:, sb_, ti * 128:(ti + 1) * 128], colap(KT, kc, r0), colap(QT, qc, r0), True, True,
                   RQKall, [Rps[sb_]])
            OP("act", "activation", out=Pb[pbi][:, 0:nt_ * 128], in_=ps[:, sb_, 0:nt_ * 128], func=AF.Exp, scale=0.125,
               reads=[Rps[sb_]], writes=[RPb[pbi]])
            OP("dve", "tensor_tensor", out=Pb[pbi][:, 0:nt_ * 128], in0=Pb[pbi][:, 0:nt_ * 128],
               in1=msk[:, 0:nt_ * 128], op=ALU.mult, reads=[RPb[pbi], Rc], writes=[RPb[pbi]])
            return pbi

        def emit_PV(pr, item, pbi):
            i = pr % 2
            eh, bi, tl, msk, last = item
            r0 = eh * 64
            zp = zpair[i]
            for ti, (kc, qc, vo, vb, osp, st) in enumerate(tl):
                lhsT = vxs[i][vo][:, vb, eh * 128:(eh + 1) * 128]
                if osp[0] == "n":
                    _, bank, off, r = osp
                    if r == 1:
                        oap = ps[:, bank, off:off + 128]
                    else:
                        oap = ps[:, bank, :].rearrange("p (n r) -> p n r", r=r)[:, :, off]
                    MM(oap, lhsT, Pb[pbi][:, ti * 128:(ti + 1) * 128], st, False,
                       [RPb[pbi], Rvxs[i][vo]], [Rps[bank]], skip_group_check=True)
                else:
                    c = osp[1]
                    for bank in range(4):
                        oap = ps[:, bank, :].rearrange("p (n r) -> p n r", r=16)[:, :, c]
                        MM(oap, lhsT, Pb[pbi][:, ti * 128 + bank * 32: ti * 128 + bank * 32 + 32], False, False,
                           [RPb[pbi], Rvxs[i][vo]], [Rps[bank]], skip_group_check=True)
            if last:
                d0 = 64 - r0
                for bank in range(4):
                    bs = slice(bank * 512, (bank + 1) * 512)
                    OP("act", "activation", out=rden[r0:r0 + 64, bs], in_=ps[d0:d0 + 64, bank, :], func=AF.Ln,
                       reads=[Rps[bank]], writes=[Rrden[bank]])
                    OP("act", "activation", out=rden[r0:r0 + 64, bs], in_=rden[r0:r0 + 64, bs], func=AF.Exp, scale=-1.0,
                       reads=[Rrden[bank]], writes=[Rrden[bank]])
                    OP("dve", "tensor_tensor", out=zp[r0:r0 + 64, bs], in0=ps[r0:r0 + 64, bank, :],
                       in1=rden[r0:r0 + 64, bs], op=ALU.mult, reads=[Rps[bank], Rrden[bank]], writes=[Rzpair[i]])

        load_pair_weights(0)
        load_pair_weights(1)
        load_pair_v(0)
        for _ in prep_gen(0):
            pass
        for pr in range(8):
            i = pr % 2
            if pr + 1 < 8:
                load_pair_v(pr + 1)
            nxt = prep_gen(pr + 1) if pr + 1 < 8 else None
            items = att_batches(pr)
            pq = [emit_S(pr, items[0]), emit_S(pr, items[1])]
            for ii, item in enumerate(items):
                if ii + 2 < len(items):
                    pq.append(emit_S(pr, items[ii + 2]))
                emit_PV(pr, item, pq.pop(0))
                if nxt is not None and ii % 2 == 1:
                    next(nxt, None)
            if nxt is not None:
                for _ in nxt:
                    pass
            if pr + 2 < 8:
                load_pair_weights(pr + 2)
            p.dma(zatt_d[pr * 128:(pr + 1) * 128, :], zpair[i], reads=[Rzpair[i]], writes=[Rzatt[pr]], semkey=("zatt", i))
        if stop_after == "ATT":
            Rfinal.extend(Rzatt)
            return finish()

        p.barrier(scratch[:])
        areset()
        cret = alloc([128, 2 * S], F32)
        Rcret = p.res("cret")
        p.dma(cret, cret_d, writes=[Rcret], semkey="cret")
        CR, SR = cret[:, 0:S], cret[:, S:2 * S]
        gretb = alloc([128, 4, 512], F32)
        Rgretb = p.res("gretb")
        p.dma(gretb.rearrange("p h v -> p (h v)"), gret_d.rearrange("h v -> (h v)").partition_broadcast(128),
              writes=[Rgretb], semkey="gretb")
        wq_r = [alloc([128, 8, 256], BF16) for i in range(2)]
        wk_r = [alloc([128, 8, 256], BF16) for i in range(2)]
        wv_r = [alloc([128, 8, 512], BF16) for i in range(2)]
        wg_r = [alloc([128, 8, 512], BF16) for i in range(2)]
        Rwr = [[p.res(f"wr{i}_{j}") for j in range(4)] for i in range(2)]
        QrT = [alloc([128, 2, 512], BF16) for i in range(2)]
        KrT = [alloc([128, 2, 512], BF16) for i in range(2)]
        Qxi = [alloc([128, 2, 512], BF16) for i in range(2)]
        RQr = [p.res(f"QrT{i}") for i in range(2)]
        RKr = [p.res(f"KrT{i}") for i in range(2)]
        RQx = [p.res(f"Qxi{i}") for i in range(2)]
        sgr = [alloc([128, 4, 512], BF16) for i in range(2)]
        Rsgr = [p.res(f"sgr{i}") for i in range(2)]
        vr_sb = [alloc([128, 512], BF16) for i in range(2)]
        Rvr = [p.res(f"vr{i}") for i in range(2)]
        kz = [alloc([128, 256], BF16) for i in range(2)]
        Rkz = [p.res(f"kz{i}") for i in range(2)]
        inm = [alloc([128, 128], BF16) for i in range(2)]
        Rinm = [p.res(f"inm{i}") for i in range(2)]
        yn = [alloc([128, 512], BF16) for i in range(2)]
        Ryn = [p.res(f"yn{i}") for i in range(2)]
        zst = [alloc([128, 4, 512], BF16) for i in range(2)]
        Rzst = [p.res(f"zst{i}") for i in range(2)]
        state = alloc([128, 2, 512], F32)
        state_bf = alloc([128, 2, 512], BF16)
        Rstate = [p.res(f"state{c}") for c in range(2)]
        Rstbf = [p.res(f"stbf{c}") for c in range(2)]
        tmp = [alloc([128, 512], F32) for i in range(4)]
        Rtmp = [p.res(f"rtmp{i}") for i in range(4)]
        ssr = alloc([128, 64], F32)
        rtr = alloc([128, 64], F32)
        rsr = alloc([128, 64], F32)
        Rzret = []

        def load_head_weights(h):
            i = h % 2
            load_w(wq_r[i], wsrc(w_in_d, 3072 + h * 256, 3072 + (h + 1) * 256), Rwr[i][0], ("wr", i, 0))
            load_w(wk_r[i], wsrc(w_in_d, 4096 + h * 256, 4096 + (h + 1) * 256), Rwr[i][1], ("wr", i, 1))
            load_w(wv_r[i], wsrc(w_in_d, 5120 + h * 512, 5120 + (h + 1) * 512), Rwr[i][2], ("wr", i, 2))
            load_w(wg_r[i], wsrc(w_in_d, 7168 + h * 512, 7168 + (h + 1) * 512), Rwr[i][3], ("wr", i, 3))

        vr3 = vr_sb + [alloc([128, 512], BF16)]
        Rvr3 = Rvr + [p.res("vr2")]
        kz3 = kz + [alloc([128, 256], BF16)]
        Rkz3 = Rkz + [p.res("kz2")]
        inm3 = inm + [alloc([128, 128], BF16)]
        Rinm3 = Rinm + [p.res("inm2")]
        INNER = ps[:, 3, 0:128]
        ysb = [alloc([128, 512], F32) for i in range(2)]
        Rysb = [p.res(f"ysb{i}") for i in range(2)]

        def proj_pieces(h, g):
            wi = h % 2
            gi = g % 2
            gs = slice(g * 512, (g + 1) * 512)
            RxG = RxnT[g * 4:(g + 1) * 4]

            def qk(which):
                wsb = (wq_r, wk_r)[which][wi]
                dst = (QrT, KrT)[which][gi]
                Rdst = (RQr, RKr)[which][gi]
                for c in range(2):
                    for k in range(8):
                        MM(ps[:, c, :], wsb[:, k, c * 128:(c + 1) * 128], xnT[:, k, gs], k == 0, k == 7,
                           [Rwr[wi][which]] + RxG, [Rps[c]])
                OP("dve", "tensor_tensor", out=tmp[0], in0=ps[:, 0, :], in1=CR[:, gs], op=ALU.mult,
                   reads=[Rps[0], Rcret], writes=[Rtmp[0]])
                OP("dve", "tensor_tensor", out=tmp[1], in0=ps[:, 1, :], in1=SR[:, gs], op=ALU.mult,
                   reads=[Rps[1], Rcret], writes=[Rtmp[1]])
                OP("pool", "tensor_tensor", out=dst[:, 0, :], in0=tmp[0], in1=tmp[1], op=ALU.subtract,
                   reads=[Rtmp[0], Rtmp[1]], writes=[Rdst])
                OP("dve", "tensor_tensor", out=tmp[2], in0=ps[:, 1, :], in1=CR[:, gs], op=ALU.mult,
                   reads=[Rps[1], Rcret], writes=[Rtmp[2]])
                OP("dve", "tensor_tensor", out=tmp[3], in0=ps[:, 0, :], in1=SR[:, gs], op=ALU.mult,
                   reads=[Rps[0], Rcret], writes=[Rtmp[3]])
                OP("pool", "tensor_tensor", out=dst[:, 1, :], in0=tmp[2], in1=tmp[3], op=ALU.add,
                   reads=[Rtmp[2], Rtmp[3]], writes=[Rdst])
                if which == 0:
                    xb = xib[:, h, :].unsqueeze(1).to_broadcast([128, 4, 128])
                    for c in range(2):
                        OP("pool", "tensor_tensor", out=Qxi[gi][:, c, :].rearrange("p (n i) -> p n i", n=4),
                           in0=dst[:, c, :].rearrange("p (n i) -> p n i", n=4), in1=xb, op=ALU.mult,
                           reads=[Rdst, Rc], writes=[RQx[gi]])

            def gate(vc):
                b = vc % 2
                for k in range(8):
                    MM(ps[:, b, :], wg_r[wi][:, k, vc * 128:(vc + 1) * 128], xnT[:, k, gs], k == 0, k == 7,
                       [Rwr[wi][3]] + RxG, [Rps[b]])
                OP("act", "activation", out=sgr[gi][:, vc, :], in_=ps[:, b, :], func=AF.Silu,
                   reads=[Rps[b]], writes=[Rsgr[gi]])

            return [lambda: qk(0), lambda: qk(1), lambda: gate(0), lambda: gate(1), lambda: gate(2), lambda: gate(3)]

        def stage_A(q):
            h, gn = q // 16, q % 16
            g, n = gn // 4, gn % 4
            wi, gi, b3 = h % 2, g % 2, q % 3
            cs = slice(n * 128, (n + 1) * 128)
            for k in range(8):
                MM(ps[:, 2, :], xnT[:, k, gn * 128:(gn + 1) * 128], wv_r[wi][:, k, :], k == 0, k == 7,
                   [Rwr[wi][2], RxnT[gn]], [Rps[2]])
            OP("act", "activation", out=vr3[b3], in_=ps[:, 2, :], func=AF.Copy, reads=[Rps[2]], writes=[Rvr3[b3]])
            for c in range(2):
                OP("pe", "transpose", out=psb(4)[:, 512 + c * 128: 512 + (c + 1) * 128], in_=KrT[gi][:, c, cs],
                   identity=identb, reads=[RKr[gi], Rc], writes=[Rps[4]])
            OP("act", "activation", out=kz3[b3], in_=psb(4)[:, 512:768], func=AF.Copy, scale=zeta[:, h:h + 1],
               reads=[Rps[4], Rc], writes=[Rkz3[b3]])
            for c in range(2):
                MM(INNER, KrT[gi][:, c, cs], QrT[gi][:, c, cs], c == 0, c == 1, [RKr[gi], RQr[gi]], [Rps[3]])
            OP("dve", "tensor_tensor", out=inm3[b3], in0=INNER, in1=decT[:, h, :], op=ALU.mult,
               reads=[Rps[3], Rc], writes=[Rinm3[b3]])

        Rst_q = {}

        def stage_B(q):
            h, gn = q // 16, q % 16
            g, n = gn // 4, gn % 4
            gi, b3, ci = g % 2, q % 3, q % 2
            cs = slice(n * 128, (n + 1) * 128)
            yb = 5
            st_i = q
            gC = float(gamma_c[h])
            MM(ps[:, yb, :], inm3[b3], vr3[b3], True, gn == 0, [Rinm3[b3], Rvr3[b3]], [Rps[yb]])
            if gn > 0:
                for c in range(2):
                    MM(ps[:, yb, :], Qxi[gi][:, c, cs], state_bf[:, c, :], False, c == 1, [RQx[gi], Rstbf[c]], [Rps[yb]])
            if gn < 15:
                for c in range(2):
                    MM(ps[:, 6 + c, :], kz3[b3][:, c * 128:(c + 1) * 128], vr3[b3], True, True,
                       [Rkz3[b3], Rvr3[b3]], [Rps[6 + c]])
                    if gn == 0:
                        OP("dve", "tensor_copy", out=state[:, c, :], in_=ps[:, 6 + c, :],
                           reads=[Rps[6 + c]], writes=[Rstate[c]])
                    else:
                        OP("dve", "scalar_tensor_tensor", out=state[:, c, :], in0=state[:, c, :], scalar=gC,
                           in1=ps[:, 6 + c, :], op0=ALU.mult, op1=ALU.add,
                           reads=[Rps[6 + c], Rstate[c]], writes=[Rstate[c]])
                    OP("pool", "tensor_copy", out=state_bf[:, c, :], in_=state[:, c, :],
                       reads=[Rstate[c]], writes=[Rstbf[c]])
            Rst = p.res()
            OP("act", "activation", out=ysb[ci], in_=ps[:, yb, :], func=AF.Copy, reads=[Rps[yb]], writes=[Rysb[ci]])
            OP("act", "activation", out=junk[:, 0:512], in_=ysb[ci], func=AF.Square,
               accum_out=ssr[:, st_i:st_i + 1], reads=[Rysb[ci]], writes=[Rjunk, Rst])
            OP("act", "activation", out=rtr[:, st_i:st_i + 1], in_=ssr[:, st_i:st_i + 1], func=AF.Sqrt,
               scale=1.0 / 512, bias=epsc[:], reads=[Rst, Rc], writes=[Rst])
            OP("dve", "reciprocal", out=rsr[:, st_i:st_i + 1], in_=rtr[:, st_i:st_i + 1], reads=[Rst], writes=[Rst])
            OP("act", "activation", out=yn[ci], in_=ysb[ci], func=AF.Copy, scale=rsr[:, st_i:st_i + 1],
               reads=[Rysb[ci], Rst], writes=[Ryn[ci]])
            OP("pool", "tensor_tensor", out=yn[ci], in0=yn[ci], in1=gretb[:, h, :], op=ALU.mult,
               reads=[Ryn[ci], Rgretb], writes=[Ryn[ci]])

        def stage_C(q):
            h, gn = q // 16, q % 16
            g, n = gn // 4, gn % 4
            gi, ci = g % 2, q % 2
            cs = slice(n * 128, (n + 1) * 128)
            gs = slice(g * 512, (g + 1) * 512)
            for vc in range(4):
                OP("pe", "transpose", out=psb(4)[:, vc * 128:(vc + 1) * 128], in_=yn[ci][:, vc * 128:(vc + 1) * 128],
                   identity=identb, reads=[Ryn[ci], Rc], writes=[Rps[4]])
            OP("dve", "tensor_tensor", out=zst[gi][:, :, cs], in0=psb(4)[:, 0:512].rearrange("p (v i) -> p v i", v=4),
               in1=sgr[gi][:, :, cs], op=ALU.mult, reads=[Rps[4], Rsgr[gi]], writes=[Rzst[gi]])
            if n == 3:
                Rz = p.res()
                Rzret.append(Rz)
                p.dma(zret_d[h * 512:(h + 1) * 512, gs].rearrange("(vc p) s -> p vc s", p=128), zst[gi],
                      reads=[Rzst[gi]], writes=[Rz], semkey=("zret", gi))

        load_head_weights(0)
        load_head_weights(1)
        units = [(h, g) for h in range(4) for g in range(NG)]
        for f in proj_pieces(*units[0]):
            f()
        pending = []
        for s_ in range(64 + 2):
            if s_ < 64:
                u, n = s_ // 4, s_ % 4
                if n == 0:
                    pending = proj_pieces(*units[u + 1]) if u + 1 < 16 else []
                if s_ % 16 == 1 and 2 <= s_ // 16 + 1 < 4:
                    load_head_weights(s_ // 16 + 1)
                stage_A(s_)
            if 0 <= s_ - 1 < 64:
                stage_B(s_ - 1)
            if 0 <= s_ - 2 < 64:
                stage_C(s_ - 2)
            if s_ < 64:
                take = 1 if n < 2 else 2
                for f in pending[:take]:
                    f()
                pending = pending[take:]
        if stop_after == "RET":
            Rfinal.extend(Rzret + Rzatt)
            return finish()

        p.barrier(scratch[:])
        areset()
        yacc = alloc([128, NT, D], F32)
        Ryacc = [p.res(f"yacc{t}") for t in range(NT)]
        moe_base = apos[0]
        za = alloc([128, 8, 512], BF16)
        zr = alloc([128, 16, 512], BF16)
        Rza, Rzr = p.res("za"), p.res("zr")
        wa = [alloc([128, 8, 128], BF16) for i in range(2)]
        wr = [alloc([128, 16, 128], BF16) for i in range(2)]
        wga = [alloc([128, 8, 128], BF16) for i in range(2)]
        wgb = [alloc([128, 8, 128], BF16) for i in range(2)]
        Rwm = [[p.res(f"wm{i}_{j}") for j in range(4)] for i in range(2)]
        mT2 = [alloc([128, 8, 512], BF16) for i in range(2)]
        RmT2 = [[p.res(f"mT{i}_{k}") for k in range(8)] for i in range(2)]
        wout = alloc([128, 8, 512], BF16)
        Rwout = p.res("wout")
        sga = alloc([128, 512], F32)
        sgb = alloc([128, 512], F32)
        ta = alloc([128, 512], F32)
        tb = alloc([128, 512], F32)
        Rsga, Rsgb, Rta, Rtb = (p.res(n) for n in ("sga", "sgb", "ta", "tb"))
        vtmp = alloc([128, 8, 128], F32)
        Rvtmp = [p.res(f"vtmp{i}") for i in range(2)]
        xlo = [alloc([128, 8, 128], BF16) for i in range(2)]
        Rxlo = [[p.res(f"xlo{i}_{hb}") for hb in range(2)] for i in range(2)]
        wrf = alloc([128, 8, 36], F32)
        wrh = alloc([128, 8, 36], BF16)
        wrl = alloc([128, 8, 36], BF16)
        Rwrf, Rwrh, Rwrl = p.res("wrf"), p.res("wrh"), p.res("wrl")
        p.dma(wrf[:, :, 0:4], wrg_d.rearrange("(k p) n -> p k n", p=128), writes=[Rwrf], semkey="wrf")
        p.dma(wrf[:, :, 4:36], wre_d.rearrange("(k p) n -> p k n", p=128), writes=[Rwrf], semkey="wrf")
        OP("dve", "tensor_copy", out=wrh, in_=wrf, reads=[Rwrf], writes=[Rwrh])
        OP("dve", "tensor_tensor", out=wrl, in0=wrf, in1=wrh, op=ALU.subtract, reads=[Rwrf, Rwrh], writes=[Rwrl])

        def load_merge_weights(fc, slot):
            cs_ = slice(fc * 128, (fc + 1) * 128)
            load_w(wa[slot], watt_d[:, cs_].rearrange("(k p) n -> p k n", p=128), Rwm[slot][0], ("wm", slot, 0))
            load_w(wr[slot], wret_d[:, cs_].rearrange("(k p) n -> p k n", p=128), Rwm[slot][1], ("wm", slot, 1))
            load_w(wga[slot], wsrc(w_in_d, 9216 + fc * 128, 9216 + (fc + 1) * 128), Rwm[slot][2], ("wm", slot, 2))
            load_w(wgb[slot], wsrc(w_in_d, 10240 + fc * 128, 10240 + (fc + 1) * 128), Rwm[slot][3], ("wm", slot, 3))

        Rstat2 = [p.res(f"stat2_{t}") for t in range(NT)]

        def post_gen(g):
            mT = mT2[g % 2]
            RmT = RmT2[g % 2]
            for tt in range(4):
                gt = g * 4 + tt
                p.dma(yacc[:, gt, :], x_d[gt * 128:(gt + 1) * 128, :], writes=[Ryacc[gt]], semkey=("xres", gt))
            for half in range(2):
                load_w(wout, wsrc(wout_d, half * 512, (half + 1) * 512), Rwout, "wout")
                for tt in range(4):
                    gt = g * 4 + tt
                    b = 4 + (tt % 2)
                    for k in range(8):
                        MM(ps[:, b, :], mT[:, k, tt * 128:(tt + 1) * 128], wout[:, k, :], k == 0, k == 7,
                           [RmT[k], Rwout], [Rps[b]])
                    OP("dve", "tensor_tensor", out=yacc[:, gt, half * 512:(half + 1) * 512], in0=ps[:, b, :],
                       in1=yacc[:, gt, half * 512:(half + 1) * 512], op=ALU.add, reads=[Rps[b], Ryacc[gt]], writes=[Ryacc[gt]])
                    yield
            Rsg = Rstat2[g * 4:(g + 1) * 4]
            for tt in range(4):
                gt = g * 4 + tt
                OP("act", "activation", out=junk[:], in_=yacc[:, gt, :], func=AF.Square, accum_out=ss[:, gt:gt + 1],
                   reads=[Ryacc[gt], Rc], writes=[Rjunk, Rstat2[gt]])
            OP("act", "activation", out=rt[:, g * 4:(g + 1) * 4], in_=ss[:, g * 4:(g + 1) * 4], func=AF.Sqrt, scale=1.0 / D,
               bias=epsc[:], reads=Rsg + [Rc], writes=Rsg)
            OP("dve", "reciprocal", out=rstd[:, g * 4:(g + 1) * 4], in_=rt[:, g * 4:(g + 1) * 4], reads=Rsg, writes=Rsg)
            yield
            for tt in range(4):
                gt = g * 4 + tt
                i = gt % 2
                for _ in norm_tile_gen(yacc[:, gt, :], Ryacc[gt], gt, gffn, Rstat2[gt], (6, 7),
                                       lo=(vtmp, Rvtmp, xlo[i], Rxlo[i]), stats_done=True):
                    yield
                n_mm = 0
                for (lh, rw, Rl, Rw_) in ((xnT[:, :, gt * 128:(gt + 1) * 128], wrh, [RxnT[gt]], Rwrh),
                                          (xnT[:, :, gt * 128:(gt + 1) * 128], wrl, [RxnT[gt]], Rwrl),
                                          (xlo[i], wrh, Rxlo[i], Rwrh)):
                    for k in range(8):
                        MM(ps[:, 5, 0:36], lh[:, k, :], rw[:, k, :], n_mm == 0, n_mm == 23, Rl + [Rw_], [Rps[5]])
                        n_mm += 1
                OP("dve", "tensor_copy", out=logit[:, gt, :], in_=ps[:, 5, 0:36], reads=[Rps[5]], writes=[Rlogit[gt]])
                yield

        widx = 0
        load_merge_weights(0, 0)
        post = None
        for g in range(NG):
            gs = slice(g * 512, (g + 1) * 512)
            RxG = RxnT[g * 4:(g + 1) * 4]
            mT = mT2[g % 2]
            RmT = RmT2[g % 2]
            p.dma(za, zatt_d[:, gs].rearrange("(k p) s -> p k s", p=128), reads=Rzatt, writes=[Rza], semkey="za")
            p.dma(zr, zret_d[:, gs].rearrange("(k p) s -> p k s", p=128), reads=Rzret, writes=[Rzr], semkey="zr")
            for fc in range(8):
                slot = widx % 2
                widx += 1
                nfc, ng_ = (fc + 1) % 8, g + (1 if fc == 7 else 0)
                if ng_ < NG:
                    load_merge_weights(nfc, widx % 2)
                for k in range(8):
                    MM(ps[:, 2, :], wga[slot][:, k, :], xnT[:, k, gs], k == 0, k == 7, [Rwm[slot][2]] + RxG, [Rps[2]])
                if post is not None:
                    next(post, None)
                for k in range(8):
                    MM(ps[:, 3, :], wgb[slot][:, k, :], xnT[:, k, gs], k == 0, k == 7, [Rwm[slot][3]] + RxG, [Rps[3]])
                if post is not None:
                    next(post, None)
                for k in range(8):
                    MM(ps[:, 0, :], wa[slot][:, k, :], za[:, k, :], k == 0, k == 7, [Rwm[slot][0], Rza], [Rps[0]])
                if post is not None:
                    next(post, None)
                for k in range(16):
                    MM(ps[:, 1, :], wr[slot][:, k, :], zr[:, k, :], k == 0, k == 15, [Rwm[slot][1], Rzr], [Rps[1]])
                OP("act", "activation", out=sga, in_=ps[:, 2, :], func=AF.Sigmoid, bias=bga[:, fc:fc + 1],
                   reads=[Rps[2], Rc], writes=[Rsga])
                OP("act", "activation", out=sgb, in_=ps[:, 3, :], func=AF.Sigmoid, bias=bgb[:, fc:fc + 1],
                   reads=[Rps[3], Rc], writes=[Rsgb])
                OP("dve", "tensor_tensor", out=ta, in0=ps[:, 0, :], in1=sga, op=ALU.mult, reads=[Rps[0], Rsga], writes=[Rta])
                OP("dve", "tensor_tensor", out=tb, in0=ps[:, 1, :], in1=sgb, op=ALU.mult, reads=[Rps[1], Rsgb], writes=[Rtb])
                OP("pool", "tensor_tensor", out=mT[:, fc, :], in0=ta, in1=tb, op=ALU.add, reads=[Rta, Rtb], writes=[RmT[fc]])
            if post is not None:
                for _ in post:
                    pass
            post = post_gen(g)
        for _ in post:
            pass
        if debug:
            Rfinal.append(p.res())
            p.dma(dbg["x1"].rearrange("(t p) d -> p t d", p=128), yacc, reads=Ryacc, writes=[Rfinal[-1]], semkey="dbg1")
            Rfinal.append(p.res())
            p.dma(dbg["logits"], logit[:].rearrange("p t e -> p (t e)"), reads=Rlogit, writes=[Rfinal[-1]], semkey="dbg2")
        if stop_after == "MERGE":
            Rfinal.extend(Ryacc + Rlogit)
            return finish()

        p.barrier(scratch[:])
        areset(moe_base)
        Rr = p.res("route")
        RW = [Rr]
        gT = alloc([128, S], BF16)
        RgT = p.res("gT")
        OP("pool", "memset", gT[64:128, :], 0.0, writes=[RgT])
        csel = alloc([128, 4096], BF16)
        Rcsel = p.res("csel")
        p.dma(csel, csel_d, writes=[Rcsel], semkey="csel")
        NSLOT = 3
        NS2 = 4
        w1s = [alloc([128, 8, 256], BF16) for i in range(NSLOT)]
        w3s = [alloc([128, 8, 256], BF16) for i in range(NSLOT)]
        w2s = [alloc([128, 2, D], BF16) for i in range(NS2)]
        Rw1 = [p.res(f"w1s{i}") for i in range(NSLOT)]
        Rw3 = [p.res(f"w3s{i}") for i in range(NSLOT)]
        Rw2 = [p.res(f"w2s{i}") for i in range(NS2)]

        def load_expert(e):
            sl = e % NSLOT
            load_w(w1s[sl], w1_d[e].rearrange("(k p) n -> p k n", p=128), Rw1[sl], ("w1s", sl))
            load_w(w3s[sl], w3_d[e].rearrange("(k p) n -> p k n", p=128), Rw3[sl], ("w3s", sl))
            load_w(w2s[e % NS2], w2_d[e].rearrange("(k p) n -> p k n", p=128), Rw2[e % NS2], ("w2s", e % NS2))

        load_expert(0)
        load_expert(1)
        route_base = apos[0]
        brb = alloc([128, 36], F32)
        p.dma(brb[:, 0:4], brg_d.partition_broadcast(128), writes=RW, semkey="brb")
        p.dma(brb[:, 4:36], bre_d.partition_broadcast(128), writes=RW, semkey="brb")
        L = alloc([128, NT, 36], F32)
        gmax = alloc([128, NT], F32)
        ohg = alloc([128, NT, 4], F32)
        tg4 = alloc([128, NT, 4], F32)
        den = alloc([128, NT], F32)
        pg = alloc([128, NT], F32)
        sel4 = alloc([128, NT, 4, 8], F32)
        ing = alloc([128, NT, 8], F32)
        ing2 = alloc([128, NT, 8], F32)
        m1 = alloc([128, NT], F32)
        m2 = alloc([128, NT], F32)
        oh1 = alloc([128, NT, 8], F32)
        oh2 = alloc([128, NT, 8], F32)
        dd = alloc([128, NT], F32)
        e2 = alloc([128, NT], F32)
        w1_ = alloc([128, NT], F32)
        w2_ = alloc([128, NT], F32)
        ge = alloc([128, NT, 8], F32)
        ge2 = alloc([128, NT, 8], F32)
        gate = alloc([128, NT, 4, 8], F32)
        glo = alloc([32, S], BF16)

        def bc3(a2, n):
            return a2.unsqueeze(2).to_broadcast([128, NT, n])

        def DV(meth, **kw):
            return OP("dve", meth, reads=RW + Rlogit, writes=RW, **kw)

        DV("tensor_tensor", out=L, in0=logit[:], in1=brb.unsqueeze(1).to_broadcast([128, NT, 36]), op=ALU.add)
        gl = L[:, :, 0:4]
        el = L[:, :, 4:36].rearrange("p t (g e) -> p t g e", g=4)
        DV("tensor_reduce", out=gmax, in_=gl, axis=AX.X, op=ALU.max)
        DV("tensor_tensor", out=ohg, in0=gl, in1=bc3(gmax, 4), op=ALU.is_equal)
        DV("tensor_tensor", out=tg4, in0=gl, in1=bc3(gmax, 4), op=ALU.subtract)
        OP("act", "activation", out=tg4, in_=tg4, func=AF.Exp, reads=RW, writes=RW)
        DV("tensor_reduce", out=den, in_=tg4, axis=AX.X, op=ALU.add)
        DV("reciprocal", out=pg, in_=den)
        DV("tensor_tensor", out=sel4, in0=el, in1=ohg.unsqueeze(3).to_broadcast([128, NT, 4, 8]), op=ALU.mult)
        DV("tensor_reduce", out=ing, in_=sel4.rearrange("p t g e -> p t e g"), axis=AX.X, op=ALU.add)
        DV("tensor_reduce", out=m1, in_=ing, axis=AX.X, op=ALU.max)
        DV("tensor_tensor", out=oh1, in0=ing, in1=bc3(m1, 8), op=ALU.is_equal)
        DV("scalar_tensor_tensor", out=ing2, in0=oh1, scalar=-1.0e30, in1=ing, op0=ALU.mult, op1=ALU.add)
        DV("tensor_reduce", out=m2, in_=ing2, axis=AX.X, op=ALU.max)
        DV("tensor_tensor", out=oh2, in0=ing2, in1=bc3(m2, 8), op=ALU.is_equal)
        DV("tensor_tensor", out=dd, in0=m2, in1=m1, op=ALU.subtract)
        OP("act", "activation", out=e2, in_=dd, func=AF.Exp, reads=RW, writes=RW)
        DV("tensor_scalar", out=w1_, in0=e2, scalar1=1.0, scalar2=None, op0=ALU.add)
        DV("reciprocal", out=w1_, in_=w1_)
        DV("tensor_tensor", out=w2_, in0=e2, in1=w1_, op=ALU.mult)
        DV("tensor_tensor", out=w1_, in0=w1_, in1=pg, op=ALU.mult)
        DV("tensor_tensor", out=w2_, in0=w2_, in1=pg, op=ALU.mult)
        DV("tensor_tensor", out=ge, in0=oh1, in1=bc3(w1_, 8), op=ALU.mult)
        DV("tensor_tensor", out=ge2, in0=oh2, in1=bc3(w2_, 8), op=ALU.mult)
        DV("tensor_tensor", out=ge, in0=ge, in1=ge2, op=ALU.add)
        DV("tensor_tensor", out=gate, in0=ohg.unsqueeze(3).to_broadcast([128, NT, 4, 8]),
           in1=ge.unsqueeze(2).to_broadcast([128, NT, 4, 8]), op=ALU.mult)
        if debug:
            Rfinal.append(p.res())
            p.dma(dbg["gate"], gate.rearrange("p t g e -> p (t g e)"), reads=RW, writes=[Rfinal[-1]], semkey="dbg3")
        for t in range(NT):
            OP("pe", "transpose", out=ps[0:32, t // 4, (t % 4) * 128:(t % 4 + 1) * 128],
               in_=gate[:, t, :, :].rearrange("p g e -> p (g e)"), identity=ident, reads=RW + [Rc], writes=[Rps[t // 4]])
        gps = ps[0:32, 0:4, :].rearrange("p a b -> p (a b)")
        OP("act", "activation", out=gT[0:32, :], in_=gps, func=AF.Copy, reads=Rps[0:4], writes=[RgT])
        OP("dve", "tensor_tensor", out=glo, in0=gps, in1=gT[0:32, :], op=ALU.subtract, reads=Rps[0:4] + [RgT], writes=RW)
        OP("dve", "tensor_copy", out=gT[32:64, :], in_=glo, reads=RW, writes=[RgT])
        if stop_after == "ROUTE":
            Rfinal.extend(Ryacc + [RgT] + RW)
            return finish()

        p.barrier(scratch[:])
        areset(route_base)
        hg = [alloc([128, 2, S], BF16) for i in range(2)]
        Rhg = [[p.res(f"hg{i}_{g}") for g in range(NG)] for i in range(2)]
        s_sb = [alloc([128, 512], F32) for i in range(2)]
        u_sb = [alloc([128, 512], F32) for i in range(2)]
        gbc = [alloc([128, 512], F32) for i in range(2)]
        Rs = [p.res(f"s{i}") for i in range(2)]
        Ru = [p.res(f"u{i}") for i in range(2)]
        Rgbc = [p.res(f"gbc{i}") for i in range(2)]

        ybank = [0]

        def down_unit(e, t, half):
            hi_ = e % 2
            b = 5 + (ybank[0] % 3)
            ybank[0] += 1
            for fc in range(2):
                MM(ps[:, b, :], hg[hi_][:, fc, t * 128:(t + 1) * 128], w2s[e % NS2][:, fc, half * 512:(half + 1) * 512],
                   fc == 0, fc == 1, [Rhg[hi_][t // 4], Rw2[e % NS2]], [Rps[b]])
            OP("dve", "tensor_tensor", out=yacc[:, t, half * 512:(half + 1) * 512],
               in0=ps[:, b, :], in1=yacc[:, t, half * 512:(half + 1) * 512], op=ALU.add,
               reads=[Rps[b], Ryacc[t]], writes=[Ryacc[t]])

        gcnt = 0
        for e in range(32):
            sl = e % NSLOT
            hi_ = e % 2
            if e + 2 < 32:
                load_expert(e + 2)
            for g in range(NG):
                gs = slice(g * 512, (g + 1) * 512)
                RxG = RxnT[g * 4:(g + 1) * 4]
                gb2 = gcnt % 2
                gcnt += 1
                dq = []
                if e > 0:
                    dq = [(t, half) for t in range(g * 4, (g + 1) * 4) for half in range(2)]
                MM(ps[:, 4, :], csel[:, e * 128:(e + 1) * 128], gT[:, gs], True, True, [Rcsel, RgT], [Rps[4]])
                OP("act", "activation", out=gbc[gb2], in_=ps[:, 4, :], func=AF.Copy, reads=[Rps[4]], writes=[Rgbc[gb2]])
                for wi_, (wsb, Rw_) in enumerate(((w1s[sl], Rw1[sl]), (w3s[sl], Rw3[sl]))):
                    for fc in range(2):
                        b = wi_ * 2 + fc
                        for k in range(8):
                            MM(ps[:, b, :], wsb[:, k, fc * 128:(fc + 1) * 128], xnT[:, k, gs], k == 0, k == 7,
                               [Rw_] + RxG, [Rps[b]])
                        for (t, half) in dq[:2]:
                            down_unit(e - 1, t, half)
                        dq = dq[2:]
                for fc in range(2):
                    OP("act", "activation", out=s_sb[fc], in_=ps[:, fc, :], func=AF.Silu, reads=[Rps[fc]], writes=[Rs[fc]])
                    OP("dve", "tensor_tensor", out=u_sb[fc], in0=ps[:, 2 + fc, :], in1=s_sb[fc], op=ALU.mult,
                       reads=[Rps[2 + fc], Rs[fc]], writes=[Ru[fc]])
                    OP("pool", "tensor_tensor", out=hg[hi_][:, fc, gs], in0=u_sb[fc], in1=gbc[gb2], op=ALU.mult,
                       reads=[Ru[fc], Rgbc[gb2]], writes=[Rhg[hi_][g]])
        for t in range(NT):
            for half in range(2):
                down_unit(31, t, half)
        for t in range(NT):
            Rf = p.res()
            Rfinal.append(Rf)
            p.dma(out_d[t * 128:(t + 1) * 128, :], yacc[:, t, :], reads=[Ryacc[t]], writes=[Rf], semkey=("out", t % 4))
        return finish()


_IN_KEYS = ["g_norm_mix", "w_in", "b_merge_gate", "g_q", "g_k", "w_branch_att", "g_ret_norm", "w_branch_ret",
            "w_out", "g_norm_ffn", "w_router_group", "b_router_group", "w_router_expert", "b_router_expert",
            "w1", "w3", "w2"]


def _run(inputs, debug=False, stop_after=None, cores=8, trace=False):
    cfd, cbd, gamma_c = _host_consts()
    cfs_arr, cfs_offs = _pack(cfd, CFS_ORDER, np.float32)
    cbs_arr, cbs_offs = _pack(cbd, CBS_ORDER, ml_dtypes.bfloat16)
    nc = build_nc(cfs_offs, cbs_offs, cfs_arr.shape[1], cbs_arr.shape[1], gamma_c, debug=debug, stop_after=stop_after)
    shared = {}
    for k in _IN_KEYS:
        a = np.asarray(inputs[k], dtype=np.float32)
        shared[k] = np.ascontiguousarray(a[0])
    shared["cfs"] = cfs_arr
    shared["cbs"] = cbs_arr
    shared["catt"] = np.ascontiguousarray(np.concatenate([cfd["CA"], cfd["SA"]], axis=1))
    shared["cret"] = np.ascontiguousarray(np.concatenate([cfd["CR"], cfd["SR"]], axis=1))
    shared["csel"] = np.ascontiguousarray(cbd["sel"].astype(ml_dtypes.bfloat16))
    x = np.asarray(inputs["x"], dtype=np.float32)
    in_maps = []
    for c in range(cores):
        m = dict(shared)
        m["x"] = np.ascontiguousarray(x[c])
        in_maps.append(m)
    res = run_bass_kernel_spmd(nc, in_maps, core_ids=list(range(cores)), trace=trace)
    return res


def kernel(**inputs):
    res = _run(inputs)
    out = np.stack([np.asarray(r["out"], dtype=np.float32) for r in res.results], axis=0)
    return out
```

```python
import numpy as np
import ml_dtypes
import concourse.bass as bass
import concourse.mybir as mybir
from concourse.bass_utils import run_bass_kernel_spmd
from contextlib import ExitStack

F32 = mybir.dt.float32
BF16 = mybir.dt.bfloat16
ALU = mybir.AluOpType
AF = mybir.ActivationFunctionType
AX = mybir.AxisListType

S = 2048
D = 1024
NT = 16
NG = 4
EPS = 1e-6
ENGS = ("pe", "act", "dve", "pool", "sp")


class Res:
    __slots__ = ("name", "writer", "readers")

    def __init__(self, name):
        self.name = name
        self.writer = None
        self.readers = []


class Op:
    __slots__ = ("eng", "fn", "deps", "signal", "value", "dma", "semkey", "idx")


class Prog:
    def __init__(self, nc, es):
        self.nc = nc
        self.es = es
        self.ops = {e: [] for e in ENGS}
        self.dma_sems = {}
        self.nres = 0
        self.excl = set()
        self.all_res = []
        self.last_barrier = None

    def res(self, name=None):
        self.nres += 1
        r = Res(name or f"r{self.nres}")
        r.writer = self.last_barrier
        self.all_res.append(r)
        return r

    def sb(self, name, shape, dt):
        return self.es.enter_context(self.nc.sbuf_tensor(name, list(shape), dt))

    def op(self, eng, meth, args=(), kw=None, reads=(), writes=(), dma=False, semkey=None):
        o = Op()
        o.eng = eng
        o.fn = (meth, tuple(args), dict(kw or {}))
        o.signal = False
        o.value = None
        o.dma = dma
        o.semkey = semkey
        if eng in ("act", "dve") and self.excl:
            extra = [r for r in reads if id(r) in self.excl and all(r is not w for w in writes)]
            if extra:
                writes = list(writes) + extra
        deps = {}
        for r in reads:
            if r.writer is not None:
                deps[id(r.writer)] = (r.writer, True)
        for w in writes:
            if w.writer is not None and id(w.writer) not in deps:
                deps[id(w.writer)] = (w.writer, False)
            for rd in w.readers:
                if id(rd) not in deps:
                    deps[id(rd)] = (rd, False)
        dl = []
        for d, raw in deps.values():
            if d is o:
                continue
            if (not d.dma) and (not dma) and d.eng == eng:
                if eng in ("pe", "sp"):
                    continue
            dl.append(d)
            d.signal = True
        o.deps = dl
        for r in reads:
            r.readers.append(o)
        for w in writes:
            w.writer = o
            w.readers = []
        if dma:
            if semkey not in self.dma_sems:
                h = self.es.enter_context(self.nc.semaphore(f"dq{len(self.dma_sems)}"))
                self.dma_sems[semkey] = [h, 0]
            ent = self.dma_sems[semkey]
            ent[1] += 16
            o.value = ent[1]
        o.idx = len(self.ops[eng])
        self.ops[eng].append(o)
        return o

    def dma(self, out, in_, reads=(), writes=(), semkey=None, eng="sp", **kw):
        kw = dict(kw)
        kw["out"] = out
        kw["in_"] = in_
        return self.op(eng, "dma_start", (), kw, reads, writes, dma=True, semkey=semkey)

    def barrier(self, scratch_ap):
        allr = list(self.all_res)
        o = self.op("dve", "memset", (scratch_ap, 0.0), None, reads=allr, writes=allr)
        self.last_barrier = o
        return o

    def emit(self):
        nc = self.nc
        esem = {e: self.es.enter_context(nc.semaphore(f"e_{e}")) for e in ENGS if e != "sp"}
        for e in ENGS:
            c = 0
            for o in self.ops[e]:
                if o.dma:
                    continue
                if o.signal:
                    c += 1
                    o.value = c
        ops = self.ops
        dma_sems = self.dma_sems

        def run(e, engobj):
            waited = {}
            for o in ops[e]:
                need = {}
                for d in o.deps:
                    if d.dma:
                        key = ("d", d.semkey)
                        h = dma_sems[d.semkey][0]
                    else:
                        key = ("e", d.eng)
                        h = esem[d.eng]
                    if key not in need or need[key][1] < d.value:
                        need[key] = (h, d.value)
                for key, (h, v) in need.items():
                    if waited.get(key, 0) >= v:
                        continue
                    waited[key] = v
                    engobj.wait_ge(h, v)
                meth, a, kw = o.fn
                if meth is None:
                    continue
                ins = getattr(engobj, meth)(*a, **kw)
                if o.dma:
                    ins.then_inc(dma_sems[o.semkey][0], 16)
                elif o.signal:
                    ins.then_inc(esem[e], 1)

        with nc.Block() as block:
            @block.tensor
            def _(eng):
                run("pe", eng)

            @block.scalar
            def _(eng):
                run("act", eng)

            @block.vector
            def _(eng):
                run("dve", eng)

            @block.gpsimd
            def _(eng):
                run("pool", eng)

            @block.sync
            def _(eng):
                run("sp", eng)


def _host_consts():
    f32 = np.float32
    t = np.arange(S, dtype=f32)
    inv_a = (f32(10000.0) ** (-np.arange(0, 64, 2, dtype=f32) / f32(64))).astype(f32)
    ang = (t[None, :] * inv_a[:, None]).astype(f32)
    p = np.arange(128)
    CA = np.cos(ang)[p % 32].astype(f32)
    sgn = np.where((p % 64) < 32, -1.0, 1.0).astype(f32)
    SA = (np.sin(ang)[p % 32] * sgn[:, None]).astype(f32)
    inv_r = (f32(1.0) / (f32(10000.0) ** np.linspace(0.0, 1.0, 128, dtype=f32))).astype(f32)
    angr = (t[None, :] * inv_r[:, None]).astype(f32)
    CR = np.cos(angr).astype(f32)
    SR = np.sin(angr).astype(f32)
    lg = np.log(f32(1.0) - np.exp2(f32(-5.0) - np.arange(4, dtype=f32))).astype(f32)
    idx = np.arange(128, dtype=f32)
    diff = idx[None, :] - idx[:, None]
    decT = np.zeros((128, 4, 128), f32)
    for h in range(4):
        decT[:, h, :] = np.where(diff >= 0, np.exp(lg[h] * np.maximum(diff, 0.0)), 0.0) / 16.0
    zeta = (np.exp(lg[None, :] * (127.0 - idx[:, None])) / 16.0).astype(f32)
    xi = np.exp(lg[:, None] * (idx[None, :] + 1.0)).astype(f32)
    xib = np.broadcast_to(xi[None], (128, 4, 128)).astype(f32)
    gamma_c = np.exp(lg * 128.0).astype(f32)
    ident = np.eye(128, dtype=f32)
    cf = {
        "CA": CA, "SA": SA, "CR": CR, "SR": SR,
        "decT": decT.reshape(128, 512), "zeta": zeta, "xib": xib.reshape(128, 512), "ident": ident,
    }
    partner = np.where((p % 64) < 32, p + 32, p - 32)
    perm = np.zeros((128, 128), f32)
    perm[partner, p] = 1.0
    bones = (p[:, None] // 64 == p[None, :] // 64).astype(f32)
    kq = np.arange(128)
    mcur = (kq[None, :] >= kq[:, None]).astype(f32)
    mprev = (kq[:, None] >= kq[None, :]).astype(f32)
    mcur4 = np.tile(mcur, (1, 4))
    mprev4 = np.tile(mprev, (1, 4))
    sel = np.zeros((128, 32, 128), f32)
    for e in range(32):
        sel[e, e, :] = 1.0
        sel[32 + e, e, :] = 1.0
    cb = {
        "perm": perm, "bones": bones, "mcur4": mcur4, "mprev4": mprev4,
        "identb": ident, "sel": sel.reshape(128, 4096),
    }
    return cf, cb, gamma_c


def _pack(dct, order, dtype):
    offs = {}
    cols = 0
    for k in order:
        offs[k] = (cols, dct[k].shape[1])
        cols += dct[k].shape[1]
    arr = np.zeros((128, cols), dtype=dtype)
    for k in order:
        a, n = offs[k]
        arr[:, a:a + n] = dct[k].astype(dtype)
    return arr, offs


CFS_ORDER = ["decT", "zeta", "xib", "ident"]
CBS_ORDER = ["perm", "bones", "mcur4", "mprev4", "identb"]


ARENA_BYTES = 152 * 1024


def build_nc(cfs_offs, cbs_offs, cfs_cols, cbs_cols, gamma_c, debug=False, stop_after=None):
    nc = bass.Bass("TRN2", target_bir_lowering=False)

    def din(name, shape, dt=F32):
        return nc.dram_tensor(name, list(shape), dt, kind="ExternalInput").ap()

    x_d = din("x", [S, D])
    gmix_d = din("g_norm_mix", [D])
    w_in_d = din("w_in", [D, 11264])
    bgate_d = din("b_merge_gate", [2 * D])
    gq_d = din("g_q", [64])
    gk_d = din("g_k", [64])
    watt_d = din("w_branch_att", [D, D])
    gret_d = din("g_ret_norm", [4, 512])
    wret_d = din("w_branch_ret", [2 * D, D])
    wout_d = din("w_out", [D, D])
    gffn_d = din("g_norm_ffn", [D])
    wrg_d = din("w_router_group", [D, 4])
    brg_d = din("b_router_group", [4])
    wre_d = din("w_router_expert", [D, 32])
    bre_d = din("b_router_expert", [32])
    w1_d = din("w1", [32, D, 256])
    w3_d = din("w3", [32, D, 256])
    w2_d = din("w2", [32, 256, D])
    cfs_d = din("cfs", [128, cfs_cols])
    cbs_d = din("cbs", [128, cbs_cols], BF16)
    catt_d = din("catt", [128, 2 * S])
    cret_d = din("cret", [128, 2 * S])
    csel_d = din("csel", [128, 4096], BF16)
    out_d = nc.dram_tensor("out", [S, D], F32, kind="ExternalOutput").ap()
    skind = "ExternalOutput" if debug else "Internal"
    vx_d = nc.dram_tensor("vx", [S, 8, 256], BF16, kind=skind).ap()
    zatt_d = nc.dram_tensor("zatt", [D, S], BF16, kind=skind).ap()
    zret_d = nc.dram_tensor("zret", [2 * D, S], BF16, kind=skind).ap()
    dbg = {}
    if debug:
        dbg["xnT"] = nc.dram_tensor("d_xnT", [128, 8 * S], BF16, kind="ExternalOutput").ap()
        dbg["x1"] = nc.dram_tensor("d_x1", [S, D], F32, kind="ExternalOutput").ap()
        dbg["logits"] = nc.dram_tensor("d_logits", [128, NT * 36], F32, kind="ExternalOutput").ap()
        dbg["gate"] = nc.dram_tensor("d_gate", [128, NT * 32], F32, kind="ExternalOutput").ap()

    with ExitStack() as es:
        p = Prog(nc, es)
        ps = es.enter_context(nc.psum_tensor("ps", [128, 8, 512], F32))
        Rps = [p.res(f"psb{b}") for b in range(8)]
        p.excl.update(id(r) for r in Rps)

        def psb(b):
            return ps[:, b, :].bitcast(BF16)

        def OP(eng, meth, *a, reads=(), writes=(), **kw):
            return p.op(eng, meth, a, kw, reads, writes)

        def MM(out, lhsT, rhs, start, stop, reads, writes, **kw):
            return p.op("pe", "matmul", (out,), dict(lhsT=lhsT, rhs=rhs, start=start, stop=stop, **kw), reads, writes)

        arena = p.sb("arena", [128, ARENA_BYTES // 2], BF16)
        apos = [0]

        def areset(pos=0):
            apos[0] = pos

        def alloc(shape, dt):
            n = 1
            for s_ in shape[1:]:
                n *= s_
            nbytes = n * (4 if dt == F32 else 2)
            nbytes = (nbytes + 63) // 64 * 64
            a = apos[0]
            assert a + nbytes <= ARENA_BYTES, ("arena overflow", a, nbytes)
            apos[0] = a + nbytes
            v = arena[0:shape[0], a // 2:(a + n * (4 if dt == F32 else 2)) // 2]
            if dt == F32:
                v = v.bitcast(F32)
            if len(shape) == 3:
                v = v.rearrange("p (a b) -> p a b", a=shape[1])
            elif len(shape) == 4:
                v = v.rearrange("p (a b c) -> p a b c", a=shape[1], b=shape[2])
            return v

        cfs = p.sb("cfs_sb", [128, cfs_cols], F32)
        cbs = p.sb("cbs_sb", [128, cbs_cols], BF16)
        Rc = p.res("consts")
        Rcs = []

        def cres():
            r = p.res()
            Rcs.append(r)
            return [r]

        def CF(k):
            a, n = cfs_offs[k]
            return cfs[:, a:a + n]

        def CB(k):
            a, n = cbs_offs[k]
            return cbs[:, a:a + n]

        p.dma(cfs[:], cfs_d, writes=cres(), semkey="c")
        p.dma(cbs[:], cbs_d, writes=cres(), semkey="c")
        gmix = p.sb("gmix", [128, 8], F32)
        gffn = p.sb("gffn", [128, 8], F32)
        bga = p.sb("bga", [128, 8], F32)
        bgb = p.sb("bgb", [128, 8], F32)
        gqk = p.sb("gqk", [128, 4], F32)
        epsc = p.sb("epsc", [128, 1], F32)
        scratch = p.sb("scratch", [128, 1], F32)
        p.dma(gmix[:], gmix_d.rearrange("(k p) -> p k", p=128), writes=cres(), semkey="c", allow_slow_non_contiguous=True, eng="act")
        p.dma(gffn[:], gffn_d.rearrange("(k p) -> p k", p=128), writes=cres(), semkey="c", allow_slow_non_contiguous=True, eng="act")
        p.dma(bga[:], bgate_d[0:D].rearrange("(k p) -> p k", p=128), writes=cres(), semkey="c", allow_slow_non_contiguous=True, eng="act")
        p.dma(bgb[:], bgate_d[D:2 * D].rearrange("(k p) -> p k", p=128), writes=cres(), semkey="c", allow_slow_non_contiguous=True, eng="act")
        for ci, gd in ((0, gq_d), (2, gk_d)):
            for half in range(2):
                p.dma(gqk[half * 64:(half + 1) * 64, ci:ci + 1], gd.rearrange("(p o) -> p o", o=1), writes=cres(), semkey="c", eng="act")
                for q4 in range(2):
                    p.dma(gqk[half * 64 + q4 * 32: half * 64 + q4 * 32 + 32, ci + 1:ci + 2],
                          gd[(1 - q4) * 32:(1 - q4) * 32 + 32].rearrange("(p o) -> p o", o=1), writes=cres(), semkey="c", eng="act")
        OP("dve", "memset", epsc[:], EPS, reads=Rcs, writes=[Rc])

        ident = CF("ident")
        identb = CB("identb")
        perm, bones = CB("perm"), CB("bones")
        mcur4, mprev4 = CB("mcur4"), CB("mprev4")
        decT = CF("decT").rearrange("p (h i) -> p h i", h=4)
        zeta = CF("zeta")
        xib = CF("xib").rearrange("p (h i) -> p h i", h=4)

        Rfinal = []
        xnT = p.sb("xnT", [128, 8, S], BF16)
        RxnT = [p.res(f"xnT{t}") for t in range(NT)]
        ss = p.sb("ss", [128, NT], F32)
        rt = p.sb("rt", [128, NT], F32)
        rstd = p.sb("rstd", [128, NT], F32)
        Rxin = [p.res(f"xin{i}") for i in range(2)]
        xs = [p.sb(f"xs{i}", [128, D], F32) for i in range(2)]
        Rxs = [p.res(f"xs{i}") for i in range(2)]
        junk = p.sb("junk", [128, D], BF16)
        Rjunk = p.res("junk")
        logit = p.sb("logit", [128, NT, 36], F32)
        Rlogit = [p.res(f"logit{t}") for t in range(NT)]

        def norm_tile_gen(src, Rsrc, t, gcol, Rstat_t, banks, lo=None, stats_done=False):
            i = t % 2
            if not stats_done:
                OP("act", "activation", out=junk[:], in_=src, func=AF.Square, accum_out=ss[:, t:t + 1],
                   reads=[Rsrc, Rc], writes=[Rjunk, Rstat_t])
                OP("act", "activation", out=rt[:, t:t + 1], in_=ss[:, t:t + 1], func=AF.Sqrt, scale=1.0 / D, bias=epsc[:],
                   reads=[Rstat_t, Rc], writes=[Rstat_t])
                OP("dve", "reciprocal", out=rstd[:, t:t + 1], in_=rt[:, t:t + 1], reads=[Rstat_t], writes=[Rstat_t])
            OP("dve", "tensor_scalar", out=xs[i][:], in0=src, scalar1=rstd[:, t:t + 1], scalar2=None, op0=ALU.mult,
               reads=[Rsrc, Rstat_t], writes=[Rxs[i]])
            yield
            for hb in range(2):
                b = banks[hb]
                for j in range(4):
                    k = hb * 4 + j
                    OP("pe", "transpose", out=ps[:, b, j * 128:(j + 1) * 128], in_=xs[i][:, k * 128:(k + 1) * 128],
                       identity=ident, reads=[Rxs[i], Rc], writes=[Rps[b]])
                gb_ = gcol[:, hb * 4:hb * 4 + 4].unsqueeze(2).to_broadcast([128, 4, 128])
                pin = ps[:, b, :].rearrange("p (j c) -> p j c", j=4)
                dsl = xnT[:, hb * 4:hb * 4 + 4, t * 128:(t + 1) * 128]
                if lo is None:
                    OP("dve", "tensor_tensor", out=dsl, in0=pin, in1=gb_, op=ALU.mult,
                       reads=[Rps[b], Rc], writes=[RxnT[t]])
                else:
                    vtmp, Rvtmp, lodst, Rlo = lo
                    OP("dve", "tensor_tensor", out=vtmp[:, hb * 4:hb * 4 + 4, :], in0=pin, in1=gb_, op=ALU.mult,
                       reads=[Rps[b], Rc], writes=[Rvtmp[hb]])
                    OP("act", "activation", out=dsl, in_=vtmp[:, hb * 4:hb * 4 + 4, :], func=AF.Copy,
                       reads=[Rvtmp[hb]], writes=[RxnT[t]])
                    OP("pool", "tensor_tensor", out=lodst[:, hb * 4:hb * 4 + 4, :], in0=vtmp[:, hb * 4:hb * 4 + 4, :],
                       in1=dsl, op=ALU.subtract, reads=[Rvtmp[hb], RxnT[t]], writes=[Rlo[hb]])
            yield

        def finish():
            p.op("sp", None, reads=list(Rfinal))
            p.emit()
            return nc

        def wsrc(wd, c0, c1):
            return wd[:, c0:c1].rearrange("(k p) n -> p k n", p=128)

        def load_w(dst_ap, src_ap, R, key):
            return p.dma(dst_ap, src_ap, writes=[R], semkey=key, eng="pool")

        Rstat = [p.res(f"stat{t}") for t in range(NT)]
        areset()
        xin = [alloc([128, D], F32) for i in range(4)]
        Rxin = [p.res(f"xin{i}") for i in range(4)]
        def vproj_tile(t):
            i = t % 2
            for cg in range(2):
                b = 4 + cg
                for k in range(8):
                    MM(ps[:, b, :], xnT[:, k, t * 128:(t + 1) * 128], wbig[cg][:, k, :], k == 0, k == 7,
                       [RxnT[t], Rwbig[cg]], [Rps[b]])
                pv = ps[:, b, :].rearrange("p (pr e d) -> p pr e d", e=2, d=64)
                OP("act", "activation", out=vst[i][:, cg * 4:(cg + 1) * 4, 0:64], in_=pv[:, :, 0, :], func=AF.Copy,
                   reads=[Rps[b]], writes=[Rvst[i]])
                OP("dve", "tensor_copy", out=vst[i][:, cg * 4:(cg + 1) * 4, 192:256], in_=pv[:, :, 1, :],
                   reads=[Rps[b]], writes=[Rvst[i]])
            p.dma(vx_d[t * 128:(t + 1) * 128, :, :], vst[i], reads=[Rvst[i]], writes=[Rvx[t]], semkey=("vx", t % 2))

        wbig = [alloc([128, 8, 512], BF16) for i in range(2)]
        Rwbig = [p.res(f"wbig{i}") for i in range(2)]
        vst = [alloc([128, 8, 256], BF16) for i in range(2)]
        Rvst = [p.res(f"vst{i}") for i in range(2)]
        Rvx = [p.res(f"vx{t}") for t in range(NT)]
        for i in range(2):
            OP("pool", "memset", vst[i], 1.0, writes=[Rvst[i]])
        for cg in range(2):
            load_w(wbig[cg], wsrc(w_in_d, 2048 + cg * 512, 2048 + (cg + 1) * 512), Rwbig[cg], ("wbig", cg))
        gens = {}
        do_v = stop_after != "A"
        for t in range(min(3, NT)):
            p.dma(xin[t % 4], x_d[t * 128:(t + 1) * 128, :], writes=[Rxin[t % 4]], semkey=("xin", t % 4))
        for step in range(NT + 2):
            if step < NT:
                t = step
                if t + 3 < NT:
                    p.dma(xin[(t + 3) % 4], x_d[(t + 3) * 128:(t + 4) * 128, :], writes=[Rxin[(t + 3) % 4]],
                          semkey=("xin", (t + 3) % 4))
                gens[t] = norm_tile_gen(xin[t % 4], Rxin[t % 4], t, gmix, Rstat[t], (6, 7))
                next(gens[t])
            if 0 <= step - 1 < NT:
                next(gens[step - 1])
            if do_v and 0 <= step - 2 < NT:
                vproj_tile(step - 2)
        if debug:
            Rfinal.append(p.res())
            p.dma(dbg["xnT"], xnT[:].rearrange("p k s -> p (k s)"), reads=RxnT, writes=[Rfinal[-1]], semkey="dbg0")
        if stop_after == "A":
            Rfinal.append(p.res("fin"))
            p.dma(out_d[0:128, :], xin[1], reads=[Rxin[1]], writes=[Rfinal[-1]], semkey="fin")
            return finish()

        if stop_after == "V":
            Rfinal.extend(Rvx)
            return finish()

        p.barrier(scratch[:])
        areset()
        catt = alloc([128, 2 * S], F32)
        Rcatt = p.res("catt")
        p.dma(catt, catt_d, writes=[Rcatt], semkey="catt")
        CA, SA = catt[:, 0:S], catt[:, S:2 * S]
        wqk = [[alloc([128, 8, 128], BF16) for j in range(2)] for i in range(2)]
        Rwqk = [[p.res(f"wqk{i}_{j}") for j in range(2)] for i in range(2)]
        QKT = [[alloc([128, S], BF16) for j in range(2)] for i in range(2)]
        RQK = [[[p.res(f"QK{i}_{j}_{g}") for g in range(NG)] for j in range(2)] for i in range(2)]
        vxs = [[alloc([128, 16, 256], BF16) for o in range(3)] for i in range(2)]
        Rvxs = [[p.res(f"vxs{i}_{o}") for o in range(3)] for i in range(2)]
        qbf = alloc([128, 512], BF16)
        sqb = alloc([128, 512], BF16)
        rtq = alloc([128, 512], F32)
        rsq = alloc([128, 512], F32)
        t1 = alloc([128, 512], F32)
        t2 = alloc([128, 512], F32)
        Rqbf, Rsqb, Rrtq, Rrsq, Rt1, Rt2 = (p.res(n) for n in ("qbf", "sqb", "rtq", "rsq", "t1", "t2"))
        Pb = [alloc([128, 512], BF16) for i in range(3)]
        RPb = [p.res(f"Pb{i}") for i in range(3)]
        rden = alloc([128, S], F32)
        Rrden = [p.res(f"rden{b}") for b in range(4)]
        zpair = [alloc([128, S], BF16) for i in range(2)]
        Rzpair = [p.res(f"zpair{i}") for i in range(2)]
        Rzatt = [p.res(f"zatt{i}") for i in range(8)]

        def load_pair_weights(pr):
            i = pr % 2
            load_w(wqk[i][0], wsrc(w_in_d, pr * 128, (pr + 1) * 128), Rwqk[i][0], ("wqk", i, 0))
            load_w(wqk[i][1], wsrc(w_in_d, 1024 + pr * 128, 1024 + (pr + 1) * 128), Rwqk[i][1], ("wqk", i, 1))

        def load_pair_v(pr):
            i = pr % 2
            src = vx_d[:, pr, :]
            p.dma(vxs[i][0], src.rearrange("(n i) m -> i n m", i=128), reads=Rvx, writes=[Rvxs[i][0]], semkey=("vxs", i, 0))
            s1 = src.rearrange("(n i c) m -> c i n m", i=128, c=4)
            for c in range(4):
                p.dma(vxs[i][1][:, c * 4:(c + 1) * 4, :], s1[c], reads=Rvx, writes=[Rvxs[i][1]], semkey=("vxs", i, 1))
            p.dma(vxs[i][2], src.rearrange("(i c) m -> i c m", c=16), reads=Rvx, writes=[Rvxs[i][2]], semkey=("vxs", i, 2))

        def colap(T, spec, r0):
            base, r, c = spec
            if r == 1:
                return T[r0:r0 + 64, base:base + 128]
            return T[r0:r0 + 64, base:base + 128 * r].rearrange("p (n r) -> p n r", r=r)[:, :, c]

        def prep_gen(pr):
            i = pr % 2
            for j, gi in ((0, 0), (1, 2)):
                dstT = QKT[i][j]
                for g in range(NG):
                    gs = slice(g * 512, (g + 1) * 512)
                    for k in range(8):
                        MM(ps[:, 6, :], wqk[i][j][:, k, :], xnT[:, k, gs], k == 0, k == 7,
                           [Rwqk[i][j]] + RxnT[g * 4:(g + 1) * 4], [Rps[6]])
                    OP("act", "activation", out=qbf, in_=ps[:, 6, :], func=AF.Copy, reads=[Rps[6]], writes=[Rqbf])
                    OP("dve", "scalar_tensor_tensor", out=t1, in0=ps[:, 6, :], scalar=gqk[:, gi:gi + 1], in1=CA[:, gs],
                       op0=ALU.mult, op1=ALU.mult, reads=[Rps[6], Rc, Rcatt], writes=[Rt1])
                    OP("pool", "tensor_tensor", out=sqb, in0=qbf, in1=qbf, op=ALU.mult, reads=[Rqbf], writes=[Rsqb])
                    yield
                    MM(ps[:, 7, :], bones, sqb, True, True, [Rsqb, Rc], [Rps[7]])
                    MM(ps[:, 6, :], perm, qbf, True, True, [Rqbf, Rc], [Rps[6]])
                    OP("act", "activation", out=rtq, in_=ps[:, 7, :], func=AF.Ln, scale=1.0 / 64, bias=epsc[:],
                       reads=[Rps[7], Rc], writes=[Rrtq])
                    OP("act", "activation", out=rsq, in_=rtq, func=AF.Exp, scale=-0.5, reads=[Rrtq], writes=[Rrsq])
                    OP("dve", "scalar_tensor_tensor", out=t2, in0=ps[:, 6, :], scalar=gqk[:, gi + 1:gi + 2], in1=SA[:, gs],
                       op0=ALU.mult, op1=ALU.mult, reads=[Rps[6], Rc, Rcatt], writes=[Rt2])
                    OP("pool", "tensor_tensor", out=t1, in0=t1, in1=t2, op=ALU.add, reads=[Rt1, Rt2], writes=[Rt1])
                    OP("dve", "tensor_tensor", out=dstT[:, gs], in0=t1, in1=rsq, op=ALU.mult,
                       reads=[Rt1, Rrsq], writes=[RQK[i][j][g]])
                    yield

        def att_batches(pr):
            def cols(r, c, n):
                return (n * 128 * r, r, c)
            tiles_cur, tiles_prev = [], []
            for n in range(16):
                tiles_cur.append((cols(1, 0, n), cols(1, 0, n), 0, n, ("n", n // 4, (n % 4) * 128, 1), n % 4 == 0))
            for c in range(4):
                for n in range(4):
                    tiles_cur.append((cols(4, c, n), cols(4, c, n), 1, c * 4 + n, ("n", n, c, 4), False))
            for c in range(16):
                tiles_cur.append((cols(16, c, 0), cols(16, c, 0), 2, c, ("s", c), False))
            for n in range(1, 16):
                tiles_prev.append((cols(1, 0, n - 1), cols(1, 0, n), 0, n - 1, ("n", n // 4, (n % 4) * 128, 1), False))
            for c in range(4):
                for n in range(1, 4):
                    tiles_prev.append((cols(4, c, n - 1), cols(4, c, n), 1, c * 4 + n - 1, ("n", n, c, 4), False))
            batches = []
            for lst, msk in ((tiles_cur, mcur4), (tiles_prev, mprev4)):
                for s0 in range(0, len(lst), 4):
                    batches.append((lst[s0:s0 + 4], msk))
            items = []
            for eh in range(2):
                for bi, (tl, msk) in enumerate(batches):
                    items.append((eh, bi, tl, msk, bi == len(batches) - 1))
            return items

        gcount = [0]

        def emit_S(pr, item):
            i = pr % 2
            eh, bi, tl, msk, last = item
            r0 = eh * 64
            gb = gcount[0]
            gcount[0] += 1
            sb_ = 4 + (gb % 2)
            pbi = gb % 3
            nt_ = len(tl)
            QT, KT = QKT[i]
            RQKall = RQK[i][0] + RQK[i][1]
            for ti, (kc, qc, vo, vb, osp, st) in enumerate(tl):
                MM(ps[:, sb_, ti * 128:(ti + 1) * 128], colap(KT, kc, r0), colap(QT, qc, r0), True, True,
                   RQKall, [Rps[sb_]])
            OP("act", "activation", out=Pb[pbi][:, 0:nt_ * 128], in_=ps[:, sb_, 0:nt_ * 128], func=AF.Exp, scale=0.125,
               reads=[Rps[sb_]], writes=[RPb[pbi]])
            OP("dve", "tensor_tensor", out=Pb[pbi][:, 0:nt_ * 128], in0=Pb[pbi][:, 0:nt_ * 128],
               in1=msk[:, 0:nt_ * 128], op=ALU.mult, reads=[RPb[pbi], Rc], writes=[RPb[pbi]])
            return pbi

        def emit_PV(pr, item, pbi):
            i = pr % 2
            eh, bi, tl, msk, last = item
            r0 = eh * 64
            zp = zpair[i]
            for ti, (kc, qc, vo, vb, osp, st) in enumerate(tl):
                lhsT = vxs[i][vo][:, vb, eh * 128:(eh + 1) * 128]
                if osp[0] == "n":
                    _, bank, off, r = osp
                    if r == 1:
                        oap = ps[:, bank, off:off + 128]
                    else:
                        oap = ps[:, bank, :].rearrange("p (n r) -> p n r", r=r)[:, :, off]
                    MM(oap, lhsT, Pb[pbi][:, ti * 128:(ti + 1) * 128], st, False,
                       [RPb[pbi], Rvxs[i][vo]], [Rps[bank]], skip_group_check=True)
                else:
                    c = osp[1]
                    for bank in range(4):
                        oap = ps[:, bank, :].rearrange("p (n r) -> p n r", r=16)[:, :, c]
                        MM(oap, lhsT, Pb[pbi][:, ti * 128 + bank * 32: ti * 128 + bank * 32 + 32], False, False,
                           [RPb[pbi], Rvxs[i][vo]], [Rps[bank]], skip_group_check=True)
            if last:
                d0 = 64 - r0
                for bank in range(4):
                    bs = slice(bank * 512, (bank + 1) * 512)
                    OP("act", "activation", out=rden[r0:r0 + 64, bs], in_=ps[d0:d0 + 64, bank, :], func=AF.Ln,
                       reads=[Rps[bank]], writes=[Rrden[bank]])
                    OP("act", "activation", out=rden[r0:r0 + 64, bs], in_=rden[r0:r0 + 64, bs], func=AF.Exp, scale=-1.0,
                       reads=[Rrden[bank]], writes=[Rrden[bank]])
                    OP("dve", "tensor_tensor", out=zp[r0:r0 + 64, bs], in0=ps[r0:r0 + 64, bank, :],
                       in1=rden[r0:r0 + 64, bs], op=ALU.mult, reads=[Rps[bank], Rrden[bank]], writes=[Rzpair[i]])

        load_pair_weights(0)
        load_pair_weights(1)
        load_pair_v(0)
        for _ in prep_gen(0):
            pass
        for pr in range(8):
            i = pr % 2
            if pr + 1 < 8:
                load_pair_v(pr + 1)
            nxt = prep_gen(pr + 1) if pr + 1 < 8 else None
            items = att_batches(pr)
            pq = [emit_S(pr, items[0]), emit_S(pr, items[1])]
            for ii, item in enumerate(items):
                if ii + 2 < len(items):
                    pq.append(emit_S(pr, items[ii + 2]))
                emit_PV(pr, item, pq.pop(0))
                if nxt is not None and ii % 2 == 1:
                    next(nxt, None)
            if nxt is not None:
                for _ in nxt:
                    pass
            if pr + 2 < 8:
                load_pair_weights(pr + 2)
            p.dma(zatt_d[pr * 128:(pr + 1) * 128, :], zpair[i], reads=[Rzpair[i]], writes=[Rzatt[pr]], semkey=("zatt", i))
        if stop_after == "ATT":
            Rfinal.extend(Rzatt)
            return finish()

        p.barrier(scratch[:])
        areset()
        cret = alloc([128, 2 * S], F32)
        Rcret = p.res("cret")
        p.dma(cret, cret_d, writes=[Rcret], semkey="cret")
        CR, SR = cret[:, 0:S], cret[:, S:2 * S]
        gretb = alloc([128, 4, 512], F32)
        Rgretb = p.res("gretb")
        p.dma(gretb.rearrange("p h v -> p (h v)"), gret_d.rearrange("h v -> (h v)").partition_broadcast(128),
              writes=[Rgretb], semkey="gretb")
        wq_r = [alloc([128, 8, 256], BF16) for i in range(2)]
        wk_r = [alloc([128, 8, 256], BF16) for i in range(2)]
        wv_r = [alloc([128, 8, 512], BF16) for i in range(2)]
        wg_r = [alloc([128, 8, 512], BF16) for i in range(2)]
        Rwr = [[p.res(f"wr{i}_{j}") for j in range(4)] for i in range(2)]
        QrT = [alloc([128, 2, 512], BF16) for i in range(2)]
        KrT = [alloc([128, 2, 512], BF16) for i in range(2)]
        Qxi = [alloc([128, 2, 512], BF16) for i in range(2)]
        RQr = [p.res(f"QrT{i}") for i in range(2)]
        RKr = [p.res(f"KrT{i}") for i in range(2)]
        RQx = [p.res(f"Qxi{i}") for i in range(2)]
        sgr = [alloc([128, 4, 512], BF16) for i in range(2)]
        Rsgr = [p.res(f"sgr{i}") for i in range(2)]
        vr_sb = [alloc([128, 512], BF16) for i in range(2)]
        Rvr = [p.res(f"vr{i}") for i in range(2)]
        kz = [alloc([128, 256], BF16) for i in range(2)]
        Rkz = [p.res(f"kz{i}") for i in range(2)]
        inm = [alloc([128, 128], BF16) for i in range(2)]
        Rinm = [p.res(f"inm{i}") for i in range(2)]
        yn = [alloc([128, 512], BF16) for i in range(2)]
        Ryn = [p.res(f"yn{i}") for i in range(2)]
        zst = [alloc([128, 4, 512], BF16) for i in range(2)]
        Rzst = [p.res(f"zst{i}") for i in range(2)]
        state = alloc([128, 2, 512], F32)
        state_bf = alloc([128, 2, 512], BF16)
        Rstate = [p.res(f"state{c}") for c in range(2)]
        Rstbf = [p.res(f"stbf{c}") for c in range(2)]
        tmp = [alloc([128, 512], F32) for i in range(4)]
        Rtmp = [p.res(f"rtmp{i}") for i in range(4)]
        ssr = alloc([128, 64], F32)
        rtr = alloc([128, 64], F32)
        rsr = alloc([128, 64], F32)
        Rzret = []

        def load_head_weights(h):
            i = h % 2
            load_w(wq_r[i], wsrc(w_in_d, 3072 + h * 256, 3072 + (h + 1) * 256), Rwr[i][0], ("wr", i, 0))
            load_w(wk_r[i], wsrc(w_in_d, 4096 + h * 256, 4096 + (h + 1) * 256), Rwr[i][1], ("wr", i, 1))
            load_w(wv_r[i], wsrc(w_in_d, 5120 + h * 512, 5120 + (h + 1) * 512), Rwr[i][2], ("wr", i, 2))
            load_w(wg_r[i], wsrc(w_in_d, 7168 + h * 512, 7168 + (h + 1) * 512), Rwr[i][3], ("wr", i, 3))

        vr3 = vr_sb + [alloc([128, 512], BF16)]
        Rvr3 = Rvr + [p.res("vr2")]
        kz3 = kz + [alloc([128, 256], BF16)]
        Rkz3 = Rkz + [p.res("kz2")]
        inm3 = inm + [alloc([128, 128], BF16)]
        Rinm3 = Rinm + [p.res("inm2")]
        INNER = ps[:, 3, 0:128]
        ysb = [alloc([128, 512], F32) for i in range(2)]
        Rysb = [p.res(f"ysb{i}") for i in range(2)]

        def proj_pieces(h, g):
            wi = h % 2
            gi = g % 2
            gs = slice(g * 512, (g + 1) * 512)
            RxG = RxnT[g * 4:(g + 1) * 4]

            def qk(which):
                wsb = (wq_r, wk_r)[which][wi]
                dst = (QrT, KrT)[which][gi]
                Rdst = (RQr, RKr)[which][gi]
                for c in range(2):
                    for k in range(8):
                        MM(ps[:, c, :], wsb[:, k, c * 128:(c + 1) * 128], xnT[:, k, gs], k == 0, k == 7,
                           [Rwr[wi][which]] + RxG, [Rps[c]])
                OP("dve", "tensor_tensor", out=tmp[0], in0=ps[:, 0, :], in1=CR[:, gs], op=ALU.mult,
                   reads=[Rps[0], Rcret], writes=[Rtmp[0]])
                OP("dve", "tensor_tensor", out=tmp[1], in0=ps[:, 1, :], in1=SR[:, gs], op=ALU.mult,
                   reads=[Rps[1], Rcret], writes=[Rtmp[1]])
                OP("pool", "tensor_tensor", out=dst[:, 0, :], in0=tmp[0], in1=tmp[1], op=ALU.subtract,
                   reads=[Rtmp[0], Rtmp[1]], writes=[Rdst])
                OP("dve", "tensor_tensor", out=tmp[2], in0=ps[:, 1, :], in1=CR[:, gs], op=ALU.mult,
                   reads=[Rps[1], Rcret], writes=[Rtmp[2]])
                OP("dve", "tensor_tensor", out=tmp[3], in0=ps[:, 0, :], in1=SR[:, gs], op=ALU.mult,
                   reads=[Rps[0], Rcret], writes=[Rtmp[3]])
                OP("pool", "tensor_tensor", out=dst[:, 1, :], in0=tmp[2], in1=tmp[3], op=ALU.add,
                   reads=[Rtmp[2], Rtmp[3]], writes=[Rdst])
                if which == 0:
                    xb = xib[:, h, :].unsqueeze(1).to_broadcast([128, 4, 128])
                    for c in range(2):
                        OP("pool", "tensor_tensor", out=Qxi[gi][:, c, :].rearrange("p (n i) -> p n i", n=4),
                           in0=dst[:, c, :].rearrange("p (n i) -> p n i", n=4), in1=xb, op=ALU.mult,
                           reads=[Rdst, Rc], writes=[RQx[gi]])

            def gate(vc):
                b = vc % 2
                for k in range(8):
                    MM(ps[:, b, :], wg_r[wi][:, k, vc * 128:(vc + 1) * 128], xnT[:, k, gs], k == 0, k == 7,
                       [Rwr[wi][3]] + RxG, [Rps[b]])
                OP("act", "activation", out=sgr[gi][:, vc, :], in_=ps[:, b, :], func=AF.Silu,
                   reads=[Rps[b]], writes=[Rsgr[gi]])

            return [lambda: qk(0), lambda: qk(1), lambda: gate(0), lambda: gate(1), lambda: gate(2), lambda: gate(3)]

        def stage_A(q):
            h, gn = q // 16, q % 16
            g, n = gn // 4, gn % 4
            wi, gi, b3 = h % 2, g % 2, q % 3
            cs = slice(n * 128, (n + 1) * 128)
            for k in range(8):
                MM(ps[:, 2, :], xnT[:, k, gn * 128:(gn + 1) * 128], wv_r[wi][:, k, :], k == 0, k == 7,
                   [Rwr[wi][2], RxnT[gn]], [Rps[2]])
            OP("act", "activation", out=vr3[b3], in_=ps[:, 2, :], func=AF.Copy, reads=[Rps[2]], writes=[Rvr3[b3]])
            for c in range(2):
                OP("pe", "transpose", out=psb(4)[:, 512 + c * 128: 512 + (c + 1) * 128], in_=KrT[gi][:, c, cs],
                   identity=identb, reads=[RKr[gi], Rc], writes=[Rps[4]])
            OP("act", "activation", out=kz3[b3], in_=psb(4)[:, 512:768], func=AF.Copy, scale=zeta[:, h:h + 1],
               reads=[Rps[4], Rc], writes=[Rkz3[b3]])
            for c in range(2):
                MM(INNER, KrT[gi][:, c, cs], QrT[gi][:, c, cs], c == 0, c == 1, [RKr[gi], RQr[gi]], [Rps[3]])
            OP("dve", "tensor_tensor", out=inm3[b3], in0=INNER, in1=decT[:, h, :], op=ALU.mult,
               reads=[Rps[3], Rc], writes=[Rinm3[b3]])

        Rst_q = {}

        def stage_B(q):
            h, gn = q // 16, q % 16
            g, n = gn // 4, gn % 4
            gi, b3, ci = g % 2, q % 3, q % 2
            cs = slice(n * 128, (n + 1) * 128)
            yb = 5
            st_i = q
            gC = float(gamma_c[h])
            MM(ps[:, yb, :], inm3[b3], vr3[b3], True, gn == 0, [Rinm3[b3], Rvr3[b3]], [Rps[yb]])
            if gn > 0:
                for c in range(2):
                    MM(ps[:, yb, :], Qxi[gi][:, c, cs], state_bf[:, c, :], False, c == 1, [RQx[gi], Rstbf[c]], [Rps[yb]])
            if gn < 15:
                for c in range(2):
                    MM(ps[:, 6 + c, :], kz3[b3][:, c * 128:(c + 1) * 128], vr3[b3], True, True,
                       [Rkz3[b3], Rvr3[b3]], [Rps[6 + c]])
                    if gn == 0:
                        OP("dve", "tensor_copy", out=state[:, c, :], in_=ps[:, 6 + c, :],
                           reads=[Rps[6 + c]], writes=[Rstate[c]])
                    else:
                        OP("dve", "scalar_tensor_tensor", out=state[:, c, :], in0=state[:, c, :], scalar=gC,
                           in1=ps[:, 6 + c, :], op0=ALU.mult, op1=ALU.add,
                           reads=[Rps[6 + c], Rstate[c]], writes=[Rstate[c]])
                    OP("pool", "tensor_copy", out=state_bf[:, c, :], in_=state[:, c, :],
                       reads=[Rstate[c]], writes=[Rstbf[c]])
            Rst = p.res()
            OP("act", "activation", out=ysb[ci], in_=ps[:, yb, :], func=AF.Copy, reads=[Rps[yb]], writes=[Rysb[ci]])
            OP("act", "activation", out=junk[:, 0:512], in_=ysb[ci], func=AF.Square,
               accum_out=ssr[:, st_i:st_i + 1], reads=[Rysb[ci]], writes=[Rjunk, Rst])
            OP("act", "activation", out=rtr[:, st_i:st_i + 1], in_=ssr[:, st_i:st_i + 1], func=AF.Sqrt,
               scale=1.0 / 512, bias=epsc[:], reads=[Rst, Rc], writes=[Rst])
            OP("dve", "reciprocal", out=rsr[:, st_i:st_i + 1], in_=rtr[:, st_i:st_i + 1], reads=[Rst], writes=[Rst])
            OP("act", "activation", out=yn[ci], in_=ysb[ci], func=AF.Copy, scale=rsr[:, st_i:st_i + 1],
               reads=[Rysb[ci], Rst], writes=[Ryn[ci]])
            OP("pool", "tensor_tensor", out=yn[ci], in0=yn[ci], in1=gretb[:, h, :], op=ALU.mult,
               reads=[Ryn[ci], Rgretb], writes=[Ryn[ci]])

        def stage_C(q):
            h, gn = q // 16, q % 16
            g, n = gn // 4, gn % 4
            gi, ci = g % 2, q % 2
            cs = slice(n * 128, (n + 1) * 128)
            gs = slice(g * 512, (g + 1) * 512)
            for vc in range(4):
                OP("pe", "transpose", out=psb(4)[:, vc * 128:(vc + 1) * 128], in_=yn[ci][:, vc * 128:(vc + 1) * 128],
                   identity=identb, reads=[Ryn[ci], Rc], writes=[Rps[4]])
            OP("dve", "tensor_tensor", out=zst[gi][:, :, cs], in0=psb(4)[:, 0:512].rearrange("p (v i) -> p v i", v=4),
               in1=sgr[gi][:, :, cs], op=ALU.mult, reads=[Rps[4], Rsgr[gi]], writes=[Rzst[gi]])
            if n == 3:
                Rz = p.res()
                Rzret.append(Rz)
                p.dma(zret_d[h * 512:(h + 1) * 512, gs].rearrange("(vc p) s -> p vc s", p=128), zst[gi],
                      reads=[Rzst[gi]], writes=[Rz], semkey=("zret", gi))

        load_head_weights(0)
        load_head_weights(1)
        units = [(h, g) for h in range(4) for g in range(NG)]
        for f in proj_pieces(*units[0]):
            f()
        pending = []
        for s_ in range(64 + 2):
            if s_ < 64:
                u, n = s_ // 4, s_ % 4
                if n == 0:
                    pending = proj_pieces(*units[u + 1]) if u + 1 < 16 else []
                if s_ % 16 == 1 and 2 <= s_ // 16 + 1 < 4:
                    load_head_weights(s_ // 16 + 1)
                stage_A(s_)
            if 0 <= s_ - 1 < 64:
                stage_B(s_ - 1)
            if 0 <= s_ - 2 < 64:
                stage_C(s_ - 2)
            if s_ < 64:
                take = 1 if n < 2 else 2
                for f in pending[:take]:
                    f()
                pending = pending[take:]
        if stop_after == "RET":
            Rfinal.extend(Rzret + Rzatt)
            return finish()

        p.barrier(scratch[:])
        areset()
        yacc = alloc([128, NT, D], F32)
        Ryacc = [p.res(f"yacc{t}") for t in range(NT)]
        moe_base = apos[0]
        za = alloc([128, 8, 512], BF16)
        zr = alloc([128, 16, 512], BF16)
        Rza, Rzr = p.res("za"), p.res("zr")
        wa = [alloc([128, 8, 128], BF16) for i in range(2)]
        wr = [alloc([128, 16, 128], BF16) for i in range(2)]
        wga = [alloc([128, 8, 128], BF16) for i in range(2)]
        wgb = [alloc([128, 8, 128], BF16) for i in range(2)]
        Rwm = [[p.res(f"wm{i}_{j}") for j in range(4)] for i in range(2)]
        mT2 = [alloc([128, 8, 512], BF16) for i in range(2)]
        RmT2 = [[p.res(f"mT{i}_{k}") for k in range(8)] for i in range(2)]
        wout = alloc([128, 8, 512], BF16)
        Rwout = p.res("wout")
        sga = alloc([128, 512], F32)
        sgb = alloc([128, 512], F32)
        ta = alloc([128, 512], F32)
        tb = alloc([128, 512], F32)
        Rsga, Rsgb, Rta, Rtb = (p.res(n) for n in ("sga", "sgb", "ta", "tb"))
        vtmp = alloc([128, 8, 128], F32)
        Rvtmp = [p.res(f"vtmp{i}") for i in range(2)]
        xlo = [alloc([128, 8, 128], BF16) for i in range(2)]
        Rxlo = [[p.res(f"xlo{i}_{hb}") for hb in range(2)] for i in range(2)]
        wrf = alloc([128, 8, 36], F32)
        wrh = alloc([128, 8, 36], BF16)
        wrl = alloc([128, 8, 36], BF16)
        Rwrf, Rwrh, Rwrl = p.res("wrf"), p.res("wrh"), p.res("wrl")
        p.dma(wrf[:, :, 0:4], wrg_d.rearrange("(k p) n -> p k n", p=128), writes=[Rwrf], semkey="wrf")
        p.dma(wrf[:, :, 4:36], wre_d.rearrange("(k p) n -> p k n", p=128), writes=[Rwrf], semkey="wrf")
        OP("dve", "tensor_copy", out=wrh, in_=wrf, reads=[Rwrf], writes=[Rwrh])
        OP("dve", "tensor_tensor", out=wrl, in0=wrf, in1=wrh, op=ALU.subtract, reads=[Rwrf, Rwrh], writes=[Rwrl])

        def load_merge_weights(fc, slot):
            cs_ = slice(fc * 128, (fc + 1) * 128)
            load_w(wa[slot], watt_d[:, cs_].rearrange("(k p) n -> p k n", p=128), Rwm[slot][0], ("wm", slot, 0))
            load_w(wr[slot], wret_d[:, cs_].rearrange("(k p) n -> p k n", p=128), Rwm[slot][1], ("wm", slot, 1))
            load_w(wga[slot], wsrc(w_in_d, 9216 + fc * 128, 9216 + (fc + 1) * 128), Rwm[slot][2], ("wm", slot, 2))
            load_w(wgb[slot], wsrc(w_in_d, 10240 + fc * 128, 10240 + (fc + 1) * 128), Rwm[slot][3], ("wm", slot, 3))

        Rstat2 = [p.res(f"stat2_{t}") for t in range(NT)]

        def post_gen(g):
            mT = mT2[g % 2]
            RmT = RmT2[g % 2]
            for tt in range(4):
                gt = g * 4 + tt
                p.dma(yacc[:, gt, :], x_d[gt * 128:(gt + 1) * 128, :], writes=[Ryacc[gt]], semkey=("xres", gt))
            for half in range(2):
                load_w(wout, wsrc(wout_d, half * 512, (half + 1) * 512), Rwout, "wout")
                for tt in range(4):
                    gt = g * 4 + tt
                    b = 4 + (tt % 2)
                    for k in range(8):
                        MM(ps[:, b, :], mT[:, k, tt * 128:(tt + 1) * 128], wout[:, k, :], k == 0, k == 7,
                           [RmT[k], Rwout], [Rps[b]])
                    OP("dve", "tensor_tensor", out=yacc[:, gt, half * 512:(half + 1) * 512], in0=ps[:, b, :],
                       in1=yacc[:, gt, half * 512:(half + 1) * 512], op=ALU.add, reads=[Rps[b], Ryacc[gt]], writes=[Ryacc[gt]])
                    yield
            for tt in range(4):
                gt = g * 4 + tt
                i = gt % 2
                for _ in norm_tile_gen(yacc[:, gt, :], Ryacc[gt], gt, gffn, Rstat2[gt], (6, 7), lo=(vtmp, Rvtmp, xlo[i], Rxlo[i])):
                    yield
                yield
                n_mm = 0
                for (lh, rw, Rl, Rw_) in ((xnT[:, :, gt * 128:(gt + 1) * 128], wrh, [RxnT[gt]], Rwrh),
                                          (xnT[:, :, gt * 128:(gt + 1) * 128], wrl, [RxnT[gt]], Rwrl),
                                          (xlo[i], wrh, Rxlo[i], Rwrh)):
                    for k in range(8):
                        MM(ps[:, 5, 0:36], lh[:, k, :], rw[:, k, :], n_mm == 0, n_mm == 23, Rl + [Rw_], [Rps[5]])
                        n_mm += 1
                OP("dve", "tensor_copy", out=logit[:, gt, :], in_=ps[:, 5, 0:36], reads=[Rps[5]], writes=[Rlogit[gt]])
                yield

        widx = 0
        load_merge_weights(0, 0)
        post = None
        for g in range(NG):
            gs = slice(g * 512, (g + 1) * 512)
            RxG = RxnT[g * 4:(g + 1) * 4]
            mT = mT2[g % 2]
            RmT = RmT2[g % 2]
            p.dma(za, zatt_d[:, gs].rearrange("(k p) s -> p k s", p=128), reads=Rzatt, writes=[Rza], semkey="za")
            p.dma(zr, zret_d[:, gs].rearrange("(k p) s -> p k s", p=128), reads=Rzret, writes=[Rzr], semkey="zr")
            for fc in range(8):
                slot = widx % 2
                widx += 1
                nfc, ng_ = (fc + 1) % 8, g + (1 if fc == 7 else 0)
                if ng_ < NG:
                    load_merge_weights(nfc, widx % 2)
                for k in range(8):
                    MM(ps[:, 2, :], wga[slot][:, k, :], xnT[:, k, gs], k == 0, k == 7, [Rwm[slot][2]] + RxG, [Rps[2]])
                for k in range(8):
                    MM(ps[:, 3, :], wgb[slot][:, k, :], xnT[:, k, gs], k == 0, k == 7, [Rwm[slot][3]] + RxG, [Rps[3]])
                for k in range(8):
                    MM(ps[:, 0, :], wa[slot][:, k, :], za[:, k, :], k == 0, k == 7, [Rwm[slot][0], Rza], [Rps[0]])
                for k in range(16):
                    MM(ps[:, 1, :], wr[slot][:, k, :], zr[:, k, :], k == 0, k == 15, [Rwm[slot][1], Rzr], [Rps[1]])
                OP("act", "activation", out=sga, in_=ps[:, 2, :], func=AF.Sigmoid, bias=bga[:, fc:fc + 1],
                   reads=[Rps[2], Rc], writes=[Rsga])
                OP("act", "activation", out=sgb, in_=ps[:, 3, :], func=AF.Sigmoid, bias=bgb[:, fc:fc + 1],
                   reads=[Rps[3], Rc], writes=[Rsgb])
                OP("dve", "tensor_tensor", out=ta, in0=ps[:, 0, :], in1=sga, op=ALU.mult, reads=[Rps[0], Rsga], writes=[Rta])
                OP("dve", "tensor_tensor", out=tb, in0=ps[:, 1, :], in1=sgb, op=ALU.mult, reads=[Rps[1], Rsgb], writes=[Rtb])
                OP("pool", "tensor_tensor", out=mT[:, fc, :], in0=ta, in1=tb, op=ALU.add, reads=[Rta, Rtb], writes=[RmT[fc]])
                if post is not None:
                    next(post, None)
                    next(post, None)
                    next(post, None)
            if post is not None:
                for _ in post:
                    pass
            post = post_gen(g)
        for _ in post:
            pass
        if debug:
            Rfinal.append(p.res())
            p.dma(dbg["x1"].rearrange("(t p) d -> p t d", p=128), yacc, reads=Ryacc, writes=[Rfinal[-1]], semkey="dbg1")
            Rfinal.append(p.res())
            p.dma(dbg["logits"], logit[:].rearrange("p t e -> p (t e)"), reads=Rlogit, writes=[Rfinal[-1]], semkey="dbg2")
        if stop_after == "MERGE":
            Rfinal.extend(Ryacc + Rlogit)
            return finish()

        p.barrier(scratch[:])
        areset(moe_base)
        Rr = p.res("route")
        RW = [Rr]
        gT = alloc([128, S], BF16)
        RgT = p.res("gT")
        OP("pool", "memset", gT[64:128, :], 0.0, writes=[RgT])
        csel = alloc([128, 4096], BF16)
        Rcsel = p.res("csel")
        p.dma(csel, csel_d, writes=[Rcsel], semkey="csel")
        NSLOT = 3
        NS2 = 4
        w1s = [alloc([128, 8, 256], BF16) for i in range(NSLOT)]
        w3s = [alloc([128, 8, 256], BF16) for i in range(NSLOT)]
        w2s = [alloc([128, 2, D], BF16) for i in range(NS2)]
        Rw1 = [p.res(f"w1s{i}") for i in range(NSLOT)]
        Rw3 = [p.res(f"w3s{i}") for i in range(NSLOT)]
        Rw2 = [p.res(f"w2s{i}") for i in range(NS2)]

        def load_expert(e):
            sl = e % NSLOT
            load_w(w1s[sl], w1_d[e].rearrange("(k p) n -> p k n", p=128), Rw1[sl], ("w1s", sl))
            load_w(w3s[sl], w3_d[e].rearrange("(k p) n -> p k n", p=128), Rw3[sl], ("w3s", sl))
            load_w(w2s[e % NS2], w2_d[e].rearrange("(k p) n -> p k n", p=128), Rw2[e % NS2], ("w2s", e % NS2))

        load_expert(0)
        load_expert(1)
        route_base = apos[0]
        brb = alloc([128, 36], F32)
        p.dma(brb[:, 0:4], brg_d.partition_broadcast(128), writes=RW, semkey="brb")
        p.dma(brb[:, 4:36], bre_d.partition_broadcast(128), writes=RW, semkey="brb")
        L = alloc([128, NT, 36], F32)
        gmax = alloc([128, NT], F32)
        ohg = alloc([128, NT, 4], F32)
        tg4 = alloc([128, NT, 4], F32)
        den = alloc([128, NT], F32)
        pg = alloc([128, NT], F32)
        sel4 = alloc([128, NT, 4, 8], F32)
        ing = alloc([128, NT, 8], F32)
        ing2 = alloc([128, NT, 8], F32)
        m1 = alloc([128, NT], F32)
        m2 = alloc([128, NT], F32)
        oh1 = alloc([128, NT, 8], F32)
        oh2 = alloc([128, NT, 8], F32)
        dd = alloc([128, NT], F32)
        e2 = alloc([128, NT], F32)
        w1_ = alloc([128, NT], F32)
        w2_ = alloc([128, NT], F32)
        ge = alloc([128, NT, 8], F32)
        ge2 = alloc([128, NT, 8], F32)
        gate = alloc([128, NT, 4, 8], F32)
        glo = alloc([32, S], BF16)

        def bc3(a2, n):
            return a2.unsqueeze(2).to_broadcast([128, NT, n])

        def DV(meth, **kw):
            return OP("dve", meth, reads=RW + Rlogit, writes=RW, **kw)

        DV("tensor_tensor", out=L, in0=logit[:], in1=brb.unsqueeze(1).to_broadcast([128, NT, 36]), op=ALU.add)
        gl = L[:, :, 0:4]
        el = L[:, :, 4:36].rearrange("p t (g e) -> p t g e", g=4)
        DV("tensor_reduce", out=gmax, in_=gl, axis=AX.X, op=ALU.max)
        DV("tensor_tensor", out=ohg, in0=gl, in1=bc3(gmax, 4), op=ALU.is_equal)
        DV("tensor_tensor", out=tg4, in0=gl, in1=bc3(gmax, 4), op=ALU.subtract)
        OP("act", "activation", out=tg4, in_=tg4, func=AF.Exp, reads=RW, writes=RW)
        DV("tensor_reduce", out=den, in_=tg4, axis=AX.X, op=ALU.add)
        DV("reciprocal", out=pg, in_=den)
        DV("tensor_tensor", out=sel4, in0=el, in1=ohg.unsqueeze(3).to_broadcast([128, NT, 4, 8]), op=ALU.mult)
        DV("tensor_reduce", out=ing, in_=sel4.rearrange("p t g e -> p t e g"), axis=AX.X, op=ALU.add)
        DV("tensor_reduce", out=m1, in_=ing, axis=AX.X, op=ALU.max)
        DV("tensor_tensor", out=oh1, in0=ing, in1=bc3(m1, 8), op=ALU.is_equal)
        DV("scalar_tensor_tensor", out=ing2, in0=oh1, scalar=-1.0e30, in1=ing, op0=ALU.mult, op1=ALU.add)
        DV("tensor_reduce", out=m2, in_=ing2, axis=AX.X, op=ALU.max)
        DV("tensor_tensor", out=oh2, in0=ing2, in1=bc3(m2, 8), op=ALU.is_equal)
        DV("tensor_tensor", out=dd, in0=m2, in1=m1, op=ALU.subtract)
        OP("act", "activation", out=e2, in_=dd, func=AF.Exp, reads=RW, writes=RW)
        DV("tensor_scalar", out=w1_, in0=e2, scalar1=1.0, scalar2=None, op0=ALU.add)
        DV("reciprocal", out=w1_, in_=w1_)
        DV("tensor_tensor", out=w2_, in0=e2, in1=w1_, op=ALU.mult)
        DV("tensor_tensor", out=w1_, in0=w1_, in1=pg, op=ALU.mult)
        DV("tensor_tensor", out=w2_, in0=w2_, in1=pg, op=ALU.mult)
        DV("tensor_tensor", out=ge, in0=oh1, in1=bc3(w1_, 8), op=ALU.mult)
        DV("tensor_tensor", out=ge2, in0=oh2, in1=bc3(w2_, 8), op=ALU.mult)
        DV("tensor_tensor", out=ge, in0=ge, in1=ge2, op=ALU.add)
        DV("tensor_tensor", out=gate, in0=ohg.unsqueeze(3).to_broadcast([128, NT, 4, 8]),
           in1=ge.unsqueeze(2).to_broadcast([128, NT, 4, 8]), op=ALU.mult)
        if debug:
            Rfinal.append(p.res())
            p.dma(dbg["gate"], gate.rearrange("p t g e -> p (t g e)"), reads=RW, writes=[Rfinal[-1]], semkey="dbg3")
        for t in range(NT):
            OP("pe", "transpose", out=ps[0:32, t // 4, (t % 4) * 128:(t % 4 + 1) * 128],
               in_=gate[:, t, :, :].rearrange("p g e -> p (g e)"), identity=ident, reads=RW + [Rc], writes=[Rps[t // 4]])
        gps = ps[0:32, 0:4, :].rearrange("p a b -> p (a b)")
        OP("act", "activation", out=gT[0:32, :], in_=gps, func=AF.Copy, reads=Rps[0:4], writes=[RgT])
        OP("dve", "tensor_tensor", out=glo, in0=gps, in1=gT[0:32, :], op=ALU.subtract, reads=Rps[0:4] + [RgT], writes=RW)
        OP("dve", "tensor_copy", out=gT[32:64, :], in_=glo, reads=RW, writes=[RgT])
        if stop_after == "ROUTE":
            Rfinal.extend(Ryacc + [RgT] + RW)
            return finish()

        p.barrier(scratch[:])
        areset(route_base)
        hg = [alloc([128, 2, S], BF16) for i in range(2)]
        Rhg = [[p.res(f"hg{i}_{g}") for g in range(NG)] for i in range(2)]
        s_sb = [alloc([128, 512], F32) for i in range(2)]
        u_sb = [alloc([128, 512], F32) for i in range(2)]
        gbc = [alloc([128, 512], F32) for i in range(2)]
        Rs = [p.res(f"s{i}") for i in range(2)]
        Ru = [p.res(f"u{i}") for i in range(2)]
        Rgbc = [p.res(f"gbc{i}") for i in range(2)]

        ybank = [0]

        def down_unit(e, t, half):
            hi_ = e % 2
            b = 5 + (ybank[0] % 3)
            ybank[0] += 1
            for fc in range(2):
                MM(ps[:, b, :], hg[hi_][:, fc, t * 128:(t + 1) * 128], w2s[e % NS2][:, fc, half * 512:(half + 1) * 512],
                   fc == 0, fc == 1, [Rhg[hi_][t // 4], Rw2[e % NS2]], [Rps[b]])
            OP("dve", "tensor_tensor", out=yacc[:, t, half * 512:(half + 1) * 512],
               in0=ps[:, b, :], in1=yacc[:, t, half * 512:(half + 1) * 512], op=ALU.add,
               reads=[Rps[b], Ryacc[t]], writes=[Ryacc[t]])

        gcnt = 0
        for e in range(32):
            sl = e % NSLOT
            hi_ = e % 2
            if e + 2 < 32:
                load_expert(e + 2)
            for g in range(NG):
                gs = slice(g * 512, (g + 1) * 512)
                RxG = RxnT[g * 4:(g + 1) * 4]
                gb2 = gcnt % 2
                gcnt += 1
                dq = []
                if e > 0:
                    dq = [(t, half) for t in range(g * 4, (g + 1) * 4) for half in range(2)]
                MM(ps[:, 4, :], csel[:, e * 128:(e + 1) * 128], gT[:, gs], True, True, [Rcsel, RgT], [Rps[4]])
                OP("act", "activation", out=gbc[gb2], in_=ps[:, 4, :], func=AF.Copy, reads=[Rps[4]], writes=[Rgbc[gb2]])
                for wi_, (wsb, Rw_) in enumerate(((w1s[sl], Rw1[sl]), (w3s[sl], Rw3[sl]))):
                    for fc in range(2):
                        b = wi_ * 2 + fc
                        for k in range(8):
                            MM(ps[:, b, :], wsb[:, k, fc * 128:(fc + 1) * 128], xnT[:, k, gs], k == 0, k == 7,
                               [Rw_] + RxG, [Rps[b]])
                        for (t, half) in dq[:2]:
                            down_unit(e - 1, t, half)
                        dq = dq[2:]
                for fc in range(2):
                    OP("act", "activation", out=s_sb[fc], in_=ps[:, fc, :], func=AF.Silu, reads=[Rps[fc]], writes=[Rs[fc]])
                    OP("dve", "tensor_tensor", out=u_sb[fc], in0=ps[:, 2 + fc, :], in1=s_sb[fc], op=ALU.mult,
                       reads=[Rps[2 + fc], Rs[fc]], writes=[Ru[fc]])
                    OP("pool", "tensor_tensor", out=hg[hi_][:, fc, gs], in0=u_sb[fc], in1=gbc[gb2], op=ALU.mult,
                       reads=[Ru[fc], Rgbc[gb2]], writes=[Rhg[hi_][g]])
        for t in range(NT):
            for half in range(2):
                down_unit(31, t, half)
        for t in range(NT):
            Rf = p.res()
            Rfinal.append(Rf)
            p.dma(out_d[t * 128:(t + 1) * 128, :], yacc[:, t, :], reads=[Ryacc[t]], writes=[Rf], semkey=("out", t % 4))
        return finish()


_IN_KEYS = ["g_norm_mix", "w_in", "b_merge_gate", "g_q", "g_k", "w_branch_att", "g_ret_norm", "w_branch_ret",
            "w_out", "g_norm_ffn", "w_router_group", "b_router_group", "w_router_expert", "b_router_expert",
            "w1", "w3", "w2"]


def _run(inputs, debug=False, stop_after=None, cores=8, trace=False):
    cfd, cbd, gamma_c = _host_consts()
    cfs_arr, cfs_offs = _pack(cfd, CFS_ORDER, np.float32)
    cbs_arr, cbs_offs = _pack(cbd, CBS_ORDER, ml_dtypes.bfloat16)
    nc = build_nc(cfs_offs, cbs_offs, cfs_arr.shape[1], cbs_arr.shape[1], gamma_c, debug=debug, stop_after=stop_after)
    shared = {}
    for k in _IN_KEYS:
        a = np.asarray(inputs[k], dtype=np.float32)
        shared[k] = np.ascontiguousarray(a[0])
    shared["cfs"] = cfs_arr
    shared["cbs"] = cbs_arr
    shared["catt"] = np.ascontiguousarray(np.concatenate([cfd["CA"], cfd["SA"]], axis=1))
    shared["cret"] = np.ascontiguousarray(np.concatenate([cfd["CR"], cfd["SR"]], axis=1))
    shared["csel"] = np.ascontiguousarray(cbd["sel"].astype(ml_dtypes.bfloat16))
    x = np.asarray(inputs["x"], dtype=np.float32)
    in_maps = []
    for c in range(cores):
        m = dict(shared)
        m["x"] = np.ascontiguousarray(x[c])
        in_maps.append(m)
    res = run_bass_kernel_spmd(nc, in_maps, core_ids=list(range(cores)), trace=trace)
    return res


def kernel(**inputs):
    res = _run(inputs)
    out = np.stack([np.asarray(r["out"], dtype=np.float32) for r in res.results], axis=0)
    return out
```

```python
import numpy as np
import ml_dtypes
import concourse.bass as bass
import concourse.mybir as mybir
from concourse.bass_utils import run_bass_kernel_spmd
from contextlib import ExitStack

F32 = mybir.dt.float32
BF16 = mybir.dt.bfloat16
ALU = mybir.AluOpType
AF = mybir.ActivationFunctionType
AX = mybir.AxisListType

S = 2048
D = 1024
NT = 16
NG = 4
EPS = 1e-6
ENGS = ("pe", "act", "dve", "pool", "sp")


class Res:
    __slots__ = ("name", "writer", "readers")

    def __init__(self, name):
        self.name = name
        self.writer = None
        self.readers = []


class Op:
    __slots__ = ("eng", "fn", "deps", "signal", "value", "dma", "semkey", "idx")


class Prog:
    def __init__(self, nc, es):
        self.nc = nc
        self.es = es
        self.ops = {e: [] for e in ENGS}
        self.dma_sems = {}
        self.nres = 0
        self.excl = set()
        self.all_res = []
        self.last_barrier = None

    def res(self, name=None):
        self.nres += 1
        r = Res(name or f"r{self.nres}")
        r.writer = self.last_barrier
        self.all_res.append(r)
        return r

    def sb(self, name, shape, dt):
        return self.es.enter_context(self.nc.sbuf_tensor(name, list(shape), dt))

    def op(self, eng, meth, args=(), kw=None, reads=(), writes=(), dma=False, semkey=None):
        o = Op()
        o.eng = eng
        o.fn = (meth, tuple(args), dict(kw or {}))
        o.signal = False
        o.value = None
        o.dma = dma
        o.semkey = semkey
        if eng in ("act", "dve") and self.excl:
            extra = [r for r in reads if id(r) in self.excl and all(r is not w for w in writes)]
            if extra:
                writes = list(writes) + extra
        deps = {}
        for r in reads:
            if r.writer is not None:
                deps[id(r.writer)] = (r.writer, True)
        for w in writes:
            if w.writer is not None and id(w.writer) not in deps:
                deps[id(w.writer)] = (w.writer, False)
            for rd in w.readers:
                if id(rd) not in deps:
                    deps[id(rd)] = (rd, False)
        dl = []
        for d, raw in deps.values():
            if d is o:
                continue
            if (not d.dma) and (not dma) and d.eng == eng:
                if eng in ("pe", "sp"):
                    continue
            dl.append(d)
            d.signal = True
        o.deps = dl
        for r in reads:
            r.readers.append(o)
        for w in writes:
            w.writer = o
            w.readers = []
        if dma:
            if semkey not in self.dma_sems:
                h = self.es.enter_context(self.nc.semaphore(f"dq{len(self.dma_sems)}"))
                self.dma_sems[semkey] = [h, 0]
            ent = self.dma_sems[semkey]
            ent[1] += 16
            o.value = ent[1]
        o.idx = len(self.ops[eng])
        self.ops[eng].append(o)
        return o

    def dma(self, out, in_, reads=(), writes=(), semkey=None, eng="sp", **kw):
        kw = dict(kw)
        kw["out"] = out
        kw["in_"] = in_
        return self.op(eng, "dma_start", (), kw, reads, writes, dma=True, semkey=semkey)

    def barrier(self, scratch_ap):
        allr = list(self.all_res)
        o = self.op("dve", "memset", (scratch_ap, 0.0), None, reads=allr, writes=allr)
        self.last_barrier = o
        return o

    def emit(self):
        nc = self.nc
        esem = {e: self.es.enter_context(nc.semaphore(f"e_{e}")) for e in ENGS if e != "sp"}
        for e in ENGS:
            c = 0
            for o in self.ops[e]:
                if o.dma:
                    continue
                if o.signal:
                    c += 1
                    o.value = c
        ops = self.ops
        dma_sems = self.dma_sems

        def run(e, engobj):
            waited = {}
            for o in ops[e]:
                need = {}
                for d in o.deps:
                    if d.dma:
                        key = ("d", d.semkey)
                        h = dma_sems[d.semkey][0]
                    else:
                        key = ("e", d.eng)
                        h = esem[d.eng]
                    if key not in need or need[key][1] < d.value:
                        need[key] = (h, d.value)
                for key, (h, v) in need.items():
                    if waited.get(key, 0) >= v:
                        continue
                    waited[key] = v
                    engobj.wait_ge(h, v)
                meth, a, kw = o.fn
                if meth is None:
                    continue
                ins = getattr(engobj, meth)(*a, **kw)
                if o.dma:
                    ins.then_inc(dma_sems[o.semkey][0], 16)
                elif o.signal:
                    ins.then_inc(esem[e], 1)

        with nc.Block() as block:
            @block.tensor
            def _(eng):
                run("pe", eng)

            @block.scalar
            def _(eng):
                run("act", eng)

            @block.vector
            def _(eng):
                run("dve", eng)

            @block.gpsimd
            def _(eng):
                run("pool", eng)

            @block.sync
            def _(eng):
                run("sp", eng)


def _host_consts():
    f32 = np.float32
    t = np.arange(S, dtype=f32)
    inv_a = (f32(10000.0) ** (-np.arange(0, 64, 2, dtype=f32) / f32(64))).astype(f32)
    ang = (t[None, :] * inv_a[:, None]).astype(f32)
    p = np.arange(128)
    CA = np.cos(ang)[p % 32].astype(f32)
    sgn = np.where((p % 64) < 32, -1.0, 1.0).astype(f32)
    SA = (np.sin(ang)[p % 32] * sgn[:, None]).astype(f32)
    inv_r = (f32(1.0) / (f32(10000.0) ** np.linspace(0.0, 1.0, 128, dtype=f32))).astype(f32)
    angr = (t[None, :] * inv_r[:, None]).astype(f32)
    CR = np.cos(angr).astype(f32)
    SR = np.sin(angr).astype(f32)
    lg = np.log(f32(1.0) - np.exp2(f32(-5.0) - np.arange(4, dtype=f32))).astype(f32)
    idx = np.arange(128, dtype=f32)
    diff = idx[None, :] - idx[:, None]
    decT = np.zeros((128, 4, 128), f32)
    for h in range(4):
        decT[:, h, :] = np.where(diff >= 0, np.exp(lg[h] * np.maximum(diff, 0.0)), 0.0) / 16.0
    zeta = (np.exp(lg[None, :] * (127.0 - idx[:, None])) / 16.0).astype(f32)
    xi = np.exp(lg[:, None] * (idx[None, :] + 1.0)).astype(f32)
    xib = np.broadcast_to(xi[None], (128, 4, 128)).astype(f32)
    gamma_c = np.exp(lg * 128.0).astype(f32)
    ident = np.eye(128, dtype=f32)
    cf = {
        "CA": CA, "SA": SA, "CR": CR, "SR": SR,
        "decT": decT.reshape(128, 512), "zeta": zeta, "xib": xib.reshape(128, 512), "ident": ident,
    }
    partner = np.where((p % 64) < 32, p + 32, p - 32)
    perm = np.zeros((128, 128), f32)
    perm[partner, p] = 1.0
    bones = (p[:, None] // 64 == p[None, :] // 64).astype(f32)
    kq = np.arange(128)
    mcur = (kq[None, :] >= kq[:, None]).astype(f32)
    mprev = (kq[:, None] >= kq[None, :]).astype(f32)
    mcur4 = np.tile(mcur, (1, 4))
    mprev4 = np.tile(mprev, (1, 4))
    sel = np.zeros((128, 32, 128), f32)
    for e in range(32):
        sel[e, e, :] = 1.0
        sel[32 + e, e, :] = 1.0
    cb = {
        "perm": perm, "bones": bones, "mcur4": mcur4, "mprev4": mprev4,
        "identb": ident, "sel": sel.reshape(128, 4096),
    }
    return cf, cb, gamma_c


def _pack(dct, order, dtype):
    offs = {}
    cols = 0
    for k in order:
        offs[k] = (cols, dct[k].shape[1])
        cols += dct[k].shape[1]
    arr = np.zeros((128, cols), dtype=dtype)
    for k in order:
        a, n = offs[k]
        arr[:, a:a + n] = dct[k].astype(dtype)
    return arr, offs


CFS_ORDER = ["decT", "zeta", "xib", "ident"]
CBS_ORDER = ["perm", "bones", "mcur4", "mprev4", "identb"]


ARENA_BYTES = 152 * 1024


def build_nc(cfs_offs, cbs_offs, cfs_cols, cbs_cols, gamma_c, debug=False, stop_after=None):
    nc = bass.Bass("TRN2", target_bir_lowering=False)

    def din(name, shape, dt=F32):
        return nc.dram_tensor(name, list(shape), dt, kind="ExternalInput").ap()

    x_d = din("x", [S, D])
    gmix_d = din("g_norm_mix", [D])
    w_in_d = din("w_in", [D, 11264])
    bgate_d = din("b_merge_gate", [2 * D])
    gq_d = din("g_q", [64])
    gk_d = din("g_k", [64])
    watt_d = din("w_branch_att", [D, D])
    gret_d = din("g_ret_norm", [4, 512])
    wret_d = din("w_branch_ret", [2 * D, D])
    wout_d = din("w_out", [D, D])
    gffn_d = din("g_norm_ffn", [D])
    wrg_d = din("w_router_group", [D, 4])
    brg_d = din("b_router_group", [4])
    wre_d = din("w_router_expert", [D, 32])
    bre_d = din("b_router_expert", [32])
    w1_d = din("w1", [32, D, 256])
    w3_d = din("w3", [32, D, 256])
    w2_d = din("w2", [32, 256, D])
    cfs_d = din("cfs", [128, cfs_cols])
    cbs_d = din("cbs", [128, cbs_cols], BF16)
    catt_d = din("catt", [128, 2 * S])
    cret_d = din("cret", [128, 2 * S])
    csel_d = din("csel", [128, 4096], BF16)
    out_d = nc.dram_tensor("out", [S, D], F32, kind="ExternalOutput").ap()
    skind = "ExternalOutput" if debug else "Internal"
    vx_d = nc.dram_tensor("vx", [S, 8, 256], BF16, kind=skind).ap()
    zatt_d = nc.dram_tensor("zatt", [D, S], BF16, kind=skind).ap()
    zret_d = nc.dram_tensor("zret", [2 * D, S], BF16, kind=skind).ap()
    dbg = {}
    if debug:
        dbg["xnT"] = nc.dram_tensor("d_xnT", [128, 8 * S], BF16, kind="ExternalOutput").ap()
        dbg["x1"] = nc.dram_tensor("d_x1", [S, D], F32, kind="ExternalOutput").ap()
        dbg["logits"] = nc.dram_tensor("d_logits", [128, NT * 36], F32, kind="ExternalOutput").ap()
        dbg["gate"] = nc.dram_tensor("d_gate", [128, NT * 32], F32, kind="ExternalOutput").ap()

    with ExitStack() as es:
        p = Prog(nc, es)
        ps = es.enter_context(nc.psum_tensor("ps", [128, 8, 512], F32))
        Rps = [p.res(f"psb{b}") for b in range(8)]
        p.excl.update(id(r) for r in Rps)

        def psb(b):
            return ps[:, b, :].bitcast(BF16)

        def OP(eng, meth, *a, reads=(), writes=(), **kw):
            return p.op(eng, meth, a, kw, reads, writes)

        def MM(out, lhsT, rhs, start, stop, reads, writes, **kw):
            return p.op("pe", "matmul", (out,), dict(lhsT=lhsT, rhs=rhs, start=start, stop=stop, **kw), reads, writes)

        arena = p.sb("arena", [128, ARENA_BYTES // 2], BF16)
        apos = [0]

        def areset(pos=0):
            apos[0] = pos

        def alloc(shape, dt):
            n = 1
            for s_ in shape[1:]:
                n *= s_
            nbytes = n * (4 if dt == F32 else 2)
            nbytes = (nbytes + 63) // 64 * 64
            a = apos[0]
            assert a + nbytes <= ARENA_BYTES, ("arena overflow", a, nbytes)
            apos[0] = a + nbytes
            v = arena[0:shape[0], a // 2:(a + n * (4 if dt == F32 else 2)) // 2]
            if dt == F32:
                v = v.bitcast(F32)
            if len(shape) == 3:
                v = v.rearrange("p (a b) -> p a b", a=shape[1])
            elif len(shape) == 4:
                v = v.rearrange("p (a b c) -> p a b c", a=shape[1], b=shape[2])
            return v

        cfs = p.sb("cfs_sb", [128, cfs_cols], F32)
        cbs = p.sb("cbs_sb", [128, cbs_cols], BF16)
        Rc = p.res("consts")
        Rcs = []

        def cres():
            r = p.res()
            Rcs.append(r)
            return [r]

        def CF(k):
            a, n = cfs_offs[k]
            return cfs[:, a:a + n]

        def CB(k):
            a, n = cbs_offs[k]
            return cbs[:, a:a + n]

        p.dma(cfs[:], cfs_d, writes=cres(), semkey="c")
        p.dma(cbs[:], cbs_d, writes=cres(), semkey="c")
        gmix = p.sb("gmix", [128, 8], F32)
        gffn = p.sb("gffn", [128, 8], F32)
        bga = p.sb("bga", [128, 8], F32)
        bgb = p.sb("bgb", [128, 8], F32)
        gqk = p.sb("gqk", [128, 4], F32)
        epsc = p.sb("epsc", [128, 1], F32)
        scratch = p.sb("scratch", [128, 1], F32)
        p.dma(gmix[:], gmix_d.rearrange("(k p) -> p k", p=128), writes=cres(), semkey="c", allow_slow_non_contiguous=True, eng="act")
        p.dma(gffn[:], gffn_d.rearrange("(k p) -> p k", p=128), writes=cres(), semkey="c", allow_slow_non_contiguous=True, eng="act")
        p.dma(bga[:], bgate_d[0:D].rearrange("(k p) -> p k", p=128), writes=cres(), semkey="c", allow_slow_non_contiguous=True, eng="act")
        p.dma(bgb[:], bgate_d[D:2 * D].rearrange("(k p) -> p k", p=128), writes=cres(), semkey="c", allow_slow_non_contiguous=True, eng="act")
        for ci, gd in ((0, gq_d), (2, gk_d)):
            for half in range(2):
                p.dma(gqk[half * 64:(half + 1) * 64, ci:ci + 1], gd.rearrange("(p o) -> p o", o=1), writes=cres(), semkey="c", eng="act")
                for q4 in range(2):
                    p.dma(gqk[half * 64 + q4 * 32: half * 64 + q4 * 32 + 32, ci + 1:ci + 2],
                          gd[(1 - q4) * 32:(1 - q4) * 32 + 32].rearrange("(p o) -> p o", o=1), writes=cres(), semkey="c", eng="act")
        OP("dve", "memset", epsc[:], EPS, reads=Rcs, writes=[Rc])

        ident = CF("ident")
        identb = CB("identb")
        perm, bones = CB("perm"), CB("bones")
        mcur4, mprev4 = CB("mcur4"), CB("mprev4")
        decT = CF("decT").rearrange("p (h i) -> p h i", h=4)
        zeta = CF("zeta")
        xib = CF("xib").rearrange("p (h i) -> p h i", h=4)

        Rfinal = []
        xnT = p.sb("xnT", [128, 8, S], BF16)
        RxnT = [p.res(f"xnT{t}") for t in range(NT)]
        ss = p.sb("ss", [128, NT], F32)
        rt = p.sb("rt", [128, NT], F32)
        rstd = p.sb("rstd", [128, NT], F32)
        Rxin = [p.res(f"xin{i}") for i in range(2)]
        xs = [p.sb(f"xs{i}", [128, D], F32) for i in range(2)]
        Rxs = [p.res(f"xs{i}") for i in range(2)]
        junk = p.sb("junk", [128, D], BF16)
        Rjunk = p.res("junk")
        logit = p.sb("logit", [128, NT, 36], F32)
        Rlogit = [p.res(f"logit{t}") for t in range(NT)]

        def norm_tile_gen(src, Rsrc, t, gcol, Rstat_t, banks, lo=None, stats_done=False):
            i = t % 2
            if not stats_done:
                OP("act", "activation", out=junk[:], in_=src, func=AF.Square, accum_out=ss[:, t:t + 1],
                   reads=[Rsrc, Rc], writes=[Rjunk, Rstat_t])
                OP("act", "activation", out=rt[:, t:t + 1], in_=ss[:, t:t + 1], func=AF.Sqrt, scale=1.0 / D, bias=epsc[:],
                   reads=[Rstat_t, Rc], writes=[Rstat_t])
                OP("dve", "reciprocal", out=rstd[:, t:t + 1], in_=rt[:, t:t + 1], reads=[Rstat_t], writes=[Rstat_t])
            OP("dve", "tensor_scalar", out=xs[i][:], in0=src, scalar1=rstd[:, t:t + 1], scalar2=None, op0=ALU.mult,
               reads=[Rsrc, Rstat_t], writes=[Rxs[i]])
            yield
            for hb in range(2):
                b = banks[hb]
                for j in range(4):
                    k = hb * 4 + j
                    OP("pe", "transpose", out=ps[:, b, j * 128:(j + 1) * 128], in_=xs[i][:, k * 128:(k + 1) * 128],
                       identity=ident, reads=[Rxs[i], Rc], writes=[Rps[b]])
                gb_ = gcol[:, hb * 4:hb * 4 + 4].unsqueeze(2).to_broadcast([128, 4, 128])
                pin = ps[:, b, :].rearrange("p (j c) -> p j c", j=4)
                dsl = xnT[:, hb * 4:hb * 4 + 4, t * 128:(t + 1) * 128]
                if lo is None:
                    OP("dve", "tensor_tensor", out=dsl, in0=pin, in1=gb_, op=ALU.mult,
                       reads=[Rps[b], Rc], writes=[RxnT[t]])
                else:
                    vtmp, Rvtmp, lodst, Rlo = lo
                    OP("dve", "tensor_tensor", out=vtmp[:, hb * 4:hb * 4 + 4, :], in0=pin, in1=gb_, op=ALU.mult,
                       reads=[Rps[b], Rc], writes=[Rvtmp[hb]])
                    OP("act", "activation", out=dsl, in_=vtmp[:, hb * 4:hb * 4 + 4, :], func=AF.Copy,
                       reads=[Rvtmp[hb]], writes=[RxnT[t]])
                    OP("pool", "tensor_tensor", out=lodst[:, hb * 4:hb * 4 + 4, :], in0=vtmp[:, hb * 4:hb * 4 + 4, :],
                       in1=dsl, op=ALU.subtract, reads=[Rvtmp[hb], RxnT[t]], writes=[Rlo[hb]])
            yield

        def finish():
            p.op("sp", None, reads=list(Rfinal))
            p.emit()
            return nc

        def wsrc(wd, c0, c1):
            return wd[:, c0:c1].rearrange("(k p) n -> p k n", p=128)

        def load_w(dst_ap, src_ap, R, key):
            return p.dma(dst_ap, src_ap, writes=[R], semkey=key, eng="pool")

        Rstat = [p.res(f"stat{t}") for t in range(NT)]
        areset()
        xin = [alloc([128, D], F32) for i in range(4)]
        Rxin = [p.res(f"xin{i}") for i in range(4)]
        def vproj_tile(t):
            i = t % 2
            for cg in range(2):
                b = 4 + cg
                for k in range(8):
                    MM(ps[:, b, :], xnT[:, k, t * 128:(t + 1) * 128], wbig[cg][:, k, :], k == 0, k == 7,
                       [RxnT[t], Rwbig[cg]], [Rps[b]])
                pv = ps[:, b, :].rearrange("p (pr e d) -> p pr e d", e=2, d=64)
                OP("act", "activation", out=vst[i][:, cg * 4:(cg + 1) * 4, 0:64], in_=pv[:, :, 0, :], func=AF.Copy,
                   reads=[Rps[b]], writes=[Rvst[i]])
                OP("dve", "tensor_copy", out=vst[i][:, cg * 4:(cg + 1) * 4, 192:256], in_=pv[:, :, 1, :],
                   reads=[Rps[b]], writes=[Rvst[i]])
            p.dma(vx_d[t * 128:(t + 1) * 128, :, :], vst[i], reads=[Rvst[i]], writes=[Rvx[t]], semkey=("vx", t % 2))

        wbig = [alloc([128, 8, 512], BF16) for i in range(2)]
        Rwbig = [p.res(f"wbig{i}") for i in range(2)]
        vst = [alloc([128, 8, 256], BF16) for i in range(2)]
        Rvst = [p.res(f"vst{i}") for i in range(2)]
        Rvx = [p.res(f"vx{t}") for t in range(NT)]
        for i in range(2):
            OP("pool", "memset", vst[i], 1.0, writes=[Rvst[i]])
        for cg in range(2):
            load_w(wbig[cg], wsrc(w_in_d, 2048 + cg * 512, 2048 + (cg + 1) * 512), Rwbig[cg], ("wbig", cg))
        gens = {}
        do_v = stop_after != "A"
        for t in range(min(3, NT)):
            p.dma(xin[t % 4], x_d[t * 128:(t + 1) * 128, :], writes=[Rxin[t % 4]], semkey=("xin", t % 4))
        for step in range(NT + 2):
            if step < NT:
                t = step
                if t + 3 < NT:
                    p.dma(xin[(t + 3) % 4], x_d[(t + 3) * 128:(t + 4) * 128, :], writes=[Rxin[(t + 3) % 4]],
                          semkey=("xin", (t + 3) % 4))
                gens[t] = norm_tile_gen(xin[t % 4], Rxin[t % 4], t, gmix, Rstat[t], (6, 7))
                next(gens[t])
            if 0 <= step - 1 < NT:
                next(gens[step - 1])
            if do_v and 0 <= step - 2 < NT:
                vproj_tile(step - 2)
        if debug:
            Rfinal.append(p.res())
            p.dma(dbg["xnT"], xnT[:].rearrange("p k s -> p (k s)"), reads=RxnT, writes=[Rfinal[-1]], semkey="dbg0")
        if stop_after == "A":
            Rfinal.append(p.res("fin"))
            p.dma(out_d[0:128, :], xin[1], reads=[Rxin[1]], writes=[Rfinal[-1]], semkey="fin")
            return finish()

        if stop_after == "V":
            Rfinal.extend(Rvx)
            return finish()

        p.barrier(scratch[:])
        areset()
        catt = alloc([128, 2 * S], F32)
        Rcatt = p.res("catt")
        p.dma(catt, catt_d, writes=[Rcatt], semkey="catt")
        CA, SA = catt[:, 0:S], catt[:, S:2 * S]
        wqk = [[alloc([128, 8, 128], BF16) for j in range(2)] for i in range(2)]
        Rwqk = [[p.res(f"wqk{i}_{j}") for j in range(2)] for i in range(2)]
        QKT = [[alloc([128, S], BF16) for j in range(2)] for i in range(2)]
        RQK = [[[p.res(f"QK{i}_{j}_{g}") for g in range(NG)] for j in range(2)] for i in range(2)]
        vxs = [[alloc([128, 16, 256], BF16) for o in range(3)] for i in range(2)]
        Rvxs = [[p.res(f"vxs{i}_{o}") for o in range(3)] for i in range(2)]
        qbf = alloc([128, 512], BF16)
        sqb = alloc([128, 512], BF16)
        rtq = alloc([128, 512], F32)
        rsq = alloc([128, 512], F32)
        t1 = alloc([128, 512], F32)
        t2 = alloc([128, 512], F32)
        Rqbf, Rsqb, Rrtq, Rrsq, Rt1, Rt2 = (p.res(n) for n in ("qbf", "sqb", "rtq", "rsq", "t1", "t2"))
        Pb = [alloc([128, 512], BF16) for i in range(3)]
        RPb = [p.res(f"Pb{i}") for i in range(3)]
        rden = alloc([128, S], F32)
        Rrden = [p.res(f"rden{b}") for b in range(4)]
        zpair = [alloc([128, S], BF16) for i in range(2)]
        Rzpair = [p.res(f"zpair{i}") for i in range(2)]
        Rzatt = [p.res(f"zatt{i}") for i in range(8)]

        def load_pair_weights(pr):
            i = pr % 2
            load_w(wqk[i][0], wsrc(w_in_d, pr * 128, (pr + 1) * 128), Rwqk[i][0], ("wqk", i, 0))
            load_w(wqk[i][1], wsrc(w_in_d, 1024 + pr * 128, 1024 + (pr + 1) * 128), Rwqk[i][1], ("wqk", i, 1))

        def load_pair_v(pr):
            i = pr % 2
            src = vx_d[:, pr, :]
            p.dma(vxs[i][0], src.rearrange("(n i) m -> i n m", i=128), reads=Rvx, writes=[Rvxs[i][0]], semkey=("vxs", i, 0))
            s1 = src.rearrange("(n i c) m -> c i n m", i=128, c=4)
            for c in range(4):
                p.dma(vxs[i][1][:, c * 4:(c + 1) * 4, :], s1[c], reads=Rvx, writes=[Rvxs[i][1]], semkey=("vxs", i, 1))
            p.dma(vxs[i][2], src.rearrange("(i c) m -> i c m", c=16), reads=Rvx, writes=[Rvxs[i][2]], semkey=("vxs", i, 2))

        def colap(T, spec, r0):
            base, r, c = spec
            if r == 1:
                return T[r0:r0 + 64, base:base + 128]
            return T[r0:r0 + 64, base:base + 128 * r].rearrange("p (n r) -> p n r", r=r)[:, :, c]

        def prep_gen(pr):
            i = pr % 2
            for j, gi in ((0, 0), (1, 2)):
                dstT = QKT[i][j]
                for g in range(NG):
                    gs = slice(g * 512, (g + 1) * 512)
                    for k in range(8):
                        MM(ps[:, 6, :], wqk[i][j][:, k, :], xnT[:, k, gs], k == 0, k == 7,
                           [Rwqk[i][j]] + RxnT[g * 4:(g + 1) * 4], [Rps[6]])
                    OP("act", "activation", out=qbf, in_=ps[:, 6, :], func=AF.Copy, reads=[Rps[6]], writes=[Rqbf])
                    OP("dve", "scalar_tensor_tensor", out=t1, in0=ps[:, 6, :], scalar=gqk[:, gi:gi + 1], in1=CA[:, gs],
                       op0=ALU.mult, op1=ALU.mult, reads=[Rps[6], Rc, Rcatt], writes=[Rt1])
                    OP("pool", "tensor_tensor", out=sqb, in0=qbf, in1=qbf, op=ALU.mult, reads=[Rqbf], writes=[Rsqb])
                    yield
                    MM(ps[:, 7, :], bones, sqb, True, True, [Rsqb, Rc], [Rps[7]])
                    MM(ps[:, 6, :], perm, qbf, True, True, [Rqbf, Rc], [Rps[6]])
                    OP("act", "activation", out=rtq, in_=ps[:, 7, :], func=AF.Ln, scale=1.0 / 64, bias=epsc[:],
                       reads=[Rps[7], Rc], writes=[Rrtq])
                    OP("act", "activation", out=rsq, in_=rtq, func=AF.Exp, scale=-0.5, reads=[Rrtq], writes=[Rrsq])
                    OP("dve", "scalar_tensor_tensor", out=t2, in0=ps[:, 6, :], scalar=gqk[:, gi + 1:gi + 2], in1=SA[:, gs],
                       op0=ALU.mult, op1=ALU.mult, reads=[Rps[6], Rc, Rcatt], writes=[Rt2])
                    OP("pool", "tensor_tensor", out=t1, in0=t1, in1=t2, op=ALU.add, reads=[Rt1, Rt2], writes=[Rt1])
                    OP("dve", "tensor_tensor", out=dstT[:, gs], in0=t1, in1=rsq, op=ALU.mult,
                       reads=[Rt1, Rrsq], writes=[RQK[i][j][g]])
                    yield

        def att_batches(pr):
            def cols(r, c, n):
                return (n * 128 * r, r, c)
            tiles_cur, tiles_prev = [], []
            for n in range(16):
                tiles_cur.append((cols(1, 0, n), cols(1, 0, n), 0, n, ("n", n // 4, (n % 4) * 128, 1), n % 4 == 0))
            for c in range(4):
                for n in range(4):
                    tiles_cur.append((cols(4, c, n), cols(4, c, n), 1, c * 4 + n, ("n", n, c, 4), False))
            for c in range(16):
                tiles_cur.append((cols(16, c, 0), cols(16, c, 0), 2, c, ("s", c), False))
            for n in range(1, 16):
                tiles_prev.append((cols(1, 0, n - 1), cols(1, 0, n), 0, n - 1, ("n", n // 4, (n % 4) * 128, 1), False))
            for c in range(4):
                for n in range(1, 4):
                    tiles_prev.append((cols(4, c, n - 1), cols(4, c, n), 1, c * 4 + n - 1, ("n", n, c, 4), False))
            batches = []
            for lst, msk in ((tiles_cur, mcur4), (tiles_prev, mprev4)):
                for s0 in range(0, len(lst), 4):
                    batches.append((lst[s0:s0 + 4], msk))
            items = []
            for eh in range(2):
                for bi, (tl, msk) in enumerate(batches):
                    items.append((eh, bi, tl, msk, bi == len(batches) - 1))
            return items

        gcount = [0]

        def emit_S(pr, item):
            i = pr % 2
            eh, bi, tl, msk, last = item
            r0 = eh * 64
            gb = gcount[0]
            gcount[0] += 1
            sb_ = 4 + (gb % 2)
            pbi = gb % 3
            nt_ = len(tl)
            QT, KT = QKT[i]
            RQKall = RQK[i][0] + RQK[i][1]
            for ti, (kc, qc, vo, vb, osp, st) in enumerate(tl):
                MM(ps[:, sb_, ti * 128:(ti + 1) * 128], colap(KT, kc, r0), colap(QT, qc, r0), True, True,
                   RQKall, [Rps[sb_]])
            OP("act", "activation", out=Pb[pbi][:, 0:nt_ * 128], in_=ps[:, sb_, 0:nt_ * 128], func=AF.Exp, scale=0.125,
               reads=[Rps[sb_]], writes=[RPb[pbi]])
            OP("dve", "tensor_tensor", out=Pb[pbi][:, 0:nt_ * 128], in0=Pb[pbi][:, 0:nt_ * 128],
               in1=msk[:, 0:nt_ * 128], op=ALU.mult, reads=[RPb[pbi], Rc], writes=[RPb[pbi]])
            return pbi

        def emit_PV(pr, item, pbi):
            i = pr % 2
            eh, bi, tl, msk, last = item
            r0 = eh * 64
            zp = zpair[i]
            for ti, (kc, qc, vo, vb, osp, st) in enumerate(tl):
                lhsT = vxs[i][vo][:, vb, eh * 128:(eh + 1) * 128]
                if osp[0] == "n":
                    _, bank, off, r = osp
                    if r == 1:
                        oap = ps[:, bank, off:off + 128]
                    else:
                        oap = ps[:, bank, :].rearrange("p (n r) -> p n r", r=r)[:, :, off]
                    MM(oap, lhsT, Pb[pbi][:, ti * 128:(ti + 1) * 128], st, False,
                       [RPb[pbi], Rvxs[i][vo]], [Rps[bank]], skip_group_check=True)
                else:
                    c = osp[1]
                    for bank in range(4):
                        oap = ps[:, bank, :].rearrange("p (n r) -> p n r", r=16)[:, :, c]
                        MM(oap, lhsT, Pb[pbi][:, ti * 128 + bank * 32: ti * 128 + bank * 32 + 32], False, False,
                           [RPb[pbi], Rvxs[i][vo]], [Rps[bank]], skip_group_check=True)
            if last:
                d0 = 64 - r0
                for bank in range(4):
                    bs = slice(bank * 512, (bank + 1) * 512)
                    OP("act", "activation", out=rden[r0:r0 + 64, bs], in_=ps[d0:d0 + 64, bank, :], func=AF.Ln,
                       reads=[Rps[bank]], writes=[Rrden[bank]])
                    OP("act", "activation", out=rden[r0:r0 + 64, bs], in_=rden[r0:r0 + 64, bs], func=AF.Exp, scale=-1.0,
                       reads=[Rrden[bank]], writes=[Rrden[bank]])
                    OP("dve", "tensor_tensor", out=zp[r0:r0 + 64, bs], in0=ps[r0:r0 + 64, bank, :],
                       in1=rden[r0:r0 + 64, bs], op=ALU.mult, reads=[Rps[bank], Rrden[bank]], writes=[Rzpair[i]])

        load_pair_weights(0)
        load_pair_weights(1)
        load_pair_v(0)
        for _ in prep_gen(0):
            pass
        for pr in range(8):
            i = pr % 2
            if pr + 1 < 8:
                load_pair_v(pr + 1)
            nxt = prep_gen(pr + 1) if pr + 1 < 8 else None
            items = att_batches(pr)
            pq = [emit_S(pr, items[0]), emit_S(pr, items[1])]
            for ii, item in enumerate(items):
                if ii + 2 < len(items):
                    pq.append(emit_S(pr, items[ii + 2]))
                emit_PV(pr, item, pq.pop(0))
                if nxt is not None and ii % 2 == 1:
                    next(nxt, None)
            if nxt is not None:
                for _ in nxt:
                    pass
            if pr + 2 < 8:
                load_pair_weights(pr + 2)
            p.dma(zatt_d[pr * 128:(pr + 1) * 128, :], zpair[i], reads=[Rzpair[i]], writes=[Rzatt[pr]], semkey=("zatt", i))
        if stop_after == "ATT":
            Rfinal.extend(Rzatt)
            return finish()

        p.barrier(scratch[:])
        areset()
        cret = alloc([128, 2 * S], F32)
        Rcret = p.res("cret")
        p.dma(cret, cret_d, writes=[Rcret], semkey="cret")
        CR, SR = cret[:, 0:S], cret[:, S:2 * S]
        gretb = alloc([128, 4, 512], F32)
        Rgretb = p.res("gretb")
        p.dma(gretb.rearrange("p h v -> p (h v)"), gret_d.rearrange("h v -> (h v)").partition_broadcast(128),
              writes=[Rgretb], semkey="gretb")
        wq_r = [alloc([128, 8, 256], BF16) for i in range(2)]
        wk_r = [alloc([128, 8, 256], BF16) for i in range(2)]
        wv_r = [alloc([128, 8, 512], BF16) for i in range(2)]
        wg_r = [alloc([128, 8, 512], BF16) for i in range(2)]
        Rwr = [[p.res(f"wr{i}_{j}") for j in range(4)] for i in range(2)]
        QrT = [alloc([128, 2, 512], BF16) for i in range(2)]
        KrT = [alloc([128, 2, 512], BF16) for i in range(2)]
        Qxi = [alloc([128, 2, 512], BF16) for i in range(2)]
        RQr = [p.res(f"QrT{i}") for i in range(2)]
        RKr = [p.res(f"KrT{i}") for i in range(2)]
        RQx = [p.res(f"Qxi{i}") for i in range(2)]
        sgr = [alloc([128, 4, 512], BF16) for i in range(2)]
        Rsgr = [p.res(f"sgr{i}") for i in range(2)]
        vr_sb = [alloc([128, 512], BF16) for i in range(2)]
        Rvr = [p.res(f"vr{i}") for i in range(2)]
        kz = [alloc([128, 256], BF16) for i in range(2)]
        Rkz = [p.res(f"kz{i}") for i in range(2)]
        inm = [alloc([128, 128], BF16) for i in range(2)]
        Rinm = [p.res(f"inm{i}") for i in range(2)]
        yn = [alloc([128, 512], BF16) for i in range(2)]
        Ryn = [p.res(f"yn{i}") for i in range(2)]
        zst = [alloc([128, 4, 512], BF16) for i in range(2)]
        Rzst = [p.res(f"zst{i}") for i in range(2)]
        state = alloc([128, 2, 512], F32)
        state_bf = alloc([128, 2, 512], BF16)
        Rstate = [p.res(f"state{c}") for c in range(2)]
        Rstbf = [p.res(f"stbf{c}") for c in range(2)]
        tmp = [alloc([128, 512], F32) for i in range(4)]
        Rtmp = [p.res(f"rtmp{i}") for i in range(4)]
        ssr = alloc([128, 64], F32)
        rtr = alloc([128, 64], F32)
        rsr = alloc([128, 64], F32)
        Rzret = []

        def load_head_weights(h):
            i = h % 2
            load_w(wq_r[i], wsrc(w_in_d, 3072 + h * 256, 3072 + (h + 1) * 256), Rwr[i][0], ("wr", i, 0))
            load_w(wk_r[i], wsrc(w_in_d, 4096 + h * 256, 4096 + (h + 1) * 256), Rwr[i][1], ("wr", i, 1))
            load_w(wv_r[i], wsrc(w_in_d, 5120 + h * 512, 5120 + (h + 1) * 512), Rwr[i][2], ("wr", i, 2))
            load_w(wg_r[i], wsrc(w_in_d, 7168 + h * 512, 7168 + (h + 1) * 512), Rwr[i][3], ("wr", i, 3))

        vr3 = vr_sb + [alloc([128, 512], BF16)]
        Rvr3 = Rvr + [p.res("vr2")]
        kz3 = kz + [alloc([128, 256], BF16)]
        Rkz3 = Rkz + [p.res("kz2")]
        inm3 = inm + [alloc([128, 128], BF16)]
        Rinm3 = Rinm + [p.res("inm2")]
        INNER = ps[:, 3, 0:128]
        ysb = [alloc([128, 512], F32) for i in range(2)]
        Rysb = [p.res(f"ysb{i}") for i in range(2)]

        def proj_pieces(h, g):
            wi = h % 2
            gi = g % 2
            gs = slice(g * 512, (g + 1) * 512)
            RxG = RxnT[g * 4:(g + 1) * 4]

            def qk(which):
                wsb = (wq_r, wk_r)[which][wi]
                dst = (QrT, KrT)[which][gi]
                Rdst = (RQr, RKr)[which][gi]
                for c in range(2):
                    for k in range(8):
                        MM(ps[:, c, :], wsb[:, k, c * 128:(c + 1) * 128], xnT[:, k, gs], k == 0, k == 7,
                           [Rwr[wi][which]] + RxG, [Rps[c]])
                OP("dve", "tensor_tensor", out=tmp[0], in0=ps[:, 0, :], in1=CR[:, gs], op=ALU.mult,
                   reads=[Rps[0], Rcret], writes=[Rtmp[0]])
                OP("dve", "tensor_tensor", out=tmp[1], in0=ps[:, 1, :], in1=SR[:, gs], op=ALU.mult,
                   reads=[Rps[1], Rcret], writes=[Rtmp[1]])
                OP("pool", "tensor_tensor", out=dst[:, 0, :], in0=tmp[0], in1=tmp[1], op=ALU.subtract,
                   reads=[Rtmp[0], Rtmp[1]], writes=[Rdst])
                OP("dve", "tensor_tensor", out=tmp[2], in0=ps[:, 1, :], in1=CR[:, gs], op=ALU.mult,
                   reads=[Rps[1], Rcret], writes=[Rtmp[2]])
                OP("dve", "tensor_tensor", out=tmp[3], in0=ps[:, 0, :], in1=SR[:, gs], op=ALU.mult,
                   reads=[Rps[0], Rcret], writes=[Rtmp[3]])
                OP("pool", "tensor_tensor", out=dst[:, 1, :], in0=tmp[2], in1=tmp[3], op=ALU.add,
                   reads=[Rtmp[2], Rtmp[3]], writes=[Rdst])
                if which == 0:
                    xb = xib[:, h, :].unsqueeze(1).to_broadcast([128, 4, 128])
                    for c in range(2):
                        OP("pool", "tensor_tensor", out=Qxi[gi][:, c, :].rearrange("p (n i) -> p n i", n=4),
                           in0=dst[:, c, :].rearrange("p (n i) -> p n i", n=4), in1=xb, op=ALU.mult,
                           reads=[Rdst, Rc], writes=[RQx[gi]])

            def gate(vc):
                b = vc % 2
                for k in range(8):
                    MM(ps[:, b, :], wg_r[wi][:, k, vc * 128:(vc + 1) * 128], xnT[:, k, gs], k == 0, k == 7,
                       [Rwr[wi][3]] + RxG, [Rps[b]])
                OP("act", "activation", out=sgr[gi][:, vc, :], in_=ps[:, b, :], func=AF.Silu,
                   reads=[Rps[b]], writes=[Rsgr[gi]])

            return [lambda: qk(0), lambda: qk(1), lambda: gate(0), lambda: gate(1), lambda: gate(2), lambda: gate(3)]

        def stage_A(q):
            h, gn = q // 16, q % 16
            g, n = gn // 4, gn % 4
            wi, gi, b3 = h % 2, g % 2, q % 3
            cs = slice(n * 128, (n + 1) * 128)
            for k in range(8):
                MM(ps[:, 2, :], xnT[:, k, gn * 128:(gn + 1) * 128], wv_r[wi][:, k, :], k == 0, k == 7,
                   [Rwr[wi][2], RxnT[gn]], [Rps[2]])
            OP("act", "activation", out=vr3[b3], in_=ps[:, 2, :], func=AF.Copy, reads=[Rps[2]], writes=[Rvr3[b3]])
            for c in range(2):
                OP("pe", "transpose", out=psb(4)[:, 512 + c * 128: 512 + (c + 1) * 128], in_=KrT[gi][:, c, cs],
                   identity=identb, reads=[RKr[gi], Rc], writes=[Rps[4]])
            OP("act", "activation", out=kz3[b3], in_=psb(4)[:, 512:768], func=AF.Copy, scale=zeta[:, h:h + 1],
               reads=[Rps[4], Rc], writes=[Rkz3[b3]])
            for c in range(2):
                MM(INNER, KrT[gi][:, c, cs], QrT[gi][:, c, cs], c == 0, c == 1, [RKr[gi], RQr[gi]], [Rps[3]])
            OP("dve", "tensor_tensor", out=inm3[b3], in0=INNER, in1=decT[:, h, :], op=ALU.mult,
               reads=[Rps[3], Rc], writes=[Rinm3[b3]])

        Rst_q = {}

        def stage_B(q):
            h, gn = q // 16, q % 16
            g, n = gn // 4, gn % 4
            gi, b3, ci = g % 2, q % 3, q % 2
            cs = slice(n * 128, (n + 1) * 128)
            yb = 5
            st_i = q
            gC = float(gamma_c[h])
            MM(ps[:, yb, :], inm3[b3], vr3[b3], True, gn == 0, [Rinm3[b3], Rvr3[b3]], [Rps[yb]])
            if gn > 0:
                for c in range(2):
                    MM(ps[:, yb, :], Qxi[gi][:, c, cs], state_bf[:, c, :], False, c == 1, [RQx[gi], Rstbf[c]], [Rps[yb]])
            if gn < 15:
                for c in range(2):
                    MM(ps[:, 6 + c, :], kz3[b3][:, c * 128:(c + 1) * 128], vr3[b3], True, True,
                       [Rkz3[b3], Rvr3[b3]], [Rps[6 + c]])
                    if gn == 0:
                        OP("dve", "tensor_copy", out=state[:, c, :], in_=ps[:, 6 + c, :],
                           reads=[Rps[6 + c]], writes=[Rstate[c]])
                    else:
                        OP("dve", "scalar_tensor_tensor", out=state[:, c, :], in0=state[:, c, :], scalar=gC,
                           in1=ps[:, 6 + c, :], op0=ALU.mult, op1=ALU.add,
                           reads=[Rps[6 + c], Rstate[c]], writes=[Rstate[c]])
                    OP("pool", "tensor_copy", out=state_bf[:, c, :], in_=state[:, c, :],
                       reads=[Rstate[c]], writes=[Rstbf[c]])
            Rst = p.res()
            OP("act", "activation", out=ysb[ci], in_=ps[:, yb, :], func=AF.Copy, reads=[Rps[yb]], writes=[Rysb[ci]])
            OP("act", "activation", out=junk[:, 0:512], in_=ysb[ci], func=AF.Square,
               accum_out=ssr[:, st_i:st_i + 1], reads=[Rysb[ci]], writes=[Rjunk, Rst])
            OP("act", "activation", out=rtr[:, st_i:st_i + 1], in_=ssr[:, st_i:st_i + 1], func=AF.Sqrt,
               scale=1.0 / 512, bias=epsc[:], reads=[Rst, Rc], writes=[Rst])
            OP("dve", "reciprocal", out=rsr[:, st_i:st_i + 1], in_=rtr[:, st_i:st_i + 1], reads=[Rst], writes=[Rst])
            OP("act", "activation", out=yn[ci], in_=ysb[ci], func=AF.Copy, scale=rsr[:, st_i:st_i + 1],
               reads=[Rysb[ci], Rst], writes=[Ryn[ci]])
            OP("pool", "tensor_tensor", out=yn[ci], in0=yn[ci], in1=gretb[:, h, :], op=ALU.mult,
               reads=[Ryn[ci], Rgretb], writes=[Ryn[ci]])

        def stage_C(q):
            h, gn = q // 16, q % 16
            g, n = gn // 4, gn % 4
            gi, ci = g % 2, q % 2
            cs = slice(n * 128, (n + 1) * 128)
            gs = slice(g * 512, (g + 1) * 512)
            for vc in range(4):
                OP("pe", "transpose", out=psb(4)[:, vc * 128:(vc + 1) * 128], in_=yn[ci][:, vc * 128:(vc + 1) * 128],
                   identity=identb, reads=[Ryn[ci], Rc], writes=[Rps[4]])
            OP("dve", "tensor_tensor", out=zst[gi][:, :, cs], in0=psb(4)[:, 0:512].rearrange("p (v i) -> p v i", v=4),
               in1=sgr[gi][:, :, cs], op=ALU.mult, reads=[Rps[4], Rsgr[gi]], writes=[Rzst[gi]])
            if n == 3:
                Rz = p.res()
                Rzret.append(Rz)
                p.dma(zret_d[h * 512:(h + 1) * 512, gs].rearrange("(vc p) s -> p vc s", p=128), zst[gi],
                      reads=[Rzst[gi]], writes=[Rz], semkey=("zret", gi))

        load_head_weights(0)
        load_head_weights(1)
        units = [(h, g) for h in range(4) for g in range(NG)]
        for f in proj_pieces(*units[0]):
            f()
        pending = []
        for s_ in range(64 + 2):
            if s_ < 64:
                u, n = s_ // 4, s_ % 4
                if n == 0:
                    pending = proj_pieces(*units[u + 1]) if u + 1 < 16 else []
                if s_ % 16 == 1 and 2 <= s_ // 16 + 1 < 4:
                    load_head_weights(s_ // 16 + 1)
                stage_A(s_)
            if 0 <= s_ - 1 < 64:
                stage_B(s_ - 1)
            if 0 <= s_ - 2 < 64:
                stage_C(s_ - 2)
            if s_ < 64:
                take = 1 if n < 2 else 2
                for f in pending[:take]:
                    f()
                pending = pending[take:]
        if stop_after == "RET":
            Rfinal.extend(Rzret + Rzatt)
            return finish()

        p.barrier(scratch[:])
        areset()
        yacc = alloc([128, NT, D], F32)
        Ryacc = [p.res(f"yacc{t}") for t in range(NT)]
        moe_base = apos[0]
        za = alloc([128, 8, 512], BF16)
        zr = alloc([128, 16, 512], BF16)
        Rza, Rzr = p.res("za"), p.res("zr")
        wa = [alloc([128, 8, 128], BF16) for i in range(2)]
        wr = [alloc([128, 16, 128], BF16) for i in range(2)]
        wga = [alloc([128, 8, 128], BF16) for i in range(2)]
        wgb = [alloc([128, 8, 128], BF16) for i in range(2)]
        Rwm = [[p.res(f"wm{i}_{j}") for j in range(4)] for i in range(2)]
        mT2 = [alloc([128, 8, 512], BF16) for i in range(2)]
        RmT2 = [[p.res(f"mT{i}_{k}") for k in range(8)] for i in range(2)]
        wout = alloc([128, 8, 512], BF16)
        Rwout = p.res("wout")
        sga = alloc([128, 512], F32)
        sgb = alloc([128, 512], F32)
        ta = alloc([128, 512], F32)
        tb = alloc([128, 512], F32)
        Rsga, Rsgb, Rta, Rtb = (p.res(n) for n in ("sga", "sgb", "ta", "tb"))
        a_alias = apos[0]
        vtmp = alloc([128, 8, 128], F32)
        Rvtmp = [p.res(f"vtmp{i}") for i in range(2)]
        xlo = [alloc([128, 8, 128], BF16) for i in range(2)]
        Rxlo = [[p.res(f"xlo{i}_{hb}") for hb in range(2)] for i in range(2)]
        assert apos[0] - a_alias == 8 * 512 * 2
        wout2 = arena[0:128, a_alias // 2: a_alias // 2 + 8 * 512].rearrange("p (a b) -> p a b", a=8)
        Rwout2 = p.res("wout2")
        Ralias = Rvtmp + Rxlo[0] + Rxlo[1]
        wrf = alloc([128, 8, 36], F32)
        wrh = alloc([128, 8, 36], BF16)
        wrl = alloc([128, 8, 36], BF16)
        Rwrf, Rwrh, Rwrl = p.res("wrf"), p.res("wrh"), p.res("wrl")
        p.dma(wrf[:, :, 0:4], wrg_d.rearrange("(k p) n -> p k n", p=128), writes=[Rwrf], semkey="wrf")
        p.dma(wrf[:, :, 4:36], wre_d.rearrange("(k p) n -> p k n", p=128), writes=[Rwrf], semkey="wrf")
        OP("dve", "tensor_copy", out=wrh, in_=wrf, reads=[Rwrf], writes=[Rwrh])
        OP("dve", "tensor_tensor", out=wrl, in0=wrf, in1=wrh, op=ALU.subtract, reads=[Rwrf, Rwrh], writes=[Rwrl])

        def load_merge_weights(fc, slot):
            cs_ = slice(fc * 128, (fc + 1) * 128)
            load_w(wa[slot], watt_d[:, cs_].rearrange("(k p) n -> p k n", p=128), Rwm[slot][0], ("wm", slot, 0))
            load_w(wr[slot], wret_d[:, cs_].rearrange("(k p) n -> p k n", p=128), Rwm[slot][1], ("wm", slot, 1))
            load_w(wga[slot], wsrc(w_in_d, 9216 + fc * 128, 9216 + (fc + 1) * 128), Rwm[slot][2], ("wm", slot, 2))
            load_w(wgb[slot], wsrc(w_in_d, 10240 + fc * 128, 10240 + (fc + 1) * 128), Rwm[slot][3], ("wm", slot, 3))

        Rstat2 = [p.res(f"stat2_{t}") for t in range(NT)]

        def post_gen(g):
            mT = mT2[g % 2]
            RmT = RmT2[g % 2]
            for tt in range(4):
                gt = g * 4 + tt
                p.dma(yacc[:, gt, :], x_d[gt * 128:(gt + 1) * 128, :], writes=[Ryacc[gt]], semkey=("xres", gt))
            for half in range(2):
                wsb_ = (wout, wout2)[half]
                Rw_ = [Rwout] if half == 0 else [Rwout2] + Ralias
                for tt in range(4):
                    gt = g * 4 + tt
                    b = 4 + (tt % 2)
                    for k in range(8):
                        MM(ps[:, b, :], mT[:, k, tt * 128:(tt + 1) * 128], wsb_[:, k, :], k == 0, k == 7,
                           [RmT[k]] + Rw_, [Rps[b]])
                    OP("dve", "tensor_tensor", out=yacc[:, gt, half * 512:(half + 1) * 512], in0=ps[:, b, :],
                       in1=yacc[:, gt, half * 512:(half + 1) * 512], op=ALU.add, reads=[Rps[b], Ryacc[gt]], writes=[Ryacc[gt]])
                    yield
            for tt in range(4):
                gt = g * 4 + tt
                i = gt % 2
                for _ in norm_tile_gen(yacc[:, gt, :], Ryacc[gt], gt, gffn, Rstat2[gt], (6, 7), lo=(vtmp, Rvtmp, xlo[i], Rxlo[i])):
                    yield
                yield
                n_mm = 0
                for (lh, rw, Rl, Rw_) in ((xnT[:, :, gt * 128:(gt + 1) * 128], wrh, [RxnT[gt]], Rwrh),
                                          (xnT[:, :, gt * 128:(gt + 1) * 128], wrl, [RxnT[gt]], Rwrl),
                                          (xlo[i], wrh, Rxlo[i], Rwrh)):
                    for k in range(8):
                        MM(ps[:, 5, 0:36], lh[:, k, :], rw[:, k, :], n_mm == 0, n_mm == 23, Rl + [Rw_], [Rps[5]])
                        n_mm += 1
                OP("dve", "tensor_copy", out=logit[:, gt, :], in_=ps[:, 5, 0:36], reads=[Rps[5]], writes=[Rlogit[gt]])
                yield
            if g + 1 < NG:
                p.dma(wout2, wsrc(wout_d, 512, 1024), writes=[Rwout2] + Ralias, semkey="wout2", eng="pool")

        widx = 0
        load_merge_weights(0, 0)
        load_w(wout, wsrc(wout_d, 0, 512), Rwout, "wout")
        p.dma(wout2, wsrc(wout_d, 512, 1024), writes=[Rwout2] + Ralias, semkey="wout2", eng="pool")
        post = None
        for g in range(NG):
            gs = slice(g * 512, (g + 1) * 512)
            RxG = RxnT[g * 4:(g + 1) * 4]
            mT = mT2[g % 2]
            RmT = RmT2[g % 2]
            p.dma(za, zatt_d[:, gs].rearrange("(k p) s -> p k s", p=128), reads=Rzatt, writes=[Rza], semkey="za")
            p.dma(zr, zret_d[:, gs].rearrange("(k p) s -> p k s", p=128), reads=Rzret, writes=[Rzr], semkey="zr")
            for fc in range(8):
                slot = widx % 2
                widx += 1
                nfc, ng_ = (fc + 1) % 8, g + (1 if fc == 7 else 0)
                if ng_ < NG:
                    load_merge_weights(nfc, widx % 2)
                for k in range(8):
                    MM(ps[:, 2, :], wga[slot][:, k, :], xnT[:, k, gs], k == 0, k == 7, [Rwm[slot][2]] + RxG, [Rps[2]])
                for k in range(8):
                    MM(ps[:, 3, :], wgb[slot][:, k, :], xnT[:, k, gs], k == 0, k == 7, [Rwm[slot][3]] + RxG, [Rps[3]])
                for k in range(8):
                    MM(ps[:, 0, :], wa[slot][:, k, :], za[:, k, :], k == 0, k == 7, [Rwm[slot][0], Rza], [Rps[0]])
                for k in range(16):
                    MM(ps[:, 1, :], wr[slot][:, k, :], zr[:, k, :], k == 0, k == 15, [Rwm[slot][1], Rzr], [Rps[1]])
                OP("act", "activation", out=sga, in_=ps[:, 2, :], func=AF.Sigmoid, bias=bga[:, fc:fc + 1],
                   reads=[Rps[2], Rc], writes=[Rsga])
                OP("act", "activation", out=sgb, in_=ps[:, 3, :], func=AF.Sigmoid, bias=bgb[:, fc:fc + 1],
                   reads=[Rps[3], Rc], writes=[Rsgb])
                OP("dve", "tensor_tensor", out=ta, in0=ps[:, 0, :], in1=sga, op=ALU.mult, reads=[Rps[0], Rsga], writes=[Rta])
                OP("dve", "tensor_tensor", out=tb, in0=ps[:, 1, :], in1=sgb, op=ALU.mult, reads=[Rps[1], Rsgb], writes=[Rtb])
                OP("pool", "tensor_tensor", out=mT[:, fc, :], in0=ta, in1=tb, op=ALU.add, reads=[Rta, Rtb], writes=[RmT[fc]])
                if post is not None:
                    next(post, None)
                    next(post, None)
                    next(post, None)
            if post is not None:
                for _ in post:
                    pass
            post = post_gen(g)
        for _ in post:
            pass
        if debug:
            Rfinal.append(p.res())
            p.dma(dbg["x1"].rearrange("(t p) d -> p t d", p=128), yacc, reads=Ryacc, writes=[Rfinal[-1]], semkey="dbg1")
            Rfinal.append(p.res())
            p.dma(dbg["logits"], logit[:].rearrange("p t e -> p (t e)"), reads=Rlogit, writes=[Rfinal[-1]], semkey="dbg2")
        if stop_after == "MERGE":
            Rfinal.extend(Ryacc + Rlogit)
            return finish()

        p.barrier(scratch[:])
        areset(moe_base)
        Rr = p.res("route")
        RW = [Rr]
        gT = alloc([128, S], BF16)
        RgT = p.res("gT")
        OP("pool", "memset", gT[64:128, :], 0.0, writes=[RgT])
        csel = alloc([128, 4096], BF16)
        Rcsel = p.res("csel")
        p.dma(csel, csel_d, writes=[Rcsel], semkey="csel")
        NSLOT = 3
        NS2 = 4
        w1s = [alloc([128, 8, 256], BF16) for i in range(NSLOT)]
        w3s = [alloc([128, 8, 256], BF16) for i in range(NSLOT)]
        w2s = [alloc([128, 2, D], BF16) for i in range(NS2)]
        Rw1 = [p.res(f"w1s{i}") for i in range(NSLOT)]
        Rw3 = [p.res(f"w3s{i}") for i in range(NSLOT)]
        Rw2 = [p.res(f"w2s{i}") for i in range(NS2)]

        def load_expert(e):
            sl = e % NSLOT
            load_w(w1s[sl], w1_d[e].rearrange("(k p) n -> p k n", p=128), Rw1[sl], ("w1s", sl))
            load_w(w3s[sl], w3_d[e].rearrange("(k p) n -> p k n", p=128), Rw3[sl], ("w3s", sl))
            load_w(w2s[e % NS2], w2_d[e].rearrange("(k p) n -> p k n", p=128), Rw2[e % NS2], ("w2s", e % NS2))

        load_expert(0)
        load_expert(1)
        route_base = apos[0]
        brb = alloc([128, 36], F32)
        p.dma(brb[:, 0:4], brg_d.partition_broadcast(128), writes=RW, semkey="brb")
        p.dma(brb[:, 4:36], bre_d.partition_broadcast(128), writes=RW, semkey="brb")
        L = alloc([128, NT, 36], F32)
        gmax = alloc([128, NT], F32)
        ohg = alloc([128, NT, 4], F32)
        tg4 = alloc([128, NT, 4], F32)
        den = alloc([128, NT], F32)
        pg = alloc([128, NT], F32)
        sel4 = alloc([128, NT, 4, 8], F32)
        ing = alloc([128, NT, 8], F32)
        ing2 = alloc([128, NT, 8], F32)
        m1 = alloc([128, NT], F32)
        m2 = alloc([128, NT], F32)
        oh1 = alloc([128, NT, 8], F32)
        oh2 = alloc([128, NT, 8], F32)
        dd = alloc([128, NT], F32)
        e2 = alloc([128, NT], F32)
        w1_ = alloc([128, NT], F32)
        w2_ = alloc([128, NT], F32)
        ge = alloc([128, NT, 8], F32)
        ge2 = alloc([128, NT, 8], F32)
        gate = alloc([128, NT, 4, 8], F32)
        glo = alloc([32, S], BF16)

        def bc3(a2, n):
            return a2.unsqueeze(2).to_broadcast([128, NT, n])

        def DV(meth, **kw):
            return OP("dve", meth, reads=RW + Rlogit, writes=RW, **kw)

        DV("tensor_tensor", out=L, in0=logit[:], in1=brb.unsqueeze(1).to_broadcast([128, NT, 36]), op=ALU.add)
        gl = L[:, :, 0:4]
        el = L[:, :, 4:36].rearrange("p t (g e) -> p t g e", g=4)
        DV("tensor_reduce", out=gmax, in_=gl, axis=AX.X, op=ALU.max)
        DV("tensor_tensor", out=ohg, in0=gl, in1=bc3(gmax, 4), op=ALU.is_equal)
        DV("tensor_tensor", out=tg4, in0=gl, in1=bc3(gmax, 4), op=ALU.subtract)
        OP("act", "activation", out=tg4, in_=tg4, func=AF.Exp, reads=RW, writes=RW)
        DV("tensor_reduce", out=den, in_=tg4, axis=AX.X, op=ALU.add)
        DV("reciprocal", out=pg, in_=den)
        DV("tensor_tensor", out=sel4, in0=el, in1=ohg.unsqueeze(3).to_broadcast([128, NT, 4, 8]), op=ALU.mult)
        DV("tensor_reduce", out=ing, in_=sel4.rearrange("p t g e -> p t e g"), axis=AX.X, op=ALU.add)
        DV("tensor_reduce", out=m1, in_=ing, axis=AX.X, op=ALU.max)
        DV("tensor_tensor", out=oh1, in0=ing, in1=bc3(m1, 8), op=ALU.is_equal)
        DV("scalar_tensor_tensor", out=ing2, in0=oh1, scalar=-1.0e30, in1=ing, op0=ALU.mult, op1=ALU.add)
        DV("tensor_reduce", out=m2, in_=ing2, axis=AX.X, op=ALU.max)
        DV("tensor_tensor", out=oh2, in0=ing2, in1=bc3(m2, 8), op=ALU.is_equal)
        DV("tensor_tensor", out=dd, in0=m2, in1=m1, op=ALU.subtract)
        OP("act", "activation", out=e2, in_=dd, func=AF.Exp, reads=RW, writes=RW)
        DV("tensor_scalar", out=w1_, in0=e2, scalar1=1.0, scalar2=None, op0=ALU.add)
        DV("reciprocal", out=w1_, in_=w1_)
        DV("tensor_tensor", out=w2_, in0=e2, in1=w1_, op=ALU.mult)
        DV("tensor_tensor", out=w1_, in0=w1_, in1=pg, op=ALU.mult)
        DV("tensor_tensor", out=w2_, in0=w2_, in1=pg, op=ALU.mult)
        DV("tensor_tensor", out=ge, in0=oh1, in1=bc3(w1_, 8), op=ALU.mult)
        DV("tensor_tensor", out=ge2, in0=oh2, in1=bc3(w2_, 8), op=ALU.mult)
        DV("tensor_tensor", out=ge, in0=ge, in1=ge2, op=ALU.add)
        DV("tensor_tensor", out=gate, in0=ohg.unsqueeze(3).to_broadcast([128, NT, 4, 8]),
           in1=ge.unsqueeze(2).to_broadcast([128, NT, 4, 8]), op=ALU.mult)
        if debug:
            Rfinal.append(p.res())
            p.dma(dbg["gate"], gate.rearrange("p t g e -> p (t g e)"), reads=RW, writes=[Rfinal[-1]], semkey="dbg3")
        for t in range(NT):
            OP("pe", "transpose", out=ps[0:32, t // 4, (t % 4) * 128:(t % 4 + 1) * 128],
               in_=gate[:, t, :, :].rearrange("p g e -> p (g e)"), identity=ident, reads=RW + [Rc], writes=[Rps[t // 4]])
        gps = ps[0:32, 0:4, :].rearrange("p a b -> p (a b)")
        OP("act", "activation", out=gT[0:32, :], in_=gps, func=AF.Copy, reads=Rps[0:4], writes=[RgT])
        OP("dve", "tensor_tensor", out=glo, in0=gps, in1=gT[0:32, :], op=ALU.subtract, reads=Rps[0:4] + [RgT], writes=RW)
        OP("dve", "tensor_copy", out=gT[32:64, :], in_=glo, reads=RW, writes=[RgT])
        if stop_after == "ROUTE":
            Rfinal.extend(Ryacc + [RgT] + RW)
            return finish()

        p.barrier(scratch[:])
        areset(route_base)
        hg = [alloc([128, 2, S], BF16) for i in range(2)]
        Rhg = [[p.res(f"hg{i}_{g}") for g in range(NG)] for i in range(2)]
        s_sb = [alloc([128, 512], F32) for i in range(2)]
        u_sb = [alloc([128, 512], F32) for i in range(2)]
        gbc = [alloc([128, 512], F32) for i in range(2)]
        Rs = [p.res(f"s{i}") for i in range(2)]
        Ru = [p.res(f"u{i}") for i in range(2)]
        Rgbc = [p.res(f"gbc{i}") for i in range(2)]

        ybank = [0]

        def down_unit(e, t, half):
            hi_ = e % 2
            b = 5 + (ybank[0] % 3)
            ybank[0] += 1
            for fc in range(2):
                MM(ps[:, b, :], hg[hi_][:, fc, t * 128:(t + 1) * 128], w2s[e % NS2][:, fc, half * 512:(half + 1) * 512],
                   fc == 0, fc == 1, [Rhg[hi_][t // 4], Rw2[e % NS2]], [Rps[b]])
            OP("dve", "tensor_tensor", out=yacc[:, t, half * 512:(half + 1) * 512],
               in0=ps[:, b, :], in1=yacc[:, t, half * 512:(half + 1) * 512], op=ALU.add,
               reads=[Rps[b], Ryacc[t]], writes=[Ryacc[t]])

        gcnt = 0
        for e in range(32):
            sl = e % NSLOT
            hi_ = e % 2
            if e + 2 < 32:
                load_expert(e + 2)
            for g in range(NG):
                gs = slice(g * 512, (g + 1) * 512)
                RxG = RxnT[g * 4:(g + 1) * 4]
                gb2 = gcnt % 2
                gcnt += 1
                dq = []
                if e > 0:
                    dq = [(t, half) for t in range(g * 4, (g + 1) * 4) for half in range(2)]
                MM(ps[:, 4, :], csel[:, e * 128:(e + 1) * 128], gT[:, gs], True, True, [Rcsel, RgT], [Rps[4]])
                OP("act", "activation", out=gbc[gb2], in_=ps[:, 4, :], func=AF.Copy, reads=[Rps[4]], writes=[Rgbc[gb2]])
                for wi_, (wsb, Rw_) in enumerate(((w1s[sl], Rw1[sl]), (w3s[sl], Rw3[sl]))):
                    for fc in range(2):
                        b = wi_ * 2 + fc
                        for k in range(8):
                            MM(ps[:, b, :], wsb[:, k, fc * 128:(fc + 1) * 128], xnT[:, k, gs], k == 0, k == 7,
                               [Rw_] + RxG, [Rps[b]])
                        for (t, half) in dq[:2]:
                            down_unit(e - 1, t, half)
                        dq = dq[2:]
                for fc in range(2):
                    OP("act", "activation", out=s_sb[fc], in_=ps[:, fc, :], func=AF.Silu, reads=[Rps[fc]], writes=[Rs[fc]])
                    OP("dve", "tensor_tensor", out=u_sb[fc], in0=ps[:, 2 + fc, :], in1=s_sb[fc], op=ALU.mult,
                       reads=[Rps[2 + fc], Rs[fc]], writes=[Ru[fc]])
                    OP("pool", "tensor_tensor", out=hg[hi_][:, fc, gs], in0=u_sb[fc], in1=gbc[gb2], op=ALU.mult,
                       reads=[Ru[fc], Rgbc[gb2]], writes=[Rhg[hi_][g]])
        for t in range(NT):
            for half in range(2):
                down_unit(31, t, half)
        for t in range(NT):
            Rf = p.res()
            Rfinal.append(Rf)
            p.dma(out_d[t * 128:(t + 1) * 128, :], yacc[:, t, :], reads=[Ryacc[t]], writes=[Rf], semkey=("out", t % 4))
        return finish()


_IN_KEYS = ["g_norm_mix", "w_in", "b_merge_gate", "g_q", "g_k", "w_branch_att", "g_ret_norm", "w_branch_ret",
            "w_out", "g_norm_ffn", "w_router_group", "b_router_group", "w_router_expert", "b_router_expert",
            "w1", "w3", "w2"]


def _run(inputs, debug=False, stop_after=None, cores=8, trace=False):
    cfd, cbd, gamma_c = _host_consts()
    cfs_arr, cfs_offs = _pack(cfd, CFS_ORDER, np.float32)
    cbs_arr, cbs_offs = _pack(cbd, CBS_ORDER, ml_dtypes.bfloat16)
    nc = build_nc(cfs_offs, cbs_offs, cfs_arr.shape[1], cbs_arr.shape[1], gamma_c, debug=debug, stop_after=stop_after)
    shared = {}
    for k in _IN_KEYS:
        a = np.asarray(inputs[k], dtype=np.float32)
        shared[k] = np.ascontiguousarray(a[0])
    shared["cfs"] = cfs_arr
    shared["cbs"] = cbs_arr
    shared["catt"] = np.ascontiguousarray(np.concatenate([cfd["CA"], cfd["SA"]], axis=1))
    shared["cret"] = np.ascontiguousarray(np.concatenate([cfd["CR"], cfd["SR"]], axis=1))
    shared["csel"] = np.ascontiguousarray(cbd["sel"].astype(ml_dtypes.bfloat16))
    x = np.asarray(inputs["x"], dtype=np.float32)
    in_maps = []
    for c in range(cores):
        m = dict(shared)
        m["x"] = np.ascontiguousarray(x[c])
        in_maps.append(m)
    res = run_bass_kernel_spmd(nc, in_maps, core_ids=list(range(cores)), trace=trace)
    return res


def kernel(**inputs):
    res = _run(inputs)
    out = np.stack([np.asarray(r["out"], dtype=np.float32) for r in res.results], axis=0)
    return out
```

```python
import numpy as np
import ml_dtypes
import concourse.bass as bass
import concourse.mybir as mybir
from concourse.bass_utils import run_bass_kernel_spmd
from contextlib import ExitStack

F32 = mybir.dt.float32
BF16 = mybir.dt.bfloat16
ALU = mybir.AluOpType
AF = mybir.ActivationFunctionType
AX = mybir.AxisListType

S = 2048
D = 1024
NT = 16
NG = 4
EPS = 1e-6
ENGS = ("pe", "act", "dve", "pool", "sp")


class Res:
    __slots__ = ("name", "writer", "readers")

    def __init__(self, name):
        self.name = name
        self.writer = None
        self.readers = []


class Op:
    __slots__ = ("eng", "fn", "deps", "signal", "value", "dma", "semkey", "idx")


class Prog:
    def __init__(self, nc, es):
        self.nc = nc
        self.es = es
        self.ops = {e: [] for e in ENGS}
        self.dma_sems = {}
        self.nres = 0
        self.excl = set()
        self.all_res = []
        self.last_barrier = None

    def res(self, name=None):
        self.nres += 1
        r = Res(name or f"r{self.nres}")
        r.writer = self.last_barrier
        self.all_res.append(r)
        return r

    def sb(self, name, shape, dt):
        return self.es.enter_context(self.nc.sbuf_tensor(name, list(shape), dt))

    def op(self, eng, meth, args=(), kw=None, reads=(), writes=(), dma=False, semkey=None):
        o = Op()
        o.eng = eng
        o.fn = (meth, tuple(args), dict(kw or {}))
        o.signal = False
        o.value = None
        o.dma = dma
        o.semkey = semkey
        if eng in ("act", "dve") and self.excl:
            extra = [r for r in reads if id(r) in self.excl and all(r is not w for w in writes)]
            if extra:
                writes = list(writes) + extra
        deps = {}
        for r in reads:
            if r.writer is not None:
                deps[id(r.writer)] = (r.writer, True)
        for w in writes:
            if w.writer is not None and id(w.writer) not in deps:
                deps[id(w.writer)] = (w.writer, False)
            for rd in w.readers:
                if id(rd) not in deps:
                    deps[id(rd)] = (rd, False)
        dl = []
        for d, raw in deps.values():
            if d is o:
                continue
            if (not d.dma) and (not dma) and d.eng == eng:
                if eng in ("pe", "sp"):
                    continue
            dl.append(d)
            d.signal = True
        o.deps = dl
        for r in reads:
            r.readers.append(o)
        for w in writes:
            w.writer = o
            w.readers = []
        if dma:
            if semkey not in self.dma_sems:
                h = self.es.enter_context(self.nc.semaphore(f"dq{len(self.dma_sems)}"))
                self.dma_sems[semkey] = [h, 0]
            ent = self.dma_sems[semkey]
            ent[1] += 16
            o.value = ent[1]
        o.idx = len(self.ops[eng])
        self.ops[eng].append(o)
        return o

    def dma(self, out, in_, reads=(), writes=(), semkey=None, eng="sp", **kw):
        kw = dict(kw)
        kw["out"] = out
        kw["in_"] = in_
        return self.op(eng, "dma_start", (), kw, reads, writes, dma=True, semkey=semkey)

    def barrier(self, scratch_ap):
        allr = list(self.all_res)
        o = self.op("dve", "memset", (scratch_ap, 0.0), None, reads=allr, writes=allr)
        self.last_barrier = o
        return o

    def emit(self):
        nc = self.nc
        esem = {e: self.es.enter_context(nc.semaphore(f"e_{e}")) for e in ENGS if e != "sp"}
        for e in ENGS:
            c = 0
            for o in self.ops[e]:
                if o.dma:
                    continue
                if o.signal:
                    c += 1
                    o.value = c
        ops = self.ops
        dma_sems = self.dma_sems

        def run(e, engobj):
            waited = {}
            for o in ops[e]:
                need = {}
                for d in o.deps:
                    if d.dma:
                        key = ("d", d.semkey)
                        h = dma_sems[d.semkey][0]
                    else:
                        key = ("e", d.eng)
                        h = esem[d.eng]
                    if key not in need or need[key][1] < d.value:
                        need[key] = (h, d.value)
                for key, (h, v) in need.items():
                    if waited.get(key, 0) >= v:
                        continue
                    waited[key] = v
                    engobj.wait_ge(h, v)
                meth, a, kw = o.fn
                if meth is None:
                    continue
                ins = getattr(engobj, meth)(*a, **kw)
                if o.dma:
                    ins.then_inc(dma_sems[o.semkey][0], 16)
                elif o.signal:
                    ins.then_inc(esem[e], 1)

        with nc.Block() as block:
            @block.tensor
            def _(eng):
                run("pe", eng)

            @block.scalar
            def _(eng):
                run("act", eng)

            @block.vector
            def _(eng):
                run("dve", eng)

            @block.gpsimd
            def _(eng):
                run("pool", eng)

            @block.sync
            def _(eng):
                run("sp", eng)


def _host_consts():
    f32 = np.float32
    t = np.arange(S, dtype=f32)
    inv_a = (f32(10000.0) ** (-np.arange(0, 64, 2, dtype=f32) / f32(64))).astype(f32)
    ang = (t[None, :] * inv_a[:, None]).astype(f32)
    p = np.arange(128)
    CA = np.cos(ang)[p % 32].astype(f32)
    sgn = np.where((p % 64) < 32, -1.0, 1.0).astype(f32)
    SA = (np.sin(ang)[p % 32] * sgn[:, None]).astype(f32)
    inv_r = (f32(1.0) / (f32(10000.0) ** np.linspace(0.0, 1.0, 128, dtype=f32))).astype(f32)
    angr = (t[None, :] * inv_r[:, None]).astype(f32)
    CR = np.cos(angr).astype(f32)
    SR = np.sin(angr).astype(f32)
    lg = np.log(f32(1.0) - np.exp2(f32(-5.0) - np.arange(4, dtype=f32))).astype(f32)
    idx = np.arange(128, dtype=f32)
    diff = idx[None, :] - idx[:, None]
    decT = np.zeros((128, 4, 128), f32)
    for h in range(4):
        decT[:, h, :] = np.where(diff >= 0, np.exp(lg[h] * np.maximum(diff, 0.0)), 0.0) / 16.0
    zeta = (np.exp(lg[None, :] * (127.0 - idx[:, None])) / 16.0).astype(f32)
    xi = np.exp(lg[:, None] * (idx[None, :] + 1.0)).astype(f32)
    xib = np.broadcast_to(xi[None], (128, 4, 128)).astype(f32)
    gamma_c = np.exp(lg * 128.0).astype(f32)
    ident = np.eye(128, dtype=f32)
    cf = {
        "CA": CA, "SA": SA, "CR": CR, "SR": SR,
        "decT": decT.reshape(128, 512), "zeta": zeta, "xib": xib.reshape(128, 512), "ident": ident,
    }
    partner = np.where((p % 64) < 32, p + 32, p - 32)
    perm = np.zeros((128, 128), f32)
    perm[partner, p] = 1.0
    bones = (p[:, None] // 64 == p[None, :] // 64).astype(f32)
    kq = np.arange(128)
    mcur = (kq[None, :] >= kq[:, None]).astype(f32)
    mprev = (kq[:, None] >= kq[None, :]).astype(f32)
    mcur4 = np.tile(mcur, (1, 4))
    mprev4 = np.tile(mprev, (1, 4))
    sel = np.zeros((128, 32, 128), f32)
    for e in range(32):
        sel[e, e, :] = 1.0
        sel[32 + e, e, :] = 1.0
    cb = {
        "perm": perm, "bones": bones, "mcur4": mcur4, "mprev4": mprev4,
        "identb": ident, "sel": sel.reshape(128, 4096),
    }
    return cf, cb, gamma_c


def _pack(dct, order, dtype):
    offs = {}
    cols = 0
    for k in order:
        offs[k] = (cols, dct[k].shape[1])
        cols += dct[k].shape[1]
    arr = np.zeros((128, cols), dtype=dtype)
    for k in order:
        a, n = offs[k]
        arr[:, a:a + n] = dct[k].astype(dtype)
    return arr, offs


CFS_ORDER = ["decT", "zeta", "xib", "ident"]
CBS_ORDER = ["perm", "bones", "mcur4", "mprev4", "identb"]


ARENA_BYTES = 152 * 1024


def build_nc(cfs_offs, cbs_offs, cfs_cols, cbs_cols, gamma_c, debug=False, stop_after=None):
    nc = bass.Bass("TRN2", target_bir_lowering=False)

    def din(name, shape, dt=F32):
        return nc.dram_tensor(name, list(shape), dt, kind="ExternalInput").ap()

    x_d = din("x", [S, D])
    gmix_d = din("g_norm_mix", [D])
    w_in_d = din("w_in", [D, 11264])
    bgate_d = din("b_merge_gate", [2 * D])
    gq_d = din("g_q", [64])
    gk_d = din("g_k", [64])
    watt_d = din("w_branch_att", [D, D])
    gret_d = din("g_ret_norm", [4, 512])
    wret_d = din("w_branch_ret", [2 * D, D])
    wout_d = din("w_out", [D, D])
    gffn_d = din("g_norm_ffn", [D])
    wrg_d = din("w_router_group", [D, 4])
    brg_d = din("b_router_group", [4])
    wre_d = din("w_router_expert", [D, 32])
    bre_d = din("b_router_expert", [32])
    w1_d = din("w1", [32, D, 256])
    w3_d = din("w3", [32, D, 256])
    w2_d = din("w2", [32, 256, D])
    cfs_d = din("cfs", [128, cfs_cols])
    cbs_d = din("cbs", [128, cbs_cols], BF16)
    catt_d = din("catt", [128, 2 * S])
    cret_d = din("cret", [128, 2 * S])
    csel_d = din("csel", [128, 4096], BF16)
    out_d = nc.dram_tensor("out", [S, D], F32, kind="ExternalOutput").ap()
    skind = "ExternalOutput" if debug else "Internal"
    vx_d = nc.dram_tensor("vx", [S, 8, 256], BF16, kind=skind).ap()
    zatt_d = nc.dram_tensor("zatt", [D, S], BF16, kind=skind).ap()
    zret_d = nc.dram_tensor("zret", [2 * D, S], BF16, kind=skind).ap()
    dbg = {}
    if debug:
        dbg["xnT"] = nc.dram_tensor("d_xnT", [128, 8 * S], BF16, kind="ExternalOutput").ap()
        dbg["x1"] = nc.dram_tensor("d_x1", [S, D], F32, kind="ExternalOutput").ap()
        dbg["logits"] = nc.dram_tensor("d_logits", [128, NT * 36], F32, kind="ExternalOutput").ap()
        dbg["gate"] = nc.dram_tensor("d_gate", [128, NT * 32], F32, kind="ExternalOutput").ap()

    with ExitStack() as es:
        p = Prog(nc, es)
        ps = es.enter_context(nc.psum_tensor("ps", [128, 8, 512], F32))
        Rps = [p.res(f"psb{b}") for b in range(8)]
        p.excl.update(id(r) for r in Rps)

        def psb(b):
            return ps[:, b, :].bitcast(BF16)

        def OP(eng, meth, *a, reads=(), writes=(), **kw):
            return p.op(eng, meth, a, kw, reads, writes)

        def MM(out, lhsT, rhs, start, stop, reads, writes, **kw):
            return p.op("pe", "matmul", (out,), dict(lhsT=lhsT, rhs=rhs, start=start, stop=stop, **kw), reads, writes)

        arena = p.sb("arena", [128, ARENA_BYTES // 2], BF16)
        apos = [0]

        def areset(pos=0):
            apos[0] = pos

        def alloc(shape, dt):
            n = 1
            for s_ in shape[1:]:
                n *= s_
            nbytes = n * (4 if dt == F32 else 2)
            nbytes = (nbytes + 63) // 64 * 64
            a = apos[0]
            assert a + nbytes <= ARENA_BYTES, ("arena overflow", a, nbytes)
            apos[0] = a + nbytes
            v = arena[0:shape[0], a // 2:(a + n * (4 if dt == F32 else 2)) // 2]
            if dt == F32:
                v = v.bitcast(F32)
            if len(shape) == 3:
                v = v.rearrange("p (a b) -> p a b", a=shape[1])
            elif len(shape) == 4:
                v = v.rearrange("p (a b c) -> p a b c", a=shape[1], b=shape[2])
            return v

        cfs = p.sb("cfs_sb", [128, cfs_cols], F32)
        cbs = p.sb("cbs_sb", [128, cbs_cols], BF16)
        Rc = p.res("consts")
        Rcs = []

        def cres():
            r = p.res()
            Rcs.append(r)
            return [r]

        def CF(k):
            a, n = cfs_offs[k]
            return cfs[:, a:a + n]

        def CB(k):
            a, n = cbs_offs[k]
            return cbs[:, a:a + n]

        p.dma(cfs[:], cfs_d, writes=cres(), semkey="c")
        p.dma(cbs[:], cbs_d, writes=cres(), semkey="c")
        gmix = p.sb("gmix", [128, 8], F32)
        gffn = p.sb("gffn", [128, 8], F32)
        bga = p.sb("bga", [128, 8], F32)
        bgb = p.sb("bgb", [128, 8], F32)
        gqk = p.sb("gqk", [128, 4], F32)
        epsc = p.sb("epsc", [128, 1], F32)
        scratch = p.sb("scratch", [128, 1], F32)
        p.dma(gmix[:], gmix_d.rearrange("(k p) -> p k", p=128), writes=cres(), semkey="c", allow_slow_non_contiguous=True, eng="act")
        p.dma(gffn[:], gffn_d.rearrange("(k p) -> p k", p=128), writes=cres(), semkey="c", allow_slow_non_contiguous=True, eng="act")
        p.dma(bga[:], bgate_d[0:D].rearrange("(k p) -> p k", p=128), writes=cres(), semkey="c", allow_slow_non_contiguous=True, eng="act")
        p.dma(bgb[:], bgate_d[D:2 * D].rearrange("(k p) -> p k", p=128), writes=cres(), semkey="c", allow_slow_non_contiguous=True, eng="act")
        for ci, gd in ((0, gq_d), (2, gk_d)):
            for half in range(2):
                p.dma(gqk[half * 64:(half + 1) * 64, ci:ci + 1], gd.rearrange("(p o) -> p o", o=1), writes=cres(), semkey="c", eng="act")
                for q4 in range(2):
                    p.dma(gqk[half * 64 + q4 * 32: half * 64 + q4 * 32 + 32, ci + 1:ci + 2],
                          gd[(1 - q4) * 32:(1 - q4) * 32 + 32].rearrange("(p o) -> p o", o=1), writes=cres(), semkey="c", eng="act")
        OP("dve", "memset", epsc[:], EPS, reads=Rcs, writes=[Rc])

        ident = CF("ident")
        identb = CB("identb")
        perm, bones = CB("perm"), CB("bones")
        mcur4, mprev4 = CB("mcur4"), CB("mprev4")
        decT = CF("decT").rearrange("p (h i) -> p h i", h=4)
        zeta = CF("zeta")
        xib = CF("xib").rearrange("p (h i) -> p h i", h=4)

        Rfinal = []
        xnT = p.sb("xnT", [128, 8, S], BF16)
        RxnT = [p.res(f"xnT{t}") for t in range(NT)]
        ss = p.sb("ss", [128, NT], F32)
        rt = p.sb("rt", [128, NT], F32)
        rstd = p.sb("rstd", [128, NT], F32)
        Rxin = [p.res(f"xin{i}") for i in range(2)]
        xs = [p.sb(f"xs{i}", [128, D], F32) for i in range(2)]
        Rxs = [p.res(f"xs{i}") for i in range(2)]
        junk = p.sb("junk", [128, D], BF16)
        Rjunk = p.res("junk")
        logit = p.sb("logit", [128, NT, 36], F32)
        Rlogit = [p.res(f"logit{t}") for t in range(NT)]

        def norm_tile_gen(src, Rsrc, t, gcol, Rstat_t, banks, lo=None, stats_done=False):
            i = t % 2
            if not stats_done:
                OP("act", "activation", out=junk[:], in_=src, func=AF.Square, accum_out=ss[:, t:t + 1],
                   reads=[Rsrc, Rc], writes=[Rjunk, Rstat_t])
                OP("act", "activation", out=rt[:, t:t + 1], in_=ss[:, t:t + 1], func=AF.Sqrt, scale=1.0 / D, bias=epsc[:],
                   reads=[Rstat_t, Rc], writes=[Rstat_t])
                OP("dve", "reciprocal", out=rstd[:, t:t + 1], in_=rt[:, t:t + 1], reads=[Rstat_t], writes=[Rstat_t])
            OP("dve", "tensor_scalar", out=xs[i][:], in0=src, scalar1=rstd[:, t:t + 1], scalar2=None, op0=ALU.mult,
               reads=[Rsrc, Rstat_t], writes=[Rxs[i]])
            yield
            for hb in range(2):
                b = banks[hb]
                for j in range(4):
                    k = hb * 4 + j
                    OP("pe", "transpose", out=ps[:, b, j * 128:(j + 1) * 128], in_=xs[i][:, k * 128:(k + 1) * 128],
                       identity=ident, reads=[Rxs[i], Rc], writes=[Rps[b]])
                gb_ = gcol[:, hb * 4:hb * 4 + 4].unsqueeze(2).to_broadcast([128, 4, 128])
                pin = ps[:, b, :].rearrange("p (j c) -> p j c", j=4)
                dsl = xnT[:, hb * 4:hb * 4 + 4, t * 128:(t + 1) * 128]
                if lo is None:
                    OP("dve", "tensor_tensor", out=dsl, in0=pin, in1=gb_, op=ALU.mult,
                       reads=[Rps[b], Rc], writes=[RxnT[t]])
                else:
                    vtmp, Rvtmp, lodst, Rlo = lo
                    OP("dve", "tensor_tensor", out=vtmp[:, hb * 4:hb * 4 + 4, :], in0=pin, in1=gb_, op=ALU.mult,
                       reads=[Rps[b], Rc], writes=[Rvtmp[hb]])
                    OP("act", "activation", out=dsl, in_=vtmp[:, hb * 4:hb * 4 + 4, :], func=AF.Copy,
                       reads=[Rvtmp[hb]], writes=[RxnT[t]])
                    OP("pool", "tensor_tensor", out=lodst[:, hb * 4:hb * 4 + 4, :], in0=vtmp[:, hb * 4:hb * 4 + 4, :],
                       in1=dsl, op=ALU.subtract, reads=[Rvtmp[hb], RxnT[t]], writes=[Rlo[hb]])
            yield

        def finish():
            p.op("sp", None, reads=list(Rfinal))
            p.emit()
            return nc

        def wsrc(wd, c0, c1):
            return wd[:, c0:c1].rearrange("(k p) n -> p k n", p=128)

        def load_w(dst_ap, src_ap, R, key):
            return p.dma(dst_ap, src_ap, writes=[R], semkey=key, eng="pool")

        Rstat = [p.res(f"stat{t}") for t in range(NT)]
        areset()
        xin = [alloc([128, D], F32) for i in range(4)]
        Rxin = [p.res(f"xin{i}") for i in range(4)]
        def vproj_tile(t):
            i = t % 2
            for cg in range(2):
                b = 4 + cg
                for k in range(8):
                    MM(ps[:, b, :], xnT[:, k, t * 128:(t + 1) * 128], wbig[cg][:, k, :], k == 0, k == 7,
                       [RxnT[t], Rwbig[cg]], [Rps[b]])
                pv = ps[:, b, :].rearrange("p (pr e d) -> p pr e d", e=2, d=64)
                OP("act", "activation", out=vst[i][:, cg * 4:(cg + 1) * 4, 0:64], in_=pv[:, :, 0, :], func=AF.Copy,
                   reads=[Rps[b]], writes=[Rvst[i]])
                OP("dve", "tensor_copy", out=vst[i][:, cg * 4:(cg + 1) * 4, 192:256], in_=pv[:, :, 1, :],
                   reads=[Rps[b]], writes=[Rvst[i]])
            p.dma(vx_d[t * 128:(t + 1) * 128, :, :], vst[i], reads=[Rvst[i]], writes=[Rvx[t]], semkey=("vx", t % 2))

        wbig = [alloc([128, 8, 512], BF16) for i in range(2)]
        Rwbig = [p.res(f"wbig{i}") for i in range(2)]
        vst = [alloc([128, 8, 256], BF16) for i in range(2)]
        Rvst = [p.res(f"vst{i}") for i in range(2)]
        Rvx = [p.res(f"vx{t}") for t in range(NT)]
        for i in range(2):
            OP("pool", "memset", vst[i], 1.0, writes=[Rvst[i]])
        for cg in range(2):
            load_w(wbig[cg], wsrc(w_in_d, 2048 + cg * 512, 2048 + (cg + 1) * 512), Rwbig[cg], ("wbig", cg))
        gens = {}
        do_v = stop_after != "A"
        for t in range(min(3, NT)):
            p.dma(xin[t % 4], x_d[t * 128:(t + 1) * 128, :], writes=[Rxin[t % 4]], semkey=("xin", t % 4))
        for step in range(NT + 2):
            if step < NT:
                t = step
                if t + 3 < NT:
                    p.dma(xin[(t + 3) % 4], x_d[(t + 3) * 128:(t + 4) * 128, :], writes=[Rxin[(t + 3) % 4]],
                          semkey=("xin", (t + 3) % 4))
                gens[t] = norm_tile_gen(xin[t % 4], Rxin[t % 4], t, gmix, Rstat[t], (6, 7))
                next(gens[t])
            if 0 <= step - 1 < NT:
                next(gens[step - 1])
            if do_v and 0 <= step - 2 < NT:
                vproj_tile(step - 2)
        if debug:
            Rfinal.append(p.res())
            p.dma(dbg["xnT"], xnT[:].rearrange("p k s -> p (k s)"), reads=RxnT, writes=[Rfinal[-1]], semkey="dbg0")
        if stop_after == "A":
            Rfinal.append(p.res("fin"))
            p.dma(out_d[0:128, :], xin[1], reads=[Rxin[1]], writes=[Rfinal[-1]], semkey="fin")
            return finish()

        if stop_after == "V":
            Rfinal.extend(Rvx)
            return finish()

        p.barrier(scratch[:])
        areset()
        catt = alloc([128, 2 * S], F32)
        Rcatt = p.res("catt")
        p.dma(catt, catt_d, writes=[Rcatt], semkey="catt")
        CA, SA = catt[:, 0:S], catt[:, S:2 * S]
        wqk = [[alloc([128, 8, 128], BF16) for j in range(2)] for i in range(2)]
        Rwqk = [[p.res(f"wqk{i}_{j}") for j in range(2)] for i in range(2)]
        QKT = [[alloc([128, S], BF16) for j in range(2)] for i in range(2)]
        RQK = [[[p.res(f"QK{i}_{j}_{g}") for g in range(NG)] for j in range(2)] for i in range(2)]
        vxs = [[alloc([128, 16, 256], BF16) for o in range(3)] for i in range(2)]
        Rvxs = [[p.res(f"vxs{i}_{o}") for o in range(3)] for i in range(2)]
        qbf = alloc([128, 512], BF16)
        sqb = alloc([128, 512], BF16)
        rtq = alloc([128, 512], F32)
        rsq = alloc([128, 512], F32)
        t1 = alloc([128, 512], F32)
        t2 = alloc([128, 512], F32)
        Rqbf, Rsqb, Rrtq, Rrsq, Rt1, Rt2 = (p.res(n) for n in ("qbf", "sqb", "rtq", "rsq", "t1", "t2"))
        Pb = [alloc([128, 512], BF16) for i in range(3)]
        RPb = [p.res(f"Pb{i}") for i in range(3)]
        rden = alloc([128, S], F32)
        Rrden = [p.res(f"rden{b}") for b in range(4)]
        zpair = [alloc([128, S], BF16) for i in range(2)]
        Rzpair = [p.res(f"zpair{i}") for i in range(2)]
        Rzatt = [p.res(f"zatt{i}") for i in range(8)]

        def load_pair_weights(pr):
            i = pr % 2
            load_w(wqk[i][0], wsrc(w_in_d, pr * 128, (pr + 1) * 128), Rwqk[i][0], ("wqk", i, 0))
            load_w(wqk[i][1], wsrc(w_in_d, 1024 + pr * 128, 1024 + (pr + 1) * 128), Rwqk[i][1], ("wqk", i, 1))

        def load_pair_v(pr):
            i = pr % 2
            src = vx_d[:, pr, :]
            p.dma(vxs[i][0], src.rearrange("(n i) m -> i n m", i=128), reads=Rvx, writes=[Rvxs[i][0]], semkey=("vxs", i, 0))
            s1 = src.rearrange("(n i c) m -> c i n m", i=128, c=4)
            for c in range(4):
                p.dma(vxs[i][1][:, c * 4:(c + 1) * 4, :], s1[c], reads=Rvx, writes=[Rvxs[i][1]], semkey=("vxs", i, 1))
            p.dma(vxs[i][2], src.rearrange("(i c) m -> i c m", c=16), reads=Rvx, writes=[Rvxs[i][2]], semkey=("vxs", i, 2))

        def colap(T, spec, r0):
            base, r, c = spec
            if r == 1:
                return T[r0:r0 + 64, base:base + 128]
            return T[r0:r0 + 64, base:base + 128 * r].rearrange("p (n r) -> p n r", r=r)[:, :, c]

        def prep_gen(pr):
            i = pr % 2
            for j, gi in ((0, 0), (1, 2)):
                dstT = QKT[i][j]
                for g in range(NG):
                    gs = slice(g * 512, (g + 1) * 512)
                    for k in range(8):
                        MM(ps[:, 6, :], wqk[i][j][:, k, :], xnT[:, k, gs], k == 0, k == 7,
                           [Rwqk[i][j]] + RxnT[g * 4:(g + 1) * 4], [Rps[6]])
                    OP("act", "activation", out=qbf, in_=ps[:, 6, :], func=AF.Copy, reads=[Rps[6]], writes=[Rqbf])
                    OP("dve", "scalar_tensor_tensor", out=t1, in0=ps[:, 6, :], scalar=gqk[:, gi:gi + 1], in1=CA[:, gs],
                       op0=ALU.mult, op1=ALU.mult, reads=[Rps[6], Rc, Rcatt], writes=[Rt1])
                    OP("pool", "tensor_tensor", out=sqb, in0=qbf, in1=qbf, op=ALU.mult, reads=[Rqbf], writes=[Rsqb])
                    yield
                    MM(ps[:, 7, :], bones, sqb, True, True, [Rsqb, Rc], [Rps[7]])
                    MM(ps[:, 6, :], perm, qbf, True, True, [Rqbf, Rc], [Rps[6]])
                    OP("act", "activation", out=rtq, in_=ps[:, 7, :], func=AF.Ln, scale=1.0 / 64, bias=epsc[:],
                       reads=[Rps[7], Rc], writes=[Rrtq])
                    OP("act", "activation", out=rsq, in_=rtq, func=AF.Exp, scale=-0.5, reads=[Rrtq], writes=[Rrsq])
                    OP("dve", "scalar_tensor_tensor", out=t2, in0=ps[:, 6, :], scalar=gqk[:, gi + 1:gi + 2], in1=SA[:, gs],
                       op0=ALU.mult, op1=ALU.mult, reads=[Rps[6], Rc, Rcatt], writes=[Rt2])
                    OP("pool", "tensor_tensor", out=t1, in0=t1, in1=t2, op=ALU.add, reads=[Rt1, Rt2], writes=[Rt1])
                    OP("dve", "tensor_tensor", out=dstT[:, gs], in0=t1, in1=rsq, op=ALU.mult,
                       reads=[Rt1, Rrsq], writes=[RQK[i][j][g]])
                    yield

        def att_batches(pr):
            def cols(r, c, n):
                return (n * 128 * r, r, c)
            tiles_cur, tiles_prev = [], []
            for n in range(16):
                tiles_cur.append((cols(1, 0, n), cols(1, 0, n), 0, n, ("n", n // 4, (n % 4) * 128, 1), n % 4 == 0))
            for c in range(4):
                for n in range(4):
                    tiles_cur.append((cols(4, c, n), cols(4, c, n), 1, c * 4 + n, ("n", n, c, 4), False))
            for c in range(16):
                tiles_cur.append((cols(16, c, 0), cols(16, c, 0), 2, c, ("s", c), False))
            for n in range(1, 16):
                tiles_prev.append((cols(1, 0, n - 1), cols(1, 0, n), 0, n - 1, ("n", n // 4, (n % 4) * 128, 1), False))
            for c in range(4):
                for n in range(1, 4):
                    tiles_prev.append((cols(4, c, n - 1), cols(4, c, n), 1, c * 4 + n - 1, ("n", n, c, 4), False))
            batches = []
            for lst, msk in ((tiles_cur, mcur4), (tiles_prev, mprev4)):
                for s0 in range(0, len(lst), 4):
                    batches.append((lst[s0:s0 + 4], msk))
            items = []
            for eh in range(2):
                for bi, (tl, msk) in enumerate(batches):
                    items.append((eh, bi, tl, msk, bi == len(batches) - 1))
            return items

        gcount = [0]

        def emit_S(pr, item):
            i = pr % 2
            eh, bi, tl, msk, last = item
            r0 = eh * 64
            gb = gcount[0]
            gcount[0] += 1
            sb_ = 4 + (gb % 2)
            pbi = gb % 3
            nt_ = len(tl)
            QT, KT = QKT[i]
            RQKall = RQK[i][0] + RQK[i][1]
            for ti, (kc, qc, vo, vb, osp, st) in enumerate(tl):
                MM(ps[:, sb_, ti * 128:(ti + 1) * 128], colap(KT, kc, r0), colap(QT, qc, r0), True, True,
                   RQKall, [Rps[sb_]])
            OP("act", "activation", out=Pb[pbi][:, 0:nt_ * 128], in_=ps[:, sb_, 0:nt_ * 128], func=AF.Exp, scale=0.125,
               reads=[Rps[sb_]], writes=[RPb[pbi]])
            OP("dve", "tensor_tensor", out=Pb[pbi][:, 0:nt_ * 128], in0=Pb[pbi][:, 0:nt_ * 128],
               in1=msk[:, 0:nt_ * 128], op=ALU.mult, reads=[RPb[pbi], Rc], writes=[RPb[pbi]])
            return pbi

        def emit_PV(pr, item, pbi):
            i = pr % 2
            eh, bi, tl, msk, last = item
            r0 = eh * 64
            zp = zpair[i]
            for ti, (kc, qc, vo, vb, osp, st) in enumerate(tl):
                lhsT = vxs[i][vo][:, vb, eh * 128:(eh + 1) * 128]
                if osp[0] == "n":
                    _, bank, off, r = osp
                    if r == 1:
                        oap = ps[:, bank, off:off + 128]
                    else:
                        oap = ps[:, bank, :].rearrange("p (n r) -> p n r", r=r)[:, :, off]
                    MM(oap, lhsT, Pb[pbi][:, ti * 128:(ti + 1) * 128], st, False,
                       [RPb[pbi], Rvxs[i][vo]], [Rps[bank]], skip_group_check=True)
                else:
                    c = osp[1]
                    for bank in range(4):
                        oap = ps[:, bank, :].rearrange("p (n r) -> p n r", r=16)[:, :, c]
                        MM(oap, lhsT, Pb[pbi][:, ti * 128 + bank * 32: ti * 128 + bank * 32 + 32], False, False,
                           [RPb[pbi], Rvxs[i][vo]], [Rps[bank]], skip_group_check=True)
            if last:
                d0 = 64 - r0
                for bank in range(4):
                    bs = slice(bank * 512, (bank + 1) * 512)
                    OP("act", "activation", out=rden[r0:r0 + 64, bs], in_=ps[d0:d0 + 64, bank, :], func=AF.Ln,
                       reads=[Rps[bank]], writes=[Rrden[bank]])
                    OP("act", "activation", out=rden[r0:r0 + 64, bs], in_=rden[r0:r0 + 64, bs], func=AF.Exp, scale=-1.0,
                       reads=[Rrden[bank]], writes=[Rrden[bank]])
                    OP("dve", "tensor_tensor", out=zp[r0:r0 + 64, bs], in0=ps[r0:r0 + 64, bank, :],
                       in1=rden[r0:r0 + 64, bs], op=ALU.mult, reads=[Rps[bank], Rrden[bank]], writes=[Rzpair[i]])

        load_pair_weights(0)
        load_pair_weights(1)
        load_pair_v(0)
        for _ in prep_gen(0):
            pass
        for pr in range(8):
            i = pr % 2
            if pr + 1 < 8:
                load_pair_v(pr + 1)
            nxt = prep_gen(pr + 1) if pr + 1 < 8 else None
            items = att_batches(pr)
            pq = [emit_S(pr, items[0]), emit_S(pr, items[1])]
            for ii, item in enumerate(items):
                if ii + 2 < len(items):
                    pq.append(emit_S(pr, items[ii + 2]))
                emit_PV(pr, item, pq.pop(0))
                if nxt is not None and ii % 2 == 1:
                    next(nxt, None)
            if nxt is not None:
                for _ in nxt:
                    pass
            if pr + 2 < 8:
                load_pair_weights(pr + 2)
            p.dma(zatt_d[pr * 128:(pr + 1) * 128, :], zpair[i], reads=[Rzpair[i]], writes=[Rzatt[pr]], semkey=("zatt", i))
        if stop_after == "ATT":
            Rfinal.extend(Rzatt)
            return finish()

        p.barrier(scratch[:])
        areset()
        cret = alloc([128, 2 * S], F32)
        Rcret = p.res("cret")
        p.dma(cret, cret_d, writes=[Rcret], semkey="cret")
        CR, SR = cret[:, 0:S], cret[:, S:2 * S]
        gretb = alloc([128, 4, 512], F32)
        Rgretb = p.res("gretb")
        p.dma(gretb.rearrange("p h v -> p (h v)"), gret_d.rearrange("h v -> (h v)").partition_broadcast(128),
              writes=[Rgretb], semkey="gretb")
        wq_r = [alloc([128, 8, 256], BF16) for i in range(2)]
        wk_r = [alloc([128, 8, 256], BF16) for i in range(2)]
        wv_r = [alloc([128, 8, 512], BF16) for i in range(2)]
        wg_r = [alloc([128, 8, 512], BF16) for i in range(2)]
        Rwr = [[p.res(f"wr{i}_{j}") for j in range(4)] for i in range(2)]
        QrT = [alloc([128, 2, 512], BF16) for i in range(2)]
        KrT = [alloc([128, 2, 512], BF16) for i in range(2)]
        Qxi = [alloc([128, 2, 512], BF16) for i in range(2)]
        RQr = [p.res(f"QrT{i}") for i in range(2)]
        RKr = [p.res(f"KrT{i}") for i in range(2)]
        RQx = [p.res(f"Qxi{i}") for i in range(2)]
        sgr = [alloc([128, 4, 512], BF16) for i in range(2)]
        Rsgr = [p.res(f"sgr{i}") for i in range(2)]
        vr_sb = [alloc([128, 512], BF16) for i in range(2)]
        Rvr = [p.res(f"vr{i}") for i in range(2)]
        kz = [alloc([128, 256], BF16) for i in range(2)]
        Rkz = [p.res(f"kz{i}") for i in range(2)]
        inm = [alloc([128, 128], BF16) for i in range(2)]
        Rinm = [p.res(f"inm{i}") for i in range(2)]
        yn = [alloc([128, 512], BF16) for i in range(2)]
        Ryn = [p.res(f"yn{i}") for i in range(2)]
        zst = [alloc([128, 4, 512], BF16) for i in range(2)]
        Rzst = [p.res(f"zst{i}") for i in range(2)]
        state = alloc([128, 2, 512], F32)
        state_bf = alloc([128, 2, 512], BF16)
        Rstate = [p.res(f"state{c}") for c in range(2)]
        Rstbf = [p.res(f"stbf{c}") for c in range(2)]
        tmp = [alloc([128, 512], F32) for i in range(4)]
        Rtmp = [p.res(f"rtmp{i}") for i in range(4)]
        ssr = alloc([128, 64], F32)
        rtr = alloc([128, 64], F32)
        rsr = alloc([128, 64], F32)
        Rzret = []

        def load_head_weights(h):
            i = h % 2
            load_w(wq_r[i], wsrc(w_in_d, 3072 + h * 256, 3072 + (h + 1) * 256), Rwr[i][0], ("wr", i, 0))
            load_w(wk_r[i], wsrc(w_in_d, 4096 + h * 256, 4096 + (h + 1) * 256), Rwr[i][1], ("wr", i, 1))
            load_w(wv_r[i], wsrc(w_in_d, 5120 + h * 512, 5120 + (h + 1) * 512), Rwr[i][2], ("wr", i, 2))
            load_w(wg_r[i], wsrc(w_in_d, 7168 + h * 512, 7168 + (h + 1) * 512), Rwr[i][3], ("wr", i, 3))

        vr3 = vr_sb + [alloc([128, 512], BF16)]
        Rvr3 = Rvr + [p.res("vr2")]
        kz3 = kz + [alloc([128, 256], BF16)]
        Rkz3 = Rkz + [p.res("kz2")]
        inm3 = inm + [alloc([128, 128], BF16)]
        Rinm3 = Rinm + [p.res("inm2")]
        INNER = ps[:, 3, 0:128]
        ysb = [alloc([128, 512], F32) for i in range(2)]
        Rysb = [p.res(f"ysb{i}") for i in range(2)]

        def proj_pieces(h, g):
            wi = h % 2
            gi = g % 2
            gs = slice(g * 512, (g + 1) * 512)
            RxG = RxnT[g * 4:(g + 1) * 4]

            def qk(which):
                wsb = (wq_r, wk_r)[which][wi]
                dst = (QrT, KrT)[which][gi]
                Rdst = (RQr, RKr)[which][gi]
                for c in range(2):
                    for k in range(8):
                        MM(ps[:, c, :], wsb[:, k, c * 128:(c + 1) * 128], xnT[:, k, gs], k == 0, k == 7,
                           [Rwr[wi][which]] + RxG, [Rps[c]])
                OP("dve", "tensor_tensor", out=tmp[0], in0=ps[:, 0, :], in1=CR[:, gs], op=ALU.mult,
                   reads=[Rps[0], Rcret], writes=[Rtmp[0]])
                OP("dve", "tensor_tensor", out=tmp[1], in0=ps[:, 1, :], in1=SR[:, gs], op=ALU.mult,
                   reads=[Rps[1], Rcret], writes=[Rtmp[1]])
                OP("pool", "tensor_tensor", out=dst[:, 0, :], in0=tmp[0], in1=tmp[1], op=ALU.subtract,
                   reads=[Rtmp[0], Rtmp[1]], writes=[Rdst])
                OP("dve", "tensor_tensor", out=tmp[2], in0=ps[:, 1, :], in1=CR[:, gs], op=ALU.mult,
                   reads=[Rps[1], Rcret], writes=[Rtmp[2]])
                OP("dve", "tensor_tensor", out=tmp[3], in0=ps[:, 0, :], in1=SR[:, gs], op=ALU.mult,
                   reads=[Rps[0], Rcret], writes=[Rtmp[3]])
                OP("pool", "tensor_tensor", out=dst[:, 1, :], in0=tmp[2], in1=tmp[3], op=ALU.add,
                   reads=[Rtmp[2], Rtmp[3]], writes=[Rdst])
                if which == 0:
                    xb = xib[:, h, :].unsqueeze(1).to_broadcast([128, 4, 128])
                    for c in range(2):
                        OP("pool", "tensor_tensor", out=Qxi[gi][:, c, :].rearrange("p (n i) -> p n i", n=4),
                           in0=dst[:, c, :].rearrange("p (n i) -> p n i", n=4), in1=xb, op=ALU.mult,
                           reads=[Rdst, Rc], writes=[RQx[gi]])

            def gate(vc):
                b = vc % 2
                for k in range(8):
                    MM(ps[:, b, :], wg_r[wi][:, k, vc * 128:(vc + 1) * 128], xnT[:, k, gs], k == 0, k == 7,
                       [Rwr[wi][3]] + RxG, [Rps[b]])
                OP("act", "activation", out=sgr[gi][:, vc, :], in_=ps[:, b, :], func=AF.Silu,
                   reads=[Rps[b]], writes=[Rsgr[gi]])

            return [lambda: qk(0), lambda: qk(1), lambda: gate(0), lambda: gate(1), lambda: gate(2), lambda: gate(3)]

        def stage_A(q):
            h, gn = q // 16, q % 16
            g, n = gn // 4, gn % 4
            wi, gi, b3 = h % 2, g % 2, q % 3
            cs = slice(n * 128, (n + 1) * 128)
            for k in range(8):
                MM(ps[:, 2, :], xnT[:, k, gn * 128:(gn + 1) * 128], wv_r[wi][:, k, :], k == 0, k == 7,
                   [Rwr[wi][2], RxnT[gn]], [Rps[2]])
            OP("act", "activation", out=vr3[b3], in_=ps[:, 2, :], func=AF.Copy, reads=[Rps[2]], writes=[Rvr3[b3]])
            for c in range(2):
                OP("pe", "transpose", out=psb(4)[:, 512 + c * 128: 512 + (c + 1) * 128], in_=KrT[gi][:, c, cs],
                   identity=identb, reads=[RKr[gi], Rc], writes=[Rps[4]])
            OP("act", "activation", out=kz3[b3], in_=psb(4)[:, 512:768], func=AF.Copy, scale=zeta[:, h:h + 1],
               reads=[Rps[4], Rc], writes=[Rkz3[b3]])
            for c in range(2):
                MM(INNER, KrT[gi][:, c, cs], QrT[gi][:, c, cs], c == 0, c == 1, [RKr[gi], RQr[gi]], [Rps[3]])
            OP("dve", "tensor_tensor", out=inm3[b3], in0=INNER, in1=decT[:, h, :], op=ALU.mult,
               reads=[Rps[3], Rc], writes=[Rinm3[b3]])

        Rst_q = {}

        def stage_B(q):
            h, gn = q // 16, q % 16
            g, n = gn // 4, gn % 4
            gi, b3, ci = g % 2, q % 3, q % 2
            cs = slice(n * 128, (n + 1) * 128)
            yb = 5
            st_i = q
            gC = float(gamma_c[h])
            MM(ps[:, yb, :], inm3[b3], vr3[b3], True, gn == 0, [Rinm3[b3], Rvr3[b3]], [Rps[yb]])
            if gn > 0:
                for c in range(2):
                    MM(ps[:, yb, :], Qxi[gi][:, c, cs], state_bf[:, c, :], False, c == 1, [RQx[gi], Rstbf[c]], [Rps[yb]])
            if gn < 15:
                for c in range(2):
                    MM(ps[:, 6 + c, :], kz3[b3][:, c * 128:(c + 1) * 128], vr3[b3], True, True,
                       [Rkz3[b3], Rvr3[b3]], [Rps[6 + c]])
                    if gn == 0:
                        OP("dve", "tensor_copy", out=state[:, c, :], in_=ps[:, 6 + c, :],
                           reads=[Rps[6 + c]], writes=[Rstate[c]])
                    else:
                        OP("dve", "scalar_tensor_tensor", out=state[:, c, :], in0=state[:, c, :], scalar=gC,
                           in1=ps[:, 6 + c, :], op0=ALU.mult, op1=ALU.add,
                           reads=[Rps[6 + c], Rstate[c]], writes=[Rstate[c]])
                    OP("pool", "tensor_copy", out=state_bf[:, c, :], in_=state[:, c, :],
                       reads=[Rstate[c]], writes=[Rstbf[c]])
            Rst = p.res()
            OP("act", "activation", out=ysb[ci], in_=ps[:, yb, :], func=AF.Copy, reads=[Rps[yb]], writes=[Rysb[ci]])
            OP("act", "activation", out=junk[:, 0:512], in_=ysb[ci], func=AF.Square,
               accum_out=ssr[:, st_i:st_i + 1], reads=[Rysb[ci]], writes=[Rjunk, Rst])
            OP("act", "activation", out=rtr[:, st_i:st_i + 1], in_=ssr[:, st_i:st_i + 1], func=AF.Sqrt,
               scale=1.0 / 512, bias=epsc[:], reads=[Rst, Rc], writes=[Rst])
            OP("dve", "reciprocal", out=rsr[:, st_i:st_i + 1], in_=rtr[:, st_i:st_i + 1], reads=[Rst], writes=[Rst])
            OP("act", "activation", out=yn[ci], in_=ysb[ci], func=AF.Copy, scale=rsr[:, st_i:st_i + 1],
               reads=[Rysb[ci], Rst], writes=[Ryn[ci]])
            OP("pool", "tensor_tensor", out=yn[ci], in0=yn[ci], in1=gretb[:, h, :], op=ALU.mult,
               reads=[Ryn[ci], Rgretb], writes=[Ryn[ci]])

        def stage_C(q):
            h, gn = q // 16, q % 16
            g, n = gn // 4, gn % 4
            gi, ci = g % 2, q % 2
            cs = slice(n * 128, (n + 1) * 128)
            gs = slice(g * 512, (g + 1) * 512)
            for vc in range(4):
                OP("pe", "transpose", out=psb(4)[:, vc * 128:(vc + 1) * 128], in_=yn[ci][:, vc * 128:(vc + 1) * 128],
                   identity=identb, reads=[Ryn[ci], Rc], writes=[Rps[4]])
            OP("dve", "tensor_tensor", out=zst[gi][:, :, cs], in0=psb(4)[:, 0:512].rearrange("p (v i) -> p v i", v=4),
               in1=sgr[gi][:, :, cs], op=ALU.mult, reads=[Rps[4], Rsgr[gi]], writes=[Rzst[gi]])
            if n == 3:
                Rz = p.res()
                Rzret.append(Rz)
                p.dma(zret_d[h * 512:(h + 1) * 512, gs].rearrange("(vc p) s -> p vc s", p=128), zst[gi],
                      reads=[Rzst[gi]], writes=[Rz], semkey=("zret", gi))

        load_head_weights(0)
        load_head_weights(1)
        units = [(h, g) for h in range(4) for g in range(NG)]
        for f in proj_pieces(*units[0]):
            f()
        pending = []
        for s_ in range(64 + 2):
            if s_ < 64:
                u, n = s_ // 4, s_ % 4
                if n == 0:
                    pending = proj_pieces(*units[u + 1]) if u + 1 < 16 else []
                if s_ % 16 == 1 and 2 <= s_ // 16 + 1 < 4:
                    load_head_weights(s_ // 16 + 1)
                stage_A(s_)
            if 0 <= s_ - 1 < 64:
                stage_B(s_ - 1)
            if 0 <= s_ - 2 < 64:
                stage_C(s_ - 2)
            if s_ < 64:
                take = 1 if n < 2 else 2
                for f in pending[:take]:
                    f()
                pending = pending[take:]
        if stop_after == "RET":
            Rfinal.extend(Rzret + Rzatt)
            return finish()

        p.barrier(scratch[:])
        areset()
        yacc = alloc([128, NT, D], F32)
        Ryacc = [p.res(f"yacc{t}") for t in range(NT)]
        moe_base = apos[0]
        za = alloc([128, 8, 512], BF16)
        zr = alloc([128, 16, 512], BF16)
        Rza, Rzr = p.res("za"), p.res("zr")
        wa = [alloc([128, 8, 128], BF16) for i in range(2)]
        wr = [alloc([128, 16, 128], BF16) for i in range(2)]
        wga = [alloc([128, 8, 128], BF16) for i in range(2)]
        wgb = [alloc([128, 8, 128], BF16) for i in range(2)]
        Rwm = [[p.res(f"wm{i}_{j}") for j in range(4)] for i in range(2)]
        mT2 = [alloc([128, 8, 512], BF16) for i in range(2)]
        RmT2 = [[p.res(f"mT{i}_{k}") for k in range(8)] for i in range(2)]
        wout = alloc([128, 8, 512], BF16)
        Rwout = p.res("wout")
        sga = alloc([128, 512], F32)
        sgb = alloc([128, 512], F32)
        ta = alloc([128, 512], F32)
        tb = alloc([128, 512], F32)
        Rsga, Rsgb, Rta, Rtb = (p.res(n) for n in ("sga", "sgb", "ta", "tb"))
        a_alias = apos[0]
        vtmp = alloc([128, 8, 128], F32)
        Rvtmp = [p.res(f"vtmp{i}") for i in range(2)]
        xlo = [alloc([128, 8, 128], BF16) for i in range(2)]
        Rxlo = [[p.res(f"xlo{i}_{hb}") for hb in range(2)] for i in range(2)]
        assert apos[0] - a_alias == 8 * 512 * 2
        wout2 = arena[0:128, a_alias // 2: a_alias // 2 + 8 * 512].rearrange("p (a b) -> p a b", a=8)
        Rwout2 = p.res("wout2")
        Ralias = Rvtmp + Rxlo[0] + Rxlo[1]
        wrf = alloc([128, 8, 36], F32)
        wrh = alloc([128, 8, 36], BF16)
        wrl = alloc([128, 8, 36], BF16)
        Rwrf, Rwrh, Rwrl = p.res("wrf"), p.res("wrh"), p.res("wrl")
        p.dma(wrf[:, :, 0:4], wrg_d.rearrange("(k p) n -> p k n", p=128), writes=[Rwrf], semkey="wrf")
        p.dma(wrf[:, :, 4:36], wre_d.rearrange("(k p) n -> p k n", p=128), writes=[Rwrf], semkey="wrf")
        OP("dve", "tensor_copy", out=wrh, in_=wrf, reads=[Rwrf], writes=[Rwrh])
        OP("dve", "tensor_tensor", out=wrl, in0=wrf, in1=wrh, op=ALU.subtract, reads=[Rwrf, Rwrh], writes=[Rwrl])

        def load_merge_weights(fc, slot):
            cs_ = slice(fc * 128, (fc + 1) * 128)
            load_w(wa[slot], watt_d[:, cs_].rearrange("(k p) n -> p k n", p=128), Rwm[slot][0], ("wm", slot, 0))
            load_w(wr[slot], wret_d[:, cs_].rearrange("(k p) n -> p k n", p=128), Rwm[slot][1], ("wm", slot, 1))
            load_w(wga[slot], wsrc(w_in_d, 9216 + fc * 128, 9216 + (fc + 1) * 128), Rwm[slot][2], ("wm", slot, 2))
            load_w(wgb[slot], wsrc(w_in_d, 10240 + fc * 128, 10240 + (fc + 1) * 128), Rwm[slot][3], ("wm", slot, 3))

        Rstat2 = [p.res(f"stat2_{t}") for t in range(NT)]

        def post_gen(g):
            mT = mT2[g % 2]
            RmT = RmT2[g % 2]
            for tt in range(4):
                gt = g * 4 + tt
                p.dma(yacc[:, gt, :], x_d[gt * 128:(gt + 1) * 128, :], writes=[Ryacc[gt]], semkey=("xres", gt))
            for half in range(2):
                wsb_ = (wout, wout2)[half]
                Rw_ = [Rwout] if half == 0 else [Rwout2] + Ralias
                for tt in range(4):
                    gt = g * 4 + tt
                    b = 4 + (tt % 2)
                    for k in range(8):
                        MM(ps[:, b, :], mT[:, k, tt * 128:(tt + 1) * 128], wsb_[:, k, :], k == 0, k == 7,
                           [RmT[k]] + Rw_, [Rps[b]])
                    OP("dve", "tensor_tensor", out=yacc[:, gt, half * 512:(half + 1) * 512], in0=ps[:, b, :],
                       in1=yacc[:, gt, half * 512:(half + 1) * 512], op=ALU.add, reads=[Rps[b], Ryacc[gt]], writes=[Ryacc[gt]])
                    yield
            def router(gt, i):
                n_mm = 0
                for (lh, rw, Rl, Rw_) in ((xnT[:, :, gt * 128:(gt + 1) * 128], wrh, [RxnT[gt]], Rwrh),
                                          (xnT[:, :, gt * 128:(gt + 1) * 128], wrl, [RxnT[gt]], Rwrl),
                                          (xlo[i], wrh, Rxlo[i], Rwrh)):
                    for k in range(8):
                        MM(ps[:, 5, 0:36], lh[:, k, :], rw[:, k, :], n_mm == 0, n_mm == 23, Rl + [Rw_], [Rps[5]])
                        n_mm += 1
                OP("dve", "tensor_copy", out=logit[:, gt, :], in_=ps[:, 5, 0:36], reads=[Rps[5]], writes=[Rlogit[gt]])

            pend = None
            for tt in range(4):
                gt = g * 4 + tt
                i = gt % 2
                for _ in norm_tile_gen(yacc[:, gt, :], Ryacc[gt], gt, gffn, Rstat2[gt], (6, 7), lo=(vtmp, Rvtmp, xlo[i], Rxlo[i])):
                    yield
                if pend is not None:
                    router(*pend)
                pend = (gt, i)
                yield
            router(*pend)
            yield
            if g + 1 < NG:
                p.dma(wout2, wsrc(wout_d, 512, 1024), writes=[Rwout2] + Ralias, semkey="wout2", eng="pool")

        widx = 0
        load_merge_weights(0, 0)
        load_w(wout, wsrc(wout_d, 0, 512), Rwout, "wout")
        p.dma(wout2, wsrc(wout_d, 512, 1024), writes=[Rwout2] + Ralias, semkey="wout2", eng="pool")
        post = None
        for g in range(NG):
            gs = slice(g * 512, (g + 1) * 512)
            RxG = RxnT[g * 4:(g + 1) * 4]
            mT = mT2[g % 2]
            RmT = RmT2[g % 2]
            p.dma(za, zatt_d[:, gs].rearrange("(k p) s -> p k s", p=128), reads=Rzatt, writes=[Rza], semkey="za")
            p.dma(zr, zret_d[:, gs].rearrange("(k p) s -> p k s", p=128), reads=Rzret, writes=[Rzr], semkey="zr")
            for fc in range(8):
                slot = widx % 2
                widx += 1
                nfc, ng_ = (fc + 1) % 8, g + (1 if fc == 7 else 0)
                if ng_ < NG:
                    load_merge_weights(nfc, widx % 2)
                for k in range(8):
                    MM(ps[:, 2, :], wga[slot][:, k, :], xnT[:, k, gs], k == 0, k == 7, [Rwm[slot][2]] + RxG, [Rps[2]])
                for k in range(8):
                    MM(ps[:, 3, :], wgb[slot][:, k, :], xnT[:, k, gs], k == 0, k == 7, [Rwm[slot][3]] + RxG, [Rps[3]])
                for k in range(8):
                    MM(ps[:, 0, :], wa[slot][:, k, :], za[:, k, :], k == 0, k == 7, [Rwm[slot][0], Rza], [Rps[0]])
                for k in range(16):
                    MM(ps[:, 1, :], wr[slot][:, k, :], zr[:, k, :], k == 0, k == 15, [Rwm[slot][1], Rzr], [Rps[1]])
                OP("act", "activation", out=sga, in_=ps[:, 2, :], func=AF.Sigmoid, bias=bga[:, fc:fc + 1],
                   reads=[Rps[2], Rc], writes=[Rsga])
                OP("act", "activation", out=sgb, in_=ps[:, 3, :], func=AF.Sigmoid, bias=bgb[:, fc:fc + 1],
                   reads=[Rps[3], Rc], writes=[Rsgb])
                OP("dve", "tensor_tensor", out=ta, in0=ps[:, 0, :], in1=sga, op=ALU.mult, reads=[Rps[0], Rsga], writes=[Rta])
                OP("dve", "tensor_tensor", out=tb, in0=ps[:, 1, :], in1=sgb, op=ALU.mult, reads=[Rps[1], Rsgb], writes=[Rtb])
                OP("pool", "tensor_tensor", out=mT[:, fc, :], in0=ta, in1=tb, op=ALU.add, reads=[Rta, Rtb], writes=[RmT[fc]])
                if post is not None:
                    next(post, None)
                    next(post, None)
                    next(post, None)
            if post is not None:
                for _ in post:
                    pass
            post = post_gen(g)
        for _ in post:
            pass
        if debug:
            Rfinal.append(p.res())
            p.dma(dbg["x1"].rearrange("(t p) d -> p t d", p=128), yacc, reads=Ryacc, writes=[Rfinal[-1]], semkey="dbg1")
            Rfinal.append(p.res())
            p.dma(dbg["logits"], logit[:].rearrange("p t e -> p (t e)"), reads=Rlogit, writes=[Rfinal[-1]], semkey="dbg2")
        if stop_after == "MERGE":
            Rfinal.extend(Ryacc + Rlogit)
            return finish()

        p.barrier(scratch[:])
        areset(moe_base)
        Rr = p.res("route")
        RW = [Rr]
        gT = alloc([128, S], BF16)
        RgT = p.res("gT")
        OP("pool", "memset", gT[64:128, :], 0.0, writes=[RgT])
        csel = alloc([128, 4096], BF16)
        Rcsel = p.res("csel")
        p.dma(csel, csel_d, writes=[Rcsel], semkey="csel")
        NSLOT = 3
        NS2 = 4
        w1s = [alloc([128, 8, 256], BF16) for i in range(NSLOT)]
        w3s = [alloc([128, 8, 256], BF16) for i in range(NSLOT)]
        w2s = [alloc([128, 2, D], BF16) for i in range(NS2)]
        Rw1 = [p.res(f"w1s{i}") for i in range(NSLOT)]
        Rw3 = [p.res(f"w3s{i}") for i in range(NSLOT)]
        Rw2 = [p.res(f"w2s{i}") for i in range(NS2)]

        def load_expert(e):
            sl = e % NSLOT
            load_w(w1s[sl], w1_d[e].rearrange("(k p) n -> p k n", p=128), Rw1[sl], ("w1s", sl))
            load_w(w3s[sl], w3_d[e].rearrange("(k p) n -> p k n", p=128), Rw3[sl], ("w3s", sl))
            load_w(w2s[e % NS2], w2_d[e].rearrange("(k p) n -> p k n", p=128), Rw2[e % NS2], ("w2s", e % NS2))

        load_expert(0)
        load_expert(1)
        route_base = apos[0]
        brb = alloc([128, 36], F32)
        p.dma(brb[:, 0:4], brg_d.partition_broadcast(128), writes=RW, semkey="brb")
        p.dma(brb[:, 4:36], bre_d.partition_broadcast(128), writes=RW, semkey="brb")
        L = alloc([128, NT, 36], F32)
        gmax = alloc([128, NT], F32)
        ohg = alloc([128, NT, 4], F32)
        tg4 = alloc([128, NT, 4], F32)
        den = alloc([128, NT], F32)
        pg = alloc([128, NT], F32)
        sel4 = alloc([128, NT, 4, 8], F32)
        ing = alloc([128, NT, 8], F32)
        ing2 = alloc([128, NT, 8], F32)
        m1 = alloc([128, NT], F32)
        m2 = alloc([128, NT], F32)
        oh1 = alloc([128, NT, 8], F32)
        oh2 = alloc([128, NT, 8], F32)
        dd = alloc([128, NT], F32)
        e2 = alloc([128, NT], F32)
        w1_ = alloc([128, NT], F32)
        w2_ = alloc([128, NT], F32)
        ge = alloc([128, NT, 8], F32)
        ge2 = alloc([128, NT, 8], F32)
        gate = alloc([128, NT, 4, 8], F32)
        glo = alloc([32, S], BF16)

        def bc3(a2, n):
            return a2.unsqueeze(2).to_broadcast([128, NT, n])

        def DV(meth, **kw):
            return OP("dve", meth, reads=RW + Rlogit, writes=RW, **kw)

        DV("tensor_tensor", out=L, in0=logit[:], in1=brb.unsqueeze(1).to_broadcast([128, NT, 36]), op=ALU.add)
        gl = L[:, :, 0:4]
        el = L[:, :, 4:36].rearrange("p t (g e) -> p t g e", g=4)
        DV("tensor_reduce", out=gmax, in_=gl, axis=AX.X, op=ALU.max)
        DV("tensor_tensor", out=ohg, in0=gl, in1=bc3(gmax, 4), op=ALU.is_equal)
        DV("tensor_tensor", out=tg4, in0=gl, in1=bc3(gmax, 4), op=ALU.subtract)
        OP("act", "activation", out=tg4, in_=tg4, func=AF.Exp, reads=RW, writes=RW)
        DV("tensor_reduce", out=den, in_=tg4, axis=AX.X, op=ALU.add)
        DV("reciprocal", out=pg, in_=den)
        DV("tensor_tensor", out=sel4, in0=el, in1=ohg.unsqueeze(3).to_broadcast([128, NT, 4, 8]), op=ALU.mult)
        DV("tensor_reduce", out=ing, in_=sel4.rearrange("p t g e -> p t e g"), axis=AX.X, op=ALU.add)
        DV("tensor_reduce", out=m1, in_=ing, axis=AX.X, op=ALU.max)
        DV("tensor_tensor", out=oh1, in0=ing, in1=bc3(m1, 8), op=ALU.is_equal)
        DV("scalar_tensor_tensor", out=ing2, in0=oh1, scalar=-1.0e30, in1=ing, op0=ALU.mult, op1=ALU.add)
        DV("tensor_reduce", out=m2, in_=ing2, axis=AX.X, op=ALU.max)
        DV("tensor_tensor", out=oh2, in0=ing2, in1=bc3(m2, 8), op=ALU.is_equal)
        DV("tensor_tensor", out=dd, in0=m2, in1=m1, op=ALU.subtract)
        OP("act", "activation", out=e2, in_=dd, func=AF.Exp, reads=RW, writes=RW)
        DV("tensor_scalar", out=w1_, in0=e2, scalar1=1.0, scalar2=None, op0=ALU.add)
        DV("reciprocal", out=w1_, in_=w1_)
        DV("tensor_tensor", out=w2_, in0=e2, in1=w1_, op=ALU.mult)
        DV("tensor_tensor", out=w1_, in0=w1_, in1=pg, op=ALU.mult)
        DV("tensor_tensor", out=w2_, in0=w2_, in1=pg, op=ALU.mult)
        DV("tensor_tensor", out=ge, in0=oh1, in1=bc3(w1_, 8), op=ALU.mult)
        DV("tensor_tensor", out=ge2, in0=oh2, in1=bc3(w2_, 8), op=ALU.mult)
        DV("tensor_tensor", out=ge, in0=ge, in1=ge2, op=ALU.add)
        DV("tensor_tensor", out=gate, in0=ohg.unsqueeze(3).to_broadcast([128, NT, 4, 8]),
           in1=ge.unsqueeze(2).to_broadcast([128, NT, 4, 8]), op=ALU.mult)
        if debug:
            Rfinal.append(p.res())
            p.dma(dbg["gate"], gate.rearrange("p t g e -> p (t g e)"), reads=RW, writes=[Rfinal[-1]], semkey="dbg3")
        for t in range(NT):
            OP("pe", "transpose", out=ps[0:32, t // 4, (t % 4) * 128:(t % 4 + 1) * 128],
               in_=gate[:, t, :, :].rearrange("p g e -> p (g e)"), identity=ident, reads=RW + [Rc], writes=[Rps[t // 4]])
        gps = ps[0:32, 0:4, :].rearrange("p a b -> p (a b)")
        OP("act", "activation", out=gT[0:32, :], in_=gps, func=AF.Copy, reads=Rps[0:4], writes=[RgT])
        OP("dve", "tensor_tensor", out=glo, in0=gps, in1=gT[0:32, :], op=ALU.subtract, reads=Rps[0:4] + [RgT], writes=RW)
        OP("dve", "tensor_copy", out=gT[32:64, :], in_=glo, reads=RW, writes=[RgT])
        if stop_after == "ROUTE":
            Rfinal.extend(Ryacc + [RgT] + RW)
            return finish()

        p.barrier(scratch[:])
        areset(route_base)
        hg = [alloc([128, 2, S], BF16) for i in range(2)]
        Rhg = [[p.res(f"hg{i}_{g}") for g in range(NG)] for i in range(2)]
        s_sb = [alloc([128, 512], F32) for i in range(2)]
        u_sb = [alloc([128, 512], F32) for i in range(2)]
        gbc = [alloc([128, 512], F32) for i in range(2)]
        Rs = [p.res(f"s{i}") for i in range(2)]
        Ru = [p.res(f"u{i}") for i in range(2)]
        Rgbc = [p.res(f"gbc{i}") for i in range(2)]

        ybank = [0]

        def down_unit(e, t, half):
            hi_ = e % 2
            b = 5 + (ybank[0] % 3)
            ybank[0] += 1
            for fc in range(2):
                MM(ps[:, b, :], hg[hi_][:, fc, t * 128:(t + 1) * 128], w2s[e % NS2][:, fc, half * 512:(half + 1) * 512],
                   fc == 0, fc == 1, [Rhg[hi_][t // 4], Rw2[e % NS2]], [Rps[b]])
            OP("dve", "tensor_tensor", out=yacc[:, t, half * 512:(half + 1) * 512],
               in0=ps[:, b, :], in1=yacc[:, t, half * 512:(half + 1) * 512], op=ALU.add,
               reads=[Rps[b], Ryacc[t]], writes=[Ryacc[t]])

        gcnt = 0
        for e in range(32):
            sl = e % NSLOT
            hi_ = e % 2
            if e + 2 < 32:
                load_expert(e + 2)
            for g in range(NG):
                gs = slice(g * 512, (g + 1) * 512)
                RxG = RxnT[g * 4:(g + 1) * 4]
                gb2 = gcnt % 2
                gcnt += 1
                dq = []
                if e > 0:
                    dq = [(t, half) for t in range(g * 4, (g + 1) * 4) for half in range(2)]
                MM(ps[:, 4, :], csel[:, e * 128:(e + 1) * 128], gT[:, gs], True, True, [Rcsel, RgT], [Rps[4]])
                OP("act", "activation", out=gbc[gb2], in_=ps[:, 4, :], func=AF.Copy, reads=[Rps[4]], writes=[Rgbc[gb2]])
                for wi_, (wsb, Rw_) in enumerate(((w1s[sl], Rw1[sl]), (w3s[sl], Rw3[sl]))):
                    for fc in range(2):
                        b = wi_ * 2 + fc
                        for k in range(8):
                            MM(ps[:, b, :], wsb[:, k, fc * 128:(fc + 1) * 128], xnT[:, k, gs], k == 0, k == 7,
                               [Rw_] + RxG, [Rps[b]])
                        for (t, half) in dq[:2]:
                            down_unit(e - 1, t, half)
                        dq = dq[2:]
                for fc in range(2):
                    OP("act", "activation", out=s_sb[fc], in_=ps[:, fc, :], func=AF.Silu, reads=[Rps[fc]], writes=[Rs[fc]])
                    OP("dve", "tensor_tensor", out=u_sb[fc], in0=ps[:, 2 + fc, :], in1=s_sb[fc], op=ALU.mult,
                       reads=[Rps[2 + fc], Rs[fc]], writes=[Ru[fc]])
                    OP("pool", "tensor_tensor", out=hg[hi_][:, fc, gs], in0=u_sb[fc], in1=gbc[gb2], op=ALU.mult,
                       reads=[Ru[fc], Rgbc[gb2]], writes=[Rhg[hi_][g]])
        for t in range(NT):
            for half in range(2):
                down_unit(31, t, half)
        for t in range(NT):
            Rf = p.res()
            Rfinal.append(Rf)
            p.dma(out_d[t * 128:(t + 1) * 128, :], yacc[:, t, :], reads=[Ryacc[t]], writes=[Rf], semkey=("out", t % 4))
        return finish()


_IN_KEYS = ["g_norm_mix", "w_in", "b_merge_gate", "g_q", "g_k", "w_branch_att", "g_ret_norm", "w_branch_ret",
            "w_out", "g_norm_ffn", "w_router_group", "b_router_group", "w_router_expert", "b_router_expert",
            "w1", "w3", "w2"]


def _run(inputs, debug=False, stop_after=None, cores=8, trace=False):
    cfd, cbd, gamma_c = _host_consts()
    cfs_arr, cfs_offs = _pack(cfd, CFS_ORDER, np.float32)
    cbs_arr, cbs_offs = _pack(cbd, CBS_ORDER, ml_dtypes.bfloat16)
    nc = build_nc(cfs_offs, cbs_offs, cfs_arr.shape[1], cbs_arr.shape[1], gamma_c, debug=debug, stop_after=stop_after)
    shared = {}
    for k in _IN_KEYS:
        a = np.asarray(inputs[k], dtype=np.float32)
        shared[k] = np.ascontiguousarray(a[0])
    shared["cfs"] = cfs_arr
    shared["cbs"] = cbs_arr
    shared["catt"] = np.ascontiguousarray(np.concatenate([cfd["CA"], cfd["SA"]], axis=1))
    shared["cret"] = np.ascontiguousarray(np.concatenate([cfd["CR"], cfd["SR"]], axis=1))
    shared["csel"] = np.ascontiguousarray(cbd["sel"].astype(ml_dtypes.bfloat16))
    x = np.asarray(inputs["x"], dtype=np.float32)
    in_maps = []
    for c in range(cores):
        m = dict(shared)
        m["x"] = np.ascontiguousarray(x[c])
        in_maps.append(m)
    res = run_bass_kernel_spmd(nc, in_maps, core_ids=list(range(cores)), trace=trace)
    return res


def kernel(**inputs):
    res = _run(inputs)
    out = np.stack([np.asarray(r["out"], dtype=np.float32) for r in res.results], axis=0)
    return out
```

```python
import numpy as np
import ml_dtypes
import concourse.bass as bass
import concourse.mybir as mybir
from concourse.bass_utils import run_bass_kernel_spmd
from contextlib import ExitStack

F32 = mybir.dt.float32
BF16 = mybir.dt.bfloat16
ALU = mybir.AluOpType
AF = mybir.ActivationFunctionType
AX = mybir.AxisListType

S = 2048
D = 1024
NT = 16
NG = 4
EPS = 1e-6
ENGS = ("pe", "act", "dve", "pool", "sp")


class Res:
    __slots__ = ("name", "writer", "readers")

    def __init__(self, name):
        self.name = name
        self.writer = None
        self.readers = []


class Op:
    __slots__ = ("eng", "fn", "deps", "signal", "value", "dma", "semkey", "idx")


class Prog:
    def __init__(self, nc, es):
        self.nc = nc
        self.es = es
        self.ops = {e: [] for e in ENGS}
        self.dma_sems = {}
        self.nres = 0
        self.excl = set()
        self.all_res = []
        self.last_barrier = None

    def res(self, name=None):
        self.nres += 1
        r = Res(name or f"r{self.nres}")
        r.writer = self.last_barrier
        self.all_res.append(r)
        return r

    def sb(self, name, shape, dt):
        return self.es.enter_context(self.nc.sbuf_tensor(name, list(shape), dt))

    def op(self, eng, meth, args=(), kw=None, reads=(), writes=(), dma=False, semkey=None):
        o = Op()
        o.eng = eng
        o.fn = (meth, tuple(args), dict(kw or {}))
        o.signal = False
        o.value = None
        o.dma = dma
        o.semkey = semkey
        if eng in ("act", "dve") and self.excl:
            extra = [r for r in reads if id(r) in self.excl and all(r is not w for w in writes)]
            if extra:
                writes = list(writes) + extra
        deps = {}
        for r in reads:
            if r.writer is not None:
                deps[id(r.writer)] = (r.writer, True)
        for w in writes:
            if w.writer is not None and id(w.writer) not in deps:
                deps[id(w.writer)] = (w.writer, False)
            for rd in w.readers:
                if id(rd) not in deps:
                    deps[id(rd)] = (rd, False)
        dl = []
        for d, raw in deps.values():
            if d is o:
                continue
            if (not d.dma) and (not dma) and d.eng == eng:
                if eng in ("pe", "sp"):
                    continue
            dl.append(d)
            d.signal = True
        o.deps = dl
        for r in reads:
            r.readers.append(o)
        for w in writes:
            w.writer = o
            w.readers = []
        if dma:
            if semkey not in self.dma_sems:
                h = self.es.enter_context(self.nc.semaphore(f"dq{len(self.dma_sems)}"))
                self.dma_sems[semkey] = [h, 0]
            ent = self.dma_sems[semkey]
            ent[1] += 16
            o.value = ent[1]
        o.idx = len(self.ops[eng])
        self.ops[eng].append(o)
        return o

    def dma(self, out, in_, reads=(), writes=(), semkey=None, eng="sp", **kw):
        kw = dict(kw)
        kw["out"] = out
        kw["in_"] = in_
        return self.op(eng, "dma_start", (), kw, reads, writes, dma=True, semkey=semkey)

    def barrier(self, scratch_ap):
        allr = list(self.all_res)
        o = self.op("dve", "memset", (scratch_ap, 0.0), None, reads=allr, writes=allr)
        self.last_barrier = o
        return o

    def emit(self):
        nc = self.nc
        esem = {e: self.es.enter_context(nc.semaphore(f"e_{e}")) for e in ENGS if e != "sp"}
        for e in ENGS:
            c = 0
            for o in self.ops[e]:
                if o.dma:
                    continue
                if o.signal:
                    c += 1
                    o.value = c
        ops = self.ops
        dma_sems = self.dma_sems

        def run(e, engobj):
            waited = {}
            for o in ops[e]:
                need = {}
                for d in o.deps:
                    if d.dma:
                        key = ("d", d.semkey)
                        h = dma_sems[d.semkey][0]
                    else:
                        key = ("e", d.eng)
                        h = esem[d.eng]
                    if key not in need or need[key][1] < d.value:
                        need[key] = (h, d.value)
                for key, (h, v) in need.items():
                    if waited.get(key, 0) >= v:
                        continue
                    waited[key] = v
                    engobj.wait_ge(h, v)
                meth, a, kw = o.fn
                if meth is None:
                    continue
                ins = getattr(engobj, meth)(*a, **kw)
                if o.dma:
                    ins.then_inc(dma_sems[o.semkey][0], 16)
                elif o.signal:
                    ins.then_inc(esem[e], 1)

        with nc.Block() as block:
            @block.tensor
            def _(eng):
                run("pe", eng)

            @block.scalar
            def _(eng):
                run("act", eng)

            @block.vector
            def _(eng):
                run("dve", eng)

            @block.gpsimd
            def _(eng):
                run("pool", eng)

            @block.sync
            def _(eng):
                run("sp", eng)


def _host_consts():
    f32 = np.float32
    t = np.arange(S, dtype=f32)
    inv_a = (f32(10000.0) ** (-np.arange(0, 64, 2, dtype=f32) / f32(64))).astype(f32)
    ang = (t[None, :] * inv_a[:, None]).astype(f32)
    p = np.arange(128)
    CA = np.cos(ang)[p % 32].astype(f32)
    sgn = np.where((p % 64) < 32, -1.0, 1.0).astype(f32)
    SA = (np.sin(ang)[p % 32] * sgn[:, None]).astype(f32)
    inv_r = (f32(1.0) / (f32(10000.0) ** np.linspace(0.0, 1.0, 128, dtype=f32))).astype(f32)
    angr = (t[None, :] * inv_r[:, None]).astype(f32)
    CR = np.cos(angr).astype(f32)
    SR = np.sin(angr).astype(f32)
    lg = np.log(f32(1.0) - np.exp2(f32(-5.0) - np.arange(4, dtype=f32))).astype(f32)
    idx = np.arange(128, dtype=f32)
    diff = idx[None, :] - idx[:, None]
    decT = np.zeros((128, 4, 128), f32)
    for h in range(4):
        decT[:, h, :] = np.where(diff >= 0, np.exp(lg[h] * np.maximum(diff, 0.0)), 0.0) / 16.0
    zeta = (np.exp(lg[None, :] * (127.0 - idx[:, None])) / 16.0).astype(f32)
    xi = np.exp(lg[:, None] * (idx[None, :] + 1.0)).astype(f32)
    xib = np.broadcast_to(xi[None], (128, 4, 128)).astype(f32)
    gamma_c = np.exp(lg * 128.0).astype(f32)
    ident = np.eye(128, dtype=f32)
    cf = {
        "CA": CA, "SA": SA, "CR": CR, "SR": SR,
        "decT": decT.reshape(128, 512), "zeta": zeta, "xib": xib.reshape(128, 512), "ident": ident,
    }
    partner = np.where((p % 64) < 32, p + 32, p - 32)
    perm = np.zeros((128, 128), f32)
    perm[partner, p] = 1.0
    bones = (p[:, None] // 64 == p[None, :] // 64).astype(f32)
    kq = np.arange(128)
    mcur = (kq[None, :] >= kq[:, None]).astype(f32)
    mprev = (kq[:, None] >= kq[None, :]).astype(f32)
    mcur4 = np.tile(mcur, (1, 4))
    mprev4 = np.tile(mprev, (1, 4))
    sel = np.zeros((128, 32, 128), f32)
    for e in range(32):
        sel[e, e, :] = 1.0
        sel[32 + e, e, :] = 1.0
    cb = {
        "perm": perm, "bones": bones, "mcur4": mcur4, "mprev4": mprev4,
        "identb": ident, "sel": sel.reshape(128, 4096),
    }
    return cf, cb, gamma_c


def _pack(dct, order, dtype):
    offs = {}
    cols = 0
    for k in order:
        offs[k] = (cols, dct[k].shape[1])
        cols += dct[k].shape[1]
    arr = np.zeros((128, cols), dtype=dtype)
    for k in order:
        a, n = offs[k]
        arr[:, a:a + n] = dct[k].astype(dtype)
    return arr, offs


CFS_ORDER = ["decT", "zeta", "xib", "ident"]
CBS_ORDER = ["perm", "bones", "mcur4", "mprev4", "identb"]


ARENA_BYTES = 152 * 1024


def build_nc(cfs_offs, cbs_offs, cfs_cols, cbs_cols, gamma_c, debug=False, stop_after=None):
    nc = bass.Bass("TRN2", target_bir_lowering=False)

    def din(name, shape, dt=F32):
        return nc.dram_tensor(name, list(shape), dt, kind="ExternalInput").ap()

    x_d = din("x", [S, D])
    gmix_d = din("g_norm_mix", [D])
    w_in_d = din("w_in", [D, 11264])
    bgate_d = din("b_merge_gate", [2 * D])
    gq_d = din("g_q", [64])
    gk_d = din("g_k", [64])
    watt_d = din("w_branch_att", [D, D])
    gret_d = din("g_ret_norm", [4, 512])
    wret_d = din("w_branch_ret", [2 * D, D])
    wout_d = din("w_out", [D, D])
    gffn_d = din("g_norm_ffn", [D])
    wrg_d = din("w_router_group", [D, 4])
    brg_d = din("b_router_group", [4])
    wre_d = din("w_router_expert", [D, 32])
    bre_d = din("b_router_expert", [32])
    w1_d = din("w1", [32, D, 256])
    w3_d = din("w3", [32, D, 256])
    w2_d = din("w2", [32, 256, D])
    cfs_d = din("cfs", [128, cfs_cols])
    cbs_d = din("cbs", [128, cbs_cols], BF16)
    catt_d = din("catt", [128, 2 * S])
    cret_d = din("cret", [128, 2 * S])
    csel_d = din("csel", [128, 4096], BF16)
    out_d = nc.dram_tensor("out", [S, D], F32, kind="ExternalOutput").ap()
    skind = "ExternalOutput" if debug else "Internal"
    vx_d = nc.dram_tensor("vx", [S, 8, 256], BF16, kind=skind).ap()
    zatt_d = nc.dram_tensor("zatt", [D, S], BF16, kind=skind).ap()
    zret_d = nc.dram_tensor("zret", [2 * D, S], BF16, kind=skind).ap()
    dbg = {}
    if debug:
        dbg["xnT"] = nc.dram_tensor("d_xnT", [128, 8 * S], BF16, kind="ExternalOutput").ap()
        dbg["x1"] = nc.dram_tensor("d_x1", [S, D], F32, kind="ExternalOutput").ap()
        dbg["logits"] = nc.dram_tensor("d_logits", [128, NT * 36], F32, kind="ExternalOutput").ap()
        dbg["gate"] = nc.dram_tensor("d_gate", [128, NT * 32], F32, kind="ExternalOutput").ap()

    with ExitStack() as es:
        p = Prog(nc, es)
        ps = es.enter_context(nc.psum_tensor("ps", [128, 8, 512], F32))
        Rps = [p.res(f"psb{b}") for b in range(8)]
        p.excl.update(id(r) for r in Rps)

        def psb(b):
            return ps[:, b, :].bitcast(BF16)

        def OP(eng, meth, *a, reads=(), writes=(), **kw):
            return p.op(eng, meth, a, kw, reads, writes)

        def MM(out, lhsT, rhs, start, stop, reads, writes, **kw):
            return p.op("pe", "matmul", (out,), dict(lhsT=lhsT, rhs=rhs, start=start, stop=stop, **kw), reads, writes)

        arena = p.sb("arena", [128, ARENA_BYTES // 2], BF16)
        apos = [0]

        def areset(pos=0):
            apos[0] = pos

        def alloc(shape, dt):
            n = 1
            for s_ in shape[1:]:
                n *= s_
            nbytes = n * (4 if dt == F32 else 2)
            nbytes = (nbytes + 63) // 64 * 64
            a = apos[0]
            assert a + nbytes <= ARENA_BYTES, ("arena overflow", a, nbytes)
            apos[0] = a + nbytes
            v = arena[0:shape[0], a // 2:(a + n * (4 if dt == F32 else 2)) // 2]
            if dt == F32:
                v = v.bitcast(F32)
            if len(shape) == 3:
                v = v.rearrange("p (a b) -> p a b", a=shape[1])
            elif len(shape) == 4:
                v = v.rearrange("p (a b c) -> p a b c", a=shape[1], b=shape[2])
            return v

        cfs = p.sb("cfs_sb", [128, cfs_cols], F32)
        cbs = p.sb("cbs_sb", [128, cbs_cols], BF16)
        Rc = p.res("consts")
        Rcs = []

        def cres():
            r = p.res()
            Rcs.append(r)
            return [r]

        def CF(k):
            a, n = cfs_offs[k]
            return cfs[:, a:a + n]

        def CB(k):
            a, n = cbs_offs[k]
            return cbs[:, a:a + n]

        p.dma(cfs[:], cfs_d, writes=cres(), semkey="c")
        p.dma(cbs[:], cbs_d, writes=cres(), semkey="c")
        gmix = p.sb("gmix", [128, 8], F32)
        gffn = p.sb("gffn", [128, 8], F32)
        bga = p.sb("bga", [128, 8], F32)
        bgb = p.sb("bgb", [128, 8], F32)
        gqk = p.sb("gqk", [128, 4], F32)
        epsc = p.sb("epsc", [128, 1], F32)
        scratch = p.sb("scratch", [128, 1], F32)
        p.dma(gmix[:], gmix_d.rearrange("(k p) -> p k", p=128), writes=cres(), semkey="c", allow_slow_non_contiguous=True, eng="act")
        p.dma(gffn[:], gffn_d.rearrange("(k p) -> p k", p=128), writes=cres(), semkey="c", allow_slow_non_contiguous=True, eng="act")
        p.dma(bga[:], bgate_d[0:D].rearrange("(k p) -> p k", p=128), writes=cres(), semkey="c", allow_slow_non_contiguous=True, eng="act")
        p.dma(bgb[:], bgate_d[D:2 * D].rearrange("(k p) -> p k", p=128), writes=cres(), semkey="c", allow_slow_non_contiguous=True, eng="act")
        for ci, gd in ((0, gq_d), (2, gk_d)):
            for half in range(2):
                p.dma(gqk[half * 64:(half + 1) * 64, ci:ci + 1], gd.rearrange("(p o) -> p o", o=1), writes=cres(), semkey="c", eng="act")
                for q4 in range(2):
                    p.dma(gqk[half * 64 + q4 * 32: half * 64 + q4 * 32 + 32, ci + 1:ci + 2],
                          gd[(1 - q4) * 32:(1 - q4) * 32 + 32].rearrange("(p o) -> p o", o=1), writes=cres(), semkey="c", eng="act")
        OP("dve", "memset", epsc[:], EPS, reads=Rcs, writes=[Rc])

        ident = CF("ident")
        identb = CB("identb")
        perm, bones = CB("perm"), CB("bones")
        mcur4, mprev4 = CB("mcur4"), CB("mprev4")
        decT = CF("decT").rearrange("p (h i) -> p h i", h=4)
        zeta = CF("zeta")
        xib = CF("xib").rearrange("p (h i) -> p h i", h=4)

        Rfinal = []
        xnT = p.sb("xnT", [128, 8, S], BF16)
        RxnT = [p.res(f"xnT{t}") for t in range(NT)]
        ss = p.sb("ss", [128, NT], F32)
        rt = p.sb("rt", [128, NT], F32)
        rstd = p.sb("rstd", [128, NT], F32)
        Rxin = [p.res(f"xin{i}") for i in range(2)]
        xs = [p.sb(f"xs{i}", [128, D], F32) for i in range(2)]
        Rxs = [p.res(f"xs{i}") for i in range(2)]
        junk = p.sb("junk", [128, D], BF16)
        Rjunk = p.res("junk")
        logit = p.sb("logit", [128, NT, 36], F32)
        Rlogit = [p.res(f"logit{t}") for t in range(NT)]

        def norm_tile_gen(src, Rsrc, t, gcol, Rstat_t, banks, lo=None, stats_done=False):
            i = t % 2
            if not stats_done:
                OP("act", "activation", out=junk[:], in_=src, func=AF.Square, accum_out=ss[:, t:t + 1],
                   reads=[Rsrc, Rc], writes=[Rjunk, Rstat_t])
                OP("act", "activation", out=rt[:, t:t + 1], in_=ss[:, t:t + 1], func=AF.Sqrt, scale=1.0 / D, bias=epsc[:],
                   reads=[Rstat_t, Rc], writes=[Rstat_t])
                OP("dve", "reciprocal", out=rstd[:, t:t + 1], in_=rt[:, t:t + 1], reads=[Rstat_t], writes=[Rstat_t])
            OP("dve", "tensor_scalar", out=xs[i][:], in0=src, scalar1=rstd[:, t:t + 1], scalar2=None, op0=ALU.mult,
               reads=[Rsrc, Rstat_t], writes=[Rxs[i]])
            yield
            for hb in range(2):
                b = banks[hb]
                for j in range(4):
                    k = hb * 4 + j
                    OP("pe", "transpose", out=ps[:, b, j * 128:(j + 1) * 128], in_=xs[i][:, k * 128:(k + 1) * 128],
                       identity=ident, reads=[Rxs[i], Rc], writes=[Rps[b]])
                gb_ = gcol[:, hb * 4:hb * 4 + 4].unsqueeze(2).to_broadcast([128, 4, 128])
                pin = ps[:, b, :].rearrange("p (j c) -> p j c", j=4)
                dsl = xnT[:, hb * 4:hb * 4 + 4, t * 128:(t + 1) * 128]
                if lo is None:
                    OP("dve", "tensor_tensor", out=dsl, in0=pin, in1=gb_, op=ALU.mult,
                       reads=[Rps[b], Rc], writes=[RxnT[t]])
                else:
                    vtmp, Rvtmp, lodst, Rlo = lo
                    OP("dve", "tensor_tensor", out=vtmp[:, hb * 4:hb * 4 + 4, :], in0=pin, in1=gb_, op=ALU.mult,
                       reads=[Rps[b], Rc], writes=[Rvtmp[hb]])
                    OP("act", "activation", out=dsl, in_=vtmp[:, hb * 4:hb * 4 + 4, :], func=AF.Copy,
                       reads=[Rvtmp[hb]], writes=[RxnT[t]])
                    OP("dve", "tensor_tensor", out=lodst[:, hb * 4:hb * 4 + 4, :], in0=vtmp[:, hb * 4:hb * 4 + 4, :],
                       in1=dsl, op=ALU.subtract, reads=[Rvtmp[hb], RxnT[t]], writes=[Rlo[hb]])
            yield

        def finish():
            p.op("sp", None, reads=list(Rfinal))
            p.emit()
            return nc

        def wsrc(wd, c0, c1):
            return wd[:, c0:c1].rearrange("(k p) n -> p k n", p=128)

        def load_w(dst_ap, src_ap, R, key):
            return p.dma(dst_ap, src_ap, writes=[R], semkey=key, eng="pool")

        Rstat = [p.res(f"stat{t}") for t in range(NT)]
        areset()
        xin = [alloc([128, D], F32) for i in range(4)]
        Rxin = [p.res(f"xin{i}") for i in range(4)]
        def vproj_tile(t):
            i = t % 2
            for cg in range(2):
                b = 4 + cg
                for k in range(8):
                    MM(ps[:, b, :], xnT[:, k, t * 128:(t + 1) * 128], wbig[cg][:, k, :], k == 0, k == 7,
                       [RxnT[t], Rwbig[cg]], [Rps[b]])
                pv = ps[:, b, :].rearrange("p (pr e d) -> p pr e d", e=2, d=64)
                OP("act", "activation", out=vst[i][:, cg * 4:(cg + 1) * 4, 0:64], in_=pv[:, :, 0, :], func=AF.Copy,
                   reads=[Rps[b]], writes=[Rvst[i]])
                OP("dve", "tensor_copy", out=vst[i][:, cg * 4:(cg + 1) * 4, 192:256], in_=pv[:, :, 1, :],
                   reads=[Rps[b]], writes=[Rvst[i]])
            p.dma(vx_d[t * 128:(t + 1) * 128, :, :], vst[i], reads=[Rvst[i]], writes=[Rvx[t]], semkey=("vx", t % 2))

        wbig = [alloc([128, 8, 512], BF16) for i in range(2)]
        Rwbig = [p.res(f"wbig{i}") for i in range(2)]
        vst = [alloc([128, 8, 256], BF16) for i in range(2)]
        Rvst = [p.res(f"vst{i}") for i in range(2)]
        Rvx = [p.res(f"vx{t}") for t in range(NT)]
        for i in range(2):
            OP("pool", "memset", vst[i], 1.0, writes=[Rvst[i]])
        for cg in range(2):
            load_w(wbig[cg], wsrc(w_in_d, 2048 + cg * 512, 2048 + (cg + 1) * 512), Rwbig[cg], ("wbig", cg))
        gens = {}
        do_v = stop_after != "A"
        for t in range(min(3, NT)):
            p.dma(xin[t % 4], x_d[t * 128:(t + 1) * 128, :], writes=[Rxin[t % 4]], semkey=("xin", t % 4))
        for step in range(NT + 2):
            if step < NT:
                t = step
                if t + 3 < NT:
                    p.dma(xin[(t + 3) % 4], x_d[(t + 3) * 128:(t + 4) * 128, :], writes=[Rxin[(t + 3) % 4]],
                          semkey=("xin", (t + 3) % 4))
                gens[t] = norm_tile_gen(xin[t % 4], Rxin[t % 4], t, gmix, Rstat[t], (6, 7))
                next(gens[t])
            if 0 <= step - 1 < NT:
                next(gens[step - 1])
            if do_v and 0 <= step - 2 < NT:
                vproj_tile(step - 2)
        if debug:
            Rfinal.append(p.res())
            p.dma(dbg["xnT"], xnT[:].rearrange("p k s -> p (k s)"), reads=RxnT, writes=[Rfinal[-1]], semkey="dbg0")
        if stop_after == "A":
            Rfinal.append(p.res("fin"))
            p.dma(out_d[0:128, :], xin[1], reads=[Rxin[1]], writes=[Rfinal[-1]], semkey="fin")
            return finish()

        if stop_after == "V":
            Rfinal.extend(Rvx)
            return finish()

        p.barrier(scratch[:])
        areset()
        catt = alloc([128, 2 * S], F32)
        Rcatt = p.res("catt")
        p.dma(catt, catt_d, writes=[Rcatt], semkey="catt")
        CA, SA = catt[:, 0:S], catt[:, S:2 * S]
        wqk = [[alloc([128, 8, 128], BF16) for j in range(2)] for i in range(2)]
        Rwqk = [[p.res(f"wqk{i}_{j}") for j in range(2)] for i in range(2)]
        QKT = [[alloc([128, S], BF16) for j in range(2)] for i in range(2)]
        RQK = [[[p.res(f"QK{i}_{j}_{g}") for g in range(NG)] for j in range(2)] for i in range(2)]
        vxs = [[alloc([128, 16, 256], BF16) for o in range(3)] for i in range(2)]
        Rvxs = [[p.res(f"vxs{i}_{o}") for o in range(3)] for i in range(2)]
        qbf = alloc([128, 512], BF16)
        sqb = alloc([128, 512], BF16)
        rtq = alloc([128, 512], F32)
        rsq = alloc([128, 512], F32)
        t1 = alloc([128, 512], F32)
        t2 = alloc([128, 512], F32)
        Rqbf, Rsqb, Rrtq, Rrsq, Rt1, Rt2 = (p.res(n) for n in ("qbf", "sqb", "rtq", "rsq", "t1", "t2"))
        Pb = [alloc([128, 512], BF16) for i in range(3)]
        RPb = [p.res(f"Pb{i}") for i in range(3)]
        rden = alloc([128, S], F32)
        Rrden = [p.res(f"rden{b}") for b in range(4)]
        zpair = [alloc([128, S], BF16) for i in range(2)]
        Rzpair = [p.res(f"zpair{i}") for i in range(2)]
        Rzatt = [p.res(f"zatt{i}") for i in range(8)]

        def load_pair_weights(pr):
            i = pr % 2
            load_w(wqk[i][0], wsrc(w_in_d, pr * 128, (pr + 1) * 128), Rwqk[i][0], ("wqk", i, 0))
            load_w(wqk[i][1], wsrc(w_in_d, 1024 + pr * 128, 1024 + (pr + 1) * 128), Rwqk[i][1], ("wqk", i, 1))

        def load_pair_v(pr):
            i = pr % 2
            src = vx_d[:, pr, :]
            p.dma(vxs[i][0], src.rearrange("(n i) m -> i n m", i=128), reads=Rvx, writes=[Rvxs[i][0]], semkey=("vxs", i, 0))
            s1 = src.rearrange("(n i c) m -> c i n m", i=128, c=4)
            for c in range(4):
                p.dma(vxs[i][1][:, c * 4:(c + 1) * 4, :], s1[c], reads=Rvx, writes=[Rvxs[i][1]], semkey=("vxs", i, 1))
            p.dma(vxs[i][2], src.rearrange("(i c) m -> i c m", c=16), reads=Rvx, writes=[Rvxs[i][2]], semkey=("vxs", i, 2))

        def colap(T, spec, r0):
            base, r, c = spec
            if r == 1:
                return T[r0:r0 + 64, base:base + 128]
            return T[r0:r0 + 64, base:base + 128 * r].rearrange("p (n r) -> p n r", r=r)[:, :, c]

        def prep_gen(pr):
            i = pr % 2
            for j, gi in ((0, 0), (1, 2)):
                dstT = QKT[i][j]
                for g in range(NG):
                    gs = slice(g * 512, (g + 1) * 512)
                    for k in range(8):
                        MM(ps[:, 6, :], wqk[i][j][:, k, :], xnT[:, k, gs], k == 0, k == 7,
                           [Rwqk[i][j]] + RxnT[g * 4:(g + 1) * 4], [Rps[6]])
                    OP("act", "activation", out=qbf, in_=ps[:, 6, :], func=AF.Copy, reads=[Rps[6]], writes=[Rqbf])
                    OP("dve", "scalar_tensor_tensor", out=t1, in0=ps[:, 6, :], scalar=gqk[:, gi:gi + 1], in1=CA[:, gs],
                       op0=ALU.mult, op1=ALU.mult, reads=[Rps[6], Rc, Rcatt], writes=[Rt1])
                    OP("pool", "tensor_tensor", out=sqb, in0=qbf, in1=qbf, op=ALU.mult, reads=[Rqbf], writes=[Rsqb])
                    yield
                    MM(ps[:, 7, :], bones, sqb, True, True, [Rsqb, Rc], [Rps[7]])
                    MM(ps[:, 6, :], perm, qbf, True, True, [Rqbf, Rc], [Rps[6]])
                    OP("act", "activation", out=rtq, in_=ps[:, 7, :], func=AF.Ln, scale=1.0 / 64, bias=epsc[:],
                       reads=[Rps[7], Rc], writes=[Rrtq])
                    OP("act", "activation", out=rsq, in_=rtq, func=AF.Exp, scale=-0.5, reads=[Rrtq], writes=[Rrsq])
                    OP("dve", "scalar_tensor_tensor", out=t2, in0=ps[:, 6, :], scalar=gqk[:, gi + 1:gi + 2], in1=SA[:, gs],
                       op0=ALU.mult, op1=ALU.mult, reads=[Rps[6], Rc, Rcatt], writes=[Rt2])
                    OP("pool", "tensor_tensor", out=t1, in0=t1, in1=t2, op=ALU.add, reads=[Rt1, Rt2], writes=[Rt1])
                    OP("dve", "tensor_tensor", out=dstT[:, gs], in0=t1, in1=rsq, op=ALU.mult,
                       reads=[Rt1, Rrsq], writes=[RQK[i][j][g]])
                    yield

        def att_batches(pr):
            def cols(r, c, n):
                return (n * 128 * r, r, c)
            tiles_cur, tiles_prev = [], []
            for n in range(16):
                tiles_cur.append((cols(1, 0, n), cols(1, 0, n), 0, n, ("n", n // 4, (n % 4) * 128, 1), n % 4 == 0))
            for c in range(4):
                for n in range(4):
                    tiles_cur.append((cols(4, c, n), cols(4, c, n), 1, c * 4 + n, ("n", n, c, 4), False))
            for c in range(16):
                tiles_cur.append((cols(16, c, 0), cols(16, c, 0), 2, c, ("s", c), False))
            for n in range(1, 16):
                tiles_prev.append((cols(1, 0, n - 1), cols(1, 0, n), 0, n - 1, ("n", n // 4, (n % 4) * 128, 1), False))
            for c in range(4):
                for n in range(1, 4):
                    tiles_prev.append((cols(4, c, n - 1), cols(4, c, n), 1, c * 4 + n - 1, ("n", n, c, 4), False))
            batches = []
            for lst, msk in ((tiles_cur, mcur4), (tiles_prev, mprev4)):
                for s0 in range(0, len(lst), 4):
                    batches.append((lst[s0:s0 + 4], msk))
            items = []
            for eh in range(2):
                for bi, (tl, msk) in enumerate(batches):
                    items.append((eh, bi, tl, msk, bi == len(batches) - 1))
            return items

        gcount = [0]

        def emit_S(pr, item):
            i = pr % 2
            eh, bi, tl, msk, last = item
            r0 = eh * 64
            gb = gcount[0]
            gcount[0] += 1
            sb_ = 4 + (gb % 2)
            pbi = gb % 3
            nt_ = len(tl)
            QT, KT = QKT[i]
            RQKall = RQK[i][0] + RQK[i][1]
            for ti, (kc, qc, vo, vb, osp, st) in enumerate(tl):
                MM(ps[:, sb_, ti * 128:(ti + 1) * 128], colap(KT, kc, r0), colap(QT, qc, r0), True, True,
                   RQKall, [Rps[sb_]])
            OP("act", "activation", out=Pb[pbi][:, 0:nt_ * 128], in_=ps[:, sb_, 0:nt_ * 128], func=AF.Exp, scale=0.125,
               reads=[Rps[sb_]], writes=[RPb[pbi]])
            OP("dve", "tensor_tensor", out=Pb[pbi][:, 0:nt_ * 128], in0=Pb[pbi][:, 0:nt_ * 128],
               in1=msk[:, 0:nt_ * 128], op=ALU.mult, reads=[RPb[pbi], Rc], writes=[RPb[pbi]])
            return pbi

        def emit_PV(pr, item, pbi):
            i = pr % 2
            eh, bi, tl, msk, last = item
            r0 = eh * 64
            zp = zpair[i]
            for ti, (kc, qc, vo, vb, osp, st) in enumerate(tl):
                lhsT = vxs[i][vo][:, vb, eh * 128:(eh + 1) * 128]
                if osp[0] == "n":
                    _, bank, off, r = osp
                    if r == 1:
                        oap = ps[:, bank, off:off + 128]
                    else:
                        oap = ps[:, bank, :].rearrange("p (n r) -> p n r", r=r)[:, :, off]
                    MM(oap, lhsT, Pb[pbi][:, ti * 128:(ti + 1) * 128], st, False,
                       [RPb[pbi], Rvxs[i][vo]], [Rps[bank]], skip_group_check=True)
                else:
                    c = osp[1]
                    for bank in range(4):
                        oap = ps[:, bank, :].rearrange("p (n r) -> p n r", r=16)[:, :, c]
                        MM(oap, lhsT, Pb[pbi][:, ti * 128 + bank * 32: ti * 128 + bank * 32 + 32], False, False,
                           [RPb[pbi], Rvxs[i][vo]], [Rps[bank]], skip_group_check=True)
            if last:
                d0 = 64 - r0
                for bank in range(4):
                    bs = slice(bank * 512, (bank + 1) * 512)
                    OP("act", "activation", out=rden[r0:r0 + 64, bs], in_=ps[d0:d0 + 64, bank, :], func=AF.Ln,
                       reads=[Rps[bank]], writes=[Rrden[bank]])
                    OP("act", "activation", out=rden[r0:r0 + 64, bs], in_=rden[r0:r0 + 64, bs], func=AF.Exp, scale=-1.0,
                       reads=[Rrden[bank]], writes=[Rrden[bank]])
                    OP("dve", "tensor_tensor", out=zp[r0:r0 + 64, bs], in0=ps[r0:r0 + 64, bank, :],
                       in1=rden[r0:r0 + 64, bs], op=ALU.mult, reads=[Rps[bank], Rrden[bank]], writes=[Rzpair[i]])

        load_pair_weights(0)
        load_pair_weights(1)
        load_pair_v(0)
        for _ in prep_gen(0):
            pass
        for pr in range(8):
            i = pr % 2
            if pr + 1 < 8:
                load_pair_v(pr + 1)
            nxt = prep_gen(pr + 1) if pr + 1 < 8 else None
            items = att_batches(pr)
            pq = [emit_S(pr, items[0]), emit_S(pr, items[1])]
            for ii, item in enumerate(items):
                if ii + 2 < len(items):
                    pq.append(emit_S(pr, items[ii + 2]))
                emit_PV(pr, item, pq.pop(0))
                if nxt is not None and ii % 2 == 1:
                    next(nxt, None)
            if nxt is not None:
                for _ in nxt:
                    pass
            if pr + 2 < 8:
                load_pair_weights(pr + 2)
            p.dma(zatt_d[pr * 128:(pr + 1) * 128, :], zpair[i], reads=[Rzpair[i]], writes=[Rzatt[pr]], semkey=("zatt", i))
        if stop_after == "ATT":
            Rfinal.extend(Rzatt)
            return finish()

        p.barrier(scratch[:])
        areset()
        cret = alloc([128, 2 * S], F32)
        Rcret = p.res("cret")
        p.dma(cret, cret_d, writes=[Rcret], semkey="cret")
        CR, SR = cret[:, 0:S], cret[:, S:2 * S]
        gretb = alloc([128, 4, 512], F32)
        Rgretb = p.res("gretb")
        p.dma(gretb.rearrange("p h v -> p (h v)"), gret_d.rearrange("h v -> (h v)").partition_broadcast(128),
              writes=[Rgretb], semkey="gretb")
        wq_r = [alloc([128, 8, 256], BF16) for i in range(2)]
        wk_r = [alloc([128, 8, 256], BF16) for i in range(2)]
        wv_r = [alloc([128, 8, 512], BF16) for i in range(2)]
        wg_r = [alloc([128, 8, 512], BF16) for i in range(2)]
        Rwr = [[p.res(f"wr{i}_{j}") for j in range(4)] for i in range(2)]
        QrT = [alloc([128, 2, 512], BF16) for i in range(2)]
        KrT = [alloc([128, 2, 512], BF16) for i in range(2)]
        Qxi = [alloc([128, 2, 512], BF16) for i in range(2)]
        RQr = [p.res(f"QrT{i}") for i in range(2)]
        RKr = [p.res(f"KrT{i}") for i in range(2)]
        RQx = [p.res(f"Qxi{i}") for i in range(2)]
        sgr = [alloc([128, 4, 512], BF16) for i in range(2)]
        Rsgr = [p.res(f"sgr{i}") for i in range(2)]
        vr_sb = [alloc([128, 512], BF16) for i in range(2)]
        Rvr = [p.res(f"vr{i}") for i in range(2)]
        kz = [alloc([128, 256], BF16) for i in range(2)]
        Rkz = [p.res(f"kz{i}") for i in range(2)]
        inm = [alloc([128, 128], BF16) for i in range(2)]
        Rinm = [p.res(f"inm{i}") for i in range(2)]
        yn = [alloc([128, 512], BF16) for i in range(2)]
        Ryn = [p.res(f"yn{i}") for i in range(2)]
        zst = [alloc([128, 4, 512], BF16) for i in range(2)]
        Rzst = [p.res(f"zst{i}") for i in range(2)]
        state = alloc([128, 2, 512], F32)
        state_bf = alloc([128, 2, 512], BF16)
        Rstate = [p.res(f"state{c}") for c in range(2)]
        Rstbf = [p.res(f"stbf{c}") for c in range(2)]
        tmp = [alloc([128, 512], F32) for i in range(4)]
        Rtmp = [p.res(f"rtmp{i}") for i in range(4)]
        ssr = alloc([128, 64], F32)
        rtr = alloc([128, 64], F32)
        rsr = alloc([128, 64], F32)
        Rzret = []

        def load_head_weights(h):
            i = h % 2
            load_w(wq_r[i], wsrc(w_in_d, 3072 + h * 256, 3072 + (h + 1) * 256), Rwr[i][0], ("wr", i, 0))
            load_w(wk_r[i], wsrc(w_in_d, 4096 + h * 256, 4096 + (h + 1) * 256), Rwr[i][1], ("wr", i, 1))
            load_w(wv_r[i], wsrc(w_in_d, 5120 + h * 512, 5120 + (h + 1) * 512), Rwr[i][2], ("wr", i, 2))
            load_w(wg_r[i], wsrc(w_in_d, 7168 + h * 512, 7168 + (h + 1) * 512), Rwr[i][3], ("wr", i, 3))

        vr3 = vr_sb + [alloc([128, 512], BF16)]
        Rvr3 = Rvr + [p.res("vr2")]
        kz3 = kz + [alloc([128, 256], BF16)]
        Rkz3 = Rkz + [p.res("kz2")]
        inm3 = inm + [alloc([128, 128], BF16)]
        Rinm3 = Rinm + [p.res("inm2")]
        INNER = ps[:, 3, 0:128]
        ysb = [alloc([128, 512], F32) for i in range(2)]
        Rysb = [p.res(f"ysb{i}") for i in range(2)]

        def proj_pieces(h, g):
            wi = h % 2
            gi = g % 2
            gs = slice(g * 512, (g + 1) * 512)
            RxG = RxnT[g * 4:(g + 1) * 4]

            def qk(which):
                wsb = (wq_r, wk_r)[which][wi]
                dst = (QrT, KrT)[which][gi]
                Rdst = (RQr, RKr)[which][gi]
                for c in range(2):
                    for k in range(8):
                        MM(ps[:, c, :], wsb[:, k, c * 128:(c + 1) * 128], xnT[:, k, gs], k == 0, k == 7,
                           [Rwr[wi][which]] + RxG, [Rps[c]])
                OP("dve", "tensor_tensor", out=tmp[0], in0=ps[:, 0, :], in1=CR[:, gs], op=ALU.mult,
                   reads=[Rps[0], Rcret], writes=[Rtmp[0]])
                OP("dve", "tensor_tensor", out=tmp[1], in0=ps[:, 1, :], in1=SR[:, gs], op=ALU.mult,
                   reads=[Rps[1], Rcret], writes=[Rtmp[1]])
                OP("pool", "tensor_tensor", out=dst[:, 0, :], in0=tmp[0], in1=tmp[1], op=ALU.subtract,
                   reads=[Rtmp[0], Rtmp[1]], writes=[Rdst])
                OP("dve", "tensor_tensor", out=tmp[2], in0=ps[:, 1, :], in1=CR[:, gs], op=ALU.mult,
                   reads=[Rps[1], Rcret], writes=[Rtmp[2]])
                OP("dve", "tensor_tensor", out=tmp[3], in0=ps[:, 0, :], in1=SR[:, gs], op=ALU.mult,
                   reads=[Rps[0], Rcret], writes=[Rtmp[3]])
                OP("pool", "tensor_tensor", out=dst[:, 1, :], in0=tmp[2], in1=tmp[3], op=ALU.add,
                   reads=[Rtmp[2], Rtmp[3]], writes=[Rdst])
                if which == 0:
                    xb = xib[:, h, :].unsqueeze(1).to_broadcast([128, 4, 128])
                    for c in range(2):
                        OP("pool", "tensor_tensor", out=Qxi[gi][:, c, :].rearrange("p (n i) -> p n i", n=4),
                           in0=dst[:, c, :].rearrange("p (n i) -> p n i", n=4), in1=xb, op=ALU.mult,
                           reads=[Rdst, Rc], writes=[RQx[gi]])

            def gate(vc):
                b = vc % 2
                for k in range(8):
                    MM(ps[:, b, :], wg_r[wi][:, k, vc * 128:(vc + 1) * 128], xnT[:, k, gs], k == 0, k == 7,
                       [Rwr[wi][3]] + RxG, [Rps[b]])
                OP("act", "activation", out=sgr[gi][:, vc, :], in_=ps[:, b, :], func=AF.Silu,
                   reads=[Rps[b]], writes=[Rsgr[gi]])

            return [lambda: qk(0), lambda: qk(1), lambda: gate(0), lambda: gate(1), lambda: gate(2), lambda: gate(3)]

        def stage_A(q):
            h, gn = q // 16, q % 16
            g, n = gn // 4, gn % 4
            wi, gi, b3 = h % 2, g % 2, q % 3
            cs = slice(n * 128, (n + 1) * 128)
            for k in range(8):
                MM(ps[:, 2, :], xnT[:, k, gn * 128:(gn + 1) * 128], wv_r[wi][:, k, :], k == 0, k == 7,
                   [Rwr[wi][2], RxnT[gn]], [Rps[2]])
            OP("act", "activation", out=vr3[b3], in_=ps[:, 2, :], func=AF.Copy, reads=[Rps[2]], writes=[Rvr3[b3]])
            for c in range(2):
                OP("pe", "transpose", out=psb(4)[:, 512 + c * 128: 512 + (c + 1) * 128], in_=KrT[gi][:, c, cs],
                   identity=identb, reads=[RKr[gi], Rc], writes=[Rps[4]])
            OP("act", "activation", out=kz3[b3], in_=psb(4)[:, 512:768], func=AF.Copy, scale=zeta[:, h:h + 1],
               reads=[Rps[4], Rc], writes=[Rkz3[b3]])
            for c in range(2):
                MM(INNER, KrT[gi][:, c, cs], QrT[gi][:, c, cs], c == 0, c == 1, [RKr[gi], RQr[gi]], [Rps[3]])
            OP("dve", "tensor_tensor", out=inm3[b3], in0=INNER, in1=decT[:, h, :], op=ALU.mult,
               reads=[Rps[3], Rc], writes=[Rinm3[b3]])

        Rst_q = {}

        def stage_B(q):
            h, gn = q // 16, q % 16
            g, n = gn // 4, gn % 4
            gi, b3, ci = g % 2, q % 3, q % 2
            cs = slice(n * 128, (n + 1) * 128)
            yb = 5
            st_i = q
            gC = float(gamma_c[h])
            MM(ps[:, yb, :], inm3[b3], vr3[b3], True, gn == 0, [Rinm3[b3], Rvr3[b3]], [Rps[yb]])
            if gn > 0:
                for c in range(2):
                    MM(ps[:, yb, :], Qxi[gi][:, c, cs], state_bf[:, c, :], False, c == 1, [RQx[gi], Rstbf[c]], [Rps[yb]])
            if gn < 15:
                for c in range(2):
                    MM(ps[:, 6 + c, :], kz3[b3][:, c * 128:(c + 1) * 128], vr3[b3], True, True,
                       [Rkz3[b3], Rvr3[b3]], [Rps[6 + c]])
                    if gn == 0:
                        OP("dve", "tensor_copy", out=state[:, c, :], in_=ps[:, 6 + c, :],
                           reads=[Rps[6 + c]], writes=[Rstate[c]])
                    else:
                        OP("dve", "scalar_tensor_tensor", out=state[:, c, :], in0=state[:, c, :], scalar=gC,
                           in1=ps[:, 6 + c, :], op0=ALU.mult, op1=ALU.add,
                           reads=[Rps[6 + c], Rstate[c]], writes=[Rstate[c]])
                    OP("pool", "tensor_copy", out=state_bf[:, c, :], in_=state[:, c, :],
                       reads=[Rstate[c]], writes=[Rstbf[c]])
            Rst = p.res()
            OP("act", "activation", out=ysb[ci], in_=ps[:, yb, :], func=AF.Copy, reads=[Rps[yb]], writes=[Rysb[ci]])
            OP("act", "activation", out=junk[:, 0:512], in_=ysb[ci], func=AF.Square,
               accum_out=ssr[:, st_i:st_i + 1], reads=[Rysb[ci]], writes=[Rjunk, Rst])
            OP("act", "activation", out=rtr[:, st_i:st_i + 1], in_=ssr[:, st_i:st_i + 1], func=AF.Sqrt,
               scale=1.0 / 512, bias=epsc[:], reads=[Rst, Rc], writes=[Rst])
            OP("dve", "reciprocal", out=rsr[:, st_i:st_i + 1], in_=rtr[:, st_i:st_i + 1], reads=[Rst], writes=[Rst])
            OP("act", "activation", out=yn[ci], in_=ysb[ci], func=AF.Copy, scale=rsr[:, st_i:st_i + 1],
               reads=[Rysb[ci], Rst], writes=[Ryn[ci]])
            OP("pool", "tensor_tensor", out=yn[ci], in0=yn[ci], in1=gretb[:, h, :], op=ALU.mult,
               reads=[Ryn[ci], Rgretb], writes=[Ryn[ci]])

        def stage_C(q):
            h, gn = q // 16, q % 16
            g, n = gn // 4, gn % 4
            gi, ci = g % 2, q % 2
            cs = slice(n * 128, (n + 1) * 128)
            gs = slice(g * 512, (g + 1) * 512)
            for vc in range(4):
                OP("pe", "transpose", out=psb(4)[:, vc * 128:(vc + 1) * 128], in_=yn[ci][:, vc * 128:(vc + 1) * 128],
                   identity=identb, reads=[Ryn[ci], Rc], writes=[Rps[4]])
            OP("dve", "tensor_tensor", out=zst[gi][:, :, cs], in0=psb(4)[:, 0:512].rearrange("p (v i) -> p v i", v=4),
               in1=sgr[gi][:, :, cs], op=ALU.mult, reads=[Rps[4], Rsgr[gi]], writes=[Rzst[gi]])
            if n == 3:
                Rz = p.res()
                Rzret.append(Rz)
                p.dma(zret_d[h * 512:(h + 1) * 512, gs].rearrange("(vc p) s -> p vc s", p=128), zst[gi],
                      reads=[Rzst[gi]], writes=[Rz], semkey=("zret", gi))

        load_head_weights(0)
        load_head_weights(1)
        units = [(h, g) for h in range(4) for g in range(NG)]
        for f in proj_pieces(*units[0]):
            f()
        pending = []
        for s_ in range(64 + 2):
            if s_ < 64:
                u, n = s_ // 4, s_ % 4
                if n == 0:
                    pending = proj_pieces(*units[u + 1]) if u + 1 < 16 else []
                if s_ % 16 == 1 and 2 <= s_ // 16 + 1 < 4:
                    load_head_weights(s_ // 16 + 1)
                stage_A(s_)
            if 0 <= s_ - 1 < 64:
                stage_B(s_ - 1)
            if 0 <= s_ - 2 < 64:
                stage_C(s_ - 2)
            if s_ < 64:
                take = 1 if n < 2 else 2
                for f in pending[:take]:
                    f()
                pending = pending[take:]
        if stop_after == "RET":
            Rfinal.extend(Rzret + Rzatt)
            return finish()

        p.barrier(scratch[:])
        areset()
        yacc = alloc([128, NT, D], F32)
        Ryacc = [p.res(f"yacc{t}") for t in range(NT)]
        moe_base = apos[0]
        za = alloc([128, 8, 512], BF16)
        zr = alloc([128, 16, 512], BF16)
        Rza, Rzr = p.res("za"), p.res("zr")
        wa = [alloc([128, 8, 128], BF16) for i in range(2)]
        wr = [alloc([128, 16, 128], BF16) for i in range(2)]
        wga = [alloc([128, 8, 128], BF16) for i in range(2)]
        wgb = [alloc([128, 8, 128], BF16) for i in range(2)]
        Rwm = [[p.res(f"wm{i}_{j}") for j in range(4)] for i in range(2)]
        mT2 = [alloc([128, 8, 512], BF16) for i in range(2)]
        RmT2 = [[p.res(f"mT{i}_{k}") for k in range(8)] for i in range(2)]
        wout = alloc([128, 8, 512], BF16)
        Rwout = p.res("wout")
        sga = alloc([128, 512], F32)
        sgb = alloc([128, 512], F32)
        ta = alloc([128, 512], F32)
        tb = alloc([128, 512], F32)
        Rsga, Rsgb, Rta, Rtb = (p.res(n) for n in ("sga", "sgb", "ta", "tb"))
        a_alias = apos[0]
        vtmp = alloc([128, 8, 128], F32)
        Rvtmp = [p.res(f"vtmp{i}") for i in range(2)]
        xlo = [alloc([128, 8, 128], BF16) for i in range(2)]
        Rxlo = [[p.res(f"xlo{i}_{hb}") for hb in range(2)] for i in range(2)]
        assert apos[0] - a_alias == 8 * 512 * 2
        wout2 = arena[0:128, a_alias // 2: a_alias // 2 + 8 * 512].rearrange("p (a b) -> p a b", a=8)
        Rwout2 = p.res("wout2")
        Ralias = Rvtmp + Rxlo[0] + Rxlo[1]
        wrf = alloc([128, 8, 36], F32)
        wrh = alloc([128, 8, 36], BF16)
        wrl = alloc([128, 8, 36], BF16)
        Rwrf, Rwrh, Rwrl = p.res("wrf"), p.res("wrh"), p.res("wrl")
        p.dma(wrf[:, :, 0:4], wrg_d.rearrange("(k p) n -> p k n", p=128), writes=[Rwrf], semkey="wrf")
        p.dma(wrf[:, :, 4:36], wre_d.rearrange("(k p) n -> p k n", p=128), writes=[Rwrf], semkey="wrf")
        OP("dve", "tensor_copy", out=wrh, in_=wrf, reads=[Rwrf], writes=[Rwrh])
        OP("dve", "tensor_tensor", out=wrl, in0=wrf, in1=wrh, op=ALU.subtract, reads=[Rwrf, Rwrh], writes=[Rwrl])

        def load_merge_weights(fc, slot):
            cs_ = slice(fc * 128, (fc + 1) * 128)
            load_w(wa[slot], watt_d[:, cs_].rearrange("(k p) n -> p k n", p=128), Rwm[slot][0], ("wm", slot, 0))
            load_w(wr[slot], wret_d[:, cs_].rearrange("(k p) n -> p k n", p=128), Rwm[slot][1], ("wm", slot, 1))
            load_w(wga[slot], wsrc(w_in_d, 9216 + fc * 128, 9216 + (fc + 1) * 128), Rwm[slot][2], ("wm", slot, 2))
            load_w(wgb[slot], wsrc(w_in_d, 10240 + fc * 128, 10240 + (fc + 1) * 128), Rwm[slot][3], ("wm", slot, 3))

        Rstat2 = [p.res(f"stat2_{t}") for t in range(NT)]

        def post_gen(g):
            mT = mT2[g % 2]
            RmT = RmT2[g % 2]
            for tt in range(4):
                gt = g * 4 + tt
                p.dma(yacc[:, gt, :], x_d[gt * 128:(gt + 1) * 128, :], writes=[Ryacc[gt]], semkey=("xres", gt))
            for half in range(2):
                wsb_ = (wout, wout2)[half]
                Rw_ = [Rwout] if half == 0 else [Rwout2] + Ralias
                for tt in range(4):
                    gt = g * 4 + tt
                    b = 4 + (tt % 2)
                    for k in range(8):
                        MM(ps[:, b, :], mT[:, k, tt * 128:(tt + 1) * 128], wsb_[:, k, :], k == 0, k == 7,
                           [RmT[k]] + Rw_, [Rps[b]])
                    OP("dve", "tensor_tensor", out=yacc[:, gt, half * 512:(half + 1) * 512], in0=ps[:, b, :],
                       in1=yacc[:, gt, half * 512:(half + 1) * 512], op=ALU.add, reads=[Rps[b], Ryacc[gt]], writes=[Ryacc[gt]])
                    yield
            def router(gt, i):
                n_mm = 0
                for (lh, rw, Rl, Rw_) in ((xnT[:, :, gt * 128:(gt + 1) * 128], wrh, [RxnT[gt]], Rwrh),
                                          (xnT[:, :, gt * 128:(gt + 1) * 128], wrl, [RxnT[gt]], Rwrl),
                                          (xlo[i], wrh, Rxlo[i], Rwrh)):
                    for k in range(8):
                        MM(ps[:, 5, 0:36], lh[:, k, :], rw[:, k, :], n_mm == 0, n_mm == 23, Rl + [Rw_], [Rps[5]])
                        n_mm += 1
                OP("dve", "tensor_copy", out=logit[:, gt, :], in_=ps[:, 5, 0:36], reads=[Rps[5]], writes=[Rlogit[gt]])

            pend = None
            for tt in range(4):
                gt = g * 4 + tt
                i = gt % 2
                for _ in norm_tile_gen(yacc[:, gt, :], Ryacc[gt], gt, gffn, Rstat2[gt], (6, 7), lo=(vtmp, Rvtmp, xlo[i], Rxlo[i])):
                    yield
                if pend is not None:
                    router(*pend)
                pend = (gt, i)
                yield
            router(*pend)
            yield
            if g + 1 < NG:
                p.dma(wout2, wsrc(wout_d, 512, 1024), writes=[Rwout2] + Ralias, semkey="wout2", eng="pool")

        widx = 0
        load_merge_weights(0, 0)
        load_w(wout, wsrc(wout_d, 0, 512), Rwout, "wout")
        p.dma(wout2, wsrc(wout_d, 512, 1024), writes=[Rwout2] + Ralias, semkey="wout2", eng="pool")
        post = None
        for g in range(NG):
            gs = slice(g * 512, (g + 1) * 512)
            RxG = RxnT[g * 4:(g + 1) * 4]
            mT = mT2[g % 2]
            RmT = RmT2[g % 2]
            p.dma(za, zatt_d[:, gs].rearrange("(k p) s -> p k s", p=128), reads=Rzatt, writes=[Rza], semkey="za")
            p.dma(zr, zret_d[:, gs].rearrange("(k p) s -> p k s", p=128), reads=Rzret, writes=[Rzr], semkey="zr")
            for fc in range(8):
                slot = widx % 2
                widx += 1
                nfc, ng_ = (fc + 1) % 8, g + (1 if fc == 7 else 0)
                if ng_ < NG:
                    load_merge_weights(nfc, widx % 2)
                for k in range(8):
                    MM(ps[:, 2, :], wga[slot][:, k, :], xnT[:, k, gs], k == 0, k == 7, [Rwm[slot][2]] + RxG, [Rps[2]])
                for k in range(8):
                    MM(ps[:, 3, :], wgb[slot][:, k, :], xnT[:, k, gs], k == 0, k == 7, [Rwm[slot][3]] + RxG, [Rps[3]])
                for k in range(8):
                    MM(ps[:, 0, :], wa[slot][:, k, :], za[:, k, :], k == 0, k == 7, [Rwm[slot][0], Rza], [Rps[0]])
                for k in range(16):
                    MM(ps[:, 1, :], wr[slot][:, k, :], zr[:, k, :], k == 0, k == 15, [Rwm[slot][1], Rzr], [Rps[1]])
                OP("act", "activation", out=sga, in_=ps[:, 2, :], func=AF.Sigmoid, bias=bga[:, fc:fc + 1],
                   reads=[Rps[2], Rc], writes=[Rsga])
                OP("act", "activation", out=sgb, in_=ps[:, 3, :], func=AF.Sigmoid, bias=bgb[:, fc:fc + 1],
                   reads=[Rps[3], Rc], writes=[Rsgb])
                OP("dve", "tensor_tensor", out=ta, in0=ps[:, 0, :], in1=sga, op=ALU.mult, reads=[Rps[0], Rsga], writes=[Rta])
                OP("dve", "tensor_tensor", out=tb, in0=ps[:, 1, :], in1=sgb, op=ALU.mult, reads=[Rps[1], Rsgb], writes=[Rtb])
                OP("pool", "tensor_tensor", out=mT[:, fc, :], in0=ta, in1=tb, op=ALU.add, reads=[Rta, Rtb], writes=[RmT[fc]])
                if post is not None:
                    next(post, None)
                    next(post, None)
                    next(post, None)
            if post is not None:
                for _ in post:
                    pass
            post = post_gen(g)
        for _ in post:
            pass
        if debug:
            Rfinal.append(p.res())
            p.dma(dbg["x1"].rearrange("(t p) d -> p t d", p=128), yacc, reads=Ryacc, writes=[Rfinal[-1]], semkey="dbg1")
            Rfinal.append(p.res())
            p.dma(dbg["logits"], logit[:].rearrange("p t e -> p (t e)"), reads=Rlogit, writes=[Rfinal[-1]], semkey="dbg2")
        if stop_after == "MERGE":
            Rfinal.extend(Ryacc + Rlogit)
            return finish()

        p.barrier(scratch[:])
        areset(moe_base)
        Rr = p.res("route")
        RW = [Rr]
        gT = alloc([128, S], BF16)
        RgT = p.res("gT")
        OP("pool", "memset", gT[64:128, :], 0.0, writes=[RgT])
        csel = alloc([128, 4096], BF16)
        Rcsel = p.res("csel")
        p.dma(csel, csel_d, writes=[Rcsel], semkey="csel")
        NSLOT = 3
        NS2 = 4
        w1s = [alloc([128, 8, 256], BF16) for i in range(NSLOT)]
        w3s = [alloc([128, 8, 256], BF16) for i in range(NSLOT)]
        w2s = [alloc([128, 2, D], BF16) for i in range(NS2)]
        Rw1 = [p.res(f"w1s{i}") for i in range(NSLOT)]
        Rw3 = [p.res(f"w3s{i}") for i in range(NSLOT)]
        Rw2 = [p.res(f"w2s{i}") for i in range(NS2)]

        def load_expert(e):
            sl = e % NSLOT
            load_w(w1s[sl], w1_d[e].rearrange("(k p) n -> p k n", p=128), Rw1[sl], ("w1s", sl))
            load_w(w3s[sl], w3_d[e].rearrange("(k p) n -> p k n", p=128), Rw3[sl], ("w3s", sl))
            load_w(w2s[e % NS2], w2_d[e].rearrange("(k p) n -> p k n", p=128), Rw2[e % NS2], ("w2s", e % NS2))

        load_expert(0)
        load_expert(1)
        route_base = apos[0]
        brb = alloc([128, 36], F32)
        p.dma(brb[:, 0:4], brg_d.partition_broadcast(128), writes=RW, semkey="brb")
        p.dma(brb[:, 4:36], bre_d.partition_broadcast(128), writes=RW, semkey="brb")
        L = alloc([128, NT, 36], F32)
        gmax = alloc([128, NT], F32)
        ohg = alloc([128, NT, 4], F32)
        tg4 = alloc([128, NT, 4], F32)
        den = alloc([128, NT], F32)
        pg = alloc([128, NT], F32)
        sel4 = alloc([128, NT, 4, 8], F32)
        ing = alloc([128, NT, 8], F32)
        ing2 = alloc([128, NT, 8], F32)
        m1 = alloc([128, NT], F32)
        m2 = alloc([128, NT], F32)
        oh1 = alloc([128, NT, 8], F32)
        oh2 = alloc([128, NT, 8], F32)
        dd = alloc([128, NT], F32)
        e2 = alloc([128, NT], F32)
        w1_ = alloc([128, NT], F32)
        w2_ = alloc([128, NT], F32)
        ge = alloc([128, NT, 8], F32)
        ge2 = alloc([128, NT, 8], F32)
        gate = alloc([128, NT, 4, 8], F32)
        glo = alloc([32, S], BF16)

        def bc3(a2, n):
            return a2.unsqueeze(2).to_broadcast([128, NT, n])

        def DV(meth, **kw):
            return OP("dve", meth, reads=RW + Rlogit, writes=RW, **kw)

        DV("tensor_tensor", out=L, in0=logit[:], in1=brb.unsqueeze(1).to_broadcast([128, NT, 36]), op=ALU.add)
        gl = L[:, :, 0:4]
        el = L[:, :, 4:36].rearrange("p t (g e) -> p t g e", g=4)
        DV("tensor_reduce", out=gmax, in_=gl, axis=AX.X, op=ALU.max)
        DV("tensor_tensor", out=ohg, in0=gl, in1=bc3(gmax, 4), op=ALU.is_equal)
        DV("tensor_tensor", out=tg4, in0=gl, in1=bc3(gmax, 4), op=ALU.subtract)
        OP("act", "activation", out=tg4, in_=tg4, func=AF.Exp, reads=RW, writes=RW)
        DV("tensor_reduce", out=den, in_=tg4, axis=AX.X, op=ALU.add)
        DV("reciprocal", out=pg, in_=den)
        DV("tensor_tensor", out=sel4, in0=el, in1=ohg.unsqueeze(3).to_broadcast([128, NT, 4, 8]), op=ALU.mult)
        DV("tensor_reduce", out=ing, in_=sel4.rearrange("p t g e -> p t e g"), axis=AX.X, op=ALU.add)
        DV("tensor_reduce", out=m1, in_=ing, axis=AX.X, op=ALU.max)
        DV("tensor_tensor", out=oh1, in0=ing, in1=bc3(m1, 8), op=ALU.is_equal)
        DV("scalar_tensor_tensor", out=ing2, in0=oh1, scalar=-1.0e30, in1=ing, op0=ALU.mult, op1=ALU.add)
        DV("tensor_reduce", out=m2, in_=ing2, axis=AX.X, op=ALU.max)
        DV("tensor_tensor", out=oh2, in0=ing2, in1=bc3(m2, 8), op=ALU.is_equal)
        DV("tensor_tensor", out=dd, in0=m2, in1=m1, op=ALU.subtract)
        OP("act", "activation", out=e2, in_=dd, func=AF.Exp, reads=RW, writes=RW)
        DV("tensor_scalar", out=w1_, in0=e2, scalar1=1.0, scalar2=None, op0=ALU.add)
        DV("reciprocal", out=w1_, in_=w1_)
        DV("tensor_tensor", out=w2_, in0=e2, in1=w1_, op=ALU.mult)
        DV("tensor_tensor", out=w1_, in0=w1_, in1=pg, op=ALU.mult)
        DV("tensor_tensor", out=w2_, in0=w2_, in1=pg, op=ALU.mult)
        DV("tensor_tensor", out=ge, in0=oh1, in1=bc3(w1_, 8), op=ALU.mult)
        DV("tensor_tensor", out=ge2, in0=oh2, in1=bc3(w2_, 8), op=ALU.mult)
        DV("tensor_tensor", out=ge, in0=ge, in1=ge2, op=ALU.add)
        DV("tensor_tensor", out=gate, in0=ohg.unsqueeze(3).to_broadcast([128, NT, 4, 8]),
           in1=ge.unsqueeze(2).to_broadcast([128, NT, 4, 8]), op=ALU.mult)
        if debug:
            Rfinal.append(p.res())
            p.dma(dbg["gate"], gate.rearrange("p t g e -> p (t g e)"), reads=RW, writes=[Rfinal[-1]], semkey="dbg3")
        for t in range(NT):
            OP("pe", "transpose", out=ps[0:32, t // 4, (t % 4) * 128:(t % 4 + 1) * 128],
               in_=gate[:, t, :, :].rearrange("p g e -> p (g e)"), identity=ident, reads=RW + [Rc], writes=[Rps[t // 4]])
        gps = ps[0:32, 0:4, :].rearrange("p a b -> p (a b)")
        OP("act", "activation", out=gT[0:32, :], in_=gps, func=AF.Copy, reads=Rps[0:4], writes=[RgT])
        OP("dve", "tensor_tensor", out=glo, in0=gps, in1=gT[0:32, :], op=ALU.subtract, reads=Rps[0:4] + [RgT], writes=RW)
        OP("dve", "tensor_copy", out=gT[32:64, :], in_=glo, reads=RW, writes=[RgT])
        if stop_after == "ROUTE":
            Rfinal.extend(Ryacc + [RgT] + RW)
            return finish()

        p.barrier(scratch[:])
        areset(route_base)
        hg = [alloc([128, 2, S], BF16) for i in range(2)]
        Rhg = [[p.res(f"hg{i}_{g}") for g in range(NG)] for i in range(2)]
        s_sb = [alloc([128, 512], F32) for i in range(2)]
        u_sb = [alloc([128, 512], F32) for i in range(2)]
        gbc = [alloc([128, 512], F32) for i in range(2)]
        Rs = [p.res(f"s{i}") for i in range(2)]
        Ru = [p.res(f"u{i}") for i in range(2)]
        Rgbc = [p.res(f"gbc{i}") for i in range(2)]

        ybank = [0]

        def down_unit(e, t, half):
            hi_ = e % 2
            b = 5 + (ybank[0] % 3)
            ybank[0] += 1
            for fc in range(2):
                MM(ps[:, b, :], hg[hi_][:, fc, t * 128:(t + 1) * 128], w2s[e % NS2][:, fc, half * 512:(half + 1) * 512],
                   fc == 0, fc == 1, [Rhg[hi_][t // 4], Rw2[e % NS2]], [Rps[b]])
            OP("dve", "tensor_tensor", out=yacc[:, t, half * 512:(half + 1) * 512],
               in0=ps[:, b, :], in1=yacc[:, t, half * 512:(half + 1) * 512], op=ALU.add,
               reads=[Rps[b], Ryacc[t]], writes=[Ryacc[t]])

        gcnt = 0
        for e in range(32):
            sl = e % NSLOT
            hi_ = e % 2
            if e + 2 < 32:
                load_expert(e + 2)
            for g in range(NG):
                gs = slice(g * 512, (g + 1) * 512)
                RxG = RxnT[g * 4:(g + 1) * 4]
                gb2 = gcnt % 2
                gcnt += 1
                dq = []
                if e > 0:
                    dq = [(t, half) for t in range(g * 4, (g + 1) * 4) for half in range(2)]
                MM(ps[:, 4, :], csel[:, e * 128:(e + 1) * 128], gT[:, gs], True, True, [Rcsel, RgT], [Rps[4]])
                OP("act", "activation", out=gbc[gb2], in_=ps[:, 4, :], func=AF.Copy, reads=[Rps[4]], writes=[Rgbc[gb2]])
                for wi_, (wsb, Rw_) in enumerate(((w1s[sl], Rw1[sl]), (w3s[sl], Rw3[sl]))):
                    for fc in range(2):
                        b = wi_ * 2 + fc
                        for k in range(8):
                            MM(ps[:, b, :], wsb[:, k, fc * 128:(fc + 1) * 128], xnT[:, k, gs], k == 0, k == 7,
                               [Rw_] + RxG, [Rps[b]])
                        for (t, half) in dq[:2]:
                            down_unit(e - 1, t, half)
                        dq = dq[2:]
                for fc in range(2):
                    OP("act", "activation", out=s_sb[fc], in_=ps[:, fc, :], func=AF.Silu, reads=[Rps[fc]], writes=[Rs[fc]])
                    OP("dve", "tensor_tensor", out=u_sb[fc], in0=ps[:, 2 + fc, :], in1=s_sb[fc], op=ALU.mult,
                       reads=[Rps[2 + fc], Rs[fc]], writes=[Ru[fc]])
                    OP("pool", "tensor_tensor", out=hg[hi_][:, fc, gs], in0=u_sb[fc], in1=gbc[gb2], op=ALU.mult,
                       reads=[Ru[fc], Rgbc[gb2]], writes=[Rhg[hi_][g]])
        for t in range(NT):
            for half in range(2):
                down_unit(31, t, half)
        for t in range(NT):
            Rf = p.res()
            Rfinal.append(Rf)
            p.dma(out_d[t * 128:(t + 1) * 128, :], yacc[:, t, :], reads=[Ryacc[t]], writes=[Rf], semkey=("out", t % 4))
        return finish()


_IN_KEYS = ["g_norm_mix", "w_in", "b_merge_gate", "g_q", "g_k", "w_branch_att", "g_ret_norm", "w_branch_ret",
            "w_out", "g_norm_ffn", "w_router_group", "b_router_group", "w_router_expert", "b_router_expert",
            "w1", "w3", "w2"]


def _run(inputs, debug=False, stop_after=None, cores=8, trace=False):
    cfd, cbd, gamma_c = _host_consts()
    cfs_arr, cfs_offs = _pack(cfd, CFS_ORDER, np.float32)
    cbs_arr, cbs_offs = _pack(cbd, CBS_ORDER, ml_dtypes.bfloat16)
    nc = build_nc(cfs_offs, cbs_offs, cfs_arr.shape[1], cbs_arr.shape[1], gamma_c, debug=debug, stop_after=stop_after)
    shared = {}
    for k in _IN_KEYS:
        a = np.asarray(inputs[k], dtype=np.float32)
        shared[k] = np.ascontiguousarray(a[0])
    shared["cfs"] = cfs_arr
    shared["cbs"] = cbs_arr
    shared["catt"] = np.ascontiguousarray(np.concatenate([cfd["CA"], cfd["SA"]], axis=1))
    shared["cret"] = np.ascontiguousarray(np.concatenate([cfd["CR"], cfd["SR"]], axis=1))
    shared["csel"] = np.ascontiguousarray(cbd["sel"].astype(ml_dtypes.bfloat16))
    x = np.asarray(inputs["x"], dtype=np.float32)
    in_maps = []
    for c in range(cores):
        m = dict(shared)
        m["x"] = np.ascontiguousarray(x[c])
        in_maps.append(m)
    res = run_bass_kernel_spmd(nc, in_maps, core_ids=list(range(cores)), trace=trace)
    return res


def kernel(**inputs):
    res = _run(inputs)
    out = np.stack([np.asarray(r["out"], dtype=np.float32) for r in res.results], axis=0)
    return out
```
